# Optimizing a Trainium2 kernel written in Bass

```python
import jax, jax.numpy as jnp
from jax import lax
import numpy as np

D_MODEL = 1024
BATCH = 8
SEQ = 4096
DEPTH = 1

EPS = 1e-6
M_HEADS = 4
M_HEAD_DIM = D_MODEL // M_HEADS
M_WIDTH = M_HEADS * M_HEAD_DIM
M_CONV = 4
M_CHUNK = 64
N_Q_HEADS = 8
N_KV_GROUPS = 2
N_HEADS_PER_GROUP = N_Q_HEADS // N_KV_GROUPS
N_HEAD_DIM = D_MODEL // N_Q_HEADS
N_WIDTH = N_Q_HEADS * N_HEAD_DIM
N_KV_WIDTH = N_KV_GROUPS * N_HEAD_DIM
CMP_BLOCK = 32
CMP_STRIDE = 16
SLC_BLOCK = 64
SLC_TOP = 8
SLC_Q_CHUNK = 32
WINDOW = 512
Q_BLOCK = 128
ROPE_THETA = 10000.0
P_HEADS = 8
P_KEYS = 128
P_EXPERTS = P_KEYS * P_KEYS
P_QUERY_DIM = 128
P_HALF = P_QUERY_DIM // 2
P_TOPK = 16
P_CHUNK = 128

IN_WIDTHS = (2 * M_WIDTH, M_WIDTH, M_WIDTH, M_HEADS, M_HEADS,
             N_WIDTH, N_KV_WIDTH, N_KV_WIDTH, N_KV_WIDTH, N_KV_WIDTH, N_KV_WIDTH, N_KV_WIDTH,
             3 * N_Q_HEADS, D_MODEL, D_MODEL)
IN_TOTAL = sum(IN_WIDTHS)

kernel_name = 'hybrid_mlstm_nsa_peer_block'


def rmsnorm(x, g):
    xf = x.astype(jnp.float32)
    y = xf * lax.rsqrt(jnp.mean(xf * xf, axis=-1, keepdims=True) + EPS)
    return (y * g.astype(jnp.float32)).astype(x.dtype)


def masked_softmax(s, mask):
    s = jnp.where(mask, s, -1e30)
    return jnp.where(mask, jax.nn.softmax(s, axis=-1), 0.0)


def split_cols(z, widths):
    outs, off = [], 0
    for w in widths:
        outs.append(z[..., off:off + w])
        off += w
    return outs


def rope_tables(seq, dim):
    inv = ROPE_THETA ** (-jnp.arange(0, dim, 2, dtype=jnp.float32) / dim)
    ang = jnp.arange(seq, dtype=jnp.float32)[:, None] * inv[None, :]
    ang = jnp.concatenate([ang, ang], axis=-1)
    return jnp.cos(ang), jnp.sin(ang)


def apply_rope(x, cos, sin):
    xf = x.astype(jnp.float32)
    x1, x2 = jnp.split(xf, 2, axis=-1)
    rot = jnp.concatenate([-x2, x1], axis=-1)
    return (xf * cos[None, :, None, :] + rot * sin[None, :, None, :]).astype(x.dtype)


def causal_dwconv(x, w, b):
    y = lax.conv_general_dilated(x, w[:, None, :].astype(x.dtype), window_strides=(1,),
                                 padding=[(M_CONV - 1, 0)],
                                 dimension_numbers=('NWC', 'WIO', 'NWC'),
                                 feature_group_count=x.shape[-1])
    return y + b.astype(y.dtype)


def mlstm_chunkwise(q, k, v, i_pre, f_pre):
    B, S, H, d = q.shape
    L = M_CHUNK
    nc = S // L

    def to_chunks(a):
        return a.astype(jnp.float32).reshape(B, nc, L, H, -1).transpose(1, 0, 3, 2, 4)

    qc, kc, vc = to_chunks(q), to_chunks(k), to_chunks(v)
    lf = jax.nn.log_sigmoid(f_pre.astype(jnp.float32)).reshape(B, nc, L, H).transpose(1, 0, 3, 2)
    li = i_pre.astype(jnp.float32).reshape(B, nc, L, H).transpose(1, 0, 3, 2)
    causal = jnp.tril(jnp.ones((L, L), dtype=bool))

    def step(carry, inp):
        C, n, m = carry
        qb, kb, vb, lfb, lib = inp
        b = jnp.cumsum(lfb, axis=-1)
        Dm = jnp.where(causal, b[..., :, None] - b[..., None, :] + lib[..., None, :], -jnp.inf)
        inter = b + m[..., None]
        mt = jnp.maximum(inter, jnp.max(Dm, axis=-1))
        Dw = jnp.exp(Dm - mt[..., None])
        iw = jnp.exp(inter - mt)
        W = jnp.einsum('bhld,bhsd->bhls', qb, kb) * Dw
        num = iw[..., None] * jnp.einsum('bhvk,bhlk->bhlv', C, qb) + jnp.einsum('bhls,bhsv->bhlv', W, vb)
        den = iw * jnp.einsum('bhk,bhlk->bhl', n, qb) + jnp.sum(W, axis=-1)
        h = num / jnp.maximum(jnp.abs(den), jnp.exp(-mt))[..., None]
        bL = b[..., -1]
        g = bL[..., None] - b + lib
        m_new = jnp.maximum(bL + m, jnp.max(g, axis=-1))
        a = jnp.exp(bL + m - m_new)
        w = jnp.exp(g - m_new[..., None])
        C_new = a[..., None, None] * C + jnp.einsum('bhlv,bhlk->bhvk', vb * w[..., None], kb)
        n_new = a[..., None] * n + jnp.einsum('bhl,bhlk->bhk', w, kb)
        return (C_new, n_new, m_new), h

    init = (jnp.zeros((B, H, d, d), jnp.float32), jnp.zeros((B, H, d), jnp.float32),
            jnp.zeros((B, H), jnp.float32))
    _, hs = lax.scan(step, init, (qc, kc, vc, lf, li))
    return hs.transpose(1, 0, 3, 2, 4).reshape(B, S, H, d)


def mlstm_branch(m_qk, m_v, m_o, m_i, m_f, conv_w, conv_b, i_bias, f_bias, norm_g):
    B, S, _ = m_v.shape
    qk = jax.nn.silu(causal_dwconv(m_qk, conv_w, conv_b))
    q, k = jnp.split(qk, 2, axis=-1)
    q = q.reshape(B, S, M_HEADS, M_HEAD_DIM)
    k = k.reshape(B, S, M_HEADS, M_HEAD_DIM) * (M_HEAD_DIM ** -0.5)
    v = m_v.reshape(B, S, M_HEADS, M_HEAD_DIM)
    i_pre = m_i + i_bias.astype(m_i.dtype)
    f_pre = m_f + f_bias.astype(m_f.dtype)
    h = mlstm_chunkwise(q, k, v, i_pre, f_pre)
    h = h * lax.rsqrt(jnp.mean(h * h, axis=-1, keepdims=True) + EPS)
    h = h.reshape(B, S, M_WIDTH) * norm_g.astype(jnp.float32) * jax.nn.sigmoid(m_o.astype(jnp.float32))
    return h.astype(m_v.dtype)


def compress(k, pos, w1, w2):
    B, S, G, d = k.shape
    n = (S - CMP_BLOCK) // CMP_STRIDE + 1
    idx = jnp.arange(n)[:, None] * CMP_STRIDE + jnp.arange(CMP_BLOCK)[None, :]
    kb = k[:, idx] + pos.astype(k.dtype)[None, None, :, None, :]
    kb = kb.transpose(0, 1, 3, 2, 4).reshape(B, n, G, CMP_BLOCK * d)
    return jax.nn.gelu(kb @ w1, approximate=False) @ w2


def selected_attention(q_r, k, v, top_idx, blk_ok):
    B, S, _, d = q_r.shape
    n_top = top_idx.shape[-1]
    nq = S // SLC_Q_CHUNK
    n_keys = n_top * SLC_BLOCK
    scale = N_HEAD_DIM ** -0.5
    qc = q_r.reshape(B, nq, SLC_Q_CHUNK, N_KV_GROUPS, N_HEADS_PER_GROUP, d).transpose(1, 0, 2, 3, 4, 5)
    ic = top_idx.reshape(B, N_KV_GROUPS, nq, SLC_Q_CHUNK, n_top).transpose(2, 0, 1, 3, 4)
    oc = blk_ok.reshape(B, N_KV_GROUPS, nq, SLC_Q_CHUNK, n_top).transpose(2, 0, 1, 3, 4)
    tc = jnp.arange(S).reshape(nq, SLC_Q_CHUNK)
    k_bg = k.transpose(0, 2, 1, 3)
    v_bg = v.transpose(0, 2, 1, 3)
    gather = jax.vmap(jax.vmap(lambda arr, ii: arr[ii]))

    def one(args):
        qb, ib, ob, tb = args
        kpos = ib[..., None] * SLC_BLOCK + jnp.arange(SLC_BLOCK)
        flat = kpos.reshape(B, N_KV_GROUPS, SLC_Q_CHUNK * n_keys)
        kg = gather(k_bg, flat).reshape(B, N_KV_GROUPS, SLC_Q_CHUNK, n_keys, d)
        vg = gather(v_bg, flat).reshape(B, N_KV_GROUPS, SLC_Q_CHUNK, n_keys, d)
        mask = ((kpos <= tb[None, None, :, None, None]) & ob[..., None]).reshape(
            B, N_KV_GROUPS, 1, SLC_Q_CHUNK, n_keys)
        s = jnp.einsum('bqghd,bgqkd->bghqk', qb, kg).astype(jnp.float32) * scale
        p = masked_softmax(s, mask)
        return jnp.einsum('bghqk,bgqkd->bqghd', p.astype(qb.dtype), vg)

    out = lax.map(one, (qc, ic, oc, tc))
    return out.transpose(1, 0, 2, 3, 4, 5).reshape(B, S, N_Q_HEADS, d)


def window_attention(q_r, k, v):
    B, S, _, d = q_r.shape
    nb = S // Q_BLOCK
    span = WINDOW + Q_BLOCK
    scale = N_HEAD_DIM ** -0.5
    qb = q_r.reshape(B, nb, Q_BLOCK, N_KV_GROUPS, N_HEADS_PER_GROUP, d).transpose(1, 0, 2, 3, 4, 5)
    kpad = jnp.pad(k, ((0, 0), (WINDOW, 0), (0, 0), (0, 0)))
    vpad = jnp.pad(v, ((0, 0), (WINDOW, 0), (0, 0), (0, 0)))

    def one(args):
        qq, bi = args
        start = bi * Q_BLOCK
        kk = lax.dynamic_slice_in_dim(kpad, start, span, axis=1)
        vv = lax.dynamic_slice_in_dim(vpad, start, span, axis=1)
        tq = start + jnp.arange(Q_BLOCK)
        kp = start - WINDOW + jnp.arange(span)
        mask = (kp[None, :] <= tq[:, None]) & (kp[None, :] > tq[:, None] - WINDOW) & (kp[None, :] >= 0)
        s = jnp.einsum('bqghd,bkgd->bghqk', qq, kk).astype(jnp.float32) * scale
        p = masked_softmax(s, mask)
        return jnp.einsum('bghqk,bkgd->bqghd', p.astype(qq.dtype), vv)

    out = lax.map(one, (qb, jnp.arange(nb)))
    return out.transpose(1, 0, 2, 3, 4, 5).reshape(B, S, N_Q_HEADS, d)


def nsa_branch(n_q, n_kc, n_vc, n_ks, n_vs, n_kw, n_vw, n_g,
               cmp_k_pos, cmp_k_w1, cmp_k_w2, cmp_v_pos, cmp_v_w1, cmp_v_w2):
    B, S, _ = n_q.shape
    dt = n_q.dtype
    d = N_HEAD_DIM
    scale = d ** -0.5
    q = n_q.reshape(B, S, N_Q_HEADS, d)
    kv = lambda a: a.reshape(B, S, N_KV_GROUPS, d)
    kc, vc, ks, vs, kw, vw = kv(n_kc), kv(n_vc), kv(n_ks), kv(n_vs), kv(n_kw), kv(n_vw)
    cos, sin = rope_tables(S, d)
    q_r = apply_rope(q, cos, sin)
    ks_r = apply_rope(ks, cos, sin)
    kw_r = apply_rope(kw, cos, sin)
    t = jnp.arange(S)
    kcmp = compress(kc, cmp_k_pos, cmp_k_w1, cmp_k_w2)
    vcmp = compress(vc, cmp_v_pos, cmp_v_w1, cmp_v_w2)
    n_cmp = kcmp.shape[1]
    cmp_start = jnp.arange(n_cmp) * CMP_STRIDE
    cmask = (cmp_start + CMP_BLOCK - 1)[None, :] <= t[:, None]
    qg = q.reshape(B, S, N_KV_GROUPS, N_HEADS_PER_GROUP, d)
    s = jnp.einsum('bsghd,bngd->bghsn', qg, kcmp).astype(jnp.float32) * scale
    p_cmp = masked_softmax(s, cmask)
    o_cmp = jnp.einsum('bghsn,bngd->bsghd', p_cmp.astype(dt), vcmp).reshape(B, S, N_Q_HEADS, d)
    n_slc = S // SLC_BLOCK
    slc_start = jnp.arange(n_slc) * SLC_BLOCK
    overlap = ((cmp_start[:, None] < slc_start[None, :] + SLC_BLOCK) &
               (cmp_start[:, None] + CMP_BLOCK > slc_start[None, :])).astype(jnp.float32)
    imp = jnp.einsum('bghsn,nj->bgsj', p_cmp, overlap)
    cur = t // SLC_BLOCK
    j = jnp.arange(n_slc)
    forced = (j[None, :] == 0) | (j[None, :] == cur[:, None]) | (j[None, :] == cur[:, None] - 1)
    valid = slc_start[None, :] <= t[:, None]
    score = jnp.where(forced, 1e9, jnp.where(valid, imp, -1e9))
    top_vals, top_idx = lax.top_k(score, min(SLC_TOP, n_slc))
    blk_ok = top_vals > -1e8
    o_slc = selected_attention(q_r, ks_r, vs, top_idx, blk_ok)
    o_win = window_attention(q_r, kw_r, vw)
    g = jax.nn.sigmoid(n_g.astype(jnp.float32)).reshape(B, S, N_Q_HEADS, 3)
    o = (g[..., 0:1] * o_cmp.astype(jnp.float32) + g[..., 1:2] * o_slc.astype(jnp.float32)
         + g[..., 2:3] * o_win.astype(jnp.float32))
    return o.reshape(B, S, N_WIDTH).astype(dt)


def token_mixer(xn, w_in, m_conv_w, m_conv_b, m_i_bias, m_f_bias, m_norm_g, w_m_out,
                cmp_k_pos, cmp_k_w1, cmp_k_w2, cmp_v_pos, cmp_v_w1, cmp_v_w2, w_n_out, w_out):
    z = xn @ w_in
    (m_qk, m_v, m_o, m_i, m_f, n_q, n_kc, n_vc, n_ks, n_vs, n_kw, n_vw,
     n_g, g_a, g_b) = split_cols(z, IN_WIDTHS)
    y_a = mlstm_branch(m_qk, m_v, m_o, m_i, m_f, m_conv_w, m_conv_b, m_i_bias, m_f_bias, m_norm_g) @ w_m_out
    y_b = nsa_branch(n_q, n_kc, n_vc, n_ks, n_vs, n_kw, n_vw, n_g,
                     cmp_k_pos, cmp_k_w1, cmp_k_w2, cmp_v_pos, cmp_v_w1, cmp_v_w2) @ w_n_out
    mix = (jax.nn.sigmoid(g_a.astype(jnp.float32)) * y_a.astype(jnp.float32)
           + jax.nn.sigmoid(g_b.astype(jnp.float32)) * y_b.astype(jnp.float32))
    return mix.astype(xn.dtype) @ w_out


def peer(xn, wq, k1, k2, u, v):
    B, S, D = xn.shape
    xs = xn.reshape((B * S) // P_CHUNK, P_CHUNK, D)

    def one(xt):
        q = (xt @ wq).reshape(P_CHUNK, P_HEADS, P_QUERY_DIM)
        q1, q2 = q[..., :P_HALF], q[..., P_HALF:]
        s1 = jnp.einsum('thd,nd->thn', q1, k1).astype(jnp.float32)
        s2 = jnp.einsum('thd,nd->thn', q2, k2).astype(jnp.float32)
        v1, i1 = lax.top_k(s1, P_TOPK)
        v2, i2 = lax.top_k(s2, P_TOPK)
        cand = (v1[..., :, None] + v2[..., None, :]).reshape(P_CHUNK, P_HEADS, P_TOPK * P_TOPK)
        sv, ci = lax.top_k(cand, P_TOPK)
        e = (jnp.take_along_axis(i1, ci // P_TOPK, axis=-1) * P_KEYS
             + jnp.take_along_axis(i2, ci % P_TOPK, axis=-1))
        gate = jax.nn.softmax(sv, axis=-1)
        act = jax.nn.gelu(jnp.einsum('td,thkd->thk', xt, u[e]).astype(jnp.float32), approximate=False)
        return jnp.einsum('thk,thkd->td', (gate * act).astype(xt.dtype), v[e])

    return lax.map(one, xs).reshape(B, S, D)


def setup_inputs(seed: int = 0) -> dict:
    key = jax.random.key(seed)
    ks = jax.random.split(key, 24)
    L = DEPTH

    def nrm(k, shape, scale):
        return jax.random.normal(k, shape, jnp.float32) * scale

    return {
        'x': nrm(ks[0], (BATCH, SEQ, D_MODEL), 1.0),
        'ln_mix_g': 1.0 + nrm(ks[1], (L, D_MODEL), 0.01),
        'w_in': nrm(ks[2], (L, D_MODEL, IN_TOTAL), D_MODEL ** -0.5),
        'm_conv_w': nrm(ks[3], (L, M_CONV, 2 * M_WIDTH), M_CONV ** -0.5),
        'm_conv_b': nrm(ks[4], (L, 2 * M_WIDTH), 0.01),
        'm_i_bias': nrm(ks[5], (L, M_HEADS), 0.1),
        'm_f_bias': jnp.linspace(3.0, 6.0, M_HEADS, dtype=jnp.float32)[None, :] + nrm(ks[6], (L, M_HEADS), 0.1),
        'm_norm_g': 1.0 + nrm(ks[7], (L, M_WIDTH), 0.01),
        'w_m_out': nrm(ks[8], (L, M_WIDTH, D_MODEL), M_WIDTH ** -0.5),
        'cmp_k_pos': nrm(ks[9], (L, CMP_BLOCK, N_HEAD_DIM), 0.1),
        'cmp_k_w1': nrm(ks[10], (L, CMP_BLOCK * N_HEAD_DIM, N_HEAD_DIM), (CMP_BLOCK * N_HEAD_DIM) ** -0.5),
        'cmp_k_w2': nrm(ks[11], (L, N_HEAD_DIM, N_HEAD_DIM), N_HEAD_DIM ** -0.5),
        'cmp_v_pos': nrm(ks[12], (L, CMP_BLOCK, N_HEAD_DIM), 0.1),
        'cmp_v_w1': nrm(ks[13], (L, CMP_BLOCK * N_HEAD_DIM, N_HEAD_DIM), (CMP_BLOCK * N_HEAD_DIM) ** -0.5),
        'cmp_v_w2': nrm(ks[14], (L, N_HEAD_DIM, N_HEAD_DIM), N_HEAD_DIM ** -0.5),
        'w_n_out': nrm(ks[15], (L, N_WIDTH, D_MODEL), N_WIDTH ** -0.5),
        'w_out': nrm(ks[16], (L, D_MODEL, D_MODEL), D_MODEL ** -0.5),
        'ln_ffn_g': 1.0 + nrm(ks[17], (L, D_MODEL), 0.01),
        'peer_wq': nrm(ks[18], (L, D_MODEL, P_HEADS * P_QUERY_DIM), D_MODEL ** -0.5),
        'peer_k1': nrm(ks[19], (L, P_KEYS, P_HALF), P_HALF ** -0.5),
        'peer_k2': nrm(ks[20], (L, P_KEYS, P_HALF), P_HALF ** -0.5),
        'peer_u': nrm(ks[21], (L, P_EXPERTS, D_MODEL), D_MODEL ** -0.5),
        'peer_v': nrm(ks[22], (L, P_EXPERTS, D_MODEL), P_HEADS ** -0.5),
        'ln_f_g': 1.0 + nrm(ks[23], (D_MODEL,), 0.01),
    }


def reference(x, ln_mix_g, w_in, m_conv_w, m_conv_b, m_i_bias, m_f_bias, m_norm_g, w_m_out,
              cmp_k_pos, cmp_k_w1, cmp_k_w2, cmp_v_pos, cmp_v_w1, cmp_v_w2, w_n_out, w_out,
              ln_ffn_g, peer_wq, peer_k1, peer_k2, peer_u, peer_v, ln_f_g):
    h = x
    for l in range(DEPTH):
        h = h + token_mixer(rmsnorm(h, ln_mix_g[l]), w_in[l], m_conv_w[l], m_conv_b[l], m_i_bias[l],
                            m_f_bias[l], m_norm_g[l], w_m_out[l], cmp_k_pos[l], cmp_k_w1[l], cmp_k_w2[l],
                            cmp_v_pos[l], cmp_v_w1[l], cmp_v_w2[l], w_n_out[l], w_out[l])
        h = h + peer(rmsnorm(h, ln_ffn_g[l]), peer_wq[l], peer_k1[l], peer_k2[l], peer_u[l], peer_v[l])
    return rmsnorm(h, ln_f_g)
```

```python
from contextlib import ExitStack
import numpy as np
import ml_dtypes
import concourse.bass as bass
import concourse.mybir as mybir
from concourse.bass_utils import run_bass_kernel_spmd

F32 = mybir.dt.float32
BF16 = mybir.dt.bfloat16
U32 = mybir.dt.uint32
ALU = mybir.AluOpType
AF = mybir.ActivationFunctionType
AX = mybir.AxisListType

S = 4096
D = 1024
NT = 32
EPS = 1e-6
N_CORES = 8
E2_VQ_ACT = False
SAME_ENGINE_WAITS = True


class Buf:
    __slots__ = ("name", "w", "r", "dsem", "dcnt", "excl")

    def __init__(self, name, excl=False):
        self.name = name
        self.excl = excl
        self.w = {}
        self.r = {}
        self.dsem = None
        self.dcnt = 0


class _Rec:
    def __init__(self):
        self.call = None

    def __getattr__(self, name):
        def f(*a, **k):
            self.call = (name, a, k)
            return self
        return f


def _bind(fn):
    rec = _Rec()
    fn(rec)
    name, a, k = rec.call

    def run(e):
        try:
            return getattr(e, name)(*a, **k)
        except Exception:
            print("FAILED OP", name, [getattr(x, "shape", x) for x in a], {kk: getattr(v, "shape", v) for kk, v in k.items()})
            raise
    return run


class Sched:
    ENG = ("pe", "act", "dve", "pool", "sp")

    def __init__(self, nc, es):
        self.nc, self.es = nc, es
        self.q = {e: [] for e in self.ENG}
        self.sems = []
        self.ecnt = {e: 0 for e in self.ENG}
        self.seen = {e: {} for e in self.ENG}
        self.pending = {e: [] for e in self.ENG}
        self.esem = {}
        for e in ("pe", "act", "dve", "pool"):
            self.esem[e] = self.new_sem("s_" + e)
        self.dbufs = []
        self.nbuf = 0

    def new_sem(self, name):
        h = self.es.enter_context(self.nc.semaphore(name))
        self.sems.append(h)
        return len(self.sems) - 1

    def buf(self, name="b"):
        self.nbuf += 1
        return Buf("%s%d" % (name, self.nbuf))

    def _waits(self, eng, reads, writes, pwrites):
        need = {}

        def add(evs):
            for s, v in evs.items():
                if need.get(s, 0) < v:
                    need[s] = v

        for b in reads:
            add(b.w)
            if b.excl:
                add(b.r)
        for b in writes:
            add(b.w)
            add(b.r)
        for b in pwrites:
            add(b.w)
            add(b.r)
        out = []
        seen = self.seen[eng]
        own = self.esem.get(eng)
        for s, v in need.items():
            if s == own and not SAME_ENGINE_WAITS:
                continue
            if seen.get(s, 0) < v:
                seen[s] = v
                out.append((s, v))
        return out

    @staticmethod
    def _commit(ev, reads, writes, pwrites):
        s, v = ev
        for b in writes:
            b.w = {s: v}
            b.r = {}
        for b in pwrites:
            if b.w.get(s, 0) < v:
                b.w[s] = v
        for b in reads:
            if b.r.get(s, 0) < v:
                b.r[s] = v

    def op(self, eng, fn, reads=(), writes=(), pwrites=(), sig=True):
        fn = _bind(fn)
        waits = self._waits(eng, reads, writes, pwrites)
        if not sig:
            self.q[eng].append((waits, fn, None))
            self.pending[eng].append((reads, writes, pwrites))
            return
        self.ecnt[eng] += 1
        ev = (self.esem[eng], self.ecnt[eng])
        self.q[eng].append((waits, fn, (ev[0], 1)))
        for (r, w, pw) in self.pending[eng]:
            self._commit(ev, r, w, pw)
        self.pending[eng] = []
        self._commit(ev, reads, writes, pwrites)

    def dma(self, out_ap, in_ap, sb, reads=(), writes=(), pwrites=(), q="sp"):
        waits = self._waits(q, reads, writes, pwrites)
        if sb.dsem is None:
            sb.dsem = self.new_sem("d_" + sb.name)
            self.dbufs.append(sb)
        sb.dcnt += 16
        ev = (sb.dsem, sb.dcnt)
        self.q[q].append((waits, lambda e: e.dma_start(out=out_ap, in_=in_ap), (sb.dsem, 16)))
        self._commit(ev, reads, writes, pwrites)

    def barrier(self):
        evs = [(self.esem[e], self.ecnt[e]) for e in ("pe", "act", "dve", "pool") if self.ecnt[e] > 0]
        evs += [(b.dsem, b.dcnt) for b in self.dbufs]
        for e in self.ENG:
            seen = self.seen[e]
            ws = []
            for s, v in evs:
                if seen.get(s, 0) < v:
                    seen[s] = v
                    ws.append((s, v))
            if ws:
                self.q[e].append((ws, None, None))

    def replay(self, name, eng):
        for waits, fn, inc in self.q[name]:
            for s, v in waits:
                eng.wait_ge(self.sems[s], v)
            if fn is None:
                continue
            ins = fn(eng)
            if inc is not None:
                ins.then_inc(self.sems[inc[0]], inc[1])


class Ring:
    def __init__(self, sc, name, aps, bufs=None):
        self.items = [(ap, sc.buf(name) if bufs is None else bufs[i]) for i, ap in enumerate(aps)]
        self.i = 0

    def next(self):
        it = self.items[self.i % len(self.items)]
        self.i += 1
        return it


class Arena:
    def __init__(self, t, nbytes):
        self.t = t
        self.cap = nbytes
        self.off = 0

    def mark(self):
        return self.off

    def reset(self, m):
        self.off = m

    def alloc(self, shape, dt):
        esz = 4 if dt in (F32, U32) else 2
        n = 1
        for s_ in shape:
            n *= s_
        nb = (n * esz + 31) // 32 * 32
        assert self.off + nb <= self.cap, ("SBUF arena overflow", self.off, nb, self.cap)
        a = self.t[:, self.off // 4:(self.off + nb) // 4]
        self.off += nb
        if esz == 2:
            a = a.bitcast(dt)
        elif dt is not F32:
            a = a.bitcast(dt)
        a = a[:, 0:n]
        if len(shape) == 2:
            return a.rearrange("p (a b) -> p a b", a=shape[0])
        if len(shape) == 3:
            return a.rearrange("p (a b c) -> p a b c", a=shape[0], b=shape[1])
        return a


IN_WIDTHS = (2048, 1024, 1024, 4, 4, 1024, 256, 256, 256, 256, 256, 256, 24, 1024, 1024)
_off = np.cumsum((0,) + IN_WIDTHS)
(O_QK, O_V, O_O, O_I, O_F, O_NQ, O_KC, O_VC, O_KS, O_VS, O_KW, O_VW, O_NG, O_GA, O_GB) = [int(v) for v in _off[:-1]]


def _rot_cols(base, nheads):
    idx = []
    for h in range(nheads):
        for d in range(128):
            idx.append(base + h * 128 + (d + 64) % 128)
    return idx


def fm_col_index():
    cols = list(range(O_QK, O_QK + 2048))
    cols += list(range(O_NQ, O_NQ + 1024)) + _rot_cols(O_NQ, 8)
    cols += list(range(O_KC, O_KC + 256)) + list(range(O_VC, O_VC + 256))
    cols += list(range(O_KS, O_KS + 256)) + _rot_cols(O_KS, 2)
    cols += list(range(O_KW, O_KW + 256)) + _rot_cols(O_KW, 2)
    cols += list(range(O_GA, O_GA + 1024)) + list(range(O_GB, O_GB + 1024))
    return np.asarray(cols)


def tm_col_index():
    cols = list(range(O_V, O_V + 1024)) + list(range(O_O, O_O + 1024))
    cols += list(range(O_VS, O_VS + 256)) + list(range(O_VW, O_VW + 256))
    return np.asarray(cols)


def sm_col_index():
    return np.asarray(list(range(O_I, O_I + 4)) + list(range(O_F, O_F + 4)) + list(range(O_NG, O_NG + 24)))


ZR_QK, ZR_Q, ZR_QR, ZR_KC, ZR_VC, ZR_KSR, ZR_KWR, ZR_GA, ZR_GB = 0, 2048, 3072, 4096, 4352, 4608, 4864, 5120, 6144
ZT_ROWS = 7168


class _Stop(Exception):
    pass


def build(dbg=(), stop=None):
    nc = bass.Bass("TRN2", target_bir_lowering=False)
    es = ExitStack()
    dbg = set(dbg)

    def check(name):
        if stop == name:
            raise _Stop()

    def din(name, shape, dt=F32):
        return nc.dram_tensor(name, list(shape), dt, kind="ExternalInput").ap()

    def dscr(name, shape, dt):
        kind = "ExternalOutput" if name in dbg else "Internal"
        return nc.dram_tensor(name, list(shape), dt, kind=kind).ap()

    x_d = din("x", [S, D])
    gmix_d = din("g_mix", [128, D])
    wfm_d = din("w_fm", [60, 128, 8, 128])
    wtm_d = din("w_tm", [5, 128, 8, 512])
    wsm_d = din("w_sm", [128, 8, 32])
    cos_d = din("cosT", [128, S])
    sin_d = din("sinT", [128, S])
    convw_d = din("conv_w", [16, 128, 5])
    ident_d = din("ident", [128, 128])
    tri_d = din("tri", [128, 128])
    mbias_d = din("m_bias", [128, 8])
    mnormg_d = din("m_norm_g", [128, 1024])
    out_d = nc.dram_tensor("out", [S, D], F32, kind="ExternalOutput").ap()

    zT_d = dscr("zT", [ZT_ROWS, S], BF16)
    ztm_d = dscr("ztm", [S, 2560], BF16)
    zsm_d = dscr("zsm", [S, 32], F32)
    hmT_d = dscr("hmT", [1024, S], BF16)
    onT_d = dscr("onT", [1024, S], BF16)
    h1_d = dscr("h1", [S, D], F32)
    xhT_d = dscr("xhT", [1024, S], BF16)
    wsq_d = din("wsq", [3, 128, 8, 1024])
    u_d = din("peer_u", [16384, 1024])
    v_d = din("peer_v", [16384, 1024])
    uT_d = dscr("uT", [128, 128, 8, 128], BF16)
    vb_d = dscr("vb", [128, 128, 1024], BF16)
    wq_d = din("wq", [128, 8, 1024])
    kblk_d = din("kblk", [128, 256])
    iota_d = din("iota", [128, 128])
    gf_d = din("g_fin", [128, D])
    gffn_d = din("g_ffn", [128, D])
    ovl_d = din("ovl", [2, 128, 64])
    eall_d = din("eall", [64, S])
    cw1_d = din("cw1", [2, 128, 32, 128])
    cw2_d = din("cw2", [2, 128, 128])
    cpos_d = din("cpos", [2, 128, 32])

    ARENA_BYTES = 204 * 1024
    arena_t = es.enter_context(nc.sbuf_tensor("arena", [128, ARENA_BYTES // 4], F32))
    psum_t = es.enter_context(nc.psum_tensor("psum", [128, 4096], F32))
    sc = Sched(nc, es)
    A = Arena(arena_t, ARENA_BYTES)

    def psbank(i, n=512, off=0):
        return psum_t[:, i * 512 + off:i * 512 + off + n]

    PB = [Buf("psb%d" % i, excl=True) for i in range(8)]

    try:
        _body(nc, sc, A, psbank, PB, check, locals())
    except _Stop:
        pass
    _finish(nc, sc, es)
    return nc


def _body(nc, sc, A, psbank, PB, check, L):
    (x_d, gmix_d, wfm_d, wtm_d, wsm_d, cos_d, sin_d, convw_d, ident_d, out_d, zT_d, ztm_d, zsm_d) = [L[k] for k in (
        "x_d", "gmix_d", "wfm_d", "wtm_d", "wsm_d", "cos_d", "sin_d", "convw_d", "ident_d", "out_d", "zT_d", "ztm_d", "zsm_d")]
    tri_d, mbias_d, mnormg_d, hmT_d = L["tri_d"], L["mbias_d"], L["mnormg_d"], L["hmT_d"]
    dbg = L["dbg"]

    def dump(name, ap, b, dt=F32):
        if name in dbg:
            d = nc.dram_tensor(name, [128] + list(ap.shape[1:]), dt, kind="ExternalOutput").ap()
            sc.dma(d, ap, b, reads=[b])

    ident_f = A.alloc([128], F32)
    ident_b = A.alloc([128], BF16)
    b_ident = sc.buf("ident")
    sc.dma(ident_f, ident_d, b_ident, writes=[b_ident])
    b_identb = sc.buf("identb")
    sc.op("dve", lambda e: e.tensor_copy(ident_b, ident_f), reads=[b_ident], writes=[b_identb])
    tri_f = A.alloc([128], F32)
    b_tri = sc.buf("tri")
    sc.dma(tri_f, tri_d, b_tri, writes=[b_tri])
    ones_f = A.alloc([128], F32)
    b_ones = sc.buf("ones")
    sc.op("pool", lambda e: e.memset(ones_f, 1.0), writes=[b_ones])
    base_mark = A.mark()
    check("const")

    xnT = A.alloc([8, S], BF16)
    b_xnT = sc.buf("xnT")
    gmix = A.alloc([D], F32)
    b_gmix = sc.buf("gmix")
    sc.dma(gmix, gmix_d, b_gmix, writes=[b_gmix])
    mA = A.mark()
    xin = Ring(sc, "xin", [A.alloc([D], F32) for _ in range(2)])
    sqr = Ring(sc, "sq", [A.alloc([D], F32) for _ in range(1)])
    xnb = Ring(sc, "xnb", [A.alloc([D], BF16) for _ in range(2)])
    stat = Ring(sc, "stat", [A.alloc([4], F32) for _ in range(2)])
    pst = Ring(sc, "pst", [psbank(i).bitcast(BF16) for i in (6, 7)], [PB[6], PB[7]])
    x_t = x_d.rearrange("(n p) d -> n p d", p=128)
    for t in range(NT):
        xa, xb = xin.next()
        sc.dma(xa, x_t[t], xb, writes=[xb])
        sq, sqb = sqr.next()
        st, stb = stat.next()
        sc.op("act", lambda e, sq=sq, xa=xa: e.activation(sq, xa, AF.Square), reads=[xb], writes=[sqb])
        sc.op("dve", lambda e, st=st, sq=sq: e.reduce_sum(st[:, 0:1], sq, axis=AX.X), reads=[sqb], writes=[stb])
        sc.op("dve", lambda e, st=st: e.tensor_scalar(st[:, 1:2], st[:, 0:1], 1.0 / D, EPS, op0=ALU.mult, op1=ALU.add),
              reads=[stb], pwrites=[stb])
        sc.op("act", lambda e, st=st: e.activation(st[:, 2:3], st[:, 1:2], AF.Ln), reads=[stb], pwrites=[stb])
        sc.op("act", lambda e, st=st: e.activation(st[:, 3:4], st[:, 2:3], AF.Exp, scale=-0.5), reads=[stb], pwrites=[stb])
        xn, xnbuf = xnb.next()
        sc.op("dve", lambda e, xn=xn, xa=xa, st=st: e.scalar_tensor_tensor(
            xn, xa, st[:, 3:4], gmix, op0=ALU.mult, op1=ALU.mult), reads=[xb, stb, b_gmix], writes=[xnbuf])
        if t == 1:
            check("norm1")
        pt, ptb = pst.next()
        for k in range(8):
            sc.op("pe", lambda e, pt=pt, xn=xn, k=k: e.transpose(pt[:, k * 128:(k + 1) * 128], xn[:, k * 128:(k + 1) * 128], ident_b),
                  reads=[xnbuf, b_identb], writes=[ptb] if k == 0 else (), pwrites=() if k == 0 else [ptb], sig=(k == 7))
        if t == 1:
            check("norm2")
        sc.op("act", lambda e, pt=pt, t=t: e.copy(xnT[:, :, t * 128:(t + 1) * 128], pt.rearrange("p (k c) -> p k c", k=8)),
              reads=[ptb], pwrites=[b_xnT])
        check("norm3_%d" % t)
    sc.barrier()
    A.reset(mA)
    check("norm")

    cosT = A.alloc([S], F32)
    sinT = A.alloc([S], F32)
    b_cos, b_sin = sc.buf("cos"), sc.buf("sin")
    sc.dma(cosT, cos_d, b_cos, writes=[b_cos])
    sc.dma(sinT, sin_d, b_sin, writes=[b_sin])

    wf32 = Ring(sc, "wf32", [A.alloc([8, 128], F32) for _ in range(4)])
    wbf = Ring(sc, "wbf", [A.alloc([8, 128], BF16) for _ in range(4)])
    zrow = Ring(sc, "zrow", [A.alloc([S], BF16) for _ in range(3)])
    z32 = Ring(sc, "z32", [A.alloc([S + 4], F32) for _ in range(1)])
    acc = Ring(sc, "acc", [A.alloc([S], F32) for _ in range(1)])
    cw = Ring(sc, "cw", [A.alloc([5], F32) for _ in range(2)])
    t1r = Ring(sc, "t1", [A.alloc([512], F32) for _ in range(2)])
    t2r = Ring(sc, "t2", [A.alloc([512], F32) for _ in range(2)])
    psA = Ring(sc, "psA", [psbank(i) for i in range(6)], PB[0:6])
    evq = [0]

    wmemo = {}

    def load_w(c):
        if c in wmemo:
            return wmemo.pop(c)
        return _load_w(c)

    def prefetch_w(cs):
        for c in cs:
            if c not in wmemo:
                wmemo[c] = _load_w(c)

    def _load_w(c):
        wa, wb = wf32.next()
        sc.dma(wa, wfm_d[c], wb, writes=[wb])
        wba, wbb = wbf.next()
        sc.op("pool", lambda e: e.tensor_copy(wba, wa), reads=[wb], writes=[wbb])
        return wba, wbb

    def proj_group(wba, wbb, g):
        ps, psb = psA.next()
        for k in range(8):
            sc.op("pe", lambda e, k=k: e.matmul(ps, wba[:, k, :], xnT[:, k, g * 512:(g + 1) * 512], start=(k == 0), stop=(k == 7)),
                  reads=[wbb, b_xnT], writes=[psb] if k == 0 else (), pwrites=() if k == 0 else [psb], sig=(k == 7))
        return ps, psb

    def store_row(zr, zrb, row0):
        sc.dma(zT_d[row0:row0 + 128, :], zr, zrb, reads=[zrb])

    def evac_copy(dst, dstb, ps, psb, func=None):
        evq[0] += 1
        if func is not None:
            sc.op("act", lambda e: e.activation(dst, ps, func), reads=[psb], pwrites=[dstb])
        elif evq[0] % 2 == 0:
            sc.op("act", lambda e: e.copy(dst, ps), reads=[psb], pwrites=[dstb])
        else:
            sc.op("dve", lambda e: e.tensor_copy(dst, ps), reads=[psb], pwrites=[dstb])

    def qk_job(c):
        wba, wbb = load_w(c)
        cwa, cwb = cw.next()
        sc.dma(cwa, convw_d[c], cwb, writes=[cwb])
        za, zb = z32.next()
        sc.op("pool", lambda e, za=za: e.memset(za[:, 0:4], 0.0), writes=[zb])
        for g in range(8):
            ps, psb = proj_group(wba, wbb, g)
            evac_copy(za[:, 4 + g * 512:4 + (g + 1) * 512], zb, ps, psb)
        aa, ab = acc.next()
        sc.op("dve", lambda e, aa=aa, za=za, cwa=cwa: e.tensor_scalar(aa, za[:, 4:4 + S], cwa[:, 3:4], cwa[:, 4:5], op0=ALU.mult, op1=ALU.add),
              reads=[zb, cwb], writes=[ab])
        sc.op("dve", lambda e, aa=aa, za=za, cwa=cwa: e.scalar_tensor_tensor(aa, za[:, 3:3 + S], cwa[:, 2:3], aa, op0=ALU.mult, op1=ALU.add),
              reads=[zb, cwb, ab], pwrites=[ab])
        sc.op("dve", lambda e, aa=aa, za=za, cwa=cwa: e.scalar_tensor_tensor(aa, za[:, 2:2 + S], cwa[:, 1:2], aa, op0=ALU.mult, op1=ALU.add),
              reads=[zb, cwb, ab], pwrites=[ab])
        sc.op("dve", lambda e, aa=aa, za=za, cwa=cwa: e.scalar_tensor_tensor(aa, za[:, 1:1 + S], cwa[:, 0:1], aa, op0=ALU.mult, op1=ALU.add),
              reads=[zb, cwb, ab], pwrites=[ab])
        zr, zrb = zrow.next()
        sc.op("act", lambda e, zr=zr, aa=aa: e.activation(zr, aa, AF.Silu), reads=[ab], writes=[zrb])
        store_row(zr, zrb, ZR_QK + c * 128)

    check("qk")
    def rope_job(c_plain, c_rot, row_plain, row_rot):
        wba, wbb = load_w(c_plain)
        wbr, wbrb = load_w(c_rot)
        if row_plain is not None:
            zp, zpb = zrow.next()
            sc.op("pool", lambda e: e.memset(zp[:, 0:1], 0.0), writes=[zpb])
        zr, zrb = zrow.next()
        sc.op("pool", lambda e: e.memset(zr[:, 0:1], 0.0), writes=[zrb])
        for g in range(8):
            sl = slice(g * 512, (g + 1) * 512)
            ps, psb = proj_group(wba, wbb, g)
            ps2, ps2b = proj_group(wbr, wbrb, g)
            if row_plain is not None:
                sc.op("act", lambda e, ps=ps, sl=sl: e.copy(zp[:, sl], ps), reads=[psb], pwrites=[zpb])
            t1, t1b = t1r.next()
            t2, t2b = t2r.next()
            sc.op("dve", lambda e, t1=t1, ps=ps, sl=sl: e.tensor_tensor(t1, ps, cosT[:, sl], op=ALU.mult), reads=[psb, b_cos], writes=[t1b])
            sc.op("dve", lambda e, t2=t2, ps2=ps2, sl=sl: e.tensor_tensor(t2, ps2, sinT[:, sl], op=ALU.mult), reads=[ps2b, b_sin], writes=[t2b])
            sc.op("pool", lambda e, t1=t1, t2=t2, sl=sl: e.tensor_tensor(zr[:, sl], t1, t2, op=ALU.add), reads=[t1b, t2b], pwrites=[zrb])
        if row_plain is not None:
            store_row(zp, zpb, row_plain)
        store_row(zr, zrb, row_rot)

    jobs = [((c,), (lambda c=c: qk_job(c))) for c in range(16)]
    for h in range(8):
        jobs.append(((16 + h, 24 + h), (lambda h=h: rope_job(16 + h, 24 + h, ZR_Q + h * 128, ZR_QR + h * 128))))
    for gI in range(2):
        jobs.append(((36 + gI, 38 + gI), (lambda gI=gI: rope_job(36 + gI, 38 + gI, None, ZR_KSR + gI * 128))))
        jobs.append(((40 + gI, 42 + gI), (lambda gI=gI: rope_job(40 + gI, 42 + gI, None, ZR_KWR + gI * 128))))

    check("rope")
    def plain_job(c, row0, func=None):
        wba, wbb = load_w(c)
        zr, zrb = zrow.next()
        sc.op("pool", lambda e: e.memset(zr[:, 0:1], 0.0), writes=[zrb])
        for g in range(8):
            ps, psb = proj_group(wba, wbb, g)
            evac_copy(zr[:, g * 512:(g + 1) * 512], zrb, ps, psb, func)
        store_row(zr, zrb, row0)

    for i in range(2):
        jobs.append(((32 + i,), (lambda i=i: plain_job(32 + i, ZR_KC + i * 128))))
        jobs.append(((34 + i,), (lambda i=i: plain_job(34 + i, ZR_VC + i * 128))))
    for i in range(8):
        jobs.append(((44 + i,), (lambda i=i: plain_job(44 + i, ZR_GA + i * 128, AF.Sigmoid))))
        jobs.append(((52 + i,), (lambda i=i: plain_job(52 + i, ZR_GB + i * 128, AF.Sigmoid))))
    prefetch_w(jobs[0][0])
    for ji, (cs, fn) in enumerate(jobs):
        if ji + 1 < len(jobs):
            prefetch_w(jobs[ji + 1][0])
        fn()

    check("fm")
    sc.barrier()
    A.reset(mA)
    wt32 = Ring(sc, "wt32", [A.alloc([8, 512], F32) for _ in range(1)])
    wtbf = Ring(sc, "wtbf", [A.alloc([8, 512], BF16) for _ in range(2)])
    ztile = Ring(sc, "ztile", [A.alloc([512], BF16) for _ in range(3)])
    zstile = Ring(sc, "zstile", [A.alloc([32], F32) for _ in range(3)])
    for blk in range(6):
        small = blk == 5
        n = 32 if small else 512
        wa, wb = wt32.next()
        wba, wbb = wtbf.next()
        if small:
            sc.dma(wa[:, :, 0:32], wsm_d, wb, writes=[wb])
        else:
            sc.dma(wa, wtm_d[blk], wb, writes=[wb])
        sc.op("pool", lambda e, wba=wba, wa=wa, n=n: e.tensor_copy(wba[:, :, 0:n], wa[:, :, 0:n]), reads=[wb], writes=[wbb])
        func = AF.Sigmoid if blk in (2, 3) else None
        for t in range(NT):
            ps, psb = psA.next()
            for k in range(8):
                sc.op("pe", lambda e, ps=ps, wba=wba, k=k, t=t, n=n: e.matmul(ps[:, 0:n], xnT[:, k, t * 128:(t + 1) * 128], wba[:, k, 0:n],
                                                                      start=(k == 0), stop=(k == 7)),
                      reads=[wbb, b_xnT], writes=[psb] if k == 0 else (), pwrites=() if k == 0 else [psb], sig=(k == 7))
            if small:
                zt, ztb = zstile.next()
                sc.op("dve", lambda e, zt=zt, ps=ps: e.tensor_copy(zt, ps[:, 0:32]), reads=[psb], writes=[ztb])
                sc.dma(zsm_d[t * 128:(t + 1) * 128, :], zt, ztb, reads=[ztb])
            else:
                zt, ztb = ztile.next()
                evq[0] += 1
                if func is not None or evq[0] % 2 == 0:
                    sc.op("act", lambda e, zt=zt, ps=ps, func=func: e.activation(zt, ps, func if func is not None else AF.Copy),
                          reads=[psb], writes=[ztb])
                else:
                    sc.op("dve", lambda e, zt=zt, ps=ps: e.tensor_copy(zt, ps), reads=[psb], writes=[ztb])
                sc.dma(ztm_d[t * 128:(t + 1) * 128, blk * 512:(blk + 1) * 512], zt, ztb, reads=[ztb])
    sc.barrier()
    A.reset(base_mark)
    check("A")
    phase_mlstm(**locals())
    check("B")
    phase_nsa(**locals())
    check("C")
    phase_mixout(**locals())
    check("D")
    phase_peer(**locals())
    check("E")


def phase_peer(nc, sc, A, psbank, PB, check, L, dump, ident_f, b_ident, ident_b, b_identb, base_mark, **_):
    xhT_d, h1_d, out_d, uT_d, vb_d = L["xhT_d"], L["h1_d"], L["out_d"], L["uT_d"], L["vb_d"]
    xh_rows = xhT_d.rearrange("(c p) t -> p c t", p=128)
    h1_t = h1_d.rearrange("(n p) d -> n p d", p=128)
    out_t = out_d.rearrange("(n p) d -> n p d", p=128)
    NEGBIG = -1e30

    aT = A.alloc([S], BF16)
    bT = A.alloc([S], BF16)
    wT = A.alloc([S], F32)
    b_aT, b_bT, b_wT = sc.buf("aT"), sc.buf("bT"), sc.buf("wT")
    iota_f = A.alloc([128], F32)
    iota_b = A.alloc([128], BF16)
    b_iota, b_iotab = sc.buf("iota"), sc.buf("iotab")
    sc.dma(iota_f, L["iota_d"], b_iota, writes=[b_iota])
    sc.op("dve", lambda e: e.tensor_copy(iota_b, iota_f), reads=[b_iota], writes=[b_iotab])
    gfin = A.alloc([1024], F32)
    b_gfin = sc.buf("gfin")
    sc.dma(gfin, L["gf_d"], b_gfin, writes=[b_gfin])
    m1 = A.mark()

    wqb = A.alloc([8, 1024], BF16)
    mwq = A.mark()
    wqf = A.alloc([8, 1024], F32)
    b_wqf, b_wqb = sc.buf("wqf"), sc.buf("wqb")
    sc.dma(wqf, L["wq_d"], b_wqf, writes=[b_wqf])
    sc.op("pool", lambda e: e.tensor_copy(wqb, wqf), reads=[b_wqf], writes=[b_wqb])
    sc.barrier()
    A.reset(mwq)
    u_ch = L["u_d"].rearrange("(a p) d -> a p d", p=128)
    v_ch = L["v_d"].rearrange("(a p) d -> a p d", p=128)
    uf = Ring(sc, "uf", [A.alloc([1024], F32) for _ in range(3)])
    ubf = Ring(sc, "ubf", [A.alloc([1024], BF16) for _ in range(2)])
    uTs = Ring(sc, "uTs", [A.alloc([8, 128], BF16) for _ in range(2)])
    vf = Ring(sc, "vf", [A.alloc([1024], F32) for _ in range(3)])
    vbf = Ring(sc, "vbf", [A.alloc([1024], BF16) for _ in range(2)])
    ptE0 = psbank(1).bitcast(BF16)
    e0l = {}

    def e0_load(a):
        ua, uab = uf.next()
        sc.dma(ua, u_ch[a], uab, writes=[uab])
        va, vab = vf.next()
        sc.dma(va, v_ch[a], vab, writes=[vab])
        e0l[a] = (ua, uab, va, vab)

    def e0_chunk(a):
        if a + 2 < 128:
            e0_load(a + 2)
        ua, uab, va, vab = e0l.pop(a)
        ub, ubb = ubf.next()
        sc.op("pool", lambda e: e.tensor_copy(ub, ua), reads=[uab], writes=[ubb])
        for k in range(8):
            sc.op("pe", lambda e, k=k: e.transpose(ptE0[:, k * 128:(k + 1) * 128], ub[:, k * 128:(k + 1) * 128], ident_b),
                  reads=[ubb, b_identb], writes=[PB[1]] if k == 0 else (), pwrites=() if k == 0 else [PB[1]], sig=(k == 7))
        us, usb = uTs.next()
        sc.op("act", lambda e: e.copy(us, ptE0.rearrange("p (k c) -> p k c", k=8)), reads=[PB[1]], writes=[usb])
        sc.dma(uT_d[a], us, usb, reads=[usb])
        vb_, vbb = vbf.next()
        sc.op("act", lambda e: e.copy(vb_, va), reads=[vab], writes=[vbb])
        sc.dma(vb_d[a], vb_, vbb, reads=[vbb])

    kbf = A.alloc([256], F32)
    kbb = A.alloc([256], BF16)
    b_kbf, b_kbb = sc.buf("kbf"), sc.buf("kbb")
    sc.dma(kbf, L["kblk_d"], b_kbf, writes=[b_kbf])
    sc.op("pool", lambda e: e.tensor_copy(kbb, kbf), reads=[b_kbf], writes=[b_kbb])
    xg = Ring(sc, "xg", [A.alloc([8, 512], BF16) for _ in range(2)])
    qTr = Ring(sc, "qTr", [A.alloc([8, 512], BF16) for _ in range(1)])
    S12r = Ring(sc, "S12", [A.alloc([8, 256], F32) for _ in range(2)])
    v12r = Ring(sc, "v12", [A.alloc([16, 16], F32) for _ in range(2)])
    i12r = Ring(sc, "i12", [A.alloc([16, 16], U32) for _ in range(2)])
    i12fr = Ring(sc, "i12f", [A.alloc([16, 16], F32) for _ in range(2)])
    tmpr = Ring(sc, "tmpk", [A.alloc([16, 128], F32) for _ in range(1)])
    candr = Ring(sc, "cand", [A.alloc([8, 256], F32) for _ in range(1)])
    tmpcr = Ring(sc, "tmpc", [A.alloc([8, 256], F32) for _ in range(1)])
    svr = Ring(sc, "sv", [A.alloc([8, 16], F32) for _ in range(2)])
    cir = Ring(sc, "ci", [A.alloc([8, 16], U32) for _ in range(2)])
    hlr = Ring(sc, "hl", [A.alloc([2, 128], U32) for _ in range(1)])
    hlfr = Ring(sc, "hlf", [A.alloc([2, 128], F32) for _ in range(1)])
    eqr = Ring(sc, "eq", [A.alloc([8, 256], F32) for _ in range(1)])
    abw = Ring(sc, "abw", [A.alloc([3, 128], F32) for _ in range(2)])
    smx = Ring(sc, "smx", [A.alloc([128], F32) for _ in range(2)])
    ssr = Ring(sc, "ss", [A.alloc([16], F32) for _ in range(2)])
    psq = Ring(sc, "psq", [psbank(i) for i in (0,)], PB[0:1])
    e0_load(0)
    e0_load(1)
    ps12 = [(psbank(i), PB[i]) for i in (2, 3, 4, 5)]
    for tg in range(8):
        xa, xab = xg.next()
        sc.dma(xa, xh_rows[:, :, tg * 512:(tg + 1) * 512], xab, writes=[xab])
        qT, qTb = qTr.next()
        sc.op("pool", lambda e: e.memset(qT[:, 0, 0:2], 0.0), writes=[qTb])
        for h in range(8):
            ps, psb = psq.next()
            for k in range(8):
                sc.op("pe", lambda e, k=k: e.matmul(ps, wqb[:, k, h * 128:(h + 1) * 128], xa[:, k, :], start=(k == 0), stop=(k == 7)),
                      reads=[b_wqb, xab], writes=[psb] if k == 0 else (), pwrites=() if k == 0 else [psb], sig=(k == 7))
            if h % 2 == 0:
                sc.op("act", lambda e: e.copy(qT[:, h, :], ps), reads=[psb], pwrites=[qTb])
            else:
                sc.op("dve", lambda e: e.tensor_copy(qT[:, h, :], ps), reads=[psb], pwrites=[qTb])
        for tt in range(4):
            t = tg * 4 + tt
            tsl = slice(tt * 128, (tt + 1) * 128)
            s12, s12b = S12r.next()
            for h in range(8):
                ps, psb = ps12[h // 2]
                sc.op("pe", lambda e: e.matmul(ps[:, (h % 2) * 256:(h % 2) * 256 + 256], qT[:, h, tsl], kbb, start=True, stop=True),
                      reads=[qTb, b_kbb], writes=[psb] if h % 2 == 0 else (), pwrites=() if h % 2 == 0 else [psb], sig=(h % 2 == 1))
                if h % 2 == 1:
                    sc.op("act", lambda e: e.copy(s12[:, h - 1:h + 1, :], ps.rearrange("p (h c) -> p h c", h=2)), reads=[psb],
                          writes=[s12b] if h == 1 else (), pwrites=() if h == 1 else [s12b])
            for a_ in range(4 * t, 4 * t + 4):
                e0_chunk(a_)
            v12, v12b = v12r.next()
            i12, i12b = i12r.next()
            tm_all, tmb = tmpr.next()
            rows = [s12[:, r // 2, (r % 2) * 128:(r % 2 + 1) * 128] for r in range(16)]
            for r in range(16):
                sc.op("dve", lambda e: e.max(out=v12[:, r, 0:8], in_=rows[r]), reads=[s12b], writes=[v12b] if r == 0 else (), pwrites=() if r == 0 else [v12b])
            for r in range(16):
                sc.op("dve", lambda e: e.max_index(out=i12[:, r, 0:8], in_max=v12[:, r, 0:8], in_values=rows[r]), reads=[s12b, v12b],
                      writes=[i12b] if r == 0 else (), pwrites=() if r == 0 else [i12b])
            for r in range(16):
                sc.op("dve", lambda e: e.match_replace(out=tm_all[:, r, :], in_to_replace=v12[:, r, 0:8], in_values=rows[r], imm_value=NEGBIG),
                      reads=[s12b, v12b], writes=[tmb] if r == 0 else (), pwrites=() if r == 0 else [tmb])
            for r in range(16):
                sc.op("dve", lambda e: e.max(out=v12[:, r, 8:16], in_=tm_all[:, r, :]), reads=[tmb], pwrites=[v12b])
            for r in range(16):
                sc.op("dve", lambda e: e.max_index(out=i12[:, r, 8:16], in_max=v12[:, r, 8:16], in_values=tm_all[:, r, :]), reads=[tmb, v12b], pwrites=[i12b])
            i12f, i12fb = i12fr.next()
            sc.op("dve", lambda e: e.tensor_copy(i12f, i12), reads=[i12b], writes=[i12fb])
            v4 = v12.rearrange("p (h two) i -> p h two i", two=2)
            i4 = i12f.rearrange("p (h two) i -> p h two i", two=2)
            cand, candb = candr.next()
            c4 = cand.rearrange("p h (i j) -> p h i j", i=16)
            sc.op("dve", lambda e: e.tensor_tensor(c4, v4[:, :, 0, :].unsqueeze(3).to_broadcast([128, 8, 16, 16]),
                                                   v4[:, :, 1, :].unsqueeze(2).to_broadcast([128, 8, 16, 16]), op=ALU.add),
                  reads=[v12b], writes=[candb])
            sv, svb = svr.next()
            ci, cib = cir.next()
            tc_all, tcb = tmpcr.next()
            for h in range(8):
                sc.op("dve", lambda e: e.max(out=sv[:, h, 0:8], in_=cand[:, h, :]), reads=[candb], writes=[svb] if h == 0 else (), pwrites=() if h == 0 else [svb])
            for h in range(8):
                sc.op("dve", lambda e: e.max_index(out=ci[:, h, 0:8], in_max=sv[:, h, 0:8], in_values=cand[:, h, :]), reads=[candb, svb],
                      writes=[cib] if h == 0 else (), pwrites=() if h == 0 else [cib])
            for h in range(8):
                sc.op("dve", lambda e: e.match_replace(out=tc_all[:, h, :], in_to_replace=sv[:, h, 0:8], in_values=cand[:, h, :], imm_value=NEGBIG),
                      reads=[candb, svb], writes=[tcb] if h == 0 else (), pwrites=() if h == 0 else [tcb])
            for h in range(8):
                sc.op("dve", lambda e: e.max(out=sv[:, h, 8:16], in_=tc_all[:, h, :]), reads=[tcb], pwrites=[svb])
            for h in range(8):
                sc.op("dve", lambda e: e.max_index(out=ci[:, h, 8:16], in_max=sv[:, h, 8:16], in_values=tc_all[:, h, :]), reads=[tcb, svb], pwrites=[cib])
            hl, hlb = hlr.next()
            cif = ci.rearrange("p h j -> p (h j)")
            sc.op("dve", lambda e: e.tensor_scalar(hl[:, 0, :], cif, 4, None, op0=ALU.logical_shift_right), reads=[cib], writes=[hlb])
            sc.op("dve", lambda e: e.tensor_scalar(hl[:, 1, :], cif, 15, None, op0=ALU.bitwise_and), reads=[cib], pwrites=[hlb])
            hlf, hlfb = hlfr.next()
            sc.op("dve", lambda e: e.tensor_copy(hlf, hl), reads=[hlb], writes=[hlfb])
            ab, abb = abw.next()
            for which in range(2):
                eq, eqb = eqr.next()
                e4 = eq.rearrange("p h (j i) -> p h j i", j=16)
                sel = hlf[:, which, :].rearrange("p (h j) -> p h j", h=8)
                sc.op("dve", lambda e: e.tensor_tensor(e4, sel.unsqueeze(3).to_broadcast([128, 8, 16, 16]),
                                                       iota_f[:, 0:16].unsqueeze(1).unsqueeze(1).to_broadcast([128, 8, 16, 16]), op=ALU.is_equal),
                      reads=[hlfb, b_iota], writes=[eqb])
                sc.op("dve", lambda e: e.tensor_tensor(e4, e4, i4[:, :, which, :].unsqueeze(2).to_broadcast([128, 8, 16, 16]), op=ALU.mult),
                      reads=[eqb, i12fb], writes=[eqb])
                sc.op("dve", lambda e: e.reduce_sum(ab[:, which, :], e4.rearrange("p h j i -> p (h j) i"), axis=AX.X),
                      reads=[eqb], writes=[abb] if which == 0 else (), pwrites=() if which == 0 else [abb])
            sx, sxb = smx.next()
            sx3 = sx.rearrange("p (h j) -> p h j", h=8)
            ss, ssb = ssr.next()
            sc.op("dve", lambda e: e.tensor_tensor(sx3, sv, sv[:, :, 0:1].to_broadcast([128, 8, 16]), op=ALU.subtract), reads=[svb], writes=[sxb])
            sc.op("act", lambda e: e.activation(sx, sx, AF.Exp), reads=[sxb], writes=[sxb])
            sc.op("dve", lambda e: e.reduce_sum(ss[:, 0:8], sx3, axis=AX.X), reads=[sxb], writes=[ssb])
            sc.op("dve", lambda e: e.reciprocal(ss[:, 8:16], ss[:, 0:8]), reads=[ssb], pwrites=[ssb])
            sc.op("dve", lambda e: e.tensor_tensor(ab[:, 2, :].rearrange("p (h j) -> p h j", h=8), sx3,
                                                   ss[:, 8:16].unsqueeze(2).to_broadcast([128, 8, 16]), op=ALU.mult),
                  reads=[sxb, ssb], pwrites=[abb])
            psT, psTb = psbank(6 + (t % 2), 384), PB[6 + (t % 2)]
            for i in range(3):
                sc.op("pe", lambda e, i=i: e.transpose(psT[:, i * 128:(i + 1) * 128], ab[:, i, :], ident_f),
                      reads=[abb, b_ident], writes=[psTb] if i == 0 else (), pwrites=() if i == 0 else [psTb], sig=(i == 2))
            tk = slice(t * 128, (t + 1) * 128)
            sc.op("act", lambda e: e.copy(aT[:, tk], psT[:, 0:128]), reads=[psTb], pwrites=[b_aT])
            sc.op("act", lambda e: e.copy(bT[:, tk], psT[:, 128:256]), reads=[psTb], pwrites=[b_bT])
            sc.op("act", lambda e: e.copy(wT[:, tk], psT[:, 256:384]), reads=[psTb], pwrites=[b_wT])
    dump("d_aT", aT, b_aT, BF16); dump("d_bT", bT, b_bT, BF16); dump("d_wT", wT, b_wT)
    sc.barrier()
    A.reset(m1)
    check("E1")

    TG = 256
    SB = 16
    GT = A.alloc([TG, 128], BF16)
    b_GT = sc.buf("GT")
    A1r = Ring(sc, "A1", [A.alloc([SB, 128], BF16) for _ in range(2)])
    B1r = Ring(sc, "B1", [A.alloc([SB, 128], BF16) for _ in range(2)])
    B1wr = Ring(sc, "B1w", [A.alloc([SB, 128], BF16) for _ in range(2)])
    ur = Ring(sc, "ur", [A.alloc([8, 128], BF16) for _ in range(4)])
    vr = Ring(sc, "vr", [A.alloc([1024], BF16) for _ in range(4)])
    ger = Ring(sc, "ge", [A.alloc([TG], F32) for _ in range(3)])
    WTr = Ring(sc, "WTe", [A.alloc([TG], BF16) for _ in range(3)])
    xgr = Ring(sc, "xge", [A.alloc([8, TG], BF16) for _ in range(2)])
    h1r = Ring(sc, "h1e", [A.alloc([1024], F32) for _ in range(2)])
    h2r = Ring(sc, "h2e", [A.alloc([1024], F32) for _ in range(1)])
    outr = Ring(sc, "oute", [A.alloc([1024], F32) for _ in range(2)])
    rings = {"sq": Ring(sc, "esq", [A.alloc([1024], F32) for _ in range(1)]), "st": Ring(sc, "est", [A.alloc([4], F32) for _ in range(2)])}
    psA = Ring(sc, "psAct", [psbank(i, TG) for i in (4, 5)], PB[4:6])
    psG = Ring(sc, "psG", [psbank(i) for i in (6, 7)], PB[6:8])
    for grp in range(S // TG):
        t0 = grp * TG
        xa, xab = xgr.next()
        sc.dma(xa, xh_rows[:, :, t0:t0 + TG], xab, writes=[xab])
        sc.op("pool", lambda e: e.memset(GT[:, 0, 0:2], 0.0), writes=[b_GT])
        def onehots(sb_):
            ts0 = t0 + sb_ * SB
            a1, a1b = A1r.next()
            b1, b1b = B1r.next()
            b1w, b1wb = B1wr.next()
            io3 = iota_b.unsqueeze(1).to_broadcast([128, SB, 128])
            sc.op("dve", lambda e: e.tensor_tensor(a1, io3, aT[:, ts0:ts0 + SB].unsqueeze(2).to_broadcast([128, SB, 128]), op=ALU.is_equal),
                  reads=[b_iotab, b_aT], writes=[a1b])
            sc.op("dve", lambda e: e.tensor_tensor(b1, io3, bT[:, ts0:ts0 + SB].unsqueeze(2).to_broadcast([128, SB, 128]), op=ALU.is_equal),
                  reads=[b_iotab, b_bT], writes=[b1b])
            sc.op("pool", lambda e: e.tensor_tensor(b1w, b1, wT[:, ts0:ts0 + SB].unsqueeze(2).to_broadcast([128, SB, 128]), op=ALU.mult),
                  reads=[b1b, b_wT], writes=[b1wb])
            return a1, a1b, b1w, b1wb

        def gmm(sb_, a1, a1b, b1w, b1wb):
            for q4 in range(SB // 4):
                pg, pgb = psG.next()
                for i in range(4):
                    tl = q4 * 4 + i
                    sc.op("pe", lambda e: e.matmul(pg[:, i * 128:(i + 1) * 128], b1w[:, tl, :], a1[:, tl, :], start=True, stop=True),
                          reads=[b1wb, a1b], writes=[pgb] if i == 0 else (), pwrites=() if i == 0 else [pgb], sig=(i == 3))
                tg0 = sb_ * SB + q4 * 4
                sc.op("act", lambda e: e.copy(GT[:, tg0:tg0 + 4, :], pg.rearrange("p (t a) -> p t a", t=4)), reads=[pgb], pwrites=[b_GT])

        cur_oh = onehots(0)
        for sb_ in range(TG // SB):
            nxt_oh = onehots(sb_ + 1) if sb_ + 1 < TG // SB else None
            gmm(sb_, *cur_oh)
            cur_oh = nxt_oh
        if grp == 0:
            dump("d_GT0", GT, b_GT, BF16)
        NPF = 3
        wl = {}

        def load_uv(a):
            ua, uab = ur.next()
            sc.dma(ua, uT_d[a], uab, writes=[uab])
            va, vab = vr.next()
            sc.dma(va, vb_d[a], vab, writes=[vab], q="act" if E2_VQ_ACT else "sp")
            wl[a] = (ua, uab, va, vab)

        def act_mm(a):
            ua, uab, _, _ = wl[a]
            pa, pab = psA.next()
            for k in range(8):
                sc.op("pe", lambda e, k=k: e.matmul(pa, ua[:, k, :], xa[:, k, :], start=(k == 0), stop=(k == 7)),
                      reads=[uab, xab], writes=[pab] if k == 0 else (), pwrites=() if k == 0 else [pab], sig=(k == 7))
            return pa, pab

        for a in range(min(NPF, 128)):
            load_uv(a)
        pend_act = [act_mm(0)]
        for a in range(128):
            if a + NPF < 128:
                load_uv(a + NPF)
            if a + 1 < 128:
                pend_act.append(act_mm(a + 1))
            pa, pab = pend_act.pop(0)
            _, _, va, vab = wl.pop(a)
            ge, geb = ger.next()
            sc.op("act", lambda e: e.activation(ge, pa, AF.Gelu), reads=[pab], writes=[geb])
            wt, wtb = WTr.next()
            sc.op("dve", lambda e: e.tensor_tensor(wt, ge, GT[:, :, a], op=ALU.mult), reads=[geb, b_GT], writes=[wtb])
            for i in range(2):
                for half in range(2):
                    bi = i * 2 + half
                    sc.op("pe", lambda e: e.matmul(psbank(bi), wt[:, i * 128:(i + 1) * 128], va[:, half * 512:(half + 1) * 512],
                                                   start=(a == 0), stop=(a == 127)),
                          reads=[wtb, vab], writes=[PB[bi]] if a == 0 else (), pwrites=() if a == 0 else [PB[bi]], sig=(bi == 3))
        for i in range(2):
            t = grp * 2 + i
            h1, h1b = h1r.next()
            sc.dma(h1, h1_t[t], h1b, writes=[h1b])
            h2, h2b = h2r.next()
            for half in range(2):
                bi = i * 2 + half
                sc.op("dve", lambda e: e.tensor_tensor(h2[:, half * 512:(half + 1) * 512], psbank(bi), h1[:, half * 512:(half + 1) * 512], op=ALU.add),
                      reads=[PB[bi], h1b], writes=[h2b] if half == 0 else (), pwrites=() if half == 0 else [h2b])
            if grp == 0 and i == 0:
                dump("d_h2", h2, h2b)
            oo, oob = outr.next()
            rmsnorm_tile(sc, rings, h2, h2b, gfin, b_gfin, oo, oob)
            sc.dma(out_t[t], oo, oob, reads=[oob])
        check("E2_%d" % grp)
    sc.barrier()
    A.reset(base_mark)


def rmsnorm_tile(sc, A_rings, src, srcb, g_bc, b_g, dst, dstb, width=1024):
    sq, sqb = A_rings["sq"].next()
    st, stb = A_rings["st"].next()
    sc.op("act", lambda e: e.activation(sq, src, AF.Square), reads=[srcb], writes=[sqb])
    sc.op("dve", lambda e: e.reduce_sum(st[:, 0:1], sq, axis=AX.X), reads=[sqb], writes=[stb])
    sc.op("dve", lambda e: e.tensor_scalar(st[:, 1:2], st[:, 0:1], 1.0 / width, EPS, op0=ALU.mult, op1=ALU.add), reads=[stb], pwrites=[stb])
    sc.op("act", lambda e: e.activation(st[:, 2:3], st[:, 1:2], AF.Ln), reads=[stb], pwrites=[stb])
    sc.op("act", lambda e: e.activation(st[:, 3:4], st[:, 2:3], AF.Exp, scale=-0.5), reads=[stb], pwrites=[stb])
    sc.op("dve", lambda e: e.scalar_tensor_tensor(dst, src, st[:, 3:4], g_bc, op0=ALU.mult, op1=ALU.mult),
          reads=[srcb, stb, b_g], writes=[dstb])


def phase_mixout(nc, sc, A, psbank, PB, check, L, dump, zT_d, x_d, ident_b, b_identb, base_mark, **_):
    hmT_d, onT_d, h1_d, xhT_d = L["hmT_d"], L["onT_d"], L["h1_d"], L["xhT_d"]
    zt_rows = zT_d.rearrange("(c p) t -> p c t", p=128)
    hm_rows = hmT_d.rearrange("(c p) t -> p c t", p=128)
    on_rows = onT_d.rearrange("(c p) t -> p c t", p=128)
    xh_rows = xhT_d.rearrange("(c p) t -> p c t", p=128)
    W = [(A.alloc([8, 1024], BF16), sc.buf("wsq")) for _ in range(3)]
    mst = A.mark()
    wst = Ring(sc, "wst", [A.alloc([8, 1024], F32) for _ in range(2)])
    for i in range(3):
        wa, wb = wst.next()
        sc.dma(wa, L["wsq_d"][i], wb, writes=[wb])
        sc.op("pool", lambda e: e.tensor_copy(W[i][0], wa), reads=[wb], writes=[W[i][1]])
    sc.barrier()
    A.reset(mst)
    gffn = A.alloc([1024], F32)
    b_gffn = sc.buf("gffn")
    sc.dma(gffn, L["gffn_d"], b_gffn, writes=[b_gffn])
    hin = Ring(sc, "hin", [A.alloc([8, 512], BF16) for _ in range(2)])
    oin = Ring(sc, "oin", [A.alloc([8, 512], BF16) for _ in range(2)])
    gain = Ring(sc, "gain", [A.alloc([8, 512], BF16) for _ in range(2)])
    gbin = Ring(sc, "gbin", [A.alloc([8, 512], BF16) for _ in range(2)])
    mixT = Ring(sc, "mixT", [A.alloc([8, 512], BF16) for _ in range(2)])
    t1r = Ring(sc, "dt1", [A.alloc([512], F32) for _ in range(2)])
    t2r = Ring(sc, "dt2", [A.alloc([512], F32) for _ in range(2)])
    xin = Ring(sc, "dxin", [A.alloc([1024], F32) for _ in range(2)])
    h1r = Ring(sc, "h1", [A.alloc([1024], F32) for _ in range(2)])
    xhb = Ring(sc, "xhb", [A.alloc([1024], BF16) for _ in range(2)])
    xhT = Ring(sc, "xhT", [A.alloc([8, 512], BF16) for _ in range(2)])
    rings = {"sq": Ring(sc, "dsq", [A.alloc([1024], F32) for _ in range(1)]), "st": Ring(sc, "dst", [A.alloc([4], F32) for _ in range(2)])}
    psr = Ring(sc, "psD", [psbank(i) for i in range(6)], PB[0:6])
    pst = Ring(sc, "pstD", [psbank(i).bitcast(BF16) for i in (6, 7)], PB[6:8])
    x_t = x_d.rearrange("(n p) d -> n p d", p=128)
    h1_t = h1_d.rearrange("(n p) d -> n p d", p=128)
    for tg in range(8):
        ts_ = slice(tg * 512, (tg + 1) * 512)
        hi, hib = hin.next()
        sc.dma(hi, hm_rows[:, :, ts_], hib, writes=[hib])
        oi, oib = oin.next()
        sc.dma(oi, on_rows[:, :, ts_], oib, writes=[oib])
        ga, gab = gain.next()
        sc.dma(ga, zt_rows[:, ZR_GA // 128:ZR_GA // 128 + 8, ts_], gab, writes=[gab])
        gb, gbb = gbin.next()
        sc.dma(gb, zt_rows[:, ZR_GB // 128:ZR_GB // 128 + 8, ts_], gbb, writes=[gbb])
        mx, mxb = mixT.next()
        sc.op("pool", lambda e: e.memset(mx[:, 0, 0:2], 0.0), writes=[mxb])
        for dc in range(8):
            psa, psab = psr.next()
            for k in range(8):
                sc.op("pe", lambda e, k=k: e.matmul(psa, W[0][0][:, k, dc * 128:(dc + 1) * 128], hi[:, k, :], start=(k == 0), stop=(k == 7)),
                      reads=[W[0][1], hib], writes=[psab] if k == 0 else (), pwrites=() if k == 0 else [psab], sig=(k == 7))
            psb_, psbb = psr.next()
            for k in range(8):
                sc.op("pe", lambda e, k=k: e.matmul(psb_, W[1][0][:, k, dc * 128:(dc + 1) * 128], oi[:, k, :], start=(k == 0), stop=(k == 7)),
                      reads=[W[1][1], oib], writes=[psbb] if k == 0 else (), pwrites=() if k == 0 else [psbb], sig=(k == 7))
            t1, t1b = t1r.next()
            t2, t2b = t2r.next()
            sc.op("dve", lambda e: e.tensor_tensor(t1, psa, ga[:, dc, :], op=ALU.mult), reads=[psab, gab], writes=[t1b])
            sc.op("dve", lambda e: e.tensor_tensor(t2, psb_, gb[:, dc, :], op=ALU.mult), reads=[psbb, gbb], writes=[t2b])
            sc.op("pool", lambda e: e.tensor_tensor(mx[:, dc, :], t1, t2, op=ALU.add), reads=[t1b, t2b], pwrites=[mxb])
        xt_, xtb = xhT.next()
        sc.op("pool", lambda e: e.memset(xt_[:, 0, 0:2], 0.0), writes=[xtb])
        for tt in range(4):
            t = tg * 4 + tt
            xa, xab = xin.next()
            sc.dma(xa, x_t[t], xab, writes=[xab])
            h1, h1b = h1r.next()
            for half in range(2):
                ps, psb2 = psr.next()
                for k in range(8):
                    sc.op("pe", lambda e, k=k: e.matmul(ps, mx[:, k, tt * 128:(tt + 1) * 128], W[2][0][:, k, half * 512:(half + 1) * 512],
                                                        start=(k == 0), stop=(k == 7)),
                          reads=[W[2][1], mxb], writes=[psb2] if k == 0 else (), pwrites=() if k == 0 else [psb2], sig=(k == 7))
                sc.op("dve", lambda e: e.tensor_tensor(h1[:, half * 512:(half + 1) * 512], ps, xa[:, half * 512:(half + 1) * 512], op=ALU.add),
                      reads=[psb2, xab], writes=[h1b] if half == 0 else (), pwrites=() if half == 0 else [h1b])
            sc.dma(h1_t[t], h1, h1b, reads=[h1b])
            xh, xhbb = xhb.next()
            rmsnorm_tile(sc, rings, h1, h1b, gffn, b_gffn, xh, xhbb)
            pt, ptb = pst.next()
            for k in range(8):
                sc.op("pe", lambda e, k=k: e.transpose(pt[:, k * 128:(k + 1) * 128], xh[:, k * 128:(k + 1) * 128], ident_b),
                      reads=[xhbb, b_identb], writes=[ptb] if k == 0 else (), pwrites=() if k == 0 else [ptb], sig=(k == 7))
            sc.op("act", lambda e: e.copy(xt_[:, :, tt * 128:(tt + 1) * 128], pt.rearrange("p (k c) -> p k c", k=8)), reads=[ptb], pwrites=[xtb])
        sc.dma(xh_rows[:, :, ts_], xt_, xtb, reads=[xtb])
    sc.barrier()
    A.reset(base_mark)


def phase_nsa(nc, sc, A, psbank, PB, check, L, dump, zT_d, ztm_d, zsm_d, ident_b, b_identb, base_mark, **_):
    SC = float(128 ** -0.5)
    NEG = -30000.0
    onT_d = L["onT_d"]
    zt_rows = zT_d.rearrange("(c p) t -> p c t", p=128)
    ztm_t = ztm_d.rearrange("(n p) c -> p n c", p=128)
    onT_rows = onT_d.rearrange("(c p) t -> p c t", p=128)

    sm = A.alloc([NT, 32], F32)
    b_sm = sc.buf("smC")
    sc.dma(sm, zsm_d.rearrange("(n p) c -> p n c", p=128), b_sm, writes=[b_sm])
    gates = A.alloc([NT, 24], F32)
    b_gates = sc.buf("gates")
    sc.op("act", lambda e: e.activation(gates, sm[:, :, 8:32], AF.Sigmoid), reads=[b_sm], writes=[b_gates])

    ovl32 = A.alloc([2, 64], F32)
    b_ovl = sc.buf("ovl")
    sc.dma(ovl32, L["ovl_d"].rearrange("t p j -> p t j"), b_ovl, writes=[b_ovl])
    Eall = A.alloc([S], BF16)
    b_Eall = sc.buf("Eall")
    kcmpT = A.alloc([2, 256], BF16)
    b_kcmpT = sc.buf("kcmpT")
    vcx = A.alloc([2, 2, 193], BF16)
    b_vcx = sc.buf("vcx")
    cmark = A.mark()
    E32 = A.alloc([S], F32)
    b_E32 = sc.buf("E32")
    sc.dma(E32[0:64, :], L["eall_d"], b_E32, writes=[b_E32])
    sc.op("pool", lambda e: e.memset(Eall[64:128, :], 0.0), writes=[b_Eall])
    sc.op("pool", lambda e: e.tensor_copy(Eall[0:64, :], E32[0:64, :]), reads=[b_E32], pwrites=[b_Eall])

    sc.barrier()
    A.reset(cmark)
    c1mark = A.mark()
    sc.op("pool", lambda e: e.memset(kcmpT, 0.0), writes=[b_kcmpT])
    sc.op("pool", lambda e: e.memset(vcx, 0.0), writes=[b_vcx])
    for g in range(2):
        sc.op("pool", lambda e, g=g: e.memset(vcx[:, 0, g, 128:129], 1.0), pwrites=[b_vcx])
        sc.op("pool", lambda e, g=g: e.memset(vcx[0:127, 1, g, 128:129], 1.0), pwrites=[b_vcx])
        sc.op("pool", lambda e, g=g: e.tensor_copy(vcx[:, :, g, 129:193], ovl32), reads=[b_ovl], pwrites=[b_vcx])
    kvc = A.alloc([4, S], BF16)
    b_kvc = sc.buf("kvc")
    sc.dma(kvc, zt_rows[:, ZR_KC // 128:ZR_KC // 128 + 4, :], b_kvc, writes=[b_kvc])
    w1f = A.alloc([32, 128], F32)
    b_w1f = sc.buf("w1f")
    w1b = A.alloc([32, 128], BF16)
    b_w1b = sc.buf("w1b")
    w2f = A.alloc([128], F32)
    w2b = A.alloc([128], BF16)
    b_w2f, b_w2b = sc.buf("w2f"), sc.buf("w2b")
    posf = A.alloc([32], F32)
    posb = A.alloc([32], BF16)
    b_posf, b_posb = sc.buf("posf"), sc.buf("posb")
    cbias = A.alloc([1], F32)
    b_cbias = sc.buf("cbias")
    gT = A.alloc([256], BF16)
    b_gT = sc.buf("gT")
    for kv in range(2):
        sc.dma(w1f, L["cw1_d"][kv], b_w1f, writes=[b_w1f])
        sc.op("pool", lambda e: e.tensor_copy(w1b, w1f), reads=[b_w1f], writes=[b_w1b])
        sc.dma(w2f, L["cw2_d"][kv], b_w2f, writes=[b_w2f])
        sc.op("pool", lambda e: e.tensor_copy(w2b, w2f), reads=[b_w2f], writes=[b_w2b])
        sc.dma(posf, L["cpos_d"][kv], b_posf, writes=[b_posf])
        sc.op("pool", lambda e: e.tensor_copy(posb, posf), reads=[b_posf], writes=[b_posb])
        psb_ = psbank(0, 1)
        for l in range(32):
            sc.op("pe", lambda e, l=l: e.matmul(psb_, w1b[:, l, :], posb[:, l:l + 1], start=(l == 0), stop=(l == 31)),
                  reads=[b_w1b, b_posb], writes=[PB[0]] if l == 0 else (), pwrites=() if l == 0 else [PB[0]], sig=(l == 31))
        sc.op("dve", lambda e: e.tensor_copy(cbias, psb_), reads=[PB[0]], writes=[b_cbias])
        for g in range(2):
            src = kvc[:, kv * 2 + g, :].rearrange("p (n s) -> p n s", s=16)
            psp = psbank(1, 255)
            for l in range(32):
                sc.op("pe", lambda e, l=l, src=src: e.matmul(psp, w1b[:, l, :], src[:, 0:255, l] if l < 16 else src[:, 1:256, l - 16], start=(l == 0), stop=(l == 31)),
                      reads=[b_w1b, b_kvc], writes=[PB[1]] if l == 0 else (), pwrites=() if l == 0 else [PB[1]], sig=(l == 31))
            sc.op("pool", lambda e: e.memset(gT, 0.0), writes=[b_gT])
            sc.op("act", lambda e: e.activation(gT[:, 0:255], psp, AF.Gelu, bias=cbias[:, 0:1]), reads=[PB[1], b_cbias], pwrites=[b_gT])
            if kv == 0:
                pso = psbank(2, 255)
                sc.op("pe", lambda e: e.matmul(pso, w2b, gT[:, 0:255], start=True, stop=True), reads=[b_w2b, b_gT], writes=[PB[2]])
                sc.op("dve", lambda e, g=g: e.tensor_copy(kcmpT[:, g, 0:255], pso), reads=[PB[2]], pwrites=[b_kcmpT])
            else:
                for tt in range(2):
                    nn = 128 if tt == 0 else 127
                    pso = psbank(2 + tt, 128)
                    sc.op("pe", lambda e, tt=tt, nn=nn, pso=pso: e.matmul(pso[0:nn, :], gT[:, tt * 128:tt * 128 + nn], w2b, start=True, stop=True),
                          reads=[b_w2b, b_gT], writes=[PB[2 + tt]])
                    sc.op("dve", lambda e, tt=tt, nn=nn, g=g, pso=pso: e.tensor_copy(vcx[0:nn, tt, g, 0:128], pso[0:nn, :]),
                          reads=[PB[2 + tt]], pwrites=[b_vcx])
    dump("d_kcmpT", kcmpT, b_kcmpT, BF16)
    dump("d_vcx", vcx, b_vcx, BF16)
    sc.barrier()
    A.reset(c1mark)
    check("C1")

    qg = A.alloc([NT, 4, 128], BF16)
    qrg = A.alloc([NT, 4, 128], BF16)
    ksw = A.alloc([2, S], BF16)
    vsx = A.alloc([NT, 129], BF16)
    vwx = A.alloc([NT, 129], BF16)
    onT = A.alloc([4, S], BF16)
    b_qg, b_qrg, b_ksw, b_vsx, b_vwx, b_onT = [sc.buf(n_) for n_ in ("qg", "qrg", "ksw", "vsx", "vwx", "onT")]
    E32r = Ring(sc, "E32r", [A.alloc([512], F32) for _ in range(2)])
    PTr = Ring(sc, "PTr", [A.alloc([512], BF16) for _ in range(4)])
    impr = Ring(sc, "imp", [A.alloc([64], F32) for _ in range(2)])
    m8r = Ring(sc, "m8", [A.alloc([8], F32) for _ in range(2)])
    negr = Ring(sc, "neg", [A.alloc([64], BF16) for _ in range(2)])
    negTr = Ring(sc, "negT", [A.alloc([512], BF16) for _ in range(2)])
    for nT_, nTb_ in negTr.items:
        sc.op("pool", lambda e: e.memset(nT_[64:128, :], 0.0), writes=[nTb_])
    zzr = Ring(sc, "zz", [A.alloc([12], F32) for _ in range(2)])
    cfr = Ring(sc, "cf", [A.alloc([12], F32) for _ in range(2)])
    oacc = Ring(sc, "oacc", [A.alloc([128], F32) for _ in range(8)])
    obf = Ring(sc, "obf", [A.alloc([4, 128], BF16) for _ in range(2)])
    sring = [0]

    SBANKS = (0, 1, 7)

    def s_bank():
        sring[0] += 1
        i = SBANKS[sring[0] % 3]
        return psbank(i), PB[i]

    def pv_accumulate(pt, ptb, vx, vxb, kt_idx, accs, width):
        for hh in range(4):
            bi, co = accs[hh]
            out = psbank(bi, width, co)
            sc.op("pe", lambda e, hh=hh, out=out: e.matmul(out, pt[:, hh * 128:(hh + 1) * 128], vx[:, kt_idx, :], start=False, stop=False,
                                                          skip_group_check=True),
                  reads=[ptb, vxb], pwrites=[PB[bi]], sig=(hh == 3))

    def masked_exp(ps, psb, base, cm, pat, cmp_op):
        ea, eb = E32r.next()
        sc.op("act", lambda e: e.activation(ea, ps, AF.Exp, scale=SC), reads=[psb], writes=[eb])
        pt, ptb = PTr.next()
        sc.op("pool", lambda e: e.affine_select(pt.rearrange("p (h t) -> p h t", h=4), ea.rearrange("p (h t) -> p h t", h=4),
                                               pattern=pat, compare_op=cmp_op, fill=0.0, base=base, channel_multiplier=cm),
              reads=[eb], writes=[ptb])
        return pt, ptb

    def run_branch(kts, s_fn, e_fn, pv_fn):
        LA = 2
        pend = [s_fn(kt) for kt in kts[:LA]]
        for i, kt in enumerate(kts):
            if i + LA < len(kts):
                pend.append(s_fn(kts[i + LA]))
            pt, ptb = e_fn(kt, *pend.pop(0))
            pv_fn(kt, pt, ptb)

    def plain_exp(ps, psb):
        pt, ptb = PTr.next()
        sc.op("act", lambda e: e.activation(pt, ps, AF.Exp, scale=SC), reads=[psb], writes=[ptb])
        return pt, ptb

    for g in range(2):
        for hh in range(4):
            sc.dma(qg[:, :, hh, :], zt_rows[:, ZR_Q // 128 + 4 * g + hh, :].rearrange("p (n t) -> p n t", t=128), b_qg,
                   writes=[b_qg] if hh == 0 else (), pwrites=() if hh == 0 else [b_qg])
            sc.dma(qrg[:, :, hh, :], zt_rows[:, ZR_QR // 128 + 4 * g + hh, :].rearrange("p (n t) -> p n t", t=128), b_qrg,
                   writes=[b_qrg] if hh == 0 else (), pwrites=() if hh == 0 else [b_qrg])
        sc.dma(ksw[:, 0, :], zt_rows[:, ZR_KSR // 128 + g, :], b_ksw, writes=[b_ksw])
        sc.dma(ksw[:, 1, :], zt_rows[:, ZR_KWR // 128 + g, :], b_ksw, pwrites=[b_ksw])
        sc.dma(vsx[:, :, 0:128], ztm_t[:, :, 2048 + g * 128:2048 + (g + 1) * 128], b_vsx, writes=[b_vsx])
        sc.op("pool", lambda e: e.memset(vsx[:, :, 128:129], 1.0), pwrites=[b_vsx])
        sc.dma(vwx[:, :, 0:128], ztm_t[:, :, 2304 + g * 128:2304 + (g + 1) * 128], b_vwx, writes=[b_vwx])
        sc.op("pool", lambda e: e.memset(vwx[:, :, 128:129], 1.0), pwrites=[b_vwx])
        sc.op("pool", lambda e: e.memset(onT[:, 0, 0:2], 0.0), writes=[b_onT])
        def front(qt):
            tq = slice(qt * 128, (qt + 1) * 128)
            qs = qg[:, qt, :, :].rearrange("p h t -> p (h t)")
            qrs = qrg[:, qt, :, :].rearrange("p h t -> p (h t)")
            for bi in (2, 3):
                sc.op("dve", lambda e, bi=bi: e.memset(psbank(bi, 386), 0.0), writes=[PB[bi]])
            accC = [(2, 0), (2, 193), (3, 0), (3, 193)]
            accW = [(4, 0), (4, 129), (5, 0), (5, 129)]
            accS = [(2, 0), (2, 129), (3, 0), (3, 129)]

            def s_cmp(kt):
                ps, psb = s_bank()
                sc.op("pe", lambda e: e.matmul(ps, kcmpT[:, g, kt * 128:(kt + 1) * 128], qs, start=True, stop=True),
                      reads=[b_kcmpT, b_qg], writes=[psb])
                return ps, psb

            run_branch(list(range(2 if qt >= 16 else 1)), s_cmp,
                       lambda kt, ps, psb: masked_exp(ps, psb, base=128 * qt - 31 - 2048 * kt, cm=-16, pat=[[0, 4], [1, 128]], cmp_op=ALU.is_ge),
                       lambda kt, pt, ptb: pv_accumulate(pt, ptb, vcx[:, :, g, :], b_vcx, kt, accC, 193))
            zz, zzb = zzr.next()
            for hh in range(4):
                bi, co = accC[hh]
                sc.op("dve", lambda e, hh=hh, bi=bi, co=co: e.tensor_scalar(zz[:, hh * 3:hh * 3 + 1], psbank(bi, 1, co + 128), 1e-30, None, op0=ALU.max),
                      reads=[PB[bi]], writes=[zzb] if hh == 0 else (), pwrites=() if hh == 0 else [zzb])
            im, imb = impr.next()
            rzc, rzcb = m8r.next()
            sc.op("dve", lambda e: e.reciprocal(rzc[:, 0:4], zz[:, 0:12:3]), reads=[zzb], writes=[rzcb])
            for hh in range(4):
                bi, co = accC[hh]
                Mh = psbank(bi, 64, co + 129)
                if hh == 0:
                    sc.op("dve", lambda e: e.tensor_scalar(im, Mh, rzc[:, 0:1], None, op0=ALU.mult), reads=[PB[bi], rzcb], writes=[imb])
                else:
                    sc.op("dve", lambda e: e.scalar_tensor_tensor(im, Mh, rzc[:, hh:hh + 1], im, op0=ALU.mult, op1=ALU.add),
                          reads=[PB[bi], rzcb, imb], pwrites=[imb])
            sc.op("pool", lambda e: e.memset(im[:, 2 * qt + 1:64], -1e9), reads=[imb], pwrites=[imb])
            sc.op("pool", lambda e: e.memset(im[:, 0:1], 1e9), pwrites=[imb])
            sc.op("pool", lambda e: e.memset(im[:, 2 * qt:2 * qt + 1], 1e9), pwrites=[imb])
            if qt >= 1:
                sc.op("pool", lambda e: e.memset(im[0:64, 2 * qt - 1:2 * qt], 1e9), pwrites=[imb])
            sc.op("pool", lambda e: e.memset(im[64:128, 2 * qt + 1:2 * qt + 2], 1e9), pwrites=[imb])
            m8, m8b = m8r.next()
            sc.op("dve", lambda e: e.max(out=m8, in_=im), reads=[imb], writes=[m8b])
            ng, ngb = negr.next()
            sc.op("dve", lambda e: e.tensor_scalar(ng, im, m8[:, 7:8], NEG, op0=ALU.is_lt, op1=ALU.mult), reads=[imb, m8b], writes=[ngb])
            dump("d_neg_%d_%d" % (g, qt), ng, ngb, BF16)
            psT = psbank(6, 64).bitcast(BF16)
            sc.op("pe", lambda e: e.transpose(psT[0:64, 0:128], ng, ident_b), reads=[ngb, b_identb], writes=[PB[6]])
            nT, nTb = negTr.next()
            sc.op("dve", lambda e: e.tensor_copy(nT[0:64, :].rearrange("p (h t) -> p h t", h=4), psT[0:64, 0:128].unsqueeze(1).to_broadcast([64, 4, 128])),
                  reads=[PB[6]], pwrites=[nTb])
            cf, cfb = cfr.next()
            sc.op("dve", lambda e: e.reciprocal(cf[:, 0:12:3], zz[:, 0:12:3]), reads=[zzb], writes=[cfb])
            sc.op("dve", lambda e: e.tensor_tensor(cf[:, 0:12:3], cf[:, 0:12:3], gates[:, qt, g * 12:g * 12 + 12:3], op=ALU.mult),
                  reads=[cfb, b_gates], pwrites=[cfb])
            oa_list = []
            for hh in range(4):
                bi, co = accC[hh]
                oa, oab = oacc.next()
                oa_list.append((oa, oab))
                sc.op("act", lambda e: e.activation(oa, psbank(bi, 128, co), AF.Copy, scale=cf[:, hh * 3:hh * 3 + 1]),
                      reads=[PB[bi], cfb], writes=[oab])
            return dict(qt=qt, tq=tq, qrs=qrs, zz=zz, zzb=zzb, cf=cf, cfb=cfb, oa_list=oa_list, nT=nT, nTb=nTb, accW=accW, accS=accS)

        def back(st):
            qt, tq, qrs, zz, zzb, cf, cfb, oa_list, nT, nTb, accW, accS = [st[k_] for k_ in (
                'qt', 'tq', 'qrs', 'zz', 'zzb', 'cf', 'cfb', 'oa_list', 'nT', 'nTb', 'accW', 'accS')]
            for bi in (4, 5):
                sc.op("dve", lambda e, bi=bi: e.memset(psbank(bi, 258), 0.0), writes=[PB[bi]])

            def s_win(kt):
                ps, psb = s_bank()
                sc.op("pe", lambda e: e.matmul(ps, ksw[:, 1, kt * 128:(kt + 1) * 128], qrs, start=True, stop=True),
                      reads=[b_ksw, b_qrg], writes=[psb])
                return ps, psb

            def e_win(kt, ps, psb):
                if kt == qt:
                    return masked_exp(ps, psb, base=0, cm=-1, pat=[[0, 4], [1, 128]], cmp_op=ALU.is_ge)
                if kt == qt - 4:
                    return masked_exp(ps, psb, base=0, cm=1, pat=[[0, 4], [-1, 128]], cmp_op=ALU.is_gt)
                return plain_exp(ps, psb)

            run_branch(list(range(max(0, qt - 4), qt + 1)), s_win, e_win,
                       lambda kt, pt, ptb: pv_accumulate(pt, ptb, vwx, b_vwx, kt, accW, 129))
            for bi in (2, 3):
                sc.op("dve", lambda e, bi=bi: e.memset(psbank(bi, 258), 0.0), writes=[PB[bi]])

            def s_slc(kt):
                ps, psb = s_bank()
                sc.op("pe", lambda e: e.matmul(ps, ksw[:, 0, kt * 128:(kt + 1) * 128], qrs, start=True, stop=False),
                      reads=[b_ksw, b_qrg], writes=[psb], sig=False)
                sc.op("pe", lambda e: e.matmul(ps, Eall[:, kt * 128:(kt + 1) * 128], nT, start=False, stop=True),
                      reads=[b_Eall, nTb], pwrites=[psb])
                return ps, psb

            run_branch(list(range(qt + 1)), s_slc,
                       lambda kt, ps, psb: (masked_exp(ps, psb, base=0, cm=-1, pat=[[0, 4], [1, 128]], cmp_op=ALU.is_ge)
                                            if kt == qt else plain_exp(ps, psb)),
                       lambda kt, pt, ptb: pv_accumulate(pt, ptb, vsx, b_vsx, kt, accS, 129))
            for hh in range(4):
                bi, co = accS[hh]
                sc.op("dve", lambda e: e.tensor_scalar(zz[:, hh * 3 + 1:hh * 3 + 2], psbank(bi, 1, co + 128), 1e-30, None, op0=ALU.max),
                      reads=[PB[bi]], pwrites=[zzb])
                bi, co = accW[hh]
                sc.op("dve", lambda e: e.tensor_scalar(zz[:, hh * 3 + 2:hh * 3 + 3], psbank(bi, 1, co + 128), 1e-30, None, op0=ALU.max),
                      reads=[PB[bi]], pwrites=[zzb])
            for br in (1, 2):
                sc.op("dve", lambda e: e.reciprocal(cf[:, br:12:3], zz[:, br:12:3]), reads=[zzb, cfb], pwrites=[cfb])
                sc.op("dve", lambda e: e.tensor_tensor(cf[:, br:12:3], cf[:, br:12:3], gates[:, qt, g * 12 + br:g * 12 + 12:3], op=ALU.mult),
                      reads=[cfb, b_gates], pwrites=[cfb])
            ob, obb = obf.next()
            for hh in range(4):
                oa, oab = oa_list[hh]
                bs, cs = accS[hh]
                bw, cw_ = accW[hh]
                sc.op("dve", lambda e: e.scalar_tensor_tensor(oa, psbank(bs, 128, cs), cf[:, hh * 3 + 1:hh * 3 + 2], oa, op0=ALU.mult, op1=ALU.add),
                      reads=[PB[bs], cfb, oab], writes=[oab])
                sc.op("dve", lambda e: e.scalar_tensor_tensor(ob[:, hh, :], psbank(bw, 128, cw_), cf[:, hh * 3 + 2:hh * 3 + 3], oa, op0=ALU.mult, op1=ALU.add),
                      reads=[PB[bw], cfb, oab], writes=[obb] if hh == 0 else (), pwrites=() if hh == 0 else [obb])
            psO = psbank(6).bitcast(BF16)[:, 256:768]
            for hh in range(4):
                sc.op("pe", lambda e: e.transpose(psO[:, hh * 128:(hh + 1) * 128], ob[:, hh, :], ident_b),
                      reads=[obb, b_identb], writes=[PB[6]] if hh == 0 else (), pwrites=() if hh == 0 else [PB[6]], sig=(hh == 3))
            sc.op("act", lambda e: e.copy(onT[:, :, tq], psO.rearrange("p (h t) -> p h t", h=4)), reads=[PB[6]], pwrites=[b_onT])

        st_cur = front(0)
        for qt in range(NT):
            st_nxt = front(qt + 1) if qt + 1 < NT else None
            back(st_cur)
            st_cur = st_nxt
        sc.dma(onT_rows[:, 4 * g:4 * g + 4, :], onT, b_onT, reads=[b_onT])
    sc.barrier()
    A.reset(base_mark)


def phase_mlstm(nc, sc, A, psbank, PB, check, L, dump, zT_d, ztm_d, zsm_d, hmT_d, tri_f, b_tri, ones_f, b_ones,
                ident_b, b_identb, mbias_d, mnormg_d, base_mark, **_):
    LN16 = float(np.log(16.0))
    sm = A.alloc([NT, 32], F32)
    b_sm = sc.buf("sm")
    sc.dma(sm, zsm_d.rearrange("(n p) c -> p n c", p=128), b_sm, writes=[b_sm])
    mbias = A.alloc([8], F32)
    b_mbias = sc.buf("mbias")
    sc.dma(mbias, mbias_d, b_mbias, writes=[b_mbias])
    fpre = A.alloc([NT, 4], F32)
    ipre = A.alloc([NT, 4], F32)
    b_fpre, b_ipre = sc.buf("fpre"), sc.buf("ipre")
    for h in range(4):
        sc.op("dve", lambda e, h=h: e.tensor_scalar(fpre[:, :, h], sm[:, :, 4 + h], mbias[:, 4 + h:5 + h], None, op0=ALU.add),
              reads=[b_sm, b_mbias], pwrites=[b_fpre])
        sc.op("dve", lambda e, h=h: e.tensor_scalar(ipre[:, :, h], sm[:, :, h], mbias[:, h:h + 1], None, op0=ALU.add),
              reads=[b_sm, b_mbias], pwrites=[b_ipre])
    fl = fpre.rearrange("p n h -> p (n h)")
    il = ipre.rearrange("p n h -> p (n h)")
    lf = A.alloc([128], F32)
    b_lf = sc.buf("lf")
    sc.op("act", lambda e: e.activation(lf, fl, AF.Exp, scale=-1.0), reads=[b_fpre], writes=[b_lf])
    sc.op("act", lambda e: e.activation(lf, lf, AF.Ln, bias=1.0), reads=[b_lf], writes=[b_lf])
    sc.op("dve", lambda e: e.tensor_scalar(lf, lf, -1.0, None, op0=ALU.mult), reads=[b_lf], writes=[b_lf])
    bcs = A.alloc([128], F32)
    ebl = A.alloc([128], F32)
    ik = A.alloc([128], F32)
    eks = A.alloc([128], F32)
    ebL = A.alloc([128], F32)
    b_bcs, b_ebl, b_ik, b_eks, b_ebL = [sc.buf(n_) for n_ in ("bcs", "ebl", "ik", "eks", "ebL")]
    ps0 = psbank(0, 128)
    sc.op("pe", lambda e: e.matmul(ps0, tri_f, lf, start=True, stop=True), reads=[b_tri, b_lf], writes=[PB[0]])
    sc.op("dve", lambda e: e.tensor_copy(bcs, ps0), reads=[PB[0]], writes=[b_bcs])
    sc.op("act", lambda e: e.activation(ebl, ps0, AF.Exp), reads=[PB[0]], writes=[b_ebl])
    ps1 = psbank(1, 128)
    sc.op("pe", lambda e: e.matmul(ps1, ones_f, lf, start=True, stop=True), reads=[b_ones, b_lf], writes=[PB[1]])
    sc.op("act", lambda e: e.activation(ebL, ps1, AF.Exp), reads=[PB[1]], writes=[b_ebL])
    sc.op("dve", lambda e: e.tensor_tensor(ik, il, bcs, op=ALU.subtract), reads=[b_ipre, b_bcs], writes=[b_ik])
    sc.op("dve", lambda e: e.tensor_scalar(ik, ik, -LN16, None, op0=ALU.add), reads=[b_ik], writes=[b_ik])
    sc.op("act", lambda e: e.activation(eks, ik, AF.Exp), reads=[b_ik], writes=[b_eks])
    mnormg = A.alloc([1024], F32)
    b_mnormg = sc.buf("mnormg")
    sc.dma(mnormg, mnormg_d, b_mnormg, writes=[b_mnormg])
    dump("d_lf", lf, b_lf); dump("d_bcs", bcs, b_bcs); dump("d_ebl", ebl, b_ebl); dump("d_ik", ik, b_ik)
    dump("d_eks", eks, b_eks); dump("d_ebL", ebL, b_ebL)
    check("Bgates")

    qkT = Ring(sc, "qkT", [A.alloc([4, S], BF16) for _ in range(2)])
    vh = Ring(sc, "vh", [A.alloc([NT, 257], BF16) for _ in range(2)])
    soh = Ring(sc, "soh", [A.alloc([NT, 256], BF16) for _ in range(2)])
    hmT = Ring(sc, "hmT", [A.alloc([2, S], BF16) for _ in range(2)])
    Cstate = Ring(sc, "Cst", [(A.alloc([2, 257], F32), A.alloc([2, 257], BF16), sc.buf("Cf"), sc.buf("Cb")) for _ in range(2)])
    DT = Ring(sc, "DT", [A.alloc([128], F32) for _ in range(2)])
    DTm = Ring(sc, "DTm", [A.alloc([128], F32) for _ in range(2)])
    WT = Ring(sc, "WT", [A.alloc([128], BF16) for _ in range(4)])
    k2 = Ring(sc, "k2", [A.alloc([256], BF16) for _ in range(4)])
    T1 = Ring(sc, "T1", [A.alloc([257], F32) for _ in range(2)])
    NUM = Ring(sc, "NUM", [A.alloc([257], F32) for _ in range(2)])
    hh = Ring(sc, "hh", [A.alloc([256], F32) for _ in range(2)])
    hfin = Ring(sc, "hfin", [A.alloc([256], BF16) for _ in range(2)])
    st8 = Ring(sc, "st8", [A.alloc([8], F32) for _ in range(4)])
    ctmp = Ring(sc, "ctmp", [A.alloc([257], F32) for _ in range(2)])
    zt_rows = zT_d.rearrange("(c p) t -> p c t", p=128)
    ztm_t = ztm_d.rearrange("(n p) c -> p n c", p=128)
    hmT_rows = hmT_d.rearrange("(c p) t -> p c t", p=128)
    def head_setup(h):
        qk, qkb = qkT.next()
        (Cf, Cb, b_Cf, b_Cb), _unused = Cstate.next()
        sc.dma(qk[:, 0:2, :], zt_rows[:, 2 * h:2 * h + 2, :], qkb, writes=[qkb])
        sc.dma(qk[:, 2:4, :], zt_rows[:, 8 + 2 * h:8 + 2 * h + 2, :], qkb, pwrites=[qkb])
        v, vb = vh.next()
        sc.dma(v[:, :, 0:256], ztm_t[:, :, h * 256:(h + 1) * 256], vb, writes=[vb])
        sc.op("pool", lambda e, v=v: e.memset(v[:, :, 256:257], 1.0), pwrites=[vb])
        so, sob = soh.next()
        sc.dma(so, ztm_t[:, :, 1024 + h * 256:1024 + (h + 1) * 256], sob, writes=[sob])
        hT, hTb = hmT.next()
        sc.op("pool", lambda e, hT=hT: e.memset(hT[:, 0, 0:2], 0.0), writes=[hTb])
        sc.op("pool", lambda e: e.memset(Cf, 0.0), writes=[b_Cf])
        sc.op("pool", lambda e: e.memset(Cb, 0.0), writes=[b_Cb])
        def stage1(n):
            col = n * 4 + h
            tk = slice(n * 128, (n + 1) * 128)
            psB = psbank(0, 128)
            sc.op("pe", lambda e, col=col: e.matmul(psB, lf[:, col:col + 1].to_broadcast([128, 128]), tri_f, start=True, stop=True),
                  reads=[b_lf, b_tri], writes=[PB[0]])
            dt, dtb = DT.next()
            sc.op("act", lambda e, dt=dt, col=col: e.activation(dt, psB, AF.Exp, bias=ik[:, col:col + 1]), reads=[PB[0], b_ik], writes=[dtb])
            dm, dmb = DTm.next()
            sc.op("pool", lambda e, dm=dm, dt=dt: e.tensor_tensor(dm, dt, tri_f, op=ALU.mult), reads=[dtb, b_tri], writes=[dmb])
            psS = psbank(1, 128)
            for c in range(2):
                sc.op("pe", lambda e, c=c, qk=qk, tk=tk: e.matmul(psS, qk[:, 2 + c, tk], qk[:, c, tk], start=(c == 0), stop=(c == 1)),
                      reads=[qkb], writes=[PB[1]] if c == 0 else (), pwrites=() if c == 0 else [PB[1]], sig=(c == 1))
            wt, wtb = WT.next()
            sc.op("dve", lambda e, wt=wt, dm=dm: e.tensor_tensor(wt, psS, dm, op=ALU.mult), reads=[PB[1], dmb], writes=[wtb])
            psK = psbank(2, 128).bitcast(BF16)
            for c in range(2):
                sc.op("pe", lambda e, c=c, qk=qk, tk=tk: e.transpose(psK[:, c * 128:(c + 1) * 128], qk[:, 2 + c, tk], ident_b),
                      reads=[qkb, b_identb], writes=[PB[2]] if c == 0 else (), pwrites=() if c == 0 else [PB[2]], sig=(c == 1))
            kk, kkb = k2.next()
            sc.op("act", lambda e, kk=kk, col=col: e.activation(kk, psK, AF.Copy, scale=eks[:, col:col + 1]), reads=[PB[2], b_eks], writes=[kkb])
            return wt, wtb, kk, kkb

        def stage2(n, wt, wtb, kk, kkb):
            col = n * 4 + h
            tk = slice(n * 128, (n + 1) * 128)
            psP1 = psbank(3, 257)
            for c in range(2):
                sc.op("pe", lambda e, c=c, qk=qk, tk=tk: e.matmul(psP1, qk[:, c, tk], Cb[:, c, :], start=(c == 0), stop=(c == 1)),
                      reads=[qkb, b_Cb], writes=[PB[3]] if c == 0 else (), pwrites=() if c == 0 else [PB[3]], sig=(c == 1))
            psP2 = psbank(4, 257)
            sc.op("pe", lambda e, wt=wt, v=v, n=n: e.matmul(psP2, wt, v[:, n, :], start=True, stop=True), reads=[wtb, vb], writes=[PB[4]])
            t1, t1b = T1.next()
            sc.op("act", lambda e, t1=t1, col=col: e.activation(t1, psP1, AF.Copy, scale=ebl[:, col:col + 1]), reads=[PB[3], b_ebl], writes=[t1b])
            nm, nmb = NUM.next()
            sc.op("dve", lambda e, nm=nm, t1=t1: e.tensor_tensor(nm, t1, psP2, op=ALU.add), reads=[t1b, PB[4]], writes=[nmb])
            s8, s8b = st8.next()
            sc.op("dve", lambda e, s8=s8, nm=nm: e.tensor_scalar(s8[:, 6:7], nm[:, 256:257], -1.0, None, op0=ALU.mult),
                  reads=[nmb], writes=[s8b])
            sc.op("dve", lambda e, s8=s8, nm=nm: e.scalar_tensor_tensor(s8[:, 0:1], nm[:, 256:257], 1.0, s8[:, 6:7], op0=ALU.max, op1=ALU.max),
                  reads=[nmb, s8b], pwrites=[s8b])
            sc.op("dve", lambda e, s8=s8: e.reciprocal(s8[:, 1:2], s8[:, 0:1]), reads=[s8b], pwrites=[s8b])
            hx, hxb = hh.next()
            sc.op("dve", lambda e, hx=hx, nm=nm, s8=s8: e.tensor_scalar(hx, nm[:, 0:256], s8[:, 1:2], None, op0=ALU.mult), reads=[nmb, s8b], writes=[hxb])
            hq, hqb = t1[:, 0:256], t1b
            sc.op("act", lambda e, hq=hq, hx=hx: e.activation(hq, hx, AF.Square), reads=[hxb, nmb], writes=[hqb])
            sc.op("dve", lambda e, s8=s8, hq=hq: e.reduce_sum(s8[:, 2:3], hq, axis=AX.X), reads=[hqb], pwrites=[s8b])
            sc.op("dve", lambda e, s8=s8: e.tensor_scalar(s8[:, 3:4], s8[:, 2:3], 1.0 / 256, EPS, op0=ALU.mult, op1=ALU.add), reads=[s8b], pwrites=[s8b])
            sc.op("act", lambda e, s8=s8: e.activation(s8[:, 4:5], s8[:, 3:4], AF.Ln), reads=[s8b], pwrites=[s8b])
            sc.op("act", lambda e, s8=s8: e.activation(s8[:, 5:6], s8[:, 4:5], AF.Exp, scale=-0.5), reads=[s8b], pwrites=[s8b])
            sc.op("dve", lambda e, hx=hx, s8=s8, h=h: e.scalar_tensor_tensor(hx, hx, s8[:, 5:6], mnormg[:, h * 256:(h + 1) * 256], op0=ALU.mult, op1=ALU.mult),
                  reads=[hxb, s8b, b_mnormg], writes=[hxb])
            hf, hfb = hfin.next()
            sc.op("pool", lambda e, hf=hf, hx=hx, so=so, n=n: e.tensor_tensor(hf, hx, so[:, n, :], op=ALU.mult), reads=[hxb, sob], writes=[hfb])
            psH = psbank(5, 128).bitcast(BF16)
            for c in range(2):
                sc.op("pe", lambda e, c=c, hf=hf: e.transpose(psH[:, c * 128:(c + 1) * 128], hf[:, c * 128:(c + 1) * 128], ident_b),
                      reads=[hfb, b_identb], writes=[PB[5]] if c == 0 else (), pwrites=() if c == 0 else [PB[5]], sig=(c == 1))
            sc.op("act", lambda e, hT=hT, tk=tk: e.copy(hT[:, :, tk], psH.rearrange("p (c t) -> p c t", c=2)), reads=[PB[5]], pwrites=[hTb])
            for c in range(2):
                psKV = psbank(6 + c, 257)
                sc.op("pe", lambda e, c=c, kk=kk, v=v, n=n, psKV=psKV: e.matmul(psKV, kk[:, c * 128:(c + 1) * 128], v[:, n, :], start=True, stop=True),
                      reads=[kkb, vb], writes=[PB[6 + c]])
                ct, ctb = ctmp.next()
                sc.op("dve", lambda e, c=c, ct=ct, psKV=psKV: e.tensor_tensor(ct, Cf[:, c, :], psKV, op=ALU.add), reads=[b_Cf, PB[6 + c]], writes=[ctb])
                sc.op("dve", lambda e, c=c, ct=ct, col=col: e.tensor_scalar(Cf[:, c, :], ct, ebL[:, col:col + 1], None, op0=ALU.mult),
                      reads=[ctb, b_ebL], pwrites=[b_Cf])
                sc.op("act", lambda e, c=c, ct=ct, col=col: e.activation(Cb[:, c, :], ct, AF.Copy, scale=ebL[:, col:col + 1]),
                      reads=[ctb, b_ebL], pwrites=[b_Cb])

        def finish():
            sc.dma(hmT_rows[:, 2 * h:2 * h + 2, :], hT, hTb, reads=[hTb])
        return stage1, stage2, finish

    for hp in range(2):
        H = [head_setup(2 * hp), head_setup(2 * hp + 1)]
        cur = [H[i][0](0) for i in range(2)]
        for n in range(NT):
            nxt_ = [H[i][0](n + 1) if n + 1 < NT else None for i in range(2)]
            for i in range(2):
                H[i][1](n, *cur[i])
            cur = nxt_
        for i in range(2):
            H[i][2]()
    sc.barrier()
    A.reset(base_mark)


def _finish(nc, sc, es):
    sc.barrier()

    with nc.Block() as block:
        @block.sync
        def _(e):
            sc.replay("sp", e)

        @block.tensor
        def _(e):
            sc.replay("pe", e)

        @block.scalar
        def _(e):
            sc.replay("act", e)

        @block.vector
        def _(e):
            sc.replay("dve", e)

        @block.gpsimd
        def _(e):
            sc.replay("pool", e)
    es.close()


def rope_tables_T():
    inv = (np.float32(10000.0) ** (-(np.arange(0, 128, 2, dtype=np.float32)) / np.float32(128))).astype(np.float32)
    ang = (np.arange(S, dtype=np.float32)[:, None] * inv[None, :]).astype(np.float32)
    ang = np.concatenate([ang, ang], axis=-1)
    cosT = np.cos(ang).astype(np.float32).T.copy()
    sinT = np.sin(ang).astype(np.float32).T.copy()
    sinT[:64, :] *= -1.0
    return np.ascontiguousarray(cosT), np.ascontiguousarray(sinT)


def _overlap_const():
    n = np.arange(256)[:, None]
    j = np.arange(64)[None, :]
    ov = ((16 * n < 64 * j + 64) & (16 * n + 32 > 64 * j) & (n < 255)).astype(np.float32)
    return np.ascontiguousarray(ov.reshape(2, 128, 64))


def _eall_const():
    j = np.arange(64)[:, None]
    p = np.arange(S)[None, :]
    return np.ascontiguousarray((p // 64 == j).astype(np.float32))


def _kblk(k1, k2):
    kb = np.zeros((128, 256), np.float32)
    kb[0:64, 0:128] = k1.T
    kb[64:128, 128:256] = k2.T
    return kb


def prep_shared(inputs):
    f = np.float32
    w_in = np.asarray(inputs["w_in"][0], dtype=f)

    def kp_layout(w):
        return np.ascontiguousarray(w.reshape(8, 128, w.shape[1]).transpose(1, 0, 2))

    wfm = w_in[:, fm_col_index()]
    w_fm = np.ascontiguousarray(kp_layout(wfm).reshape(128, 8, 60, 128).transpose(2, 0, 1, 3))
    wtm = w_in[:, tm_col_index()]
    w_tm = np.ascontiguousarray(kp_layout(wtm).reshape(128, 8, 5, 512).transpose(2, 0, 1, 3))
    w_sm = kp_layout(w_in[:, sm_col_index()])
    cosT, sinT = rope_tables_T()
    conv_w = np.asarray(inputs["m_conv_w"][0], dtype=f)
    conv_b = np.asarray(inputs["m_conv_b"][0], dtype=f)
    cw = np.concatenate([conv_w.T, conv_b[:, None]], axis=1)
    shared = {
        "g_mix": np.ascontiguousarray(np.broadcast_to(np.asarray(inputs["ln_mix_g"][0], dtype=f)[None, :], (128, D))),
        "w_fm": w_fm, "w_tm": w_tm, "w_sm": w_sm, "cosT": cosT, "sinT": sinT,
        "conv_w": np.ascontiguousarray(cw.reshape(16, 128, 5)),
        "ident": np.eye(128, dtype=f),
        "tri": np.triu(np.ones((128, 128), dtype=f)),
        "ovl": _overlap_const(), "eall": _eall_const(),
        "peer_u": np.ascontiguousarray(np.asarray(inputs["peer_u"][0], dtype=f)),
        "peer_v": np.ascontiguousarray(np.asarray(inputs["peer_v"][0], dtype=f)),
        "wq": kp_layout(np.asarray(inputs["peer_wq"][0], dtype=f)),
        "kblk": _kblk(np.asarray(inputs["peer_k1"][0], dtype=f), np.asarray(inputs["peer_k2"][0], dtype=f)),
        "iota": np.ascontiguousarray(np.broadcast_to(np.arange(128, dtype=f)[None, :], (128, 128))),
        "g_fin": np.ascontiguousarray(np.broadcast_to(np.asarray(inputs["ln_f_g"], dtype=f)[None, :], (128, D))),
        "wsq": np.ascontiguousarray(np.stack([kp_layout(np.asarray(inputs[k][0], dtype=f)) for k in ("w_m_out", "w_n_out", "w_out")])),
        "g_ffn": np.ascontiguousarray(np.broadcast_to(np.asarray(inputs["ln_ffn_g"][0], dtype=f)[None, :], (128, D))),
        "cw1": np.ascontiguousarray(np.stack([np.asarray(inputs[k][0], dtype=f).reshape(32, 128, 128).transpose(1, 0, 2)
                                              for k in ("cmp_k_w1", "cmp_v_w1")])),
        "cw2": np.ascontiguousarray(np.stack([np.asarray(inputs[k][0], dtype=f) for k in ("cmp_k_w2", "cmp_v_w2")])),
        "cpos": np.ascontiguousarray(np.stack([np.asarray(inputs[k][0], dtype=f).T for k in ("cmp_k_pos", "cmp_v_pos")])),
        "m_bias": np.ascontiguousarray(np.broadcast_to(np.concatenate([np.asarray(inputs["m_i_bias"][0], dtype=f),
                                                                       np.asarray(inputs["m_f_bias"][0], dtype=f)])[None, :], (128, 8))),
        "m_norm_g": np.ascontiguousarray(np.broadcast_to(np.asarray(inputs["m_norm_g"][0], dtype=f)[None, :], (128, 1024))),
    }
    return shared


def kernel(**inputs):
    shared = prep_shared(inputs)
    x = np.asarray(inputs["x"], dtype=np.float32)
    nc = build()
    in_maps = []
    for c in range(N_CORES):
        m = dict(shared)
        m["x"] = np.ascontiguousarray(x[c])
        in_maps.append(m)
    res = run_bass_kernel_spmd(nc, in_maps, core_ids=list(range(N_CORES)))
    return np.stack([np.asarray(r["out"], dtype=np.float32) for r in res.results], axis=0)
```

```python
from contextlib import ExitStack
import numpy as np
import ml_dtypes
import concourse.bass as bass
import concourse.mybir as mybir
from concourse.bass_utils import run_bass_kernel_spmd

F32 = mybir.dt.float32
BF16 = mybir.dt.bfloat16
U32 = mybir.dt.uint32
ALU = mybir.AluOpType
AF = mybir.ActivationFunctionType
AX = mybir.AxisListType

S = 4096
D = 1024
NT = 32
EPS = 1e-6
N_CORES = 8
E2_VQ_ACT = False
SAME_ENGINE_WAITS = True


class Buf:
    __slots__ = ("name", "w", "r", "dsem", "dcnt", "excl")

    def __init__(self, name, excl=False):
        self.name = name
        self.excl = excl
        self.w = {}
        self.r = {}
        self.dsem = None
        self.dcnt = 0


class _Rec:
    def __init__(self):
        self.call = None

    def __getattr__(self, name):
        def f(*a, **k):
            self.call = (name, a, k)
            return self
        return f


def _bind(fn):
    rec = _Rec()
    fn(rec)
    name, a, k = rec.call

    def run(e):
        try:
            return getattr(e, name)(*a, **k)
        except Exception:
            print("FAILED OP", name, [getattr(x, "shape", x) for x in a], {kk: getattr(v, "shape", v) for kk, v in k.items()})
            raise
    return run


class Sched:
    ENG = ("pe", "act", "dve", "pool", "sp")

    def __init__(self, nc, es):
        self.nc, self.es = nc, es
        self.q = {e: [] for e in self.ENG}
        self.sems = []
        self.ecnt = {e: 0 for e in self.ENG}
        self.seen = {e: {} for e in self.ENG}
        self.pending = {e: [] for e in self.ENG}
        self.esem = {}
        for e in ("pe", "act", "dve", "pool"):
            self.esem[e] = self.new_sem("s_" + e)
        self.dbufs = []
        self.sem_pool = []
        self.nbuf = 0

    def new_sem(self, name):
        h = self.es.enter_context(self.nc.semaphore(name))
        self.sems.append(h)
        return len(self.sems) - 1

    def buf(self, name="b"):
        self.nbuf += 1
        return Buf("%s%d" % (name, self.nbuf))

    def _waits(self, eng, reads, writes, pwrites):
        need = {}

        def add(evs):
            for s, v in evs.items():
                if need.get(s, 0) < v:
                    need[s] = v

        for b in reads:
            add(b.w)
            if b.excl:
                add(b.r)
        for b in writes:
            add(b.w)
            add(b.r)
        for b in pwrites:
            add(b.w)
            add(b.r)
        out = []
        seen = self.seen[eng]
        own = self.esem.get(eng)
        for s, v in need.items():
            if s == own and not SAME_ENGINE_WAITS:
                continue
            if seen.get(s, 0) < v:
                seen[s] = v
                out.append((s, v))
        return out

    @staticmethod
    def _commit(ev, reads, writes, pwrites):
        s, v = ev
        for b in writes:
            b.w = {s: v}
            b.r = {}
        for b in pwrites:
            if b.w.get(s, 0) < v:
                b.w[s] = v
        for b in reads:
            if b.r.get(s, 0) < v:
                b.r[s] = v

    def op(self, eng, fn, reads=(), writes=(), pwrites=(), sig=True):
        fn = _bind(fn)
        waits = self._waits(eng, reads, writes, pwrites)
        if not sig:
            self.q[eng].append((waits, fn, None))
            self.pending[eng].append((reads, writes, pwrites))
            return
        self.ecnt[eng] += 1
        ev = (self.esem[eng], self.ecnt[eng])
        self.q[eng].append((waits, fn, (ev[0], 1)))
        for (r, w, pw) in self.pending[eng]:
            self._commit(ev, r, w, pw)
        self.pending[eng] = []
        self._commit(ev, reads, writes, pwrites)

    def dma(self, out_ap, in_ap, sb, reads=(), writes=(), pwrites=(), q="sp"):
        waits = self._waits(q, reads, writes, pwrites)
        if sb.dsem is None:
            if self.sem_pool:
                sb.dsem, sb.dcnt = self.sem_pool.pop()
            else:
                sb.dsem = self.new_sem("d_" + sb.name)
            self.dbufs.append(sb)
        sb.dcnt += 16
        ev = (sb.dsem, sb.dcnt)
        self.q[q].append((waits, lambda e: e.dma_start(out=out_ap, in_=in_ap), (sb.dsem, 16)))
        self._commit(ev, reads, writes, pwrites)

    def barrier(self):
        evs = [(self.esem[e], self.ecnt[e]) for e in ("pe", "act", "dve", "pool") if self.ecnt[e] > 0]
        evs += [(b.dsem, b.dcnt) for b in self.dbufs]
        for e in self.ENG:
            seen = self.seen[e]
            ws = []
            for s, v in evs:
                if seen.get(s, 0) < v:
                    seen[s] = v
                    ws.append((s, v))
            if ws:
                self.q[e].append((ws, None, None))
        for b in self.dbufs:
            self.sem_pool.append((b.dsem, b.dcnt))
            b.dsem = None
        self.dbufs = []

    def replay(self, name, eng):
        for waits, fn, inc in self.q[name]:
            for s, v in waits:
                eng.wait_ge(self.sems[s], v)
            if fn is None:
                continue
            ins = fn(eng)
            if inc is not None:
                ins.then_inc(self.sems[inc[0]], inc[1])


class Ring:
    def __init__(self, sc, name, aps, bufs=None):
        self.items = [(ap, sc.buf(name) if bufs is None else bufs[i]) for i, ap in enumerate(aps)]
        self.i = 0

    def next(self):
        it = self.items[self.i % len(self.items)]
        self.i += 1
        return it


class Arena:
    def __init__(self, t, nbytes):
        self.t = t
        self.cap = nbytes
        self.off = 0

    def mark(self):
        return self.off

    def reset(self, m):
        self.off = m

    def alloc(self, shape, dt):
        esz = 4 if dt in (F32, U32) else 2
        n = 1
        for s_ in shape:
            n *= s_
        nb = (n * esz + 31) // 32 * 32
        assert self.off + nb <= self.cap, ("SBUF arena overflow", self.off, nb, self.cap)
        a = self.t[:, self.off // 4:(self.off + nb) // 4]
        self.off += nb
        if esz == 2:
            a = a.bitcast(dt)
        elif dt is not F32:
            a = a.bitcast(dt)
        a = a[:, 0:n]
        if len(shape) == 2:
            return a.rearrange("p (a b) -> p a b", a=shape[0])
        if len(shape) == 3:
            return a.rearrange("p (a b c) -> p a b c", a=shape[0], b=shape[1])
        return a


IN_WIDTHS = (2048, 1024, 1024, 4, 4, 1024, 256, 256, 256, 256, 256, 256, 24, 1024, 1024)
_off = np.cumsum((0,) + IN_WIDTHS)
(O_QK, O_V, O_O, O_I, O_F, O_NQ, O_KC, O_VC, O_KS, O_VS, O_KW, O_VW, O_NG, O_GA, O_GB) = [int(v) for v in _off[:-1]]


def _rot_cols(base, nheads):
    idx = []
    for h in range(nheads):
        for d in range(128):
            idx.append(base + h * 128 + (d + 64) % 128)
    return idx


def fm_col_index():
    cols = list(range(O_QK, O_QK + 2048))
    cols += list(range(O_NQ, O_NQ + 1024)) + _rot_cols(O_NQ, 8)
    cols += list(range(O_KC, O_KC + 256)) + list(range(O_VC, O_VC + 256))
    cols += list(range(O_KS, O_KS + 256)) + _rot_cols(O_KS, 2)
    cols += list(range(O_KW, O_KW + 256)) + _rot_cols(O_KW, 2)
    cols += list(range(O_GA, O_GA + 1024)) + list(range(O_GB, O_GB + 1024))
    return np.asarray(cols)


def tm_col_index():
    cols = list(range(O_V, O_V + 1024)) + list(range(O_O, O_O + 1024))
    cols += list(range(O_VS, O_VS + 256)) + list(range(O_VW, O_VW + 256))
    return np.asarray(cols)


def sm_col_index():
    return np.asarray(list(range(O_I, O_I + 4)) + list(range(O_F, O_F + 4)) + list(range(O_NG, O_NG + 24)))


ZR_QK, ZR_Q, ZR_QR, ZR_KC, ZR_VC, ZR_KSR, ZR_KWR, ZR_GA, ZR_GB = 0, 2048, 3072, 4096, 4352, 4608, 4864, 5120, 6144
ZT_ROWS = 7168


class _Stop(Exception):
    pass


def build(dbg=(), stop=None):
    nc = bass.Bass("TRN2", target_bir_lowering=False)
    es = ExitStack()
    dbg = set(dbg)

    def check(name):
        if stop == name:
            raise _Stop()

    def din(name, shape, dt=F32):
        return nc.dram_tensor(name, list(shape), dt, kind="ExternalInput").ap()

    def dscr(name, shape, dt):
        kind = "ExternalOutput" if name in dbg else "Internal"
        return nc.dram_tensor(name, list(shape), dt, kind=kind).ap()

    x_d = din("x", [S, D])
    gmix_d = din("g_mix", [128, D])
    wfm_d = din("w_fm", [60, 128, 8, 128])
    wtm_d = din("w_tm", [5, 128, 8, 512])
    wsm_d = din("w_sm", [128, 8, 32])
    cos_d = din("cosT", [128, S])
    sin_d = din("sinT", [128, S])
    convw_d = din("conv_w", [16, 128, 5])
    ident_d = din("ident", [128, 128])
    tri_d = din("tri", [128, 128])
    mbias_d = din("m_bias", [128, 8])
    mnormg_d = din("m_norm_g", [128, 1024])
    out_d = nc.dram_tensor("out", [S, D], F32, kind="ExternalOutput").ap()

    zT_d = dscr("zT", [ZT_ROWS, S], BF16)
    ztm_d = dscr("ztm", [S, 2560], BF16)
    zsm_d = dscr("zsm", [S, 32], F32)
    hmT_d = dscr("hmT", [1024, S], BF16)
    onT_d = dscr("onT", [1024, S], BF16)
    h1_d = dscr("h1", [S, D], F32)
    xhT_d = dscr("xhT", [1024, S], BF16)
    wsq_d = din("wsq", [3, 128, 8, 1024])
    u_d = din("peer_u", [16384, 1024])
    v_d = din("peer_v", [16384, 1024])
    uT_d = dscr("uT", [128, 128, 8, 128], BF16)
    vb_d = dscr("vb", [128, 128, 1024], BF16)
    wq_d = din("wq", [128, 8, 1024])
    kblk_d = din("kblk", [128, 256])
    iota_d = din("iota", [128, 128])
    gf_d = din("g_fin", [128, D])
    gffn_d = din("g_ffn", [128, D])
    ovl_d = din("ovl", [2, 128, 64])
    eall_d = din("eall", [64, S])
    cw1_d = din("cw1", [2, 128, 32, 128])
    cw2_d = din("cw2", [2, 128, 128])
    cpos_d = din("cpos", [2, 128, 32])

    ARENA_BYTES = 204 * 1024
    arena_t = es.enter_context(nc.sbuf_tensor("arena", [128, ARENA_BYTES // 4], F32))
    psum_t = es.enter_context(nc.psum_tensor("psum", [128, 4096], F32))
    sc = Sched(nc, es)
    A = Arena(arena_t, ARENA_BYTES)

    def psbank(i, n=512, off=0):
        return psum_t[:, i * 512 + off:i * 512 + off + n]

    PB = [Buf("psb%d" % i, excl=True) for i in range(8)]

    try:
        _body(nc, sc, A, psbank, PB, check, locals())
    except _Stop:
        pass
    _finish(nc, sc, es)
    return nc


def _body(nc, sc, A, psbank, PB, check, L):
    (x_d, gmix_d, wfm_d, wtm_d, wsm_d, cos_d, sin_d, convw_d, ident_d, out_d, zT_d, ztm_d, zsm_d) = [L[k] for k in (
        "x_d", "gmix_d", "wfm_d", "wtm_d", "wsm_d", "cos_d", "sin_d", "convw_d", "ident_d", "out_d", "zT_d", "ztm_d", "zsm_d")]
    tri_d, mbias_d, mnormg_d, hmT_d = L["tri_d"], L["mbias_d"], L["mnormg_d"], L["hmT_d"]
    dbg = L["dbg"]

    def dump(name, ap, b, dt=F32):
        if name in dbg:
            d = nc.dram_tensor(name, [128] + list(ap.shape[1:]), dt, kind="ExternalOutput").ap()
            sc.dma(d, ap, b, reads=[b])

    ident_f = A.alloc([128], F32)
    ident_b = A.alloc([128], BF16)
    b_ident = sc.buf("ident")
    sc.dma(ident_f, ident_d, b_ident, writes=[b_ident])
    b_identb = sc.buf("identb")
    sc.op("dve", lambda e: e.tensor_copy(ident_b, ident_f), reads=[b_ident], writes=[b_identb])
    tri_f = A.alloc([128], F32)
    b_tri = sc.buf("tri")
    sc.dma(tri_f, tri_d, b_tri, writes=[b_tri])
    ones_f = A.alloc([128], F32)
    b_ones = sc.buf("ones")
    sc.op("pool", lambda e: e.memset(ones_f, 1.0), writes=[b_ones])
    base_mark = A.mark()
    check("const")

    xnT = A.alloc([8, S], BF16)
    b_xnT = sc.buf("xnT")
    gmix = A.alloc([D], F32)
    b_gmix = sc.buf("gmix")
    sc.dma(gmix, gmix_d, b_gmix, writes=[b_gmix])
    mA = A.mark()
    xin = Ring(sc, "xin", [A.alloc([D], F32) for _ in range(2)])
    sqr = Ring(sc, "sq", [A.alloc([D], F32) for _ in range(1)])
    xnb = Ring(sc, "xnb", [A.alloc([D], BF16) for _ in range(2)])
    stat = Ring(sc, "stat", [A.alloc([4], F32) for _ in range(2)])
    pst = Ring(sc, "pst", [psbank(i).bitcast(BF16) for i in (6, 7)], [PB[6], PB[7]])
    x_t = x_d.rearrange("(n p) d -> n p d", p=128)
    for t in range(NT):
        xa, xb = xin.next()
        sc.dma(xa, x_t[t], xb, writes=[xb])
        sq, sqb = sqr.next()
        st, stb = stat.next()
        sc.op("act", lambda e, sq=sq, xa=xa: e.activation(sq, xa, AF.Square), reads=[xb], writes=[sqb])
        sc.op("dve", lambda e, st=st, sq=sq: e.reduce_sum(st[:, 0:1], sq, axis=AX.X), reads=[sqb], writes=[stb])
        sc.op("dve", lambda e, st=st: e.tensor_scalar(st[:, 1:2], st[:, 0:1], 1.0 / D, EPS, op0=ALU.mult, op1=ALU.add),
              reads=[stb], pwrites=[stb])
        sc.op("act", lambda e, st=st: e.activation(st[:, 2:3], st[:, 1:2], AF.Ln), reads=[stb], pwrites=[stb])
        sc.op("act", lambda e, st=st: e.activation(st[:, 3:4], st[:, 2:3], AF.Exp, scale=-0.5), reads=[stb], pwrites=[stb])
        xn, xnbuf = xnb.next()
        sc.op("dve", lambda e, xn=xn, xa=xa, st=st: e.scalar_tensor_tensor(
            xn, xa, st[:, 3:4], gmix, op0=ALU.mult, op1=ALU.mult), reads=[xb, stb, b_gmix], writes=[xnbuf])
        if t == 1:
            check("norm1")
        pt, ptb = pst.next()
        for k in range(8):
            sc.op("pe", lambda e, pt=pt, xn=xn, k=k: e.transpose(pt[:, k * 128:(k + 1) * 128], xn[:, k * 128:(k + 1) * 128], ident_b),
                  reads=[xnbuf, b_identb], writes=[ptb] if k == 0 else (), pwrites=() if k == 0 else [ptb], sig=(k == 7))
        if t == 1:
            check("norm2")
        sc.op("act", lambda e, pt=pt, t=t: e.copy(xnT[:, :, t * 128:(t + 1) * 128], pt.rearrange("p (k c) -> p k c", k=8)),
              reads=[ptb], pwrites=[b_xnT])
        check("norm3_%d" % t)
    sc.barrier()
    A.reset(mA)
    check("norm")

    cosT = A.alloc([S], F32)
    sinT = A.alloc([S], F32)
    b_cos, b_sin = sc.buf("cos"), sc.buf("sin")
    sc.dma(cosT, cos_d, b_cos, writes=[b_cos])
    sc.dma(sinT, sin_d, b_sin, writes=[b_sin])

    wf32 = Ring(sc, "wf32", [A.alloc([8, 128], F32) for _ in range(4)])
    wbf = Ring(sc, "wbf", [A.alloc([8, 128], BF16) for _ in range(4)])
    zrow = Ring(sc, "zrow", [A.alloc([S], BF16) for _ in range(3)])
    z32 = Ring(sc, "z32", [A.alloc([S + 4], F32) for _ in range(1)])
    acc = Ring(sc, "acc", [A.alloc([S], F32) for _ in range(1)])
    cw = Ring(sc, "cw", [A.alloc([5], F32) for _ in range(2)])
    t1r = Ring(sc, "t1", [A.alloc([512], F32) for _ in range(2)])
    t2r = Ring(sc, "t2", [A.alloc([512], F32) for _ in range(2)])
    psA = Ring(sc, "psA", [psbank(i) for i in range(6)], PB[0:6])
    evq = [0]

    wmemo = {}

    def load_w(c):
        if c in wmemo:
            return wmemo.pop(c)
        return _load_w(c)

    def prefetch_w(cs):
        for c in cs:
            if c not in wmemo:
                wmemo[c] = _load_w(c)

    def _load_w(c):
        wa, wb = wf32.next()
        sc.dma(wa, wfm_d[c], wb, writes=[wb])
        wba, wbb = wbf.next()
        sc.op("pool", lambda e: e.tensor_copy(wba, wa), reads=[wb], writes=[wbb])
        return wba, wbb

    def proj_group(wba, wbb, g):
        ps, psb = psA.next()
        for k in range(8):
            sc.op("pe", lambda e, k=k: e.matmul(ps, wba[:, k, :], xnT[:, k, g * 512:(g + 1) * 512], start=(k == 0), stop=(k == 7)),
                  reads=[wbb, b_xnT], writes=[psb] if k == 0 else (), pwrites=() if k == 0 else [psb], sig=(k == 7))
        return ps, psb

    def store_row(zr, zrb, row0):
        sc.dma(zT_d[row0:row0 + 128, :], zr, zrb, reads=[zrb])

    def evac_copy(dst, dstb, ps, psb, func=None):
        evq[0] += 1
        if func is not None:
            sc.op("act", lambda e: e.activation(dst, ps, func), reads=[psb], pwrites=[dstb])
        elif evq[0] % 2 == 0:
            sc.op("act", lambda e: e.copy(dst, ps), reads=[psb], pwrites=[dstb])
        else:
            sc.op("dve", lambda e: e.tensor_copy(dst, ps), reads=[psb], pwrites=[dstb])

    def qk_job(c):
        wba, wbb = load_w(c)
        cwa, cwb = cw.next()
        sc.dma(cwa, convw_d[c], cwb, writes=[cwb])
        za, zb = z32.next()
        sc.op("pool", lambda e, za=za: e.memset(za[:, 0:4], 0.0), writes=[zb])
        for g in range(8):
            ps, psb = proj_group(wba, wbb, g)
            evac_copy(za[:, 4 + g * 512:4 + (g + 1) * 512], zb, ps, psb)
        aa, ab = acc.next()
        sc.op("dve", lambda e, aa=aa, za=za, cwa=cwa: e.tensor_scalar(aa, za[:, 4:4 + S], cwa[:, 3:4], cwa[:, 4:5], op0=ALU.mult, op1=ALU.add),
              reads=[zb, cwb], writes=[ab])
        sc.op("dve", lambda e, aa=aa, za=za, cwa=cwa: e.scalar_tensor_tensor(aa, za[:, 3:3 + S], cwa[:, 2:3], aa, op0=ALU.mult, op1=ALU.add),
              reads=[zb, cwb, ab], pwrites=[ab])
        sc.op("dve", lambda e, aa=aa, za=za, cwa=cwa: e.scalar_tensor_tensor(aa, za[:, 2:2 + S], cwa[:, 1:2], aa, op0=ALU.mult, op1=ALU.add),
              reads=[zb, cwb, ab], pwrites=[ab])
        sc.op("dve", lambda e, aa=aa, za=za, cwa=cwa: e.scalar_tensor_tensor(aa, za[:, 1:1 + S], cwa[:, 0:1], aa, op0=ALU.mult, op1=ALU.add),
              reads=[zb, cwb, ab], pwrites=[ab])
        zr, zrb = zrow.next()
        sc.op("act", lambda e, zr=zr, aa=aa: e.activation(zr, aa, AF.Silu), reads=[ab], writes=[zrb])
        store_row(zr, zrb, ZR_QK + c * 128)

    check("qk")
    def rope_job(c_plain, c_rot, row_plain, row_rot):
        wba, wbb = load_w(c_plain)
        wbr, wbrb = load_w(c_rot)
        if row_plain is not None:
            zp, zpb = zrow.next()
            sc.op("pool", lambda e: e.memset(zp[:, 0:1], 0.0), writes=[zpb])
        zr, zrb = zrow.next()
        sc.op("pool", lambda e: e.memset(zr[:, 0:1], 0.0), writes=[zrb])
        for g in range(8):
            sl = slice(g * 512, (g + 1) * 512)
            ps, psb = proj_group(wba, wbb, g)
            ps2, ps2b = proj_group(wbr, wbrb, g)
            if row_plain is not None:
                sc.op("act", lambda e, ps=ps, sl=sl: e.copy(zp[:, sl], ps), reads=[psb], pwrites=[zpb])
            t1, t1b = t1r.next()
            t2, t2b = t2r.next()
            sc.op("dve", lambda e, t1=t1, ps=ps, sl=sl: e.tensor_tensor(t1, ps, cosT[:, sl], op=ALU.mult), reads=[psb, b_cos], writes=[t1b])
            sc.op("dve", lambda e, t2=t2, ps2=ps2, sl=sl: e.tensor_tensor(t2, ps2, sinT[:, sl], op=ALU.mult), reads=[ps2b, b_sin], writes=[t2b])
            sc.op("pool", lambda e, t1=t1, t2=t2, sl=sl: e.tensor_tensor(zr[:, sl], t1, t2, op=ALU.add), reads=[t1b, t2b], pwrites=[zrb])
        if row_plain is not None:
            store_row(zp, zpb, row_plain)
        store_row(zr, zrb, row_rot)

    jobs = [((c,), (lambda c=c: qk_job(c))) for c in range(16)]
    for h in range(8):
        jobs.append(((16 + h, 24 + h), (lambda h=h: rope_job(16 + h, 24 + h, ZR_Q + h * 128, ZR_QR + h * 128))))
    for gI in range(2):
        jobs.append(((36 + gI, 38 + gI), (lambda gI=gI: rope_job(36 + gI, 38 + gI, None, ZR_KSR + gI * 128))))
        jobs.append(((40 + gI, 42 + gI), (lambda gI=gI: rope_job(40 + gI, 42 + gI, None, ZR_KWR + gI * 128))))

    check("rope")
    def plain_job(c, row0, func=None):
        wba, wbb = load_w(c)
        zr, zrb = zrow.next()
        sc.op("pool", lambda e: e.memset(zr[:, 0:1], 0.0), writes=[zrb])
        for g in range(8):
            ps, psb = proj_group(wba, wbb, g)
            evac_copy(zr[:, g * 512:(g + 1) * 512], zrb, ps, psb, func)
        store_row(zr, zrb, row0)

    for i in range(2):
        jobs.append(((32 + i,), (lambda i=i: plain_job(32 + i, ZR_KC + i * 128))))
        jobs.append(((34 + i,), (lambda i=i: plain_job(34 + i, ZR_VC + i * 128))))
    for i in range(8):
        jobs.append(((44 + i,), (lambda i=i: plain_job(44 + i, ZR_GA + i * 128, AF.Sigmoid))))
        jobs.append(((52 + i,), (lambda i=i: plain_job(52 + i, ZR_GB + i * 128, AF.Sigmoid))))
    prefetch_w(jobs[0][0])
    for ji, (cs, fn) in enumerate(jobs):
        if ji + 1 < len(jobs):
            prefetch_w(jobs[ji + 1][0])
        fn()

    check("fm")
    sc.barrier()
    A.reset(mA)
    wt32 = Ring(sc, "wt32", [A.alloc([8, 512], F32) for _ in range(1)])
    wtbf = Ring(sc, "wtbf", [A.alloc([8, 512], BF16) for _ in range(2)])
    ztile = Ring(sc, "ztile", [A.alloc([512], BF16) for _ in range(3)])
    zstile = Ring(sc, "zstile", [A.alloc([32], F32) for _ in range(3)])
    for blk in range(6):
        small = blk == 5
        n = 32 if small else 512
        wa, wb = wt32.next()
        wba, wbb = wtbf.next()
        if small:
            sc.dma(wa[:, :, 0:32], wsm_d, wb, writes=[wb])
        else:
            sc.dma(wa, wtm_d[blk], wb, writes=[wb])
        sc.op("pool", lambda e, wba=wba, wa=wa, n=n: e.tensor_copy(wba[:, :, 0:n], wa[:, :, 0:n]), reads=[wb], writes=[wbb])
        func = AF.Sigmoid if blk in (2, 3) else None
        for t in range(NT):
            ps, psb = psA.next()
            for k in range(8):
                sc.op("pe", lambda e, ps=ps, wba=wba, k=k, t=t, n=n: e.matmul(ps[:, 0:n], xnT[:, k, t * 128:(t + 1) * 128], wba[:, k, 0:n],
                                                                      start=(k == 0), stop=(k == 7)),
                      reads=[wbb, b_xnT], writes=[psb] if k == 0 else (), pwrites=() if k == 0 else [psb], sig=(k == 7))
            if small:
                zt, ztb = zstile.next()
                sc.op("dve", lambda e, zt=zt, ps=ps: e.tensor_copy(zt, ps[:, 0:32]), reads=[psb], writes=[ztb])
                sc.dma(zsm_d[t * 128:(t + 1) * 128, :], zt, ztb, reads=[ztb])
            else:
                zt, ztb = ztile.next()
                evq[0] += 1
                if func is not None or evq[0] % 2 == 0:
                    sc.op("act", lambda e, zt=zt, ps=ps, func=func: e.activation(zt, ps, func if func is not None else AF.Copy),
                          reads=[psb], writes=[ztb])
                else:
                    sc.op("dve", lambda e, zt=zt, ps=ps: e.tensor_copy(zt, ps), reads=[psb], writes=[ztb])
                sc.dma(ztm_d[t * 128:(t + 1) * 128, blk * 512:(blk + 1) * 512], zt, ztb, reads=[ztb])
    sc.barrier()
    A.reset(base_mark)
    check("A")
    phase_mlstm(**locals())
    check("B")
    phase_nsa(**locals())
    check("C")
    phase_mixout(**locals())
    check("D")
    phase_peer(**locals())
    check("E")


def phase_peer(nc, sc, A, psbank, PB, check, L, dump, ident_f, b_ident, ident_b, b_identb, base_mark, **_):
    xhT_d, h1_d, out_d, uT_d, vb_d = L["xhT_d"], L["h1_d"], L["out_d"], L["uT_d"], L["vb_d"]
    xh_rows = xhT_d.rearrange("(c p) t -> p c t", p=128)
    h1_t = h1_d.rearrange("(n p) d -> n p d", p=128)
    out_t = out_d.rearrange("(n p) d -> n p d", p=128)
    NEGBIG = -1e30

    aT = A.alloc([S], BF16)
    bT = A.alloc([S], BF16)
    wT = A.alloc([S], F32)
    b_aT, b_bT, b_wT = sc.buf("aT"), sc.buf("bT"), sc.buf("wT")
    iota_f = A.alloc([128], F32)
    iota_b = A.alloc([128], BF16)
    b_iota, b_iotab = sc.buf("iota"), sc.buf("iotab")
    sc.dma(iota_f, L["iota_d"], b_iota, writes=[b_iota])
    sc.op("dve", lambda e: e.tensor_copy(iota_b, iota_f), reads=[b_iota], writes=[b_iotab])
    gfin = A.alloc([1024], F32)
    b_gfin = sc.buf("gfin")
    sc.dma(gfin, L["gf_d"], b_gfin, writes=[b_gfin])
    m1 = A.mark()

    wqb = A.alloc([8, 1024], BF16)
    mwq = A.mark()
    wqf = A.alloc([8, 1024], F32)
    b_wqf, b_wqb = sc.buf("wqf"), sc.buf("wqb")
    sc.dma(wqf, L["wq_d"], b_wqf, writes=[b_wqf])
    sc.op("pool", lambda e: e.tensor_copy(wqb, wqf), reads=[b_wqf], writes=[b_wqb])
    sc.barrier()
    A.reset(mwq)
    u_ch = L["u_d"].rearrange("(a p) d -> a p d", p=128)
    v_ch = L["v_d"].rearrange("(a p) d -> a p d", p=128)
    uf = Ring(sc, "uf", [A.alloc([1024], F32) for _ in range(3)])
    ubf = Ring(sc, "ubf", [A.alloc([1024], BF16) for _ in range(2)])
    uTs = Ring(sc, "uTs", [A.alloc([8, 128], BF16) for _ in range(2)])
    vf = Ring(sc, "vf", [A.alloc([1024], F32) for _ in range(3)])
    vbf = Ring(sc, "vbf", [A.alloc([1024], BF16) for _ in range(2)])
    ptE0 = psbank(1).bitcast(BF16)
    e0l = {}

    def e0_load(a):
        ua, uab = uf.next()
        sc.dma(ua, u_ch[a], uab, writes=[uab])
        va, vab = vf.next()
        sc.dma(va, v_ch[a], vab, writes=[vab])
        e0l[a] = (ua, uab, va, vab)

    def e0_chunk(a):
        if a + 2 < 128:
            e0_load(a + 2)
        ua, uab, va, vab = e0l.pop(a)
        ub, ubb = ubf.next()
        sc.op("pool", lambda e: e.tensor_copy(ub, ua), reads=[uab], writes=[ubb])
        for k in range(8):
            sc.op("pe", lambda e, k=k: e.transpose(ptE0[:, k * 128:(k + 1) * 128], ub[:, k * 128:(k + 1) * 128], ident_b),
                  reads=[ubb, b_identb], writes=[PB[1]] if k == 0 else (), pwrites=() if k == 0 else [PB[1]], sig=(k == 7))
        us, usb = uTs.next()
        sc.op("act", lambda e: e.copy(us, ptE0.rearrange("p (k c) -> p k c", k=8)), reads=[PB[1]], writes=[usb])
        sc.dma(uT_d[a], us, usb, reads=[usb])
        vb_, vbb = vbf.next()
        sc.op("act", lambda e: e.copy(vb_, va), reads=[vab], writes=[vbb])
        sc.dma(vb_d[a], vb_, vbb, reads=[vbb])

    kbf = A.alloc([256], F32)
    kbb = A.alloc([256], BF16)
    b_kbf, b_kbb = sc.buf("kbf"), sc.buf("kbb")
    sc.dma(kbf, L["kblk_d"], b_kbf, writes=[b_kbf])
    sc.op("pool", lambda e: e.tensor_copy(kbb, kbf), reads=[b_kbf], writes=[b_kbb])
    xg = Ring(sc, "xg", [A.alloc([8, 512], BF16) for _ in range(2)])
    qTr = Ring(sc, "qTr", [A.alloc([8, 512], BF16) for _ in range(1)])
    S12r = Ring(sc, "S12", [A.alloc([8, 256], F32) for _ in range(2)])
    v12r = Ring(sc, "v12", [A.alloc([16, 16], F32) for _ in range(2)])
    i12r = Ring(sc, "i12", [A.alloc([16, 16], U32) for _ in range(2)])
    i12fr = Ring(sc, "i12f", [A.alloc([16, 16], F32) for _ in range(2)])
    tmpr = Ring(sc, "tmpk", [A.alloc([16, 128], F32) for _ in range(1)])
    candr = Ring(sc, "cand", [A.alloc([8, 256], F32) for _ in range(1)])
    tmpcr = Ring(sc, "tmpc", [A.alloc([8, 256], F32) for _ in range(1)])
    svr = Ring(sc, "sv", [A.alloc([8, 16], F32) for _ in range(2)])
    cir = Ring(sc, "ci", [A.alloc([8, 16], U32) for _ in range(2)])
    hlr = Ring(sc, "hl", [A.alloc([2, 128], U32) for _ in range(1)])
    hlfr = Ring(sc, "hlf", [A.alloc([2, 128], F32) for _ in range(1)])
    eqr = Ring(sc, "eq", [A.alloc([8, 256], F32) for _ in range(1)])
    abw = Ring(sc, "abw", [A.alloc([3, 128], F32) for _ in range(2)])
    smx = Ring(sc, "smx", [A.alloc([128], F32) for _ in range(2)])
    ssr = Ring(sc, "ss", [A.alloc([16], F32) for _ in range(2)])
    psq = Ring(sc, "psq", [psbank(i) for i in (0,)], PB[0:1])
    e0_load(0)
    e0_load(1)
    ps12 = [(psbank(i), PB[i]) for i in (2, 3, 4, 5)]
    for tg in range(8):
        xa, xab = xg.next()
        sc.dma(xa, xh_rows[:, :, tg * 512:(tg + 1) * 512], xab, writes=[xab])
        qT, qTb = qTr.next()
        sc.op("pool", lambda e: e.memset(qT[:, 0, 0:2], 0.0), writes=[qTb])
        for h in range(8):
            ps, psb = psq.next()
            for k in range(8):
                sc.op("pe", lambda e, k=k: e.matmul(ps, wqb[:, k, h * 128:(h + 1) * 128], xa[:, k, :], start=(k == 0), stop=(k == 7)),
                      reads=[b_wqb, xab], writes=[psb] if k == 0 else (), pwrites=() if k == 0 else [psb], sig=(k == 7))
            if h % 2 == 0:
                sc.op("act", lambda e: e.copy(qT[:, h, :], ps), reads=[psb], pwrites=[qTb])
            else:
                sc.op("dve", lambda e: e.tensor_copy(qT[:, h, :], ps), reads=[psb], pwrites=[qTb])
        for tt in range(4):
            t = tg * 4 + tt
            tsl = slice(tt * 128, (tt + 1) * 128)
            s12, s12b = S12r.next()
            for h in range(8):
                ps, psb = ps12[h // 2]
                sc.op("pe", lambda e: e.matmul(ps[:, (h % 2) * 256:(h % 2) * 256 + 256], qT[:, h, tsl], kbb, start=True, stop=True),
                      reads=[qTb, b_kbb], writes=[psb] if h % 2 == 0 else (), pwrites=() if h % 2 == 0 else [psb], sig=(h % 2 == 1))
                if h % 2 == 1:
                    sc.op("act", lambda e: e.copy(s12[:, h - 1:h + 1, :], ps.rearrange("p (h c) -> p h c", h=2)), reads=[psb],
                          writes=[s12b] if h == 1 else (), pwrites=() if h == 1 else [s12b])
            for a_ in range(4 * t, 4 * t + 4):
                e0_chunk(a_)
            v12, v12b = v12r.next()
            i12, i12b = i12r.next()
            tm_all, tmb = tmpr.next()
            rows = [s12[:, r // 2, (r % 2) * 128:(r % 2 + 1) * 128] for r in range(16)]
            for r in range(16):
                sc.op("dve", lambda e: e.max(out=v12[:, r, 0:8], in_=rows[r]), reads=[s12b], writes=[v12b] if r == 0 else (), pwrites=() if r == 0 else [v12b])
            for r in range(16):
                sc.op("dve", lambda e: e.max_index(out=i12[:, r, 0:8], in_max=v12[:, r, 0:8], in_values=rows[r]), reads=[s12b, v12b],
                      writes=[i12b] if r == 0 else (), pwrites=() if r == 0 else [i12b])
            for r in range(16):
                sc.op("dve", lambda e: e.match_replace(out=tm_all[:, r, :], in_to_replace=v12[:, r, 0:8], in_values=rows[r], imm_value=NEGBIG),
                      reads=[s12b, v12b], writes=[tmb] if r == 0 else (), pwrites=() if r == 0 else [tmb])
            for r in range(16):
                sc.op("dve", lambda e: e.max(out=v12[:, r, 8:16], in_=tm_all[:, r, :]), reads=[tmb], pwrites=[v12b])
            for r in range(16):
                sc.op("dve", lambda e: e.max_index(out=i12[:, r, 8:16], in_max=v12[:, r, 8:16], in_values=tm_all[:, r, :]), reads=[tmb, v12b], pwrites=[i12b])
            i12f, i12fb = i12fr.next()
            sc.op("dve", lambda e: e.tensor_copy(i12f, i12), reads=[i12b], writes=[i12fb])
            v4 = v12.rearrange("p (h two) i -> p h two i", two=2)
            i4 = i12f.rearrange("p (h two) i -> p h two i", two=2)
            cand, candb = candr.next()
            c4 = cand.rearrange("p h (i j) -> p h i j", i=16)
            sc.op("dve", lambda e: e.tensor_tensor(c4, v4[:, :, 0, :].unsqueeze(3).to_broadcast([128, 8, 16, 16]),
                                                   v4[:, :, 1, :].unsqueeze(2).to_broadcast([128, 8, 16, 16]), op=ALU.add),
                  reads=[v12b], writes=[candb])
            sv, svb = svr.next()
            ci, cib = cir.next()
            tc_all, tcb = tmpcr.next()
            for h in range(8):
                sc.op("dve", lambda e: e.max(out=sv[:, h, 0:8], in_=cand[:, h, :]), reads=[candb], writes=[svb] if h == 0 else (), pwrites=() if h == 0 else [svb])
            for h in range(8):
                sc.op("dve", lambda e: e.max_index(out=ci[:, h, 0:8], in_max=sv[:, h, 0:8], in_values=cand[:, h, :]), reads=[candb, svb],
                      writes=[cib] if h == 0 else (), pwrites=() if h == 0 else [cib])
            for h in range(8):
                sc.op("dve", lambda e: e.match_replace(out=tc_all[:, h, :], in_to_replace=sv[:, h, 0:8], in_values=cand[:, h, :], imm_value=NEGBIG),
                      reads=[candb, svb], writes=[tcb] if h == 0 else (), pwrites=() if h == 0 else [tcb])
            for h in range(8):
                sc.op("dve", lambda e: e.max(out=sv[:, h, 8:16], in_=tc_all[:, h, :]), reads=[tcb], pwrites=[svb])
            for h in range(8):
                sc.op("dve", lambda e: e.max_index(out=ci[:, h, 8:16], in_max=sv[:, h, 8:16], in_values=tc_all[:, h, :]), reads=[tcb, svb], pwrites=[cib])
            hl, hlb = hlr.next()
            cif = ci.rearrange("p h j -> p (h j)")
            sc.op("dve", lambda e: e.tensor_scalar(hl[:, 0, :], cif, 4, None, op0=ALU.logical_shift_right), reads=[cib], writes=[hlb])
            sc.op("dve", lambda e: e.tensor_scalar(hl[:, 1, :], cif, 15, None, op0=ALU.bitwise_and), reads=[cib], pwrites=[hlb])
            hlf, hlfb = hlfr.next()
            sc.op("dve", lambda e: e.tensor_copy(hlf, hl), reads=[hlb], writes=[hlfb])
            ab, abb = abw.next()
            for which in range(2):
                eq, eqb = eqr.next()
                e4 = eq.rearrange("p h (j i) -> p h j i", j=16)
                sel = hlf[:, which, :].rearrange("p (h j) -> p h j", h=8)
                sc.op("dve", lambda e: e.tensor_tensor(e4, sel.unsqueeze(3).to_broadcast([128, 8, 16, 16]),
                                                       iota_f[:, 0:16].unsqueeze(1).unsqueeze(1).to_broadcast([128, 8, 16, 16]), op=ALU.is_equal),
                      reads=[hlfb, b_iota], writes=[eqb])
                sc.op("dve", lambda e: e.tensor_tensor(e4, e4, i4[:, :, which, :].unsqueeze(2).to_broadcast([128, 8, 16, 16]), op=ALU.mult),
                      reads=[eqb, i12fb], writes=[eqb])
                sc.op("dve", lambda e: e.reduce_sum(ab[:, which, :], e4.rearrange("p h j i -> p (h j) i"), axis=AX.X),
                      reads=[eqb], writes=[abb] if which == 0 else (), pwrites=() if which == 0 else [abb])
            sx, sxb = smx.next()
            sx3 = sx.rearrange("p (h j) -> p h j", h=8)
            ss, ssb = ssr.next()
            sc.op("dve", lambda e: e.tensor_tensor(sx3, sv, sv[:, :, 0:1].to_broadcast([128, 8, 16]), op=ALU.subtract), reads=[svb], writes=[sxb])
            sc.op("act", lambda e: e.activation(sx, sx, AF.Exp), reads=[sxb], writes=[sxb])
            sc.op("dve", lambda e: e.reduce_sum(ss[:, 0:8], sx3, axis=AX.X), reads=[sxb], writes=[ssb])
            sc.op("dve", lambda e: e.reciprocal(ss[:, 8:16], ss[:, 0:8]), reads=[ssb], pwrites=[ssb])
            sc.op("dve", lambda e: e.tensor_tensor(ab[:, 2, :].rearrange("p (h j) -> p h j", h=8), sx3,
                                                   ss[:, 8:16].unsqueeze(2).to_broadcast([128, 8, 16]), op=ALU.mult),
                  reads=[sxb, ssb], pwrites=[abb])
            psT, psTb = psbank(6 + (t % 2), 384), PB[6 + (t % 2)]
            for i in range(3):
                sc.op("pe", lambda e, i=i: e.transpose(psT[:, i * 128:(i + 1) * 128], ab[:, i, :], ident_f),
                      reads=[abb, b_ident], writes=[psTb] if i == 0 else (), pwrites=() if i == 0 else [psTb], sig=(i == 2))
            tk = slice(t * 128, (t + 1) * 128)
            sc.op("act", lambda e: e.copy(aT[:, tk], psT[:, 0:128]), reads=[psTb], pwrites=[b_aT])
            sc.op("act", lambda e: e.copy(bT[:, tk], psT[:, 128:256]), reads=[psTb], pwrites=[b_bT])
            sc.op("act", lambda e: e.copy(wT[:, tk], psT[:, 256:384]), reads=[psTb], pwrites=[b_wT])
    dump("d_aT", aT, b_aT, BF16); dump("d_bT", bT, b_bT, BF16); dump("d_wT", wT, b_wT)
    sc.barrier()
    A.reset(m1)
    check("E1")

    TG = 256
    SB = 16
    GT = A.alloc([TG, 128], BF16)
    b_GT = sc.buf("GT")
    A1r = Ring(sc, "A1", [A.alloc([SB, 128], BF16) for _ in range(2)])
    B1r = Ring(sc, "B1", [A.alloc([SB, 128], BF16) for _ in range(2)])
    B1wr = Ring(sc, "B1w", [A.alloc([SB, 128], BF16) for _ in range(2)])
    ur = Ring(sc, "ur", [A.alloc([8, 128], BF16) for _ in range(7)])
    vr = Ring(sc, "vr", [A.alloc([1024], BF16) for _ in range(7)])
    ger = Ring(sc, "ge", [A.alloc([TG], F32) for _ in range(4)])
    WTr = Ring(sc, "WTe", [A.alloc([TG], BF16) for _ in range(4)])
    xgr = Ring(sc, "xge", [A.alloc([8, TG], BF16) for _ in range(2)])
    h1r = Ring(sc, "h1e", [A.alloc([1024], F32) for _ in range(2)])
    h2r = Ring(sc, "h2e", [A.alloc([1024], F32) for _ in range(1)])
    outr = Ring(sc, "oute", [A.alloc([1024], F32) for _ in range(2)])
    rings = {"sq": Ring(sc, "esq", [A.alloc([1024], F32) for _ in range(1)]), "st": Ring(sc, "est", [A.alloc([4], F32) for _ in range(2)])}
    psA = Ring(sc, "psAct", [psbank(i, TG) for i in (4, 5, 7)], [PB[4], PB[5], PB[7]])
    psG = Ring(sc, "psG", [psbank(i) for i in (6, 7)], PB[6:8])
    for grp in range(S // TG):
        t0 = grp * TG
        xa, xab = xgr.next()
        sc.dma(xa, xh_rows[:, :, t0:t0 + TG], xab, writes=[xab])
        sc.op("pool", lambda e: e.memset(GT[:, 0, 0:2], 0.0), writes=[b_GT])
        def onehots(sb_):
            ts0 = t0 + sb_ * SB
            a1, a1b = A1r.next()
            b1, b1b = B1r.next()
            b1w, b1wb = B1wr.next()
            io3 = iota_b.unsqueeze(1).to_broadcast([128, SB, 128])
            sc.op("dve", lambda e: e.tensor_tensor(a1, io3, aT[:, ts0:ts0 + SB].unsqueeze(2).to_broadcast([128, SB, 128]), op=ALU.is_equal),
                  reads=[b_iotab, b_aT], writes=[a1b])
            sc.op("dve", lambda e: e.tensor_tensor(b1, io3, bT[:, ts0:ts0 + SB].unsqueeze(2).to_broadcast([128, SB, 128]), op=ALU.is_equal),
                  reads=[b_iotab, b_bT], writes=[b1b])
            sc.op("pool", lambda e: e.tensor_tensor(b1w, b1, wT[:, ts0:ts0 + SB].unsqueeze(2).to_broadcast([128, SB, 128]), op=ALU.mult),
                  reads=[b1b, b_wT], writes=[b1wb])
            return a1, a1b, b1w, b1wb

        def gmm(sb_, a1, a1b, b1w, b1wb):
            for q4 in range(SB // 4):
                pg, pgb = psG.next()
                for i in range(4):
                    tl = q4 * 4 + i
                    sc.op("pe", lambda e: e.matmul(pg[:, i * 128:(i + 1) * 128], b1w[:, tl, :], a1[:, tl, :], start=True, stop=True),
                          reads=[b1wb, a1b], writes=[pgb] if i == 0 else (), pwrites=() if i == 0 else [pgb], sig=(i == 3))
                tg0 = sb_ * SB + q4 * 4
                sc.op("act", lambda e: e.copy(GT[:, tg0:tg0 + 4, :], pg.rearrange("p (t a) -> p t a", t=4)), reads=[pgb], pwrites=[b_GT])

        cur_oh = onehots(0)
        for sb_ in range(TG // SB):
            nxt_oh = onehots(sb_ + 1) if sb_ + 1 < TG // SB else None
            gmm(sb_, *cur_oh)
            cur_oh = nxt_oh
        if grp == 0:
            dump("d_GT0", GT, b_GT, BF16)
        NPF = 6
        wl = {}

        def load_uv(a):
            ua, uab = ur.next()
            sc.dma(ua, uT_d[a], uab, writes=[uab])
            va, vab = vr.next()
            sc.dma(va, vb_d[a], vab, writes=[vab], q="act" if E2_VQ_ACT else "sp")
            wl[a] = (ua, uab, va, vab)

        def act_mm(a):
            ua, uab, _, _ = wl[a]
            pa, pab = psA.next()
            for k in range(8):
                sc.op("pe", lambda e, k=k: e.matmul(pa, ua[:, k, :], xa[:, k, :], start=(k == 0), stop=(k == 7)),
                      reads=[uab, xab], writes=[pab] if k == 0 else (), pwrites=() if k == 0 else [pab], sig=(k == 7))
            return pa, pab

        for a in range(min(NPF, 128)):
            load_uv(a)
        pend_act = [act_mm(0), act_mm(1)]
        for a in range(128):
            if a + NPF < 128:
                load_uv(a + NPF)
            if a + 2 < 128:
                pend_act.append(act_mm(a + 2))
            pa, pab = pend_act.pop(0)
            _, _, va, vab = wl.pop(a)
            ge, geb = ger.next()
            sc.op("act", lambda e: e.activation(ge, pa, AF.Gelu), reads=[pab], writes=[geb])
            wt, wtb = WTr.next()
            sc.op("dve", lambda e: e.tensor_tensor(wt, ge, GT[:, :, a], op=ALU.mult), reads=[geb, b_GT], writes=[wtb])
            for i in range(2):
                for half in range(2):
                    bi = i * 2 + half
                    sc.op("pe", lambda e: e.matmul(psbank(bi), wt[:, i * 128:(i + 1) * 128], va[:, half * 512:(half + 1) * 512],
                                                   start=(a == 0), stop=(a == 127)),
                          reads=[wtb, vab], writes=[PB[bi]] if a == 0 else (), pwrites=() if a == 0 else [PB[bi]], sig=(bi == 3))
        for i in range(2):
            t = grp * 2 + i
            h1, h1b = h1r.next()
            sc.dma(h1, h1_t[t], h1b, writes=[h1b])
            h2, h2b = h2r.next()
            for half in range(2):
                bi = i * 2 + half
                sc.op("dve", lambda e: e.tensor_tensor(h2[:, half * 512:(half + 1) * 512], psbank(bi), h1[:, half * 512:(half + 1) * 512], op=ALU.add),
                      reads=[PB[bi], h1b], writes=[h2b] if half == 0 else (), pwrites=() if half == 0 else [h2b])
            if grp == 0 and i == 0:
                dump("d_h2", h2, h2b)
            oo, oob = outr.next()
            rmsnorm_tile(sc, rings, h2, h2b, gfin, b_gfin, oo, oob)
            sc.dma(out_t[t], oo, oob, reads=[oob])
        check("E2_%d" % grp)
    sc.barrier()
    A.reset(base_mark)


def rmsnorm_tile(sc, A_rings, src, srcb, g_bc, b_g, dst, dstb, width=1024):
    sq, sqb = A_rings["sq"].next()
    st, stb = A_rings["st"].next()
    sc.op("act", lambda e: e.activation(sq, src, AF.Square), reads=[srcb], writes=[sqb])
    sc.op("dve", lambda e: e.reduce_sum(st[:, 0:1], sq, axis=AX.X), reads=[sqb], writes=[stb])
    sc.op("dve", lambda e: e.tensor_scalar(st[:, 1:2], st[:, 0:1], 1.0 / width, EPS, op0=ALU.mult, op1=ALU.add), reads=[stb], pwrites=[stb])
    sc.op("act", lambda e: e.activation(st[:, 2:3], st[:, 1:2], AF.Ln), reads=[stb], pwrites=[stb])
    sc.op("act", lambda e: e.activation(st[:, 3:4], st[:, 2:3], AF.Exp, scale=-0.5), reads=[stb], pwrites=[stb])
    sc.op("dve", lambda e: e.scalar_tensor_tensor(dst, src, st[:, 3:4], g_bc, op0=ALU.mult, op1=ALU.mult),
          reads=[srcb, stb, b_g], writes=[dstb])


def phase_mixout(nc, sc, A, psbank, PB, check, L, dump, zT_d, x_d, ident_b, b_identb, base_mark, **_):
    hmT_d, onT_d, h1_d, xhT_d = L["hmT_d"], L["onT_d"], L["h1_d"], L["xhT_d"]
    zt_rows = zT_d.rearrange("(c p) t -> p c t", p=128)
    hm_rows = hmT_d.rearrange("(c p) t -> p c t", p=128)
    on_rows = onT_d.rearrange("(c p) t -> p c t", p=128)
    xh_rows = xhT_d.rearrange("(c p) t -> p c t", p=128)
    W = [(A.alloc([8, 1024], BF16), sc.buf("wsq")) for _ in range(3)]
    mst = A.mark()
    wst = Ring(sc, "wst", [A.alloc([8, 1024], F32) for _ in range(2)])
    for i in range(3):
        wa, wb = wst.next()
        sc.dma(wa, L["wsq_d"][i], wb, writes=[wb])
        sc.op("pool", lambda e: e.tensor_copy(W[i][0], wa), reads=[wb], writes=[W[i][1]])
    sc.barrier()
    A.reset(mst)
    gffn = A.alloc([1024], F32)
    b_gffn = sc.buf("gffn")
    sc.dma(gffn, L["gffn_d"], b_gffn, writes=[b_gffn])
    hin = Ring(sc, "hin", [A.alloc([8, 512], BF16) for _ in range(2)])
    oin = Ring(sc, "oin", [A.alloc([8, 512], BF16) for _ in range(2)])
    gain = Ring(sc, "gain", [A.alloc([8, 512], BF16) for _ in range(2)])
    gbin = Ring(sc, "gbin", [A.alloc([8, 512], BF16) for _ in range(2)])
    mixT = Ring(sc, "mixT", [A.alloc([8, 512], BF16) for _ in range(2)])
    t1r = Ring(sc, "dt1", [A.alloc([512], F32) for _ in range(2)])
    t2r = Ring(sc, "dt2", [A.alloc([512], F32) for _ in range(2)])
    xin = Ring(sc, "dxin", [A.alloc([1024], F32) for _ in range(2)])
    h1r = Ring(sc, "h1", [A.alloc([1024], F32) for _ in range(2)])
    xhb = Ring(sc, "xhb", [A.alloc([1024], BF16) for _ in range(2)])
    xhT = Ring(sc, "xhT", [A.alloc([8, 512], BF16) for _ in range(2)])
    rings = {"sq": Ring(sc, "dsq", [A.alloc([1024], F32) for _ in range(1)]), "st": Ring(sc, "dst", [A.alloc([4], F32) for _ in range(2)])}
    psr = Ring(sc, "psD", [psbank(i) for i in range(6)], PB[0:6])
    pst = Ring(sc, "pstD", [psbank(i).bitcast(BF16) for i in (6, 7)], PB[6:8])
    x_t = x_d.rearrange("(n p) d -> n p d", p=128)
    h1_t = h1_d.rearrange("(n p) d -> n p d", p=128)
    for tg in range(8):
        ts_ = slice(tg * 512, (tg + 1) * 512)
        hi, hib = hin.next()
        sc.dma(hi, hm_rows[:, :, ts_], hib, writes=[hib])
        oi, oib = oin.next()
        sc.dma(oi, on_rows[:, :, ts_], oib, writes=[oib])
        ga, gab = gain.next()
        sc.dma(ga, zt_rows[:, ZR_GA // 128:ZR_GA // 128 + 8, ts_], gab, writes=[gab])
        gb, gbb = gbin.next()
        sc.dma(gb, zt_rows[:, ZR_GB // 128:ZR_GB // 128 + 8, ts_], gbb, writes=[gbb])
        mx, mxb = mixT.next()
        sc.op("pool", lambda e: e.memset(mx[:, 0, 0:2], 0.0), writes=[mxb])
        for dc in range(8):
            psa, psab = psr.next()
            for k in range(8):
                sc.op("pe", lambda e, k=k: e.matmul(psa, W[0][0][:, k, dc * 128:(dc + 1) * 128], hi[:, k, :], start=(k == 0), stop=(k == 7)),
                      reads=[W[0][1], hib], writes=[psab] if k == 0 else (), pwrites=() if k == 0 else [psab], sig=(k == 7))
            psb_, psbb = psr.next()
            for k in range(8):
                sc.op("pe", lambda e, k=k: e.matmul(psb_, W[1][0][:, k, dc * 128:(dc + 1) * 128], oi[:, k, :], start=(k == 0), stop=(k == 7)),
                      reads=[W[1][1], oib], writes=[psbb] if k == 0 else (), pwrites=() if k == 0 else [psbb], sig=(k == 7))
            t1, t1b = t1r.next()
            t2, t2b = t2r.next()
            sc.op("dve", lambda e: e.tensor_tensor(t1, psa, ga[:, dc, :], op=ALU.mult), reads=[psab, gab], writes=[t1b])
            sc.op("dve", lambda e: e.tensor_tensor(t2, psb_, gb[:, dc, :], op=ALU.mult), reads=[psbb, gbb], writes=[t2b])
            sc.op("pool", lambda e: e.tensor_tensor(mx[:, dc, :], t1, t2, op=ALU.add), reads=[t1b, t2b], pwrites=[mxb])
        xt_, xtb = xhT.next()
        sc.op("pool", lambda e: e.memset(xt_[:, 0, 0:2], 0.0), writes=[xtb])
        for tt in range(4):
            t = tg * 4 + tt
            xa, xab = xin.next()
            sc.dma(xa, x_t[t], xab, writes=[xab])
            h1, h1b = h1r.next()
            for half in range(2):
                ps, psb2 = psr.next()
                for k in range(8):
                    sc.op("pe", lambda e, k=k: e.matmul(ps, mx[:, k, tt * 128:(tt + 1) * 128], W[2][0][:, k, half * 512:(half + 1) * 512],
                                                        start=(k == 0), stop=(k == 7)),
                          reads=[W[2][1], mxb], writes=[psb2] if k == 0 else (), pwrites=() if k == 0 else [psb2], sig=(k == 7))
                sc.op("dve", lambda e: e.tensor_tensor(h1[:, half * 512:(half + 1) * 512], ps, xa[:, half * 512:(half + 1) * 512], op=ALU.add),
                      reads=[psb2, xab], writes=[h1b] if half == 0 else (), pwrites=() if half == 0 else [h1b])
            sc.dma(h1_t[t], h1, h1b, reads=[h1b])
            xh, xhbb = xhb.next()
            rmsnorm_tile(sc, rings, h1, h1b, gffn, b_gffn, xh, xhbb)
            pt, ptb = pst.next()
            for k in range(8):
                sc.op("pe", lambda e, k=k: e.transpose(pt[:, k * 128:(k + 1) * 128], xh[:, k * 128:(k + 1) * 128], ident_b),
                      reads=[xhbb, b_identb], writes=[ptb] if k == 0 else (), pwrites=() if k == 0 else [ptb], sig=(k == 7))
            sc.op("act", lambda e: e.copy(xt_[:, :, tt * 128:(tt + 1) * 128], pt.rearrange("p (k c) -> p k c", k=8)), reads=[ptb], pwrites=[xtb])
        sc.dma(xh_rows[:, :, ts_], xt_, xtb, reads=[xtb])
    sc.barrier()
    A.reset(base_mark)


def phase_nsa(nc, sc, A, psbank, PB, check, L, dump, zT_d, ztm_d, zsm_d, ident_b, b_identb, base_mark, **_):
    SC = float(128 ** -0.5)
    NEG = -30000.0
    onT_d = L["onT_d"]
    zt_rows = zT_d.rearrange("(c p) t -> p c t", p=128)
    ztm_t = ztm_d.rearrange("(n p) c -> p n c", p=128)
    onT_rows = onT_d.rearrange("(c p) t -> p c t", p=128)

    sm = A.alloc([NT, 32], F32)
    b_sm = sc.buf("smC")
    sc.dma(sm, zsm_d.rearrange("(n p) c -> p n c", p=128), b_sm, writes=[b_sm])
    gates = A.alloc([NT, 24], F32)
    b_gates = sc.buf("gates")
    sc.op("act", lambda e: e.activation(gates, sm[:, :, 8:32], AF.Sigmoid), reads=[b_sm], writes=[b_gates])

    ovl32 = A.alloc([2, 64], F32)
    b_ovl = sc.buf("ovl")
    sc.dma(ovl32, L["ovl_d"].rearrange("t p j -> p t j"), b_ovl, writes=[b_ovl])
    Eall = A.alloc([S], BF16)
    b_Eall = sc.buf("Eall")
    kcmpT = A.alloc([2, 256], BF16)
    b_kcmpT = sc.buf("kcmpT")
    vcx = A.alloc([2, 2, 193], BF16)
    b_vcx = sc.buf("vcx")
    cmark = A.mark()
    E32 = A.alloc([S], F32)
    b_E32 = sc.buf("E32")
    sc.dma(E32[0:64, :], L["eall_d"], b_E32, writes=[b_E32])
    sc.op("pool", lambda e: e.memset(Eall[64:128, :], 0.0), writes=[b_Eall])
    sc.op("pool", lambda e: e.tensor_copy(Eall[0:64, :], E32[0:64, :]), reads=[b_E32], pwrites=[b_Eall])

    sc.barrier()
    A.reset(cmark)
    c1mark = A.mark()
    sc.op("pool", lambda e: e.memset(kcmpT, 0.0), writes=[b_kcmpT])
    sc.op("pool", lambda e: e.memset(vcx, 0.0), writes=[b_vcx])
    for g in range(2):
        sc.op("pool", lambda e, g=g: e.memset(vcx[:, 0, g, 128:129], 1.0), pwrites=[b_vcx])
        sc.op("pool", lambda e, g=g: e.memset(vcx[0:127, 1, g, 128:129], 1.0), pwrites=[b_vcx])
        sc.op("pool", lambda e, g=g: e.tensor_copy(vcx[:, :, g, 129:193], ovl32), reads=[b_ovl], pwrites=[b_vcx])
    kvc = A.alloc([4, S], BF16)
    b_kvc = sc.buf("kvc")
    sc.dma(kvc, zt_rows[:, ZR_KC // 128:ZR_KC // 128 + 4, :], b_kvc, writes=[b_kvc])
    w1f = A.alloc([32, 128], F32)
    b_w1f = sc.buf("w1f")
    w1b = A.alloc([32, 128], BF16)
    b_w1b = sc.buf("w1b")
    w2f = A.alloc([128], F32)
    w2b = A.alloc([128], BF16)
    b_w2f, b_w2b = sc.buf("w2f"), sc.buf("w2b")
    posf = A.alloc([32], F32)
    posb = A.alloc([32], BF16)
    b_posf, b_posb = sc.buf("posf"), sc.buf("posb")
    cbias = A.alloc([1], F32)
    b_cbias = sc.buf("cbias")
    gT = A.alloc([256], BF16)
    b_gT = sc.buf("gT")
    for kv in range(2):
        sc.dma(w1f, L["cw1_d"][kv], b_w1f, writes=[b_w1f])
        sc.op("pool", lambda e: e.tensor_copy(w1b, w1f), reads=[b_w1f], writes=[b_w1b])
        sc.dma(w2f, L["cw2_d"][kv], b_w2f, writes=[b_w2f])
        sc.op("pool", lambda e: e.tensor_copy(w2b, w2f), reads=[b_w2f], writes=[b_w2b])
        sc.dma(posf, L["cpos_d"][kv], b_posf, writes=[b_posf])
        sc.op("pool", lambda e: e.tensor_copy(posb, posf), reads=[b_posf], writes=[b_posb])
        psb_ = psbank(0, 1)
        for l in range(32):
            sc.op("pe", lambda e, l=l: e.matmul(psb_, w1b[:, l, :], posb[:, l:l + 1], start=(l == 0), stop=(l == 31)),
                  reads=[b_w1b, b_posb], writes=[PB[0]] if l == 0 else (), pwrites=() if l == 0 else [PB[0]], sig=(l == 31))
        sc.op("dve", lambda e: e.tensor_copy(cbias, psb_), reads=[PB[0]], writes=[b_cbias])
        for g in range(2):
            src = kvc[:, kv * 2 + g, :].rearrange("p (n s) -> p n s", s=16)
            psp = psbank(1, 255)
            for l in range(32):
                sc.op("pe", lambda e, l=l, src=src: e.matmul(psp, w1b[:, l, :], src[:, 0:255, l] if l < 16 else src[:, 1:256, l - 16], start=(l == 0), stop=(l == 31)),
                      reads=[b_w1b, b_kvc], writes=[PB[1]] if l == 0 else (), pwrites=() if l == 0 else [PB[1]], sig=(l == 31))
            sc.op("pool", lambda e: e.memset(gT, 0.0), writes=[b_gT])
            sc.op("act", lambda e: e.activation(gT[:, 0:255], psp, AF.Gelu, bias=cbias[:, 0:1]), reads=[PB[1], b_cbias], pwrites=[b_gT])
            if kv == 0:
                pso = psbank(2, 255)
                sc.op("pe", lambda e: e.matmul(pso, w2b, gT[:, 0:255], start=True, stop=True), reads=[b_w2b, b_gT], writes=[PB[2]])
                sc.op("dve", lambda e, g=g: e.tensor_copy(kcmpT[:, g, 0:255], pso), reads=[PB[2]], pwrites=[b_kcmpT])
            else:
                for tt in range(2):
                    nn = 128 if tt == 0 else 127
                    pso = psbank(2 + tt, 128)
                    sc.op("pe", lambda e, tt=tt, nn=nn, pso=pso: e.matmul(pso[0:nn, :], gT[:, tt * 128:tt * 128 + nn], w2b, start=True, stop=True),
                          reads=[b_w2b, b_gT], writes=[PB[2 + tt]])
                    sc.op("dve", lambda e, tt=tt, nn=nn, g=g, pso=pso: e.tensor_copy(vcx[0:nn, tt, g, 0:128], pso[0:nn, :]),
                          reads=[PB[2 + tt]], pwrites=[b_vcx])
    dump("d_kcmpT", kcmpT, b_kcmpT, BF16)
    dump("d_vcx", vcx, b_vcx, BF16)
    sc.barrier()
    A.reset(c1mark)
    check("C1")

    qg = A.alloc([NT, 4, 128], BF16)
    qrg = A.alloc([NT, 4, 128], BF16)
    ksw = A.alloc([2, S], BF16)
    vsx = A.alloc([NT, 129], BF16)
    vwx = A.alloc([NT, 129], BF16)
    onT = A.alloc([4, S], BF16)
    b_qg, b_qrg, b_ksw, b_vsx, b_vwx, b_onT = [sc.buf(n_) for n_ in ("qg", "qrg", "ksw", "vsx", "vwx", "onT")]
    E32r = Ring(sc, "E32r", [A.alloc([512], F32) for _ in range(2)])
    PTr = Ring(sc, "PTr", [A.alloc([512], BF16) for _ in range(4)])
    impr = Ring(sc, "imp", [A.alloc([64], F32) for _ in range(2)])
    m8r = Ring(sc, "m8", [A.alloc([8], F32) for _ in range(2)])
    negr = Ring(sc, "neg", [A.alloc([64], BF16) for _ in range(2)])
    negTr = Ring(sc, "negT", [A.alloc([512], BF16) for _ in range(2)])
    for nT_, nTb_ in negTr.items:
        sc.op("pool", lambda e: e.memset(nT_[64:128, :], 0.0), writes=[nTb_])
    zzr = Ring(sc, "zz", [A.alloc([12], F32) for _ in range(2)])
    cfr = Ring(sc, "cf", [A.alloc([12], F32) for _ in range(2)])
    oacc = Ring(sc, "oacc", [A.alloc([128], F32) for _ in range(8)])
    obf = Ring(sc, "obf", [A.alloc([4, 128], BF16) for _ in range(2)])
    sring = [0]

    SBANKS = (0, 1, 7)

    def s_bank():
        sring[0] += 1
        i = SBANKS[sring[0] % 3]
        return psbank(i), PB[i]

    def pv_accumulate(pt, ptb, vx, vxb, kt_idx, accs, width):
        for hh in range(4):
            bi, co = accs[hh]
            out = psbank(bi, width, co)
            sc.op("pe", lambda e, hh=hh, out=out: e.matmul(out, pt[:, hh * 128:(hh + 1) * 128], vx[:, kt_idx, :], start=False, stop=False,
                                                          skip_group_check=True),
                  reads=[ptb, vxb], pwrites=[PB[bi]], sig=(hh == 3))

    def masked_exp(ps, psb, base, cm, pat, cmp_op):
        ea, eb = E32r.next()
        sc.op("act", lambda e: e.activation(ea, ps, AF.Exp, scale=SC), reads=[psb], writes=[eb])
        pt, ptb = PTr.next()
        sc.op("pool", lambda e: e.affine_select(pt.rearrange("p (h t) -> p h t", h=4), ea.rearrange("p (h t) -> p h t", h=4),
                                               pattern=pat, compare_op=cmp_op, fill=0.0, base=base, channel_multiplier=cm),
              reads=[eb], writes=[ptb])
        return pt, ptb

    def run_branch(kts, s_fn, e_fn, pv_fn):
        LA = 2
        pend = [s_fn(kt) for kt in kts[:LA]]
        for i, kt in enumerate(kts):
            if i + LA < len(kts):
                pend.append(s_fn(kts[i + LA]))
            pt, ptb = e_fn(kt, *pend.pop(0))
            pv_fn(kt, pt, ptb)

    def plain_exp(ps, psb):
        pt, ptb = PTr.next()
        sc.op("act", lambda e: e.activation(pt, ps, AF.Exp, scale=SC), reads=[psb], writes=[ptb])
        return pt, ptb

    for g in range(2):
        for hh in range(4):
            sc.dma(qg[:, :, hh, :], zt_rows[:, ZR_Q // 128 + 4 * g + hh, :].rearrange("p (n t) -> p n t", t=128), b_qg,
                   writes=[b_qg] if hh == 0 else (), pwrites=() if hh == 0 else [b_qg])
            sc.dma(qrg[:, :, hh, :], zt_rows[:, ZR_QR // 128 + 4 * g + hh, :].rearrange("p (n t) -> p n t", t=128), b_qrg,
                   writes=[b_qrg] if hh == 0 else (), pwrites=() if hh == 0 else [b_qrg])
        sc.dma(ksw[:, 0, :], zt_rows[:, ZR_KSR // 128 + g, :], b_ksw, writes=[b_ksw])
        sc.dma(ksw[:, 1, :], zt_rows[:, ZR_KWR // 128 + g, :], b_ksw, pwrites=[b_ksw])
        sc.dma(vsx[:, :, 0:128], ztm_t[:, :, 2048 + g * 128:2048 + (g + 1) * 128], b_vsx, writes=[b_vsx])
        sc.op("pool", lambda e: e.memset(vsx[:, :, 128:129], 1.0), pwrites=[b_vsx])
        sc.dma(vwx[:, :, 0:128], ztm_t[:, :, 2304 + g * 128:2304 + (g + 1) * 128], b_vwx, writes=[b_vwx])
        sc.op("pool", lambda e: e.memset(vwx[:, :, 128:129], 1.0), pwrites=[b_vwx])
        sc.op("pool", lambda e: e.memset(onT[:, 0, 0:2], 0.0), writes=[b_onT])
        for qt in range(NT):
            tq = slice(qt * 128, (qt + 1) * 128)
            qs = qg[:, qt, :, :].rearrange("p h t -> p (h t)")
            qrs = qrg[:, qt, :, :].rearrange("p h t -> p (h t)")
            for bi in (2, 3):
                sc.op("dve", lambda e, bi=bi: e.memset(psbank(bi, 386), 0.0), writes=[PB[bi]])
            accC = [(2, 0), (2, 193), (3, 0), (3, 193)]
            accW = [(4, 0), (4, 129), (5, 0), (5, 129)]
            accS = [(2, 0), (2, 129), (3, 0), (3, 129)]

            def s_cmp(kt):
                ps, psb = s_bank()
                sc.op("pe", lambda e: e.matmul(ps, kcmpT[:, g, kt * 128:(kt + 1) * 128], qs, start=True, stop=True),
                      reads=[b_kcmpT, b_qg], writes=[psb])
                return ps, psb

            run_branch(list(range(2 if qt >= 16 else 1)), s_cmp,
                       lambda kt, ps, psb: masked_exp(ps, psb, base=128 * qt - 31 - 2048 * kt, cm=-16, pat=[[0, 4], [1, 128]], cmp_op=ALU.is_ge),
                       lambda kt, pt, ptb: pv_accumulate(pt, ptb, vcx[:, :, g, :], b_vcx, kt, accC, 193))
            for bi in (4, 5):
                sc.op("dve", lambda e, bi=bi: e.memset(psbank(bi, 258), 0.0), writes=[PB[bi]])
            zz, zzb = zzr.next()
            for hh in range(4):
                bi, co = accC[hh]
                sc.op("dve", lambda e, hh=hh, bi=bi, co=co: e.tensor_scalar(zz[:, hh * 3:hh * 3 + 1], psbank(bi, 1, co + 128), 1e-30, None, op0=ALU.max),
                      reads=[PB[bi]], writes=[zzb] if hh == 0 else (), pwrites=() if hh == 0 else [zzb])
            im, imb = impr.next()
            rzc, rzcb = m8r.next()
            sc.op("dve", lambda e: e.reciprocal(rzc[:, 0:4], zz[:, 0:12:3]), reads=[zzb], writes=[rzcb])
            for hh in range(4):
                bi, co = accC[hh]
                Mh = psbank(bi, 64, co + 129)
                if hh == 0:
                    sc.op("dve", lambda e: e.tensor_scalar(im, Mh, rzc[:, 0:1], None, op0=ALU.mult), reads=[PB[bi], rzcb], writes=[imb])
                else:
                    sc.op("dve", lambda e: e.scalar_tensor_tensor(im, Mh, rzc[:, hh:hh + 1], im, op0=ALU.mult, op1=ALU.add),
                          reads=[PB[bi], rzcb, imb], pwrites=[imb])
            sc.op("pool", lambda e: e.memset(im[:, 2 * qt + 1:64], -1e9), reads=[imb], pwrites=[imb])
            sc.op("pool", lambda e: e.memset(im[:, 0:1], 1e9), pwrites=[imb])
            sc.op("pool", lambda e: e.memset(im[:, 2 * qt:2 * qt + 1], 1e9), pwrites=[imb])
            if qt >= 1:
                sc.op("pool", lambda e: e.memset(im[0:64, 2 * qt - 1:2 * qt], 1e9), pwrites=[imb])
            sc.op("pool", lambda e: e.memset(im[64:128, 2 * qt + 1:2 * qt + 2], 1e9), pwrites=[imb])
            m8, m8b = m8r.next()
            sc.op("dve", lambda e: e.max(out=m8, in_=im), reads=[imb], writes=[m8b])
            ng, ngb = negr.next()
            sc.op("dve", lambda e: e.tensor_scalar(ng, im, m8[:, 7:8], NEG, op0=ALU.is_lt, op1=ALU.mult), reads=[imb, m8b], writes=[ngb])
            dump("d_neg_%d_%d" % (g, qt), ng, ngb, BF16)

            def s_win(kt):
                ps, psb = s_bank()
                sc.op("pe", lambda e: e.matmul(ps, ksw[:, 1, kt * 128:(kt + 1) * 128], qrs, start=True, stop=True),
                      reads=[b_ksw, b_qrg], writes=[psb])
                return ps, psb

            def e_win(kt, ps, psb):
                if kt == qt:
                    return masked_exp(ps, psb, base=0, cm=-1, pat=[[0, 4], [1, 128]], cmp_op=ALU.is_ge)
                if kt == qt - 4:
                    return masked_exp(ps, psb, base=0, cm=1, pat=[[0, 4], [-1, 128]], cmp_op=ALU.is_gt)
                return plain_exp(ps, psb)

            run_branch(list(range(max(0, qt - 4), qt + 1)), s_win, e_win,
                       lambda kt, pt, ptb: pv_accumulate(pt, ptb, vwx, b_vwx, kt, accW, 129))
            psT = psbank(6, 64).bitcast(BF16)
            sc.op("pe", lambda e: e.transpose(psT[0:64, 0:128], ng, ident_b), reads=[ngb, b_identb], writes=[PB[6]])
            nT, nTb = negTr.next()
            sc.op("dve", lambda e: e.tensor_copy(nT[0:64, :].rearrange("p (h t) -> p h t", h=4), psT[0:64, 0:128].unsqueeze(1).to_broadcast([64, 4, 128])),
                  reads=[PB[6]], pwrites=[nTb])
            cf, cfb = cfr.next()
            sc.op("dve", lambda e: e.reciprocal(cf[:, 0:12:3], zz[:, 0:12:3]), reads=[zzb], writes=[cfb])
            sc.op("dve", lambda e: e.tensor_tensor(cf[:, 0:12:3], cf[:, 0:12:3], gates[:, qt, g * 12:g * 12 + 12:3], op=ALU.mult),
                  reads=[cfb, b_gates], pwrites=[cfb])
            oa_list = []
            for hh in range(4):
                bi, co = accC[hh]
                oa, oab = oacc.next()
                oa_list.append((oa, oab))
                sc.op("act", lambda e: e.activation(oa, psbank(bi, 128, co), AF.Copy, scale=cf[:, hh * 3:hh * 3 + 1]),
                      reads=[PB[bi], cfb], writes=[oab])
            for bi in (2, 3):
                sc.op("dve", lambda e, bi=bi: e.memset(psbank(bi, 258), 0.0), writes=[PB[bi]])

            def s_slc(kt):
                ps, psb = s_bank()
                sc.op("pe", lambda e: e.matmul(ps, ksw[:, 0, kt * 128:(kt + 1) * 128], qrs, start=True, stop=False),
                      reads=[b_ksw, b_qrg], writes=[psb], sig=False)
                sc.op("pe", lambda e: e.matmul(ps, Eall[:, kt * 128:(kt + 1) * 128], nT, start=False, stop=True),
                      reads=[b_Eall, nTb], pwrites=[psb])
                return ps, psb

            run_branch(list(range(qt + 1)), s_slc,
                       lambda kt, ps, psb: (masked_exp(ps, psb, base=0, cm=-1, pat=[[0, 4], [1, 128]], cmp_op=ALU.is_ge)
                                            if kt == qt else plain_exp(ps, psb)),
                       lambda kt, pt, ptb: pv_accumulate(pt, ptb, vsx, b_vsx, kt, accS, 129))
            for hh in range(4):
                bi, co = accS[hh]
                sc.op("dve", lambda e: e.tensor_scalar(zz[:, hh * 3 + 1:hh * 3 + 2], psbank(bi, 1, co + 128), 1e-30, None, op0=ALU.max),
                      reads=[PB[bi]], pwrites=[zzb])
                bi, co = accW[hh]
                sc.op("dve", lambda e: e.tensor_scalar(zz[:, hh * 3 + 2:hh * 3 + 3], psbank(bi, 1, co + 128), 1e-30, None, op0=ALU.max),
                      reads=[PB[bi]], pwrites=[zzb])
            for br in (1, 2):
                sc.op("dve", lambda e: e.reciprocal(cf[:, br:12:3], zz[:, br:12:3]), reads=[zzb, cfb], pwrites=[cfb])
                sc.op("dve", lambda e: e.tensor_tensor(cf[:, br:12:3], cf[:, br:12:3], gates[:, qt, g * 12 + br:g * 12 + 12:3], op=ALU.mult),
                      reads=[cfb, b_gates], pwrites=[cfb])
            ob, obb = obf.next()
            for hh in range(4):
                oa, oab = oa_list[hh]
                bs, cs = accS[hh]
                bw, cw_ = accW[hh]
                sc.op("dve", lambda e: e.scalar_tensor_tensor(oa, psbank(bs, 128, cs), cf[:, hh * 3 + 1:hh * 3 + 2], oa, op0=ALU.mult, op1=ALU.add),
                      reads=[PB[bs], cfb, oab], writes=[oab])
                sc.op("dve", lambda e: e.scalar_tensor_tensor(ob[:, hh, :], psbank(bw, 128, cw_), cf[:, hh * 3 + 2:hh * 3 + 3], oa, op0=ALU.mult, op1=ALU.add),
                      reads=[PB[bw], cfb, oab], writes=[obb] if hh == 0 else (), pwrites=() if hh == 0 else [obb])
            psO = psbank(6).bitcast(BF16)[:, 256:768]
            for hh in range(4):
                sc.op("pe", lambda e: e.transpose(psO[:, hh * 128:(hh + 1) * 128], ob[:, hh, :], ident_b),
                      reads=[obb, b_identb], writes=[PB[6]] if hh == 0 else (), pwrites=() if hh == 0 else [PB[6]], sig=(hh == 3))
            sc.op("act", lambda e: e.copy(onT[:, :, tq], psO.rearrange("p (h t) -> p h t", h=4)), reads=[PB[6]], pwrites=[b_onT])
        sc.dma(onT_rows[:, 4 * g:4 * g + 4, :], onT, b_onT, reads=[b_onT])
    sc.barrier()
    A.reset(base_mark)


def phase_mlstm(nc, sc, A, psbank, PB, check, L, dump, zT_d, ztm_d, zsm_d, hmT_d, tri_f, b_tri, ones_f, b_ones,
                ident_b, b_identb, mbias_d, mnormg_d, base_mark, **_):
    LN16 = float(np.log(16.0))
    sm = A.alloc([NT, 32], F32)
    b_sm = sc.buf("sm")
    sc.dma(sm, zsm_d.rearrange("(n p) c -> p n c", p=128), b_sm, writes=[b_sm])
    mbias = A.alloc([8], F32)
    b_mbias = sc.buf("mbias")
    sc.dma(mbias, mbias_d, b_mbias, writes=[b_mbias])
    fpre = A.alloc([NT, 4], F32)
    ipre = A.alloc([NT, 4], F32)
    b_fpre, b_ipre = sc.buf("fpre"), sc.buf("ipre")
    for h in range(4):
        sc.op("dve", lambda e, h=h: e.tensor_scalar(fpre[:, :, h], sm[:, :, 4 + h], mbias[:, 4 + h:5 + h], None, op0=ALU.add),
              reads=[b_sm, b_mbias], pwrites=[b_fpre])
        sc.op("dve", lambda e, h=h: e.tensor_scalar(ipre[:, :, h], sm[:, :, h], mbias[:, h:h + 1], None, op0=ALU.add),
              reads=[b_sm, b_mbias], pwrites=[b_ipre])
    fl = fpre.rearrange("p n h -> p (n h)")
    il = ipre.rearrange("p n h -> p (n h)")
    lf = A.alloc([128], F32)
    b_lf = sc.buf("lf")
    sc.op("act", lambda e: e.activation(lf, fl, AF.Exp, scale=-1.0), reads=[b_fpre], writes=[b_lf])
    sc.op("act", lambda e: e.activation(lf, lf, AF.Ln, bias=1.0), reads=[b_lf], writes=[b_lf])
    sc.op("dve", lambda e: e.tensor_scalar(lf, lf, -1.0, None, op0=ALU.mult), reads=[b_lf], writes=[b_lf])
    bcs = A.alloc([128], F32)
    ebl = A.alloc([128], F32)
    ik = A.alloc([128], F32)
    eks = A.alloc([128], F32)
    ebL = A.alloc([128], F32)
    b_bcs, b_ebl, b_ik, b_eks, b_ebL = [sc.buf(n_) for n_ in ("bcs", "ebl", "ik", "eks", "ebL")]
    ps0 = psbank(0, 128)
    sc.op("pe", lambda e: e.matmul(ps0, tri_f, lf, start=True, stop=True), reads=[b_tri, b_lf], writes=[PB[0]])
    sc.op("dve", lambda e: e.tensor_copy(bcs, ps0), reads=[PB[0]], writes=[b_bcs])
    sc.op("act", lambda e: e.activation(ebl, ps0, AF.Exp), reads=[PB[0]], writes=[b_ebl])
    ps1 = psbank(1, 128)
    sc.op("pe", lambda e: e.matmul(ps1, ones_f, lf, start=True, stop=True), reads=[b_ones, b_lf], writes=[PB[1]])
    sc.op("act", lambda e: e.activation(ebL, ps1, AF.Exp), reads=[PB[1]], writes=[b_ebL])
    sc.op("dve", lambda e: e.tensor_tensor(ik, il, bcs, op=ALU.subtract), reads=[b_ipre, b_bcs], writes=[b_ik])
    sc.op("dve", lambda e: e.tensor_scalar(ik, ik, -LN16, None, op0=ALU.add), reads=[b_ik], writes=[b_ik])
    sc.op("act", lambda e: e.activation(eks, ik, AF.Exp), reads=[b_ik], writes=[b_eks])
    mnormg = A.alloc([1024], F32)
    b_mnormg = sc.buf("mnormg")
    sc.dma(mnormg, mnormg_d, b_mnormg, writes=[b_mnormg])
    dump("d_lf", lf, b_lf); dump("d_bcs", bcs, b_bcs); dump("d_ebl", ebl, b_ebl); dump("d_ik", ik, b_ik)
    dump("d_eks", eks, b_eks); dump("d_ebL", ebL, b_ebL)
    check("Bgates")

    qkT = Ring(sc, "qkT", [A.alloc([4, S], BF16) for _ in range(2)])
    vh = Ring(sc, "vh", [A.alloc([NT, 257], BF16) for _ in range(2)])
    soh = Ring(sc, "soh", [A.alloc([NT, 256], BF16) for _ in range(2)])
    hmT = Ring(sc, "hmT", [A.alloc([2, S], BF16) for _ in range(2)])
    Cstate = Ring(sc, "Cst", [(A.alloc([2, 257], F32), A.alloc([2, 257], BF16), sc.buf("Cf"), sc.buf("Cb")) for _ in range(2)])
    DT = Ring(sc, "DT", [A.alloc([128], F32) for _ in range(2)])
    DTm = Ring(sc, "DTm", [A.alloc([128], F32) for _ in range(2)])
    WT = Ring(sc, "WT", [A.alloc([128], BF16) for _ in range(4)])
    k2 = Ring(sc, "k2", [A.alloc([256], BF16) for _ in range(4)])
    T1 = Ring(sc, "T1", [A.alloc([257], F32) for _ in range(2)])
    NUM = Ring(sc, "NUM", [A.alloc([257], F32) for _ in range(2)])
    hh = Ring(sc, "hh", [A.alloc([256], F32) for _ in range(2)])
    hfin = Ring(sc, "hfin", [A.alloc([256], BF16) for _ in range(2)])
    st8 = Ring(sc, "st8", [A.alloc([8], F32) for _ in range(4)])
    ctmp = Ring(sc, "ctmp", [A.alloc([257], F32) for _ in range(2)])
    zt_rows = zT_d.rearrange("(c p) t -> p c t", p=128)
    ztm_t = ztm_d.rearrange("(n p) c -> p n c", p=128)
    hmT_rows = hmT_d.rearrange("(c p) t -> p c t", p=128)
    def head_setup(h):
        qk, qkb = qkT.next()
        (Cf, Cb, b_Cf, b_Cb), _unused = Cstate.next()
        sc.dma(qk[:, 0:2, :], zt_rows[:, 2 * h:2 * h + 2, :], qkb, writes=[qkb])
        sc.dma(qk[:, 2:4, :], zt_rows[:, 8 + 2 * h:8 + 2 * h + 2, :], qkb, pwrites=[qkb])
        v, vb = vh.next()
        sc.dma(v[:, :, 0:256], ztm_t[:, :, h * 256:(h + 1) * 256], vb, writes=[vb])
        sc.op("pool", lambda e, v=v: e.memset(v[:, :, 256:257], 1.0), pwrites=[vb])
        so, sob = soh.next()
        sc.dma(so, ztm_t[:, :, 1024 + h * 256:1024 + (h + 1) * 256], sob, writes=[sob])
        hT, hTb = hmT.next()
        sc.op("pool", lambda e, hT=hT: e.memset(hT[:, 0, 0:2], 0.0), writes=[hTb])
        sc.op("pool", lambda e: e.memset(Cf, 0.0), writes=[b_Cf])
        sc.op("pool", lambda e: e.memset(Cb, 0.0), writes=[b_Cb])
        def stage1(n):
            col = n * 4 + h
            tk = slice(n * 128, (n + 1) * 128)
            psB = psbank(0, 128)
            sc.op("pe", lambda e, col=col: e.matmul(psB, lf[:, col:col + 1].to_broadcast([128, 128]), tri_f, start=True, stop=True),
                  reads=[b_lf, b_tri], writes=[PB[0]])
            dt, dtb = DT.next()
            sc.op("act", lambda e, dt=dt, col=col: e.activation(dt, psB, AF.Exp, bias=ik[:, col:col + 1]), reads=[PB[0], b_ik], writes=[dtb])
            dm, dmb = DTm.next()
            sc.op("pool", lambda e, dm=dm, dt=dt: e.tensor_tensor(dm, dt, tri_f, op=ALU.mult), reads=[dtb, b_tri], writes=[dmb])
            psS = psbank(1, 128)
            for c in range(2):
                sc.op("pe", lambda e, c=c, qk=qk, tk=tk: e.matmul(psS, qk[:, 2 + c, tk], qk[:, c, tk], start=(c == 0), stop=(c == 1)),
                      reads=[qkb], writes=[PB[1]] if c == 0 else (), pwrites=() if c == 0 else [PB[1]], sig=(c == 1))
            wt, wtb = WT.next()
            sc.op("dve", lambda e, wt=wt, dm=dm: e.tensor_tensor(wt, psS, dm, op=ALU.mult), reads=[PB[1], dmb], writes=[wtb])
            psK = psbank(2, 128).bitcast(BF16)
            for c in range(2):
                sc.op("pe", lambda e, c=c, qk=qk, tk=tk: e.transpose(psK[:, c * 128:(c + 1) * 128], qk[:, 2 + c, tk], ident_b),
                      reads=[qkb, b_identb], writes=[PB[2]] if c == 0 else (), pwrites=() if c == 0 else [PB[2]], sig=(c == 1))
            kk, kkb = k2.next()
            sc.op("act", lambda e, kk=kk, col=col: e.activation(kk, psK, AF.Copy, scale=eks[:, col:col + 1]), reads=[PB[2], b_eks], writes=[kkb])
            return wt, wtb, kk, kkb

        def stage2(n, wt, wtb, kk, kkb):
            col = n * 4 + h
            tk = slice(n * 128, (n + 1) * 128)
            psP1 = psbank(3, 257)
            for c in range(2):
                sc.op("pe", lambda e, c=c, qk=qk, tk=tk: e.matmul(psP1, qk[:, c, tk], Cb[:, c, :], start=(c == 0), stop=(c == 1)),
                      reads=[qkb, b_Cb], writes=[PB[3]] if c == 0 else (), pwrites=() if c == 0 else [PB[3]], sig=(c == 1))
            psP2 = psbank(4, 257)
            sc.op("pe", lambda e, wt=wt, v=v, n=n: e.matmul(psP2, wt, v[:, n, :], start=True, stop=True), reads=[wtb, vb], writes=[PB[4]])
            t1, t1b = T1.next()
            sc.op("act", lambda e, t1=t1, col=col: e.activation(t1, psP1, AF.Copy, scale=ebl[:, col:col + 1]), reads=[PB[3], b_ebl], writes=[t1b])
            nm, nmb = NUM.next()
            sc.op("dve", lambda e, nm=nm, t1=t1: e.tensor_tensor(nm, t1, psP2, op=ALU.add), reads=[t1b, PB[4]], writes=[nmb])
            s8, s8b = st8.next()
            sc.op("dve", lambda e, s8=s8, nm=nm: e.tensor_scalar(s8[:, 6:7], nm[:, 256:257], -1.0, None, op0=ALU.mult),
                  reads=[nmb], writes=[s8b])
            sc.op("dve", lambda e, s8=s8, nm=nm: e.scalar_tensor_tensor(s8[:, 0:1], nm[:, 256:257], 1.0, s8[:, 6:7], op0=ALU.max, op1=ALU.max),
                  reads=[nmb, s8b], pwrites=[s8b])
            sc.op("dve", lambda e, s8=s8: e.reciprocal(s8[:, 1:2], s8[:, 0:1]), reads=[s8b], pwrites=[s8b])
            hx, hxb = hh.next()
            sc.op("dve", lambda e, hx=hx, nm=nm, s8=s8: e.tensor_scalar(hx, nm[:, 0:256], s8[:, 1:2], None, op0=ALU.mult), reads=[nmb, s8b], writes=[hxb])
            hq, hqb = t1[:, 0:256], t1b
            sc.op("act", lambda e, hq=hq, hx=hx: e.activation(hq, hx, AF.Square), reads=[hxb, nmb], writes=[hqb])
            sc.op("dve", lambda e, s8=s8, hq=hq: e.reduce_sum(s8[:, 2:3], hq, axis=AX.X), reads=[hqb], pwrites=[s8b])
            sc.op("dve", lambda e, s8=s8: e.tensor_scalar(s8[:, 3:4], s8[:, 2:3], 1.0 / 256, EPS, op0=ALU.mult, op1=ALU.add), reads=[s8b], pwrites=[s8b])
            sc.op("act", lambda e, s8=s8: e.activation(s8[:, 4:5], s8[:, 3:4], AF.Ln), reads=[s8b], pwrites=[s8b])
            sc.op("act", lambda e, s8=s8: e.activation(s8[:, 5:6], s8[:, 4:5], AF.Exp, scale=-0.5), reads=[s8b], pwrites=[s8b])
            sc.op("dve", lambda e, hx=hx, s8=s8, h=h: e.scalar_tensor_tensor(hx, hx, s8[:, 5:6], mnormg[:, h * 256:(h + 1) * 256], op0=ALU.mult, op1=ALU.mult),
                  reads=[hxb, s8b, b_mnormg], writes=[hxb])
            hf, hfb = hfin.next()
            sc.op("pool", lambda e, hf=hf, hx=hx, so=so, n=n: e.tensor_tensor(hf, hx, so[:, n, :], op=ALU.mult), reads=[hxb, sob], writes=[hfb])
            psH = psbank(5, 128).bitcast(BF16)
            for c in range(2):
                sc.op("pe", lambda e, c=c, hf=hf: e.transpose(psH[:, c * 128:(c + 1) * 128], hf[:, c * 128:(c + 1) * 128], ident_b),
                      reads=[hfb, b_identb], writes=[PB[5]] if c == 0 else (), pwrites=() if c == 0 else [PB[5]], sig=(c == 1))
            sc.op("act", lambda e, hT=hT, tk=tk: e.copy(hT[:, :, tk], psH.rearrange("p (c t) -> p c t", c=2)), reads=[PB[5]], pwrites=[hTb])
            for c in range(2):
                psKV = psbank(6 + c, 257)
                sc.op("pe", lambda e, c=c, kk=kk, v=v, n=n, psKV=psKV: e.matmul(psKV, kk[:, c * 128:(c + 1) * 128], v[:, n, :], start=True, stop=True),
                      reads=[kkb, vb], writes=[PB[6 + c]])
                ct, ctb = ctmp.next()
                sc.op("dve", lambda e, c=c, ct=ct, psKV=psKV: e.tensor_tensor(ct, Cf[:, c, :], psKV, op=ALU.add), reads=[b_Cf, PB[6 + c]], writes=[ctb])
                sc.op("dve", lambda e, c=c, ct=ct, col=col: e.tensor_scalar(Cf[:, c, :], ct, ebL[:, col:col + 1], None, op0=ALU.mult),
                      reads=[ctb, b_ebL], pwrites=[b_Cf])
                sc.op("act", lambda e, c=c, ct=ct, col=col: e.activation(Cb[:, c, :], ct, AF.Copy, scale=ebL[:, col:col + 1]),
                      reads=[ctb, b_ebL], pwrites=[b_Cb])

        def finish():
            sc.dma(hmT_rows[:, 2 * h:2 * h + 2, :], hT, hTb, reads=[hTb])
        return stage1, stage2, finish

    for hp in range(2):
        H = [head_setup(2 * hp), head_setup(2 * hp + 1)]
        cur = [H[i][0](0) for i in range(2)]
        for n in range(NT):
            nxt_ = [H[i][0](n + 1) if n + 1 < NT else None for i in range(2)]
            for i in range(2):
                H[i][1](n, *cur[i])
            cur = nxt_
        for i in range(2):
            H[i][2]()
    sc.barrier()
    A.reset(base_mark)


def _finish(nc, sc, es):
    sc.barrier()

    with nc.Block() as block:
        @block.sync
        def _(e):
            sc.replay("sp", e)

        @block.tensor
        def _(e):
            sc.replay("pe", e)

        @block.scalar
        def _(e):
            sc.replay("act", e)

        @block.vector
        def _(e):
            sc.replay("dve", e)

        @block.gpsimd
        def _(e):
            sc.replay("pool", e)
    es.close()


def rope_tables_T():
    inv = (np.float32(10000.0) ** (-(np.arange(0, 128, 2, dtype=np.float32)) / np.float32(128))).astype(np.float32)
    ang = (np.arange(S, dtype=np.float32)[:, None] * inv[None, :]).astype(np.float32)
    ang = np.concatenate([ang, ang], axis=-1)
    cosT = np.cos(ang).astype(np.float32).T.copy()
    sinT = np.sin(ang).astype(np.float32).T.copy()
    sinT[:64, :] *= -1.0
    return np.ascontiguousarray(cosT), np.ascontiguousarray(sinT)


def _overlap_const():
    n = np.arange(256)[:, None]
    j = np.arange(64)[None, :]
    ov = ((16 * n < 64 * j + 64) & (16 * n + 32 > 64 * j) & (n < 255)).astype(np.float32)
    return np.ascontiguousarray(ov.reshape(2, 128, 64))


def _eall_const():
    j = np.arange(64)[:, None]
    p = np.arange(S)[None, :]
    return np.ascontiguousarray((p // 64 == j).astype(np.float32))


def _kblk(k1, k2):
    kb = np.zeros((128, 256), np.float32)
    kb[0:64, 0:128] = k1.T
    kb[64:128, 128:256] = k2.T
    return kb


def prep_shared(inputs):
    f = np.float32
    w_in = np.asarray(inputs["w_in"][0], dtype=f)

    def kp_layout(w):
        return np.ascontiguousarray(w.reshape(8, 128, w.shape[1]).transpose(1, 0, 2))

    wfm = w_in[:, fm_col_index()]
    w_fm = np.ascontiguousarray(kp_layout(wfm).reshape(128, 8, 60, 128).transpose(2, 0, 1, 3))
    wtm = w_in[:, tm_col_index()]
    w_tm = np.ascontiguousarray(kp_layout(wtm).reshape(128, 8, 5, 512).transpose(2, 0, 1, 3))
    w_sm = kp_layout(w_in[:, sm_col_index()])
    cosT, sinT = rope_tables_T()
    conv_w = np.asarray(inputs["m_conv_w"][0], dtype=f)
    conv_b = np.asarray(inputs["m_conv_b"][0], dtype=f)
    cw = np.concatenate([conv_w.T, conv_b[:, None]], axis=1)
    shared = {
        "g_mix": np.ascontiguousarray(np.broadcast_to(np.asarray(inputs["ln_mix_g"][0], dtype=f)[None, :], (128, D))),
        "w_fm": w_fm, "w_tm": w_tm, "w_sm": w_sm, "cosT": cosT, "sinT": sinT,
        "conv_w": np.ascontiguousarray(cw.reshape(16, 128, 5)),
        "ident": np.eye(128, dtype=f),
        "tri": np.triu(np.ones((128, 128), dtype=f)),
        "ovl": _overlap_const(), "eall": _eall_const(),
        "peer_u": np.ascontiguousarray(np.asarray(inputs["peer_u"][0], dtype=f)),
        "peer_v": np.ascontiguousarray(np.asarray(inputs["peer_v"][0], dtype=f)),
        "wq": kp_layout(np.asarray(inputs["peer_wq"][0], dtype=f)),
        "kblk": _kblk(np.asarray(inputs["peer_k1"][0], dtype=f), np.asarray(inputs["peer_k2"][0], dtype=f)),
        "iota": np.ascontiguousarray(np.broadcast_to(np.arange(128, dtype=f)[None, :], (128, 128))),
        "g_fin": np.ascontiguousarray(np.broadcast_to(np.asarray(inputs["ln_f_g"], dtype=f)[None, :], (128, D))),
        "wsq": np.ascontiguousarray(np.stack([kp_layout(np.asarray(inputs[k][0], dtype=f)) for k in ("w_m_out", "w_n_out", "w_out")])),
        "g_ffn": np.ascontiguousarray(np.broadcast_to(np.asarray(inputs["ln_ffn_g"][0], dtype=f)[None, :], (128, D))),
        "cw1": np.ascontiguousarray(np.stack([np.asarray(inputs[k][0], dtype=f).reshape(32, 128, 128).transpose(1, 0, 2)
                                              for k in ("cmp_k_w1", "cmp_v_w1")])),
        "cw2": np.ascontiguousarray(np.stack([np.asarray(inputs[k][0], dtype=f) for k in ("cmp_k_w2", "cmp_v_w2")])),
        "cpos": np.ascontiguousarray(np.stack([np.asarray(inputs[k][0], dtype=f).T for k in ("cmp_k_pos", "cmp_v_pos")])),
        "m_bias": np.ascontiguousarray(np.broadcast_to(np.concatenate([np.asarray(inputs["m_i_bias"][0], dtype=f),
                                                                       np.asarray(inputs["m_f_bias"][0], dtype=f)])[None, :], (128, 8))),
        "m_norm_g": np.ascontiguousarray(np.broadcast_to(np.asarray(inputs["m_norm_g"][0], dtype=f)[None, :], (128, 1024))),
    }
    return shared


def kernel(**inputs):
    shared = prep_shared(inputs)
    x = np.asarray(inputs["x"], dtype=np.float32)
    nc = build()
    in_maps = []
    for c in range(N_CORES):
        m = dict(shared)
        m["x"] = np.ascontiguousarray(x[c])
        in_maps.append(m)
    res = run_bass_kernel_spmd(nc, in_maps, core_ids=list(range(N_CORES)))
    return np.stack([np.asarray(r["out"], dtype=np.float32) for r in res.results], axis=0)
```

```python
from contextlib import ExitStack
import numpy as np
import ml_dtypes
import concourse.bass as bass
import concourse.mybir as mybir
from concourse.bass_utils import run_bass_kernel_spmd

F32 = mybir.dt.float32
BF16 = mybir.dt.bfloat16
U32 = mybir.dt.uint32
ALU = mybir.AluOpType
AF = mybir.ActivationFunctionType
AX = mybir.AxisListType

S = 4096
D = 1024
NT = 32
EPS = 1e-6
N_CORES = 8
E2_VQ_ACT = False
SAME_ENGINE_WAITS = True


class Buf:
    __slots__ = ("name", "w", "r", "dsem", "dcnt", "excl")

    def __init__(self, name, excl=False):
        self.name = name
        self.excl = excl
        self.w = {}
        self.r = {}
        self.dsem = None
        self.dcnt = 0


class _Rec:
    def __init__(self):
        self.call = None

    def __getattr__(self, name):
        def f(*a, **k):
            self.call = (name, a, k)
            return self
        return f


def _bind(fn):
    rec = _Rec()
    fn(rec)
    name, a, k = rec.call

    def run(e):
        try:
            return getattr(e, name)(*a, **k)
        except Exception:
            print("FAILED OP", name, [getattr(x, "shape", x) for x in a], {kk: getattr(v, "shape", v) for kk, v in k.items()})
            raise
    return run


class Sched:
    ENG = ("pe", "act", "dve", "pool", "sp")

    def __init__(self, nc, es):
        self.nc, self.es = nc, es
        self.q = {e: [] for e in self.ENG}
        self.sems = []
        self.ecnt = {e: 0 for e in self.ENG}
        self.seen = {e: {} for e in self.ENG}
        self.pending = {e: [] for e in self.ENG}
        self.esem = {}
        for e in ("pe", "act", "dve", "pool"):
            self.esem[e] = self.new_sem("s_" + e)
        self.dbufs = []
        self.sem_pool = []
        self.nbuf = 0

    def new_sem(self, name):
        h = self.es.enter_context(self.nc.semaphore(name))
        self.sems.append(h)
        return len(self.sems) - 1

    def buf(self, name="b"):
        self.nbuf += 1
        return Buf("%s%d" % (name, self.nbuf))

    def _waits(self, eng, reads, writes, pwrites):
        need = {}

        def add(evs):
            for s, v in evs.items():
                if need.get(s, 0) < v:
                    need[s] = v

        for b in reads:
            add(b.w)
            if b.excl:
                add(b.r)
        for b in writes:
            add(b.w)
            add(b.r)
        for b in pwrites:
            add(b.w)
            add(b.r)
        out = []
        seen = self.seen[eng]
        own = self.esem.get(eng)
        for s, v in need.items():
            if s == own and not SAME_ENGINE_WAITS:
                continue
            if seen.get(s, 0) < v:
                seen[s] = v
                out.append((s, v))
        return out

    @staticmethod
    def _commit(ev, reads, writes, pwrites):
        s, v = ev
        for b in writes:
            b.w = {s: v}
            b.r = {}
        for b in pwrites:
            if b.w.get(s, 0) < v:
                b.w[s] = v
        for b in reads:
            if b.r.get(s, 0) < v:
                b.r[s] = v

    def op(self, eng, fn, reads=(), writes=(), pwrites=(), sig=True):
        fn = _bind(fn)
        waits = self._waits(eng, reads, writes, pwrites)
        if not sig:
            self.q[eng].append((waits, fn, None))
            self.pending[eng].append((reads, writes, pwrites))
            return
        self.ecnt[eng] += 1
        ev = (self.esem[eng], self.ecnt[eng])
        self.q[eng].append((waits, fn, (ev[0], 1)))
        for (r, w, pw) in self.pending[eng]:
            self._commit(ev, r, w, pw)
        self.pending[eng] = []
        self._commit(ev, reads, writes, pwrites)

    def dma(self, out_ap, in_ap, sb, reads=(), writes=(), pwrites=(), q="sp"):
        waits = self._waits(q, reads, writes, pwrites)
        if sb.dsem is None:
            if self.sem_pool:
                sb.dsem, sb.dcnt = self.sem_pool.pop()
            else:
                sb.dsem = self.new_sem("d_" + sb.name)
            self.dbufs.append(sb)
        sb.dcnt += 16
        ev = (sb.dsem, sb.dcnt)
        self.q[q].append((waits, lambda e: e.dma_start(out=out_ap, in_=in_ap), (sb.dsem, 16)))
        self._commit(ev, reads, writes, pwrites)

    def barrier(self):
        evs = [(self.esem[e], self.ecnt[e]) for e in ("pe", "act", "dve", "pool") if self.ecnt[e] > 0]
        evs += [(b.dsem, b.dcnt) for b in self.dbufs]
        for e in self.ENG:
            seen = self.seen[e]
            ws = []
            for s, v in evs:
                if seen.get(s, 0) < v:
                    seen[s] = v
                    ws.append((s, v))
            if ws:
                self.q[e].append((ws, None, None))
        for b in self.dbufs:
            self.sem_pool.append((b.dsem, b.dcnt))
            b.dsem = None
        self.dbufs = []

    def replay(self, name, eng):
        for waits, fn, inc in self.q[name]:
            for s, v in waits:
                eng.wait_ge(self.sems[s], v)
            if fn is None:
                continue
            ins = fn(eng)
            if inc is not None:
                ins.then_inc(self.sems[inc[0]], inc[1])


class Ring:
    def __init__(self, sc, name, aps, bufs=None):
        self.items = [(ap, sc.buf(name) if bufs is None else bufs[i]) for i, ap in enumerate(aps)]
        self.i = 0

    def next(self):
        it = self.items[self.i % len(self.items)]
        self.i += 1
        return it


class Arena:
    def __init__(self, t, nbytes):
        self.t = t
        self.cap = nbytes
        self.off = 0

    def mark(self):
        return self.off

    def reset(self, m):
        self.off = m

    def alloc(self, shape, dt):
        esz = 4 if dt in (F32, U32) else 2
        n = 1
        for s_ in shape:
            n *= s_
        nb = (n * esz + 31) // 32 * 32
        assert self.off + nb <= self.cap, ("SBUF arena overflow", self.off, nb, self.cap)
        a = self.t[:, self.off // 4:(self.off + nb) // 4]
        self.off += nb
        if esz == 2:
            a = a.bitcast(dt)
        elif dt is not F32:
            a = a.bitcast(dt)
        a = a[:, 0:n]
        if len(shape) == 2:
            return a.rearrange("p (a b) -> p a b", a=shape[0])
        if len(shape) == 3:
            return a.rearrange("p (a b c) -> p a b c", a=shape[0], b=shape[1])
        return a


IN_WIDTHS = (2048, 1024, 1024, 4, 4, 1024, 256, 256, 256, 256, 256, 256, 24, 1024, 1024)
_off = np.cumsum((0,) + IN_WIDTHS)
(O_QK, O_V, O_O, O_I, O_F, O_NQ, O_KC, O_VC, O_KS, O_VS, O_KW, O_VW, O_NG, O_GA, O_GB) = [int(v) for v in _off[:-1]]


def _rot_cols(base, nheads):
    idx = []
    for h in range(nheads):
        for d in range(128):
            idx.append(base + h * 128 + (d + 64) % 128)
    return idx


def fm_col_index():
    cols = list(range(O_QK, O_QK + 2048))
    cols += list(range(O_NQ, O_NQ + 1024)) + _rot_cols(O_NQ, 8)
    cols += list(range(O_KC, O_KC + 256)) + list(range(O_VC, O_VC + 256))
    cols += list(range(O_KS, O_KS + 256)) + _rot_cols(O_KS, 2)
    cols += list(range(O_KW, O_KW + 256)) + _rot_cols(O_KW, 2)
    cols += list(range(O_GA, O_GA + 1024)) + list(range(O_GB, O_GB + 1024))
    return np.asarray(cols)


def tm_col_index():
    cols = list(range(O_V, O_V + 1024)) + list(range(O_O, O_O + 1024))
    cols += list(range(O_VS, O_VS + 256)) + list(range(O_VW, O_VW + 256))
    return np.asarray(cols)


def sm_col_index():
    return np.asarray(list(range(O_I, O_I + 4)) + list(range(O_F, O_F + 4)) + list(range(O_NG, O_NG + 24)))


ZR_QK, ZR_Q, ZR_QR, ZR_KC, ZR_VC, ZR_KSR, ZR_KWR, ZR_GA, ZR_GB = 0, 2048, 3072, 4096, 4352, 4608, 4864, 5120, 6144
ZT_ROWS = 7168


class _Stop(Exception):
    pass


def build(dbg=(), stop=None):
    nc = bass.Bass("TRN2", target_bir_lowering=False)
    es = ExitStack()
    dbg = set(dbg)

    def check(name):
        if stop == name:
            raise _Stop()

    def din(name, shape, dt=F32):
        return nc.dram_tensor(name, list(shape), dt, kind="ExternalInput").ap()

    def dscr(name, shape, dt):
        kind = "ExternalOutput" if name in dbg else "Internal"
        return nc.dram_tensor(name, list(shape), dt, kind=kind).ap()

    x_d = din("x", [S, D])
    gmix_d = din("g_mix", [128, D])
    wfm_d = din("w_fm", [60, 128, 8, 128])
    wtm_d = din("w_tm", [5, 128, 8, 512])
    wsm_d = din("w_sm", [128, 8, 32])
    cos_d = din("cosT", [128, S])
    sin_d = din("sinT", [128, S])
    convw_d = din("conv_w", [16, 128, 5])
    ident_d = din("ident", [128, 128])
    tri_d = din("tri", [128, 128])
    mbias_d = din("m_bias", [128, 8])
    mnormg_d = din("m_norm_g", [128, 1024])
    out_d = nc.dram_tensor("out", [S, D], F32, kind="ExternalOutput").ap()

    zT_d = dscr("zT", [ZT_ROWS, S], BF16)
    ztm_d = dscr("ztm", [S, 2560], BF16)
    zsm_d = dscr("zsm", [S, 32], F32)
    hmT_d = dscr("hmT", [1024, S], BF16)
    onT_d = dscr("onT", [1024, S], BF16)
    h1_d = dscr("h1", [S, D], F32)
    xhT_d = dscr("xhT", [1024, S], BF16)
    wsq_d = din("wsq", [3, 128, 8, 1024])
    u_d = din("peer_u", [16384, 1024])
    v_d = din("peer_v", [16384, 1024])
    uT_d = dscr("uT", [128, 128, 8, 128], BF16)
    vb_d = dscr("vb", [128, 128, 1024], BF16)
    wq_d = din("wq", [128, 8, 1024])
    kblk_d = din("kblk", [128, 256])
    iota_d = din("iota", [128, 128])
    gf_d = din("g_fin", [128, D])
    gffn_d = din("g_ffn", [128, D])
    ovl_d = din("ovl", [2, 128, 64])
    eall_d = din("eall", [64, S])
    cw1_d = din("cw1", [2, 128, 32, 128])
    cw2_d = din("cw2", [2, 128, 128])
    cpos_d = din("cpos", [2, 128, 32])

    ARENA_BYTES = 204 * 1024
    arena_t = es.enter_context(nc.sbuf_tensor("arena", [128, ARENA_BYTES // 4], F32))
    psum_t = es.enter_context(nc.psum_tensor("psum", [128, 4096], F32))
    sc = Sched(nc, es)
    A = Arena(arena_t, ARENA_BYTES)

    def psbank(i, n=512, off=0):
        return psum_t[:, i * 512 + off:i * 512 + off + n]

    PB = [Buf("psb%d" % i, excl=True) for i in range(8)]

    try:
        _body(nc, sc, A, psbank, PB, check, locals())
    except _Stop:
        pass
    _finish(nc, sc, es)
    return nc


def _body(nc, sc, A, psbank, PB, check, L):
    (x_d, gmix_d, wfm_d, wtm_d, wsm_d, cos_d, sin_d, convw_d, ident_d, out_d, zT_d, ztm_d, zsm_d) = [L[k] for k in (
        "x_d", "gmix_d", "wfm_d", "wtm_d", "wsm_d", "cos_d", "sin_d", "convw_d", "ident_d", "out_d", "zT_d", "ztm_d", "zsm_d")]
    tri_d, mbias_d, mnormg_d, hmT_d = L["tri_d"], L["mbias_d"], L["mnormg_d"], L["hmT_d"]
    dbg = L["dbg"]

    def dump(name, ap, b, dt=F32):
        if name in dbg:
            d = nc.dram_tensor(name, [128] + list(ap.shape[1:]), dt, kind="ExternalOutput").ap()
            sc.dma(d, ap, b, reads=[b])

    ident_f = A.alloc([128], F32)
    ident_b = A.alloc([128], BF16)
    b_ident = sc.buf("ident")
    sc.dma(ident_f, ident_d, b_ident, writes=[b_ident])
    b_identb = sc.buf("identb")
    sc.op("dve", lambda e: e.tensor_copy(ident_b, ident_f), reads=[b_ident], writes=[b_identb])
    tri_f = A.alloc([128], F32)
    b_tri = sc.buf("tri")
    sc.dma(tri_f, tri_d, b_tri, writes=[b_tri])
    ones_f = A.alloc([128], F32)
    b_ones = sc.buf("ones")
    sc.op("pool", lambda e: e.memset(ones_f, 1.0), writes=[b_ones])
    base_mark = A.mark()
    check("const")

    xnT = A.alloc([8, S], BF16)
    b_xnT = sc.buf("xnT")
    gmix = A.alloc([D], F32)
    b_gmix = sc.buf("gmix")
    sc.dma(gmix, gmix_d, b_gmix, writes=[b_gmix])
    mA = A.mark()
    xin = Ring(sc, "xin", [A.alloc([D], F32) for _ in range(2)])
    sqr = Ring(sc, "sq", [A.alloc([D], F32) for _ in range(1)])
    xnb = Ring(sc, "xnb", [A.alloc([D], BF16) for _ in range(2)])
    stat = Ring(sc, "stat", [A.alloc([4], F32) for _ in range(2)])
    pst = Ring(sc, "pst", [psbank(i).bitcast(BF16) for i in (6, 7)], [PB[6], PB[7]])
    x_t = x_d.rearrange("(n p) d -> n p d", p=128)
    for t in range(NT):
        xa, xb = xin.next()
        sc.dma(xa, x_t[t], xb, writes=[xb])
        sq, sqb = sqr.next()
        st, stb = stat.next()
        sc.op("act", lambda e, sq=sq, xa=xa: e.activation(sq, xa, AF.Square), reads=[xb], writes=[sqb])
        sc.op("dve", lambda e, st=st, sq=sq: e.reduce_sum(st[:, 0:1], sq, axis=AX.X), reads=[sqb], writes=[stb])
        sc.op("dve", lambda e, st=st: e.tensor_scalar(st[:, 1:2], st[:, 0:1], 1.0 / D, EPS, op0=ALU.mult, op1=ALU.add),
              reads=[stb], pwrites=[stb])
        sc.op("act", lambda e, st=st: e.activation(st[:, 2:3], st[:, 1:2], AF.Ln), reads=[stb], pwrites=[stb])
        sc.op("act", lambda e, st=st: e.activation(st[:, 3:4], st[:, 2:3], AF.Exp, scale=-0.5), reads=[stb], pwrites=[stb])
        xn, xnbuf = xnb.next()
        sc.op("dve", lambda e, xn=xn, xa=xa, st=st: e.scalar_tensor_tensor(
            xn, xa, st[:, 3:4], gmix, op0=ALU.mult, op1=ALU.mult), reads=[xb, stb, b_gmix], writes=[xnbuf])
        if t == 1:
            check("norm1")
        pt, ptb = pst.next()
        for k in range(8):
            sc.op("pe", lambda e, pt=pt, xn=xn, k=k: e.transpose(pt[:, k * 128:(k + 1) * 128], xn[:, k * 128:(k + 1) * 128], ident_b),
                  reads=[xnbuf, b_identb], writes=[ptb] if k == 0 else (), pwrites=() if k == 0 else [ptb], sig=(k == 7))
        if t == 1:
            check("norm2")
        sc.op("act", lambda e, pt=pt, t=t: e.copy(xnT[:, :, t * 128:(t + 1) * 128], pt.rearrange("p (k c) -> p k c", k=8)),
              reads=[ptb], pwrites=[b_xnT])
        check("norm3_%d" % t)
    sc.barrier()
    A.reset(mA)
    check("norm")

    cosT = A.alloc([S], F32)
    sinT = A.alloc([S], F32)
    b_cos, b_sin = sc.buf("cos"), sc.buf("sin")
    sc.dma(cosT, cos_d, b_cos, writes=[b_cos])
    sc.dma(sinT, sin_d, b_sin, writes=[b_sin])

    wf32 = Ring(sc, "wf32", [A.alloc([8, 128], F32) for _ in range(4)])
    wbf = Ring(sc, "wbf", [A.alloc([8, 128], BF16) for _ in range(4)])
    zrow = Ring(sc, "zrow", [A.alloc([S], BF16) for _ in range(3)])
    z32 = Ring(sc, "z32", [A.alloc([S + 4], F32) for _ in range(1)])
    acc = Ring(sc, "acc", [A.alloc([S], F32) for _ in range(1)])
    cw = Ring(sc, "cw", [A.alloc([5], F32) for _ in range(2)])
    t1r = Ring(sc, "t1", [A.alloc([512], F32) for _ in range(2)])
    t2r = Ring(sc, "t2", [A.alloc([512], F32) for _ in range(2)])
    psA = Ring(sc, "psA", [psbank(i) for i in range(6)], PB[0:6])
    evq = [0]

    wmemo = {}

    def load_w(c):
        if c in wmemo:
            return wmemo.pop(c)
        return _load_w(c)

    def prefetch_w(cs):
        for c in cs:
            if c not in wmemo:
                wmemo[c] = _load_w(c)

    def _load_w(c):
        wa, wb = wf32.next()
        sc.dma(wa, wfm_d[c], wb, writes=[wb])
        wba, wbb = wbf.next()
        sc.op("pool", lambda e: e.tensor_copy(wba, wa), reads=[wb], writes=[wbb])
        return wba, wbb

    def proj_group(wba, wbb, g):
        ps, psb = psA.next()
        for k in range(8):
            sc.op("pe", lambda e, k=k: e.matmul(ps, wba[:, k, :], xnT[:, k, g * 512:(g + 1) * 512], start=(k == 0), stop=(k == 7)),
                  reads=[wbb, b_xnT], writes=[psb] if k == 0 else (), pwrites=() if k == 0 else [psb], sig=(k == 7))
        return ps, psb

    def store_row(zr, zrb, row0):
        sc.dma(zT_d[row0:row0 + 128, :], zr, zrb, reads=[zrb])

    def evac_copy(dst, dstb, ps, psb, func=None):
        evq[0] += 1
        if func is not None:
            sc.op("act", lambda e: e.activation(dst, ps, func), reads=[psb], pwrites=[dstb])
        elif evq[0] % 2 == 0:
            sc.op("act", lambda e: e.copy(dst, ps), reads=[psb], pwrites=[dstb])
        else:
            sc.op("dve", lambda e: e.tensor_copy(dst, ps), reads=[psb], pwrites=[dstb])

    def qk_job(c):
        wba, wbb = load_w(c)
        cwa, cwb = cw.next()
        sc.dma(cwa, convw_d[c], cwb, writes=[cwb])
        za, zb = z32.next()
        sc.op("pool", lambda e, za=za: e.memset(za[:, 0:4], 0.0), writes=[zb])
        for g in range(8):
            ps, psb = proj_group(wba, wbb, g)
            evac_copy(za[:, 4 + g * 512:4 + (g + 1) * 512], zb, ps, psb)
        aa, ab = acc.next()
        sc.op("dve", lambda e, aa=aa, za=za, cwa=cwa: e.tensor_scalar(aa, za[:, 4:4 + S], cwa[:, 3:4], cwa[:, 4:5], op0=ALU.mult, op1=ALU.add),
              reads=[zb, cwb], writes=[ab])
        sc.op("dve", lambda e, aa=aa, za=za, cwa=cwa: e.scalar_tensor_tensor(aa, za[:, 3:3 + S], cwa[:, 2:3], aa, op0=ALU.mult, op1=ALU.add),
              reads=[zb, cwb, ab], pwrites=[ab])
        sc.op("dve", lambda e, aa=aa, za=za, cwa=cwa: e.scalar_tensor_tensor(aa, za[:, 2:2 + S], cwa[:, 1:2], aa, op0=ALU.mult, op1=ALU.add),
              reads=[zb, cwb, ab], pwrites=[ab])
        sc.op("dve", lambda e, aa=aa, za=za, cwa=cwa: e.scalar_tensor_tensor(aa, za[:, 1:1 + S], cwa[:, 0:1], aa, op0=ALU.mult, op1=ALU.add),
              reads=[zb, cwb, ab], pwrites=[ab])
        zr, zrb = zrow.next()
        sc.op("act", lambda e, zr=zr, aa=aa: e.activation(zr, aa, AF.Silu), reads=[ab], writes=[zrb])
        store_row(zr, zrb, ZR_QK + c * 128)

    check("qk")
    def rope_job(c_plain, c_rot, row_plain, row_rot):
        wba, wbb = load_w(c_plain)
        wbr, wbrb = load_w(c_rot)
        if row_plain is not None:
            zp, zpb = zrow.next()
            sc.op("pool", lambda e: e.memset(zp[:, 0:1], 0.0), writes=[zpb])
        zr, zrb = zrow.next()
        sc.op("pool", lambda e: e.memset(zr[:, 0:1], 0.0), writes=[zrb])
        for g in range(8):
            sl = slice(g * 512, (g + 1) * 512)
            ps, psb = proj_group(wba, wbb, g)
            ps2, ps2b = proj_group(wbr, wbrb, g)
            if row_plain is not None:
                sc.op("act", lambda e, ps=ps, sl=sl: e.copy(zp[:, sl], ps), reads=[psb], pwrites=[zpb])
            t1, t1b = t1r.next()
            t2, t2b = t2r.next()
            sc.op("dve", lambda e, t1=t1, ps=ps, sl=sl: e.tensor_tensor(t1, ps, cosT[:, sl], op=ALU.mult), reads=[psb, b_cos], writes=[t1b])
            sc.op("dve", lambda e, t2=t2, ps2=ps2, sl=sl: e.tensor_tensor(t2, ps2, sinT[:, sl], op=ALU.mult), reads=[ps2b, b_sin], writes=[t2b])
            sc.op("pool", lambda e, t1=t1, t2=t2, sl=sl: e.tensor_tensor(zr[:, sl], t1, t2, op=ALU.add), reads=[t1b, t2b], pwrites=[zrb])
        if row_plain is not None:
            store_row(zp, zpb, row_plain)
        store_row(zr, zrb, row_rot)

    jobs = [((c,), (lambda c=c: qk_job(c))) for c in range(16)]
    for h in range(8):
        jobs.append(((16 + h, 24 + h), (lambda h=h: rope_job(16 + h, 24 + h, ZR_Q + h * 128, ZR_QR + h * 128))))
    for gI in range(2):
        jobs.append(((36 + gI, 38 + gI), (lambda gI=gI: rope_job(36 + gI, 38 + gI, None, ZR_KSR + gI * 128))))
        jobs.append(((40 + gI, 42 + gI), (lambda gI=gI: rope_job(40 + gI, 42 + gI, None, ZR_KWR + gI * 128))))

    check("rope")
    def plain_job(c, row0, func=None):
        wba, wbb = load_w(c)
        zr, zrb = zrow.next()
        sc.op("pool", lambda e: e.memset(zr[:, 0:1], 0.0), writes=[zrb])
        for g in range(8):
            ps, psb = proj_group(wba, wbb, g)
            evac_copy(zr[:, g * 512:(g + 1) * 512], zrb, ps, psb, func)
        store_row(zr, zrb, row0)

    for i in range(2):
        jobs.append(((32 + i,), (lambda i=i: plain_job(32 + i, ZR_KC + i * 128))))
        jobs.append(((34 + i,), (lambda i=i: plain_job(34 + i, ZR_VC + i * 128))))
    for i in range(8):
        jobs.append(((44 + i,), (lambda i=i: plain_job(44 + i, ZR_GA + i * 128, AF.Sigmoid))))
        jobs.append(((52 + i,), (lambda i=i: plain_job(52 + i, ZR_GB + i * 128, AF.Sigmoid))))
    prefetch_w(jobs[0][0])
    for ji, (cs, fn) in enumerate(jobs):
        if ji + 1 < len(jobs):
            prefetch_w(jobs[ji + 1][0])
        fn()

    check("fm")
    sc.barrier()
    A.reset(mA)
    wt32 = Ring(sc, "wt32", [A.alloc([8, 512], F32) for _ in range(1)])
    wtbf = Ring(sc, "wtbf", [A.alloc([8, 512], BF16) for _ in range(2)])
    ztile = Ring(sc, "ztile", [A.alloc([512], BF16) for _ in range(3)])
    zstile = Ring(sc, "zstile", [A.alloc([32], F32) for _ in range(3)])
    for blk in range(6):
        small = blk == 5
        n = 32 if small else 512
        wa, wb = wt32.next()
        wba, wbb = wtbf.next()
        if small:
            sc.dma(wa[:, :, 0:32], wsm_d, wb, writes=[wb])
        else:
            sc.dma(wa, wtm_d[blk], wb, writes=[wb])
        sc.op("pool", lambda e, wba=wba, wa=wa, n=n: e.tensor_copy(wba[:, :, 0:n], wa[:, :, 0:n]), reads=[wb], writes=[wbb])
        func = AF.Sigmoid if blk in (2, 3) else None
        for t in range(NT):
            ps, psb = psA.next()
            for k in range(8):
                sc.op("pe", lambda e, ps=ps, wba=wba, k=k, t=t, n=n: e.matmul(ps[:, 0:n], xnT[:, k, t * 128:(t + 1) * 128], wba[:, k, 0:n],
                                                                      start=(k == 0), stop=(k == 7)),
                      reads=[wbb, b_xnT], writes=[psb] if k == 0 else (), pwrites=() if k == 0 else [psb], sig=(k == 7))
            if small:
                zt, ztb = zstile.next()
                sc.op("dve", lambda e, zt=zt, ps=ps: e.tensor_copy(zt, ps[:, 0:32]), reads=[psb], writes=[ztb])
                sc.dma(zsm_d[t * 128:(t + 1) * 128, :], zt, ztb, reads=[ztb])
            else:
                zt, ztb = ztile.next()
                evq[0] += 1
                if func is not None or evq[0] % 2 == 0:
                    sc.op("act", lambda e, zt=zt, ps=ps, func=func: e.activation(zt, ps, func if func is not None else AF.Copy),
                          reads=[psb], writes=[ztb])
                else:
                    sc.op("dve", lambda e, zt=zt, ps=ps: e.tensor_copy(zt, ps), reads=[psb], writes=[ztb])
                sc.dma(ztm_d[t * 128:(t + 1) * 128, blk * 512:(blk + 1) * 512], zt, ztb, reads=[ztb])
    sc.barrier()
    A.reset(base_mark)
    check("A")
    phase_mlstm(**locals())
    check("B")
    phase_nsa(**locals())
    check("C")
    phase_mixout(**locals())
    check("D")
    phase_peer(**locals())
    check("E")


def phase_peer(nc, sc, A, psbank, PB, check, L, dump, ident_f, b_ident, ident_b, b_identb, base_mark, **_):
    xhT_d, h1_d, out_d, uT_d, vb_d = L["xhT_d"], L["h1_d"], L["out_d"], L["uT_d"], L["vb_d"]
    xh_rows = xhT_d.rearrange("(c p) t -> p c t", p=128)
    h1_t = h1_d.rearrange("(n p) d -> n p d", p=128)
    out_t = out_d.rearrange("(n p) d -> n p d", p=128)
    NEGBIG = -1e30

    aT = A.alloc([S], BF16)
    bT = A.alloc([S], BF16)
    wT = A.alloc([S], F32)
    b_aT, b_bT, b_wT = sc.buf("aT"), sc.buf("bT"), sc.buf("wT")
    iota_f = A.alloc([128], F32)
    iota_b = A.alloc([128], BF16)
    b_iota, b_iotab = sc.buf("iota"), sc.buf("iotab")
    sc.dma(iota_f, L["iota_d"], b_iota, writes=[b_iota])
    sc.op("dve", lambda e: e.tensor_copy(iota_b, iota_f), reads=[b_iota], writes=[b_iotab])
    gfin = A.alloc([1024], F32)
    b_gfin = sc.buf("gfin")
    sc.dma(gfin, L["gf_d"], b_gfin, writes=[b_gfin])
    m1 = A.mark()

    wqb = A.alloc([8, 1024], BF16)
    mwq = A.mark()
    wqf = A.alloc([8, 1024], F32)
    b_wqf, b_wqb = sc.buf("wqf"), sc.buf("wqb")
    sc.dma(wqf, L["wq_d"], b_wqf, writes=[b_wqf])
    sc.op("pool", lambda e: e.tensor_copy(wqb, wqf), reads=[b_wqf], writes=[b_wqb])
    sc.barrier()
    A.reset(mwq)
    u_ch = L["u_d"].rearrange("(a p) d -> a p d", p=128)
    v_ch = L["v_d"].rearrange("(a p) d -> a p d", p=128)
    uf = Ring(sc, "uf", [A.alloc([1024], F32) for _ in range(3)])
    ubf = Ring(sc, "ubf", [A.alloc([1024], BF16) for _ in range(2)])
    uTs = Ring(sc, "uTs", [A.alloc([8, 128], BF16) for _ in range(2)])
    vf = Ring(sc, "vf", [A.alloc([1024], F32) for _ in range(3)])
    vbf = Ring(sc, "vbf", [A.alloc([1024], BF16) for _ in range(2)])
    ptE0 = psbank(1).bitcast(BF16)
    e0l = {}

    def e0_load(a):
        ua, uab = uf.next()
        sc.dma(ua, u_ch[a], uab, writes=[uab])
        va, vab = vf.next()
        sc.dma(va, v_ch[a], vab, writes=[vab])
        e0l[a] = (ua, uab, va, vab)

    def e0_chunk(a):
        if a + 2 < 128:
            e0_load(a + 2)
        ua, uab, va, vab = e0l.pop(a)
        ub, ubb = ubf.next()
        sc.op("pool", lambda e: e.tensor_copy(ub, ua), reads=[uab], writes=[ubb])
        for k in range(8):
            sc.op("pe", lambda e, k=k: e.transpose(ptE0[:, k * 128:(k + 1) * 128], ub[:, k * 128:(k + 1) * 128], ident_b),
                  reads=[ubb, b_identb], writes=[PB[1]] if k == 0 else (), pwrites=() if k == 0 else [PB[1]], sig=(k == 7))
        us, usb = uTs.next()
        sc.op("act", lambda e: e.copy(us, ptE0.rearrange("p (k c) -> p k c", k=8)), reads=[PB[1]], writes=[usb])
        sc.dma(uT_d[a], us, usb, reads=[usb])
        vb_, vbb = vbf.next()
        sc.op("act", lambda e: e.copy(vb_, va), reads=[vab], writes=[vbb])
        sc.dma(vb_d[a], vb_, vbb, reads=[vbb])

    kbf = A.alloc([256], F32)
    kbb = A.alloc([256], BF16)
    b_kbf, b_kbb = sc.buf("kbf"), sc.buf("kbb")
    sc.dma(kbf, L["kblk_d"], b_kbf, writes=[b_kbf])
    sc.op("pool", lambda e: e.tensor_copy(kbb, kbf), reads=[b_kbf], writes=[b_kbb])
    xg = Ring(sc, "xg", [A.alloc([8, 512], BF16) for _ in range(2)])
    qTr = Ring(sc, "qTr", [A.alloc([8, 512], BF16) for _ in range(1)])
    S12r = Ring(sc, "S12", [A.alloc([8, 256], F32) for _ in range(2)])
    v12r = Ring(sc, "v12", [A.alloc([16, 16], F32) for _ in range(2)])
    i12r = Ring(sc, "i12", [A.alloc([16, 16], U32) for _ in range(2)])
    i12fr = Ring(sc, "i12f", [A.alloc([16, 16], F32) for _ in range(2)])
    tmpr = Ring(sc, "tmpk", [A.alloc([16, 128], F32) for _ in range(1)])
    candr = Ring(sc, "cand", [A.alloc([8, 256], F32) for _ in range(1)])
    tmpcr = Ring(sc, "tmpc", [A.alloc([8, 256], F32) for _ in range(1)])
    svr = Ring(sc, "sv", [A.alloc([8, 16], F32) for _ in range(2)])
    cir = Ring(sc, "ci", [A.alloc([8, 16], U32) for _ in range(2)])
    hlr = Ring(sc, "hl", [A.alloc([2, 128], U32) for _ in range(1)])
    hlfr = Ring(sc, "hlf", [A.alloc([2, 128], F32) for _ in range(1)])
    eqr = Ring(sc, "eq", [A.alloc([8, 256], F32) for _ in range(1)])
    abw = Ring(sc, "abw", [A.alloc([3, 128], F32) for _ in range(2)])
    smx = Ring(sc, "smx", [A.alloc([128], F32) for _ in range(2)])
    ssr = Ring(sc, "ss", [A.alloc([16], F32) for _ in range(2)])
    psq = Ring(sc, "psq", [psbank(i) for i in (0,)], PB[0:1])
    e0_load(0)
    e0_load(1)
    ps12 = [(psbank(i), PB[i]) for i in (2, 3, 4, 5)]
    for tg in range(8):
        xa, xab = xg.next()
        sc.dma(xa, xh_rows[:, :, tg * 512:(tg + 1) * 512], xab, writes=[xab])
        qT, qTb = qTr.next()
        sc.op("pool", lambda e: e.memset(qT[:, 0, 0:2], 0.0), writes=[qTb])
        for h in range(8):
            ps, psb = psq.next()
            for k in range(8):
                sc.op("pe", lambda e, k=k: e.matmul(ps, wqb[:, k, h * 128:(h + 1) * 128], xa[:, k, :], start=(k == 0), stop=(k == 7)),
                      reads=[b_wqb, xab], writes=[psb] if k == 0 else (), pwrites=() if k == 0 else [psb], sig=(k == 7))
            if h % 2 == 0:
                sc.op("act", lambda e: e.copy(qT[:, h, :], ps), reads=[psb], pwrites=[qTb])
            else:
                sc.op("dve", lambda e: e.tensor_copy(qT[:, h, :], ps), reads=[psb], pwrites=[qTb])
        for tt in range(4):
            t = tg * 4 + tt
            tsl = slice(tt * 128, (tt + 1) * 128)
            s12, s12b = S12r.next()
            for h in range(8):
                ps, psb = ps12[h // 2]
                sc.op("pe", lambda e: e.matmul(ps[:, (h % 2) * 256:(h % 2) * 256 + 256], qT[:, h, tsl], kbb, start=True, stop=True),
                      reads=[qTb, b_kbb], writes=[psb] if h % 2 == 0 else (), pwrites=() if h % 2 == 0 else [psb], sig=(h % 2 == 1))
                if h % 2 == 1:
                    sc.op("act", lambda e: e.copy(s12[:, h - 1:h + 1, :], ps.rearrange("p (h c) -> p h c", h=2)), reads=[psb],
                          writes=[s12b] if h == 1 else (), pwrites=() if h == 1 else [s12b])
            for a_ in range(4 * t, 4 * t + 4):
                e0_chunk(a_)
            v12, v12b = v12r.next()
            i12, i12b = i12r.next()
            tm_all, tmb = tmpr.next()
            rows = [s12[:, r // 2, (r % 2) * 128:(r % 2 + 1) * 128] for r in range(16)]
            for r in range(16):
                sc.op("dve", lambda e: e.max(out=v12[:, r, 0:8], in_=rows[r]), reads=[s12b], writes=[v12b] if r == 0 else (), pwrites=() if r == 0 else [v12b])
            for r in range(16):
                sc.op("dve", lambda e: e.max_index(out=i12[:, r, 0:8], in_max=v12[:, r, 0:8], in_values=rows[r]), reads=[s12b, v12b],
                      writes=[i12b] if r == 0 else (), pwrites=() if r == 0 else [i12b])
            for r in range(16):
                sc.op("dve", lambda e: e.match_replace(out=tm_all[:, r, :], in_to_replace=v12[:, r, 0:8], in_values=rows[r], imm_value=NEGBIG),
                      reads=[s12b, v12b], writes=[tmb] if r == 0 else (), pwrites=() if r == 0 else [tmb])
            for r in range(16):
                sc.op("dve", lambda e: e.max(out=v12[:, r, 8:16], in_=tm_all[:, r, :]), reads=[tmb], pwrites=[v12b])
            for r in range(16):
                sc.op("dve", lambda e: e.max_index(out=i12[:, r, 8:16], in_max=v12[:, r, 8:16], in_values=tm_all[:, r, :]), reads=[tmb, v12b], pwrites=[i12b])
            i12f, i12fb = i12fr.next()
            sc.op("dve", lambda e: e.tensor_copy(i12f, i12), reads=[i12b], writes=[i12fb])
            v4 = v12.rearrange("p (h two) i -> p h two i", two=2)
            i4 = i12f.rearrange("p (h two) i -> p h two i", two=2)
            cand, candb = candr.next()
            c4 = cand.rearrange("p h (i j) -> p h i j", i=16)
            sc.op("dve", lambda e: e.tensor_tensor(c4, v4[:, :, 0, :].unsqueeze(3).to_broadcast([128, 8, 16, 16]),
                                                   v4[:, :, 1, :].unsqueeze(2).to_broadcast([128, 8, 16, 16]), op=ALU.add),
                  reads=[v12b], writes=[candb])
            sv, svb = svr.next()
            ci, cib = cir.next()
            tc_all, tcb = tmpcr.next()
            for h in range(8):
                sc.op("dve", lambda e: e.max(out=sv[:, h, 0:8], in_=cand[:, h, :]), reads=[candb], writes=[svb] if h == 0 else (), pwrites=() if h == 0 else [svb])
            for h in range(8):
                sc.op("dve", lambda e: e.max_index(out=ci[:, h, 0:8], in_max=sv[:, h, 0:8], in_values=cand[:, h, :]), reads=[candb, svb],
                      writes=[cib] if h == 0 else (), pwrites=() if h == 0 else [cib])
            for h in range(8):
                sc.op("dve", lambda e: e.match_replace(out=tc_all[:, h, :], in_to_replace=sv[:, h, 0:8], in_values=cand[:, h, :], imm_value=NEGBIG),
                      reads=[candb, svb], writes=[tcb] if h == 0 else (), pwrites=() if h == 0 else [tcb])
            for h in range(8):
                sc.op("dve", lambda e: e.max(out=sv[:, h, 8:16], in_=tc_all[:, h, :]), reads=[tcb], pwrites=[svb])
            for h in range(8):
                sc.op("dve", lambda e: e.max_index(out=ci[:, h, 8:16], in_max=sv[:, h, 8:16], in_values=tc_all[:, h, :]), reads=[tcb, svb], pwrites=[cib])
            hl, hlb = hlr.next()
            cif = ci.rearrange("p h j -> p (h j)")
            sc.op("dve", lambda e: e.tensor_scalar(hl[:, 0, :], cif, 4, None, op0=ALU.logical_shift_right), reads=[cib], writes=[hlb])
            sc.op("dve", lambda e: e.tensor_scalar(hl[:, 1, :], cif, 15, None, op0=ALU.bitwise_and), reads=[cib], pwrites=[hlb])
            hlf, hlfb = hlfr.next()
            sc.op("dve", lambda e: e.tensor_copy(hlf, hl), reads=[hlb], writes=[hlfb])
            ab, abb = abw.next()
            for which in range(2):
                eq, eqb = eqr.next()
                e4 = eq.rearrange("p h (j i) -> p h j i", j=16)
                sel = hlf[:, which, :].rearrange("p (h j) -> p h j", h=8)
                sc.op("dve", lambda e: e.tensor_tensor(e4, sel.unsqueeze(3).to_broadcast([128, 8, 16, 16]),
                                                       iota_f[:, 0:16].unsqueeze(1).unsqueeze(1).to_broadcast([128, 8, 16, 16]), op=ALU.is_equal),
                      reads=[hlfb, b_iota], writes=[eqb])
                sc.op("dve", lambda e: e.tensor_tensor(e4, e4, i4[:, :, which, :].unsqueeze(2).to_broadcast([128, 8, 16, 16]), op=ALU.mult),
                      reads=[eqb, i12fb], writes=[eqb])
                sc.op("dve", lambda e: e.reduce_sum(ab[:, which, :], e4.rearrange("p h j i -> p (h j) i"), axis=AX.X),
                      reads=[eqb], writes=[abb] if which == 0 else (), pwrites=() if which == 0 else [abb])
            sx, sxb = smx.next()
            sx3 = sx.rearrange("p (h j) -> p h j", h=8)
            ss, ssb = ssr.next()
            sc.op("dve", lambda e: e.tensor_tensor(sx3, sv, sv[:, :, 0:1].to_broadcast([128, 8, 16]), op=ALU.subtract), reads=[svb], writes=[sxb])
            sc.op("act", lambda e: e.activation(sx, sx, AF.Exp), reads=[sxb], writes=[sxb])
            sc.op("dve", lambda e: e.reduce_sum(ss[:, 0:8], sx3, axis=AX.X), reads=[sxb], writes=[ssb])
            sc.op("dve", lambda e: e.reciprocal(ss[:, 8:16], ss[:, 0:8]), reads=[ssb], pwrites=[ssb])
            sc.op("dve", lambda e: e.tensor_tensor(ab[:, 2, :].rearrange("p (h j) -> p h j", h=8), sx3,
                                                   ss[:, 8:16].unsqueeze(2).to_broadcast([128, 8, 16]), op=ALU.mult),
                  reads=[sxb, ssb], pwrites=[abb])
            psT, psTb = psbank(6 + (t % 2), 384), PB[6 + (t % 2)]
            for i in range(3):
                sc.op("pe", lambda e, i=i: e.transpose(psT[:, i * 128:(i + 1) * 128], ab[:, i, :], ident_f),
                      reads=[abb, b_ident], writes=[psTb] if i == 0 else (), pwrites=() if i == 0 else [psTb], sig=(i == 2))
            tk = slice(t * 128, (t + 1) * 128)
            sc.op("act", lambda e: e.copy(aT[:, tk], psT[:, 0:128]), reads=[psTb], pwrites=[b_aT])
            sc.op("act", lambda e: e.copy(bT[:, tk], psT[:, 128:256]), reads=[psTb], pwrites=[b_bT])
            sc.op("act", lambda e: e.copy(wT[:, tk], psT[:, 256:384]), reads=[psTb], pwrites=[b_wT])
    dump("d_aT", aT, b_aT, BF16); dump("d_bT", bT, b_bT, BF16); dump("d_wT", wT, b_wT)
    sc.barrier()
    A.reset(m1)
    check("E1")

    TG = 256
    SB = 16
    GT = A.alloc([TG, 128], BF16)
    b_GT = sc.buf("GT")
    A1r = Ring(sc, "A1", [A.alloc([SB, 128], BF16) for _ in range(2)])
    B1r = Ring(sc, "B1", [A.alloc([SB, 128], BF16) for _ in range(2)])
    B1wr = Ring(sc, "B1w", [A.alloc([SB, 128], BF16) for _ in range(2)])
    ur = Ring(sc, "ur", [A.alloc([8, 128], BF16) for _ in range(7)])
    vr = Ring(sc, "vr", [A.alloc([1024], BF16) for _ in range(7)])
    ger = Ring(sc, "ge", [A.alloc([TG], F32) for _ in range(4)])
    WTr = Ring(sc, "WTe", [A.alloc([TG], BF16) for _ in range(4)])
    xgr = Ring(sc, "xge", [A.alloc([8, TG], BF16) for _ in range(2)])
    h1r = Ring(sc, "h1e", [A.alloc([1024], F32) for _ in range(2)])
    h2r = Ring(sc, "h2e", [A.alloc([1024], F32) for _ in range(1)])
    outr = Ring(sc, "oute", [A.alloc([1024], F32) for _ in range(2)])
    rings = {"sq": Ring(sc, "esq", [A.alloc([1024], F32) for _ in range(1)]), "st": Ring(sc, "est", [A.alloc([4], F32) for _ in range(2)])}
    psA = Ring(sc, "psAct", [psbank(i, TG) for i in (4, 5, 7)], [PB[4], PB[5], PB[7]])
    psG = Ring(sc, "psG", [psbank(i) for i in (6, 7)], PB[6:8])
    for grp in range(S // TG):
        t0 = grp * TG
        xa, xab = xgr.next()
        sc.dma(xa, xh_rows[:, :, t0:t0 + TG], xab, writes=[xab])
        sc.op("pool", lambda e: e.memset(GT[:, 0, 0:2], 0.0), writes=[b_GT])
        def onehots(sb_):
            ts0 = t0 + sb_ * SB
            a1, a1b = A1r.next()
            b1, b1b = B1r.next()
            b1w, b1wb = B1wr.next()
            io3 = iota_b.unsqueeze(1).to_broadcast([128, SB, 128])
            sc.op("dve", lambda e: e.tensor_tensor(a1, io3, aT[:, ts0:ts0 + SB].unsqueeze(2).to_broadcast([128, SB, 128]), op=ALU.is_equal),
                  reads=[b_iotab, b_aT], writes=[a1b])
            sc.op("dve", lambda e: e.tensor_tensor(b1, io3, bT[:, ts0:ts0 + SB].unsqueeze(2).to_broadcast([128, SB, 128]), op=ALU.is_equal),
                  reads=[b_iotab, b_bT], writes=[b1b])
            sc.op("dve", lambda e: e.tensor_tensor(b1w, b1, wT[:, ts0:ts0 + SB].unsqueeze(2).to_broadcast([128, SB, 128]), op=ALU.mult),
                  reads=[b1b, b_wT], writes=[b1wb])
            return a1, a1b, b1w, b1wb

        def gmm(sb_, a1, a1b, b1w, b1wb):
            for q4 in range(SB // 4):
                pg, pgb = psG.next()
                for i in range(4):
                    tl = q4 * 4 + i
                    sc.op("pe", lambda e: e.matmul(pg[:, i * 128:(i + 1) * 128], b1w[:, tl, :], a1[:, tl, :], start=True, stop=True),
                          reads=[b1wb, a1b], writes=[pgb] if i == 0 else (), pwrites=() if i == 0 else [pgb], sig=(i == 3))
                tg0 = sb_ * SB + q4 * 4
                sc.op("act", lambda e: e.copy(GT[:, tg0:tg0 + 4, :], pg.rearrange("p (t a) -> p t a", t=4)), reads=[pgb], pwrites=[b_GT])

        cur_oh = onehots(0)
        for sb_ in range(TG // SB):
            nxt_oh = onehots(sb_ + 1) if sb_ + 1 < TG // SB else None
            gmm(sb_, *cur_oh)
            cur_oh = nxt_oh
        if grp == 0:
            dump("d_GT0", GT, b_GT, BF16)
        NPF = 6
        wl = {}

        def load_uv(a):
            ua, uab = ur.next()
            sc.dma(ua, uT_d[a], uab, writes=[uab])
            va, vab = vr.next()
            sc.dma(va, vb_d[a], vab, writes=[vab], q="act" if E2_VQ_ACT else "sp")
            wl[a] = (ua, uab, va, vab)

        def act_mm(a):
            ua, uab, _, _ = wl[a]
            pa, pab = psA.next()
            for k in range(8):
                sc.op("pe", lambda e, k=k: e.matmul(pa, ua[:, k, :], xa[:, k, :], start=(k == 0), stop=(k == 7)),
                      reads=[uab, xab], writes=[pab] if k == 0 else (), pwrites=() if k == 0 else [pab], sig=(k == 7))
            return pa, pab

        for a in range(min(NPF, 128)):
            load_uv(a)
        pend_act = [act_mm(0), act_mm(1)]
        for a in range(128):
            if a + NPF < 128:
                load_uv(a + NPF)
            if a + 2 < 128:
                pend_act.append(act_mm(a + 2))
            pa, pab = pend_act.pop(0)
            _, _, va, vab = wl.pop(a)
            ge, geb = ger.next()
            sc.op("act", lambda e: e.activation(ge, pa, AF.Gelu), reads=[pab], writes=[geb])
            wt, wtb = WTr.next()
            sc.op("dve", lambda e: e.tensor_tensor(wt, ge, GT[:, :, a], op=ALU.mult), reads=[geb, b_GT], writes=[wtb])
            for i in range(2):
                for half in range(2):
                    bi = i * 2 + half
                    sc.op("pe", lambda e: e.matmul(psbank(bi), wt[:, i * 128:(i + 1) * 128], va[:, half * 512:(half + 1) * 512],
                                                   start=(a == 0), stop=(a == 127)),
                          reads=[wtb, vab], writes=[PB[bi]] if a == 0 else (), pwrites=() if a == 0 else [PB[bi]], sig=(bi == 3))
        for i in range(2):
            t = grp * 2 + i
            h1, h1b = h1r.next()
            sc.dma(h1, h1_t[t], h1b, writes=[h1b])
            h2, h2b = h2r.next()
            for half in range(2):
                bi = i * 2 + half
                sc.op("dve", lambda e: e.tensor_tensor(h2[:, half * 512:(half + 1) * 512], psbank(bi), h1[:, half * 512:(half + 1) * 512], op=ALU.add),
                      reads=[PB[bi], h1b], writes=[h2b] if half == 0 else (), pwrites=() if half == 0 else [h2b])
            if grp == 0 and i == 0:
                dump("d_h2", h2, h2b)
            oo, oob = outr.next()
            rmsnorm_tile(sc, rings, h2, h2b, gfin, b_gfin, oo, oob)
            sc.dma(out_t[t], oo, oob, reads=[oob])
        check("E2_%d" % grp)
    sc.barrier()
    A.reset(base_mark)


def rmsnorm_tile(sc, A_rings, src, srcb, g_bc, b_g, dst, dstb, width=1024):
    sq, sqb = A_rings["sq"].next()
    st, stb = A_rings["st"].next()
    sc.op("act", lambda e: e.activation(sq, src, AF.Square), reads=[srcb], writes=[sqb])
    sc.op("dve", lambda e: e.reduce_sum(st[:, 0:1], sq, axis=AX.X), reads=[sqb], writes=[stb])
    sc.op("dve", lambda e: e.tensor_scalar(st[:, 1:2], st[:, 0:1], 1.0 / width, EPS, op0=ALU.mult, op1=ALU.add), reads=[stb], pwrites=[stb])
    sc.op("act", lambda e: e.activation(st[:, 2:3], st[:, 1:2], AF.Ln), reads=[stb], pwrites=[stb])
    sc.op("act", lambda e: e.activation(st[:, 3:4], st[:, 2:3], AF.Exp, scale=-0.5), reads=[stb], pwrites=[stb])
    sc.op("dve", lambda e: e.scalar_tensor_tensor(dst, src, st[:, 3:4], g_bc, op0=ALU.mult, op1=ALU.mult),
          reads=[srcb, stb, b_g], writes=[dstb])


def phase_mixout(nc, sc, A, psbank, PB, check, L, dump, zT_d, x_d, ident_b, b_identb, base_mark, **_):
    hmT_d, onT_d, h1_d, xhT_d = L["hmT_d"], L["onT_d"], L["h1_d"], L["xhT_d"]
    zt_rows = zT_d.rearrange("(c p) t -> p c t", p=128)
    hm_rows = hmT_d.rearrange("(c p) t -> p c t", p=128)
    on_rows = onT_d.rearrange("(c p) t -> p c t", p=128)
    xh_rows = xhT_d.rearrange("(c p) t -> p c t", p=128)
    W = [(A.alloc([8, 1024], BF16), sc.buf("wsq")) for _ in range(3)]
    mst = A.mark()
    wst = Ring(sc, "wst", [A.alloc([8, 1024], F32) for _ in range(2)])
    for i in range(3):
        wa, wb = wst.next()
        sc.dma(wa, L["wsq_d"][i], wb, writes=[wb])
        sc.op("pool", lambda e: e.tensor_copy(W[i][0], wa), reads=[wb], writes=[W[i][1]])
    sc.barrier()
    A.reset(mst)
    gffn = A.alloc([1024], F32)
    b_gffn = sc.buf("gffn")
    sc.dma(gffn, L["gffn_d"], b_gffn, writes=[b_gffn])
    hin = Ring(sc, "hin", [A.alloc([8, 512], BF16) for _ in range(2)])
    oin = Ring(sc, "oin", [A.alloc([8, 512], BF16) for _ in range(2)])
    gain = Ring(sc, "gain", [A.alloc([8, 512], BF16) for _ in range(2)])
    gbin = Ring(sc, "gbin", [A.alloc([8, 512], BF16) for _ in range(2)])
    mixT = Ring(sc, "mixT", [A.alloc([8, 512], BF16) for _ in range(2)])
    t1r = Ring(sc, "dt1", [A.alloc([512], F32) for _ in range(2)])
    t2r = Ring(sc, "dt2", [A.alloc([512], F32) for _ in range(2)])
    xin = Ring(sc, "dxin", [A.alloc([1024], F32) for _ in range(2)])
    h1r = Ring(sc, "h1", [A.alloc([1024], F32) for _ in range(2)])
    xhb = Ring(sc, "xhb", [A.alloc([1024], BF16) for _ in range(2)])
    xhT = Ring(sc, "xhT", [A.alloc([8, 512], BF16) for _ in range(2)])
    rings = {"sq": Ring(sc, "dsq", [A.alloc([1024], F32) for _ in range(1)]), "st": Ring(sc, "dst", [A.alloc([4], F32) for _ in range(2)])}
    psr = Ring(sc, "psD", [psbank(i) for i in range(6)], PB[0:6])
    pst = Ring(sc, "pstD", [psbank(i).bitcast(BF16) for i in (6, 7)], PB[6:8])
    x_t = x_d.rearrange("(n p) d -> n p d", p=128)
    h1_t = h1_d.rearrange("(n p) d -> n p d", p=128)
    for tg in range(8):
        ts_ = slice(tg * 512, (tg + 1) * 512)
        hi, hib = hin.next()
        sc.dma(hi, hm_rows[:, :, ts_], hib, writes=[hib])
        oi, oib = oin.next()
        sc.dma(oi, on_rows[:, :, ts_], oib, writes=[oib])
        ga, gab = gain.next()
        sc.dma(ga, zt_rows[:, ZR_GA // 128:ZR_GA // 128 + 8, ts_], gab, writes=[gab])
        gb, gbb = gbin.next()
        sc.dma(gb, zt_rows[:, ZR_GB // 128:ZR_GB // 128 + 8, ts_], gbb, writes=[gbb])
        mx, mxb = mixT.next()
        sc.op("pool", lambda e: e.memset(mx[:, 0, 0:2], 0.0), writes=[mxb])
        for dc in range(8):
            psa, psab = psr.next()
            for k in range(8):
                sc.op("pe", lambda e, k=k: e.matmul(psa, W[0][0][:, k, dc * 128:(dc + 1) * 128], hi[:, k, :], start=(k == 0), stop=(k == 7)),
                      reads=[W[0][1], hib], writes=[psab] if k == 0 else (), pwrites=() if k == 0 else [psab], sig=(k == 7))
            psb_, psbb = psr.next()
            for k in range(8):
                sc.op("pe", lambda e, k=k: e.matmul(psb_, W[1][0][:, k, dc * 128:(dc + 1) * 128], oi[:, k, :], start=(k == 0), stop=(k == 7)),
                      reads=[W[1][1], oib], writes=[psbb] if k == 0 else (), pwrites=() if k == 0 else [psbb], sig=(k == 7))
            t1, t1b = t1r.next()
            t2, t2b = t2r.next()
            sc.op("dve", lambda e: e.tensor_tensor(t1, psa, ga[:, dc, :], op=ALU.mult), reads=[psab, gab], writes=[t1b])
            sc.op("dve", lambda e: e.tensor_tensor(t2, psb_, gb[:, dc, :], op=ALU.mult), reads=[psbb, gbb], writes=[t2b])
            sc.op("pool", lambda e: e.tensor_tensor(mx[:, dc, :], t1, t2, op=ALU.add), reads=[t1b, t2b], pwrites=[mxb])
        xt_, xtb = xhT.next()
        sc.op("pool", lambda e: e.memset(xt_[:, 0, 0:2], 0.0), writes=[xtb])
        for tt in range(4):
            t = tg * 4 + tt
            xa, xab = xin.next()
            sc.dma(xa, x_t[t], xab, writes=[xab])
            h1, h1b = h1r.next()
            for half in range(2):
                ps, psb2 = psr.next()
                for k in range(8):
                    sc.op("pe", lambda e, k=k: e.matmul(ps, mx[:, k, tt * 128:(tt + 1) * 128], W[2][0][:, k, half * 512:(half + 1) * 512],
                                                        start=(k == 0), stop=(k == 7)),
                          reads=[W[2][1], mxb], writes=[psb2] if k == 0 else (), pwrites=() if k == 0 else [psb2], sig=(k == 7))
                sc.op("dve", lambda e: e.tensor_tensor(h1[:, half * 512:(half + 1) * 512], ps, xa[:, half * 512:(half + 1) * 512], op=ALU.add),
                      reads=[psb2, xab], writes=[h1b] if half == 0 else (), pwrites=() if half == 0 else [h1b])
            sc.dma(h1_t[t], h1, h1b, reads=[h1b])
            xh, xhbb = xhb.next()
            rmsnorm_tile(sc, rings, h1, h1b, gffn, b_gffn, xh, xhbb)
            pt, ptb = pst.next()
            for k in range(8):
                sc.op("pe", lambda e, k=k: e.transpose(pt[:, k * 128:(k + 1) * 128], xh[:, k * 128:(k + 1) * 128], ident_b),
                      reads=[xhbb, b_identb], writes=[ptb] if k == 0 else (), pwrites=() if k == 0 else [ptb], sig=(k == 7))
            sc.op("act", lambda e: e.copy(xt_[:, :, tt * 128:(tt + 1) * 128], pt.rearrange("p (k c) -> p k c", k=8)), reads=[ptb], pwrites=[xtb])
        sc.dma(xh_rows[:, :, ts_], xt_, xtb, reads=[xtb])
    sc.barrier()
    A.reset(base_mark)


def phase_nsa(nc, sc, A, psbank, PB, check, L, dump, zT_d, ztm_d, zsm_d, ident_b, b_identb, base_mark, **_):
    SC = float(128 ** -0.5)
    NEG = -30000.0
    onT_d = L["onT_d"]
    zt_rows = zT_d.rearrange("(c p) t -> p c t", p=128)
    ztm_t = ztm_d.rearrange("(n p) c -> p n c", p=128)
    onT_rows = onT_d.rearrange("(c p) t -> p c t", p=128)

    sm = A.alloc([NT, 32], F32)
    b_sm = sc.buf("smC")
    sc.dma(sm, zsm_d.rearrange("(n p) c -> p n c", p=128), b_sm, writes=[b_sm])
    gates = A.alloc([NT, 24], F32)
    b_gates = sc.buf("gates")
    sc.op("act", lambda e: e.activation(gates, sm[:, :, 8:32], AF.Sigmoid), reads=[b_sm], writes=[b_gates])

    ovl32 = A.alloc([2, 64], F32)
    b_ovl = sc.buf("ovl")
    sc.dma(ovl32, L["ovl_d"].rearrange("t p j -> p t j"), b_ovl, writes=[b_ovl])
    Eall = A.alloc([S], BF16)
    b_Eall = sc.buf("Eall")
    kcmpT = A.alloc([2, 256], BF16)
    b_kcmpT = sc.buf("kcmpT")
    vcx = A.alloc([2, 2, 193], BF16)
    b_vcx = sc.buf("vcx")
    cmark = A.mark()
    E32 = A.alloc([S], F32)
    b_E32 = sc.buf("E32")
    sc.dma(E32[0:64, :], L["eall_d"], b_E32, writes=[b_E32])
    sc.op("pool", lambda e: e.memset(Eall[64:128, :], 0.0), writes=[b_Eall])
    sc.op("pool", lambda e: e.tensor_copy(Eall[0:64, :], E32[0:64, :]), reads=[b_E32], pwrites=[b_Eall])

    sc.barrier()
    A.reset(cmark)
    c1mark = A.mark()
    sc.op("pool", lambda e: e.memset(kcmpT, 0.0), writes=[b_kcmpT])
    sc.op("pool", lambda e: e.memset(vcx, 0.0), writes=[b_vcx])
    for g in range(2):
        sc.op("pool", lambda e, g=g: e.memset(vcx[:, 0, g, 128:129], 1.0), pwrites=[b_vcx])
        sc.op("pool", lambda e, g=g: e.memset(vcx[0:127, 1, g, 128:129], 1.0), pwrites=[b_vcx])
        sc.op("pool", lambda e, g=g: e.tensor_copy(vcx[:, :, g, 129:193], ovl32), reads=[b_ovl], pwrites=[b_vcx])
    kvc = A.alloc([4, S], BF16)
    b_kvc = sc.buf("kvc")
    sc.dma(kvc, zt_rows[:, ZR_KC // 128:ZR_KC // 128 + 4, :], b_kvc, writes=[b_kvc])
    w1f = A.alloc([32, 128], F32)
    b_w1f = sc.buf("w1f")
    w1b = A.alloc([32, 128], BF16)
    b_w1b = sc.buf("w1b")
    w2f = A.alloc([128], F32)
    w2b = A.alloc([128], BF16)
    b_w2f, b_w2b = sc.buf("w2f"), sc.buf("w2b")
    posf = A.alloc([32], F32)
    posb = A.alloc([32], BF16)
    b_posf, b_posb = sc.buf("posf"), sc.buf("posb")
    cbias = A.alloc([1], F32)
    b_cbias = sc.buf("cbias")
    gT = A.alloc([256], BF16)
    b_gT = sc.buf("gT")
    for kv in range(2):
        sc.dma(w1f, L["cw1_d"][kv], b_w1f, writes=[b_w1f])
        sc.op("pool", lambda e: e.tensor_copy(w1b, w1f), reads=[b_w1f], writes=[b_w1b])
        sc.dma(w2f, L["cw2_d"][kv], b_w2f, writes=[b_w2f])
        sc.op("pool", lambda e: e.tensor_copy(w2b, w2f), reads=[b_w2f], writes=[b_w2b])
        sc.dma(posf, L["cpos_d"][kv], b_posf, writes=[b_posf])
        sc.op("pool", lambda e: e.tensor_copy(posb, posf), reads=[b_posf], writes=[b_posb])
        psb_ = psbank(0, 1)
        for l in range(32):
            sc.op("pe", lambda e, l=l: e.matmul(psb_, w1b[:, l, :], posb[:, l:l + 1], start=(l == 0), stop=(l == 31)),
                  reads=[b_w1b, b_posb], writes=[PB[0]] if l == 0 else (), pwrites=() if l == 0 else [PB[0]], sig=(l == 31))
        sc.op("dve", lambda e: e.tensor_copy(cbias, psb_), reads=[PB[0]], writes=[b_cbias])
        for g in range(2):
            src = kvc[:, kv * 2 + g, :].rearrange("p (n s) -> p n s", s=16)
            psp = psbank(1, 255)
            for l in range(32):
                sc.op("pe", lambda e, l=l, src=src: e.matmul(psp, w1b[:, l, :], src[:, 0:255, l] if l < 16 else src[:, 1:256, l - 16], start=(l == 0), stop=(l == 31)),
                      reads=[b_w1b, b_kvc], writes=[PB[1]] if l == 0 else (), pwrites=() if l == 0 else [PB[1]], sig=(l == 31))
            sc.op("pool", lambda e: e.memset(gT, 0.0), writes=[b_gT])
            sc.op("act", lambda e: e.activation(gT[:, 0:255], psp, AF.Gelu, bias=cbias[:, 0:1]), reads=[PB[1], b_cbias], pwrites=[b_gT])
            if kv == 0:
                pso = psbank(2, 255)
                sc.op("pe", lambda e: e.matmul(pso, w2b, gT[:, 0:255], start=True, stop=True), reads=[b_w2b, b_gT], writes=[PB[2]])
                sc.op("dve", lambda e, g=g: e.tensor_copy(kcmpT[:, g, 0:255], pso), reads=[PB[2]], pwrites=[b_kcmpT])
            else:
                for tt in range(2):
                    nn = 128 if tt == 0 else 127
                    pso = psbank(2 + tt, 128)
                    sc.op("pe", lambda e, tt=tt, nn=nn, pso=pso: e.matmul(pso[0:nn, :], gT[:, tt * 128:tt * 128 + nn], w2b, start=True, stop=True),
                          reads=[b_w2b, b_gT], writes=[PB[2 + tt]])
                    sc.op("dve", lambda e, tt=tt, nn=nn, g=g, pso=pso: e.tensor_copy(vcx[0:nn, tt, g, 0:128], pso[0:nn, :]),
                          reads=[PB[2 + tt]], pwrites=[b_vcx])
    dump("d_kcmpT", kcmpT, b_kcmpT, BF16)
    dump("d_vcx", vcx, b_vcx, BF16)
    sc.barrier()
    A.reset(c1mark)
    check("C1")

    qg = A.alloc([NT, 4, 128], BF16)
    qrg = A.alloc([NT, 4, 128], BF16)
    ksw = A.alloc([2, S], BF16)
    vsx = A.alloc([NT, 129], BF16)
    vwx = A.alloc([NT, 129], BF16)
    onT = A.alloc([4, S], BF16)
    b_qg, b_qrg, b_ksw, b_vsx, b_vwx, b_onT = [sc.buf(n_) for n_ in ("qg", "qrg", "ksw", "vsx", "vwx", "onT")]
    E32r = Ring(sc, "E32r", [A.alloc([512], F32) for _ in range(2)])
    PTr = Ring(sc, "PTr", [A.alloc([512], BF16) for _ in range(4)])
    impr = Ring(sc, "imp", [A.alloc([64], F32) for _ in range(2)])
    m8r = Ring(sc, "m8", [A.alloc([8], F32) for _ in range(2)])
    negr = Ring(sc, "neg", [A.alloc([64], BF16) for _ in range(2)])
    negTr = Ring(sc, "negT", [A.alloc([512], BF16) for _ in range(2)])
    for nT_, nTb_ in negTr.items:
        sc.op("pool", lambda e: e.memset(nT_[64:128, :], 0.0), writes=[nTb_])
    zzr = Ring(sc, "zz", [A.alloc([12], F32) for _ in range(2)])
    cfr = Ring(sc, "cf", [A.alloc([12], F32) for _ in range(2)])
    oacc = Ring(sc, "oacc", [A.alloc([128], F32) for _ in range(8)])
    obf = Ring(sc, "obf", [A.alloc([4, 128], BF16) for _ in range(2)])
    sring = [0]

    SBANKS = (0, 1, 7)

    def s_bank():
        sring[0] += 1
        i = SBANKS[sring[0] % 3]
        return psbank(i), PB[i]

    def pv_accumulate(pt, ptb, vx, vxb, kt_idx, accs, width):
        for hh in range(4):
            bi, co = accs[hh]
            out = psbank(bi, width, co)
            sc.op("pe", lambda e, hh=hh, out=out: e.matmul(out, pt[:, hh * 128:(hh + 1) * 128], vx[:, kt_idx, :], start=False, stop=False,
                                                          skip_group_check=True),
                  reads=[ptb, vxb], pwrites=[PB[bi]], sig=(hh == 3))

    def masked_exp(ps, psb, base, cm, pat, cmp_op):
        ea, eb = E32r.next()
        sc.op("act", lambda e: e.activation(ea, ps, AF.Exp, scale=SC), reads=[psb], writes=[eb])
        pt, ptb = PTr.next()
        sc.op("pool", lambda e: e.affine_select(pt.rearrange("p (h t) -> p h t", h=4), ea.rearrange("p (h t) -> p h t", h=4),
                                               pattern=pat, compare_op=cmp_op, fill=0.0, base=base, channel_multiplier=cm),
              reads=[eb], writes=[ptb])
        return pt, ptb

    def run_branch(kts, s_fn, e_fn, pv_fn):
        LA = 2
        pend = [s_fn(kt) for kt in kts[:LA]]
        for i, kt in enumerate(kts):
            if i + LA < len(kts):
                pend.append(s_fn(kts[i + LA]))
            pt, ptb = e_fn(kt, *pend.pop(0))
            pv_fn(kt, pt, ptb)

    def plain_exp(ps, psb):
        pt, ptb = PTr.next()
        sc.op("act", lambda e: e.activation(pt, ps, AF.Exp, scale=SC), reads=[psb], writes=[ptb])
        return pt, ptb

    for g in range(2):
        for hh in range(4):
            sc.dma(qg[:, :, hh, :], zt_rows[:, ZR_Q // 128 + 4 * g + hh, :].rearrange("p (n t) -> p n t", t=128), b_qg,
                   writes=[b_qg] if hh == 0 else (), pwrites=() if hh == 0 else [b_qg])
            sc.dma(qrg[:, :, hh, :], zt_rows[:, ZR_QR // 128 + 4 * g + hh, :].rearrange("p (n t) -> p n t", t=128), b_qrg,
                   writes=[b_qrg] if hh == 0 else (), pwrites=() if hh == 0 else [b_qrg])
        sc.dma(ksw[:, 0, :], zt_rows[:, ZR_KSR // 128 + g, :], b_ksw, writes=[b_ksw])
        sc.dma(ksw[:, 1, :], zt_rows[:, ZR_KWR // 128 + g, :], b_ksw, pwrites=[b_ksw])
        sc.dma(vsx[:, :, 0:128], ztm_t[:, :, 2048 + g * 128:2048 + (g + 1) * 128], b_vsx, writes=[b_vsx])
        sc.op("pool", lambda e: e.memset(vsx[:, :, 128:129], 1.0), pwrites=[b_vsx])
        sc.dma(vwx[:, :, 0:128], ztm_t[:, :, 2304 + g * 128:2304 + (g + 1) * 128], b_vwx, writes=[b_vwx])
        sc.op("pool", lambda e: e.memset(vwx[:, :, 128:129], 1.0), pwrites=[b_vwx])
        sc.op("pool", lambda e: e.memset(onT[:, 0, 0:2], 0.0), writes=[b_onT])
        for qt in range(NT):
            tq = slice(qt * 128, (qt + 1) * 128)
            qs = qg[:, qt, :, :].rearrange("p h t -> p (h t)")
            qrs = qrg[:, qt, :, :].rearrange("p h t -> p (h t)")
            for bi in (2, 3):
                sc.op("dve", lambda e, bi=bi: e.memset(psbank(bi, 386), 0.0), writes=[PB[bi]])
            accC = [(2, 0), (2, 193), (3, 0), (3, 193)]
            accW = [(4, 0), (4, 129), (5, 0), (5, 129)]
            accS = [(2, 0), (2, 129), (3, 0), (3, 129)]

            def s_cmp(kt):
                ps, psb = s_bank()
                sc.op("pe", lambda e: e.matmul(ps, kcmpT[:, g, kt * 128:(kt + 1) * 128], qs, start=True, stop=True),
                      reads=[b_kcmpT, b_qg], writes=[psb])
                return ps, psb

            run_branch(list(range(2 if qt >= 16 else 1)), s_cmp,
                       lambda kt, ps, psb: masked_exp(ps, psb, base=128 * qt - 31 - 2048 * kt, cm=-16, pat=[[0, 4], [1, 128]], cmp_op=ALU.is_ge),
                       lambda kt, pt, ptb: pv_accumulate(pt, ptb, vcx[:, :, g, :], b_vcx, kt, accC, 193))
            for bi in (4, 5):
                sc.op("dve", lambda e, bi=bi: e.memset(psbank(bi, 258), 0.0), writes=[PB[bi]])
            zz, zzb = zzr.next()
            for hh in range(4):
                bi, co = accC[hh]
                sc.op("dve", lambda e, hh=hh, bi=bi, co=co: e.tensor_scalar(zz[:, hh * 3:hh * 3 + 1], psbank(bi, 1, co + 128), 1e-30, None, op0=ALU.max),
                      reads=[PB[bi]], writes=[zzb] if hh == 0 else (), pwrites=() if hh == 0 else [zzb])
            im, imb = impr.next()
            rzc, rzcb = m8r.next()
            sc.op("dve", lambda e: e.reciprocal(rzc[:, 0:4], zz[:, 0:12:3]), reads=[zzb], writes=[rzcb])
            for hh in range(4):
                bi, co = accC[hh]
                Mh = psbank(bi, 64, co + 129)
                if hh == 0:
                    sc.op("dve", lambda e: e.tensor_scalar(im, Mh, rzc[:, 0:1], None, op0=ALU.mult), reads=[PB[bi], rzcb], writes=[imb])
                else:
                    sc.op("dve", lambda e: e.scalar_tensor_tensor(im, Mh, rzc[:, hh:hh + 1], im, op0=ALU.mult, op1=ALU.add),
                          reads=[PB[bi], rzcb, imb], pwrites=[imb])
            sc.op("pool", lambda e: e.memset(im[:, 2 * qt + 1:64], -1e9), reads=[imb], pwrites=[imb])
            sc.op("pool", lambda e: e.memset(im[:, 0:1], 1e9), pwrites=[imb])
            sc.op("pool", lambda e: e.memset(im[:, 2 * qt:2 * qt + 1], 1e9), pwrites=[imb])
            if qt >= 1:
                sc.op("pool", lambda e: e.memset(im[0:64, 2 * qt - 1:2 * qt], 1e9), pwrites=[imb])
            sc.op("pool", lambda e: e.memset(im[64:128, 2 * qt + 1:2 * qt + 2], 1e9), pwrites=[imb])
            m8, m8b = m8r.next()
            sc.op("dve", lambda e: e.max(out=m8, in_=im), reads=[imb], writes=[m8b])
            ng, ngb = negr.next()
            sc.op("dve", lambda e: e.tensor_scalar(ng, im, m8[:, 7:8], NEG, op0=ALU.is_lt, op1=ALU.mult), reads=[imb, m8b], writes=[ngb])
            dump("d_neg_%d_%d" % (g, qt), ng, ngb, BF16)

            def s_win(kt):
                ps, psb = s_bank()
                sc.op("pe", lambda e: e.matmul(ps, ksw[:, 1, kt * 128:(kt + 1) * 128], qrs, start=True, stop=True),
                      reads=[b_ksw, b_qrg], writes=[psb])
                return ps, psb

            def e_win(kt, ps, psb):
                if kt == qt:
                    return masked_exp(ps, psb, base=0, cm=-1, pat=[[0, 4], [1, 128]], cmp_op=ALU.is_ge)
                if kt == qt - 4:
                    return masked_exp(ps, psb, base=0, cm=1, pat=[[0, 4], [-1, 128]], cmp_op=ALU.is_gt)
                return plain_exp(ps, psb)

            run_branch(list(range(max(0, qt - 4), qt + 1)), s_win, e_win,
                       lambda kt, pt, ptb: pv_accumulate(pt, ptb, vwx, b_vwx, kt, accW, 129))
            psT = psbank(6, 64).bitcast(BF16)
            sc.op("pe", lambda e: e.transpose(psT[0:64, 0:128], ng, ident_b), reads=[ngb, b_identb], writes=[PB[6]])
            nT, nTb = negTr.next()
            sc.op("dve", lambda e: e.tensor_copy(nT[0:64, :].rearrange("p (h t) -> p h t", h=4), psT[0:64, 0:128].unsqueeze(1).to_broadcast([64, 4, 128])),
                  reads=[PB[6]], pwrites=[nTb])
            cf, cfb = cfr.next()
            sc.op("dve", lambda e: e.reciprocal(cf[:, 0:12:3], zz[:, 0:12:3]), reads=[zzb], writes=[cfb])
            sc.op("dve", lambda e: e.tensor_tensor(cf[:, 0:12:3], cf[:, 0:12:3], gates[:, qt, g * 12:g * 12 + 12:3], op=ALU.mult),
                  reads=[cfb, b_gates], pwrites=[cfb])
            oa_list = []
            for hh in range(4):
                bi, co = accC[hh]
                oa, oab = oacc.next()
                oa_list.append((oa, oab))
                sc.op("act", lambda e: e.activation(oa, psbank(bi, 128, co), AF.Copy, scale=cf[:, hh * 3:hh * 3 + 1]),
                      reads=[PB[bi], cfb], writes=[oab])
            for bi in (2, 3):
                sc.op("dve", lambda e, bi=bi: e.memset(psbank(bi, 258), 0.0), writes=[PB[bi]])

            def s_slc(kt):
                ps, psb = s_bank()
                sc.op("pe", lambda e: e.matmul(ps, ksw[:, 0, kt * 128:(kt + 1) * 128], qrs, start=True, stop=False),
                      reads=[b_ksw, b_qrg], writes=[psb], sig=False)
                sc.op("pe", lambda e: e.matmul(ps, Eall[:, kt * 128:(kt + 1) * 128], nT, start=False, stop=True),
                      reads=[b_Eall, nTb], pwrites=[psb])
                return ps, psb

            run_branch(list(range(qt + 1)), s_slc,
                       lambda kt, ps, psb: (masked_exp(ps, psb, base=0, cm=-1, pat=[[0, 4], [1, 128]], cmp_op=ALU.is_ge)
                                            if kt == qt else plain_exp(ps, psb)),
                       lambda kt, pt, ptb: pv_accumulate(pt, ptb, vsx, b_vsx, kt, accS, 129))
            for hh in range(4):
                bi, co = accS[hh]
                sc.op("dve", lambda e: e.tensor_scalar(zz[:, hh * 3 + 1:hh * 3 + 2], psbank(bi, 1, co + 128), 1e-30, None, op0=ALU.max),
                      reads=[PB[bi]], pwrites=[zzb])
                bi, co = accW[hh]
                sc.op("dve", lambda e: e.tensor_scalar(zz[:, hh * 3 + 2:hh * 3 + 3], psbank(bi, 1, co + 128), 1e-30, None, op0=ALU.max),
                      reads=[PB[bi]], pwrites=[zzb])
            for br in (1, 2):
                sc.op("dve", lambda e: e.reciprocal(cf[:, br:12:3], zz[:, br:12:3]), reads=[zzb, cfb], pwrites=[cfb])
                sc.op("dve", lambda e: e.tensor_tensor(cf[:, br:12:3], cf[:, br:12:3], gates[:, qt, g * 12 + br:g * 12 + 12:3], op=ALU.mult),
                      reads=[cfb, b_gates], pwrites=[cfb])
            ob, obb = obf.next()
            for hh in range(4):
                oa, oab = oa_list[hh]
                bs, cs = accS[hh]
                bw, cw_ = accW[hh]
                sc.op("dve", lambda e: e.scalar_tensor_tensor(oa, psbank(bs, 128, cs), cf[:, hh * 3 + 1:hh * 3 + 2], oa, op0=ALU.mult, op1=ALU.add),
                      reads=[PB[bs], cfb, oab], writes=[oab])
                sc.op("dve", lambda e: e.scalar_tensor_tensor(ob[:, hh, :], psbank(bw, 128, cw_), cf[:, hh * 3 + 2:hh * 3 + 3], oa, op0=ALU.mult, op1=ALU.add),
                      reads=[PB[bw], cfb, oab], writes=[obb] if hh == 0 else (), pwrites=() if hh == 0 else [obb])
            psO = psbank(6).bitcast(BF16)[:, 256:768]
            for hh in range(4):
                sc.op("pe", lambda e: e.transpose(psO[:, hh * 128:(hh + 1) * 128], ob[:, hh, :], ident_b),
                      reads=[obb, b_identb], writes=[PB[6]] if hh == 0 else (), pwrites=() if hh == 0 else [PB[6]], sig=(hh == 3))
            sc.op("act", lambda e: e.copy(onT[:, :, tq], psO.rearrange("p (h t) -> p h t", h=4)), reads=[PB[6]], pwrites=[b_onT])
        sc.dma(onT_rows[:, 4 * g:4 * g + 4, :], onT, b_onT, reads=[b_onT])
    sc.barrier()
    A.reset(base_mark)


def phase_mlstm(nc, sc, A, psbank, PB, check, L, dump, zT_d, ztm_d, zsm_d, hmT_d, tri_f, b_tri, ones_f, b_ones,
                ident_b, b_identb, mbias_d, mnormg_d, base_mark, **_):
    LN16 = float(np.log(16.0))
    sm = A.alloc([NT, 32], F32)
    b_sm = sc.buf("sm")
    sc.dma(sm, zsm_d.rearrange("(n p) c -> p n c", p=128), b_sm, writes=[b_sm])
    mbias = A.alloc([8], F32)
    b_mbias = sc.buf("mbias")
    sc.dma(mbias, mbias_d, b_mbias, writes=[b_mbias])
    fpre = A.alloc([NT, 4], F32)
    ipre = A.alloc([NT, 4], F32)
    b_fpre, b_ipre = sc.buf("fpre"), sc.buf("ipre")
    for h in range(4):
        sc.op("dve", lambda e, h=h: e.tensor_scalar(fpre[:, :, h], sm[:, :, 4 + h], mbias[:, 4 + h:5 + h], None, op0=ALU.add),
              reads=[b_sm, b_mbias], pwrites=[b_fpre])
        sc.op("dve", lambda e, h=h: e.tensor_scalar(ipre[:, :, h], sm[:, :, h], mbias[:, h:h + 1], None, op0=ALU.add),
              reads=[b_sm, b_mbias], pwrites=[b_ipre])
    fl = fpre.rearrange("p n h -> p (n h)")
    il = ipre.rearrange("p n h -> p (n h)")
    lf = A.alloc([128], F32)
    b_lf = sc.buf("lf")
    sc.op("act", lambda e: e.activation(lf, fl, AF.Exp, scale=-1.0), reads=[b_fpre], writes=[b_lf])
    sc.op("act", lambda e: e.activation(lf, lf, AF.Ln, bias=1.0), reads=[b_lf], writes=[b_lf])
    sc.op("dve", lambda e: e.tensor_scalar(lf, lf, -1.0, None, op0=ALU.mult), reads=[b_lf], writes=[b_lf])
    bcs = A.alloc([128], F32)
    ebl = A.alloc([128], F32)
    ik = A.alloc([128], F32)
    eks = A.alloc([128], F32)
    ebL = A.alloc([128], F32)
    b_bcs, b_ebl, b_ik, b_eks, b_ebL = [sc.buf(n_) for n_ in ("bcs", "ebl", "ik", "eks", "ebL")]
    ps0 = psbank(0, 128)
    sc.op("pe", lambda e: e.matmul(ps0, tri_f, lf, start=True, stop=True), reads=[b_tri, b_lf], writes=[PB[0]])
    sc.op("dve", lambda e: e.tensor_copy(bcs, ps0), reads=[PB[0]], writes=[b_bcs])
    sc.op("act", lambda e: e.activation(ebl, ps0, AF.Exp), reads=[PB[0]], writes=[b_ebl])
    ps1 = psbank(1, 128)
    sc.op("pe", lambda e: e.matmul(ps1, ones_f, lf, start=True, stop=True), reads=[b_ones, b_lf], writes=[PB[1]])
    sc.op("act", lambda e: e.activation(ebL, ps1, AF.Exp), reads=[PB[1]], writes=[b_ebL])
    sc.op("dve", lambda e: e.tensor_tensor(ik, il, bcs, op=ALU.subtract), reads=[b_ipre, b_bcs], writes=[b_ik])
    sc.op("dve", lambda e: e.tensor_scalar(ik, ik, -LN16, None, op0=ALU.add), reads=[b_ik], writes=[b_ik])
    sc.op("act", lambda e: e.activation(eks, ik, AF.Exp), reads=[b_ik], writes=[b_eks])
    mnormg = A.alloc([1024], F32)
    b_mnormg = sc.buf("mnormg")
    sc.dma(mnormg, mnormg_d, b_mnormg, writes=[b_mnormg])
    dump("d_lf", lf, b_lf); dump("d_bcs", bcs, b_bcs); dump("d_ebl", ebl, b_ebl); dump("d_ik", ik, b_ik)
    dump("d_eks", eks, b_eks); dump("d_ebL", ebL, b_ebL)
    check("Bgates")

    qkT = Ring(sc, "qkT", [A.alloc([4, S], BF16) for _ in range(2)])
    vh = Ring(sc, "vh", [A.alloc([NT, 257], BF16) for _ in range(2)])
    soh = Ring(sc, "soh", [A.alloc([NT, 256], BF16) for _ in range(2)])
    hmT = Ring(sc, "hmT", [A.alloc([2, S], BF16) for _ in range(2)])
    Cstate = Ring(sc, "Cst", [(A.alloc([2, 257], F32), A.alloc([2, 257], BF16), sc.buf("Cf"), sc.buf("Cb")) for _ in range(2)])
    DT = Ring(sc, "DT", [A.alloc([128], F32) for _ in range(2)])
    DTm = Ring(sc, "DTm", [A.alloc([128], F32) for _ in range(2)])
    WT = Ring(sc, "WT", [A.alloc([128], BF16) for _ in range(4)])
    k2 = Ring(sc, "k2", [A.alloc([256], BF16) for _ in range(4)])
    T1 = Ring(sc, "T1", [A.alloc([257], F32) for _ in range(2)])
    NUM = Ring(sc, "NUM", [A.alloc([257], F32) for _ in range(2)])
    hh = Ring(sc, "hh", [A.alloc([256], F32) for _ in range(2)])
    hfin = Ring(sc, "hfin", [A.alloc([256], BF16) for _ in range(2)])
    st8 = Ring(sc, "st8", [A.alloc([8], F32) for _ in range(4)])
    ctmp = Ring(sc, "ctmp", [A.alloc([257], F32) for _ in range(2)])
    zt_rows = zT_d.rearrange("(c p) t -> p c t", p=128)
    ztm_t = ztm_d.rearrange("(n p) c -> p n c", p=128)
    hmT_rows = hmT_d.rearrange("(c p) t -> p c t", p=128)
    def head_setup(h):
        qk, qkb = qkT.next()
        (Cf, Cb, b_Cf, b_Cb), _unused = Cstate.next()
        sc.dma(qk[:, 0:2, :], zt_rows[:, 2 * h:2 * h + 2, :], qkb, writes=[qkb])
        sc.dma(qk[:, 2:4, :], zt_rows[:, 8 + 2 * h:8 + 2 * h + 2, :], qkb, pwrites=[qkb])
        v, vb = vh.next()
        sc.dma(v[:, :, 0:256], ztm_t[:, :, h * 256:(h + 1) * 256], vb, writes=[vb])
        sc.op("pool", lambda e, v=v: e.memset(v[:, :, 256:257], 1.0), pwrites=[vb])
        so, sob = soh.next()
        sc.dma(so, ztm_t[:, :, 1024 + h * 256:1024 + (h + 1) * 256], sob, writes=[sob])
        hT, hTb = hmT.next()
        sc.op("pool", lambda e, hT=hT: e.memset(hT[:, 0, 0:2], 0.0), writes=[hTb])
        sc.op("pool", lambda e: e.memset(Cf, 0.0), writes=[b_Cf])
        sc.op("pool", lambda e: e.memset(Cb, 0.0), writes=[b_Cb])
        def stage1(n):
            col = n * 4 + h
            tk = slice(n * 128, (n + 1) * 128)
            psB = psbank(0, 128)
            sc.op("pe", lambda e, col=col: e.matmul(psB, lf[:, col:col + 1].to_broadcast([128, 128]), tri_f, start=True, stop=True),
                  reads=[b_lf, b_tri], writes=[PB[0]])
            dt, dtb = DT.next()
            sc.op("act", lambda e, dt=dt, col=col: e.activation(dt, psB, AF.Exp, bias=ik[:, col:col + 1]), reads=[PB[0], b_ik], writes=[dtb])
            dm, dmb = DTm.next()
            sc.op("pool", lambda e, dm=dm, dt=dt: e.tensor_tensor(dm, dt, tri_f, op=ALU.mult), reads=[dtb, b_tri], writes=[dmb])
            psS = psbank(1, 128)
            for c in range(2):
                sc.op("pe", lambda e, c=c, qk=qk, tk=tk: e.matmul(psS, qk[:, 2 + c, tk], qk[:, c, tk], start=(c == 0), stop=(c == 1)),
                      reads=[qkb], writes=[PB[1]] if c == 0 else (), pwrites=() if c == 0 else [PB[1]], sig=(c == 1))
            wt, wtb = WT.next()
            sc.op("dve", lambda e, wt=wt, dm=dm: e.tensor_tensor(wt, psS, dm, op=ALU.mult), reads=[PB[1], dmb], writes=[wtb])
            psK = psbank(2, 128).bitcast(BF16)
            for c in range(2):
                sc.op("pe", lambda e, c=c, qk=qk, tk=tk: e.transpose(psK[:, c * 128:(c + 1) * 128], qk[:, 2 + c, tk], ident_b),
                      reads=[qkb, b_identb], writes=[PB[2]] if c == 0 else (), pwrites=() if c == 0 else [PB[2]], sig=(c == 1))
            kk, kkb = k2.next()
            sc.op("act", lambda e, kk=kk, col=col: e.activation(kk, psK, AF.Copy, scale=eks[:, col:col + 1]), reads=[PB[2], b_eks], writes=[kkb])
            return wt, wtb, kk, kkb

        def stage2(n, wt, wtb, kk, kkb):
            col = n * 4 + h
            tk = slice(n * 128, (n + 1) * 128)
            psP1 = psbank(3, 257)
            for c in range(2):
                sc.op("pe", lambda e, c=c, qk=qk, tk=tk: e.matmul(psP1, qk[:, c, tk], Cb[:, c, :], start=(c == 0), stop=(c == 1)),
                      reads=[qkb, b_Cb], writes=[PB[3]] if c == 0 else (), pwrites=() if c == 0 else [PB[3]], sig=(c == 1))
            psP2 = psbank(4, 257)
            sc.op("pe", lambda e, wt=wt, v=v, n=n: e.matmul(psP2, wt, v[:, n, :], start=True, stop=True), reads=[wtb, vb], writes=[PB[4]])
            t1, t1b = T1.next()
            sc.op("act", lambda e, t1=t1, col=col: e.activation(t1, psP1, AF.Copy, scale=ebl[:, col:col + 1]), reads=[PB[3], b_ebl], writes=[t1b])
            nm, nmb = NUM.next()
            sc.op("dve", lambda e, nm=nm, t1=t1: e.tensor_tensor(nm, t1, psP2, op=ALU.add), reads=[t1b, PB[4]], writes=[nmb])
            s8, s8b = st8.next()
            sc.op("dve", lambda e, s8=s8, nm=nm: e.tensor_scalar(s8[:, 6:7], nm[:, 256:257], -1.0, None, op0=ALU.mult),
                  reads=[nmb], writes=[s8b])
            sc.op("dve", lambda e, s8=s8, nm=nm: e.scalar_tensor_tensor(s8[:, 0:1], nm[:, 256:257], 1.0, s8[:, 6:7], op0=ALU.max, op1=ALU.max),
                  reads=[nmb, s8b], pwrites=[s8b])
            sc.op("dve", lambda e, s8=s8: e.reciprocal(s8[:, 1:2], s8[:, 0:1]), reads=[s8b], pwrites=[s8b])
            hx, hxb = hh.next()
            sc.op("dve", lambda e, hx=hx, nm=nm, s8=s8: e.tensor_scalar(hx, nm[:, 0:256], s8[:, 1:2], None, op0=ALU.mult), reads=[nmb, s8b], writes=[hxb])
            hq, hqb = t1[:, 0:256], t1b
            sc.op("act", lambda e, hq=hq, hx=hx: e.activation(hq, hx, AF.Square), reads=[hxb, nmb], writes=[hqb])
            sc.op("dve", lambda e, s8=s8, hq=hq: e.reduce_sum(s8[:, 2:3], hq, axis=AX.X), reads=[hqb], pwrites=[s8b])
            sc.op("dve", lambda e, s8=s8: e.tensor_scalar(s8[:, 3:4], s8[:, 2:3], 1.0 / 256, EPS, op0=ALU.mult, op1=ALU.add), reads=[s8b], pwrites=[s8b])
            sc.op("act", lambda e, s8=s8: e.activation(s8[:, 4:5], s8[:, 3:4], AF.Ln), reads=[s8b], pwrites=[s8b])
            sc.op("act", lambda e, s8=s8: e.activation(s8[:, 5:6], s8[:, 4:5], AF.Exp, scale=-0.5), reads=[s8b], pwrites=[s8b])
            sc.op("dve", lambda e, hx=hx, s8=s8, h=h: e.scalar_tensor_tensor(hx, hx, s8[:, 5:6], mnormg[:, h * 256:(h + 1) * 256], op0=ALU.mult, op1=ALU.mult),
                  reads=[hxb, s8b, b_mnormg], writes=[hxb])
            hf, hfb = hfin.next()
            sc.op("pool", lambda e, hf=hf, hx=hx, so=so, n=n: e.tensor_tensor(hf, hx, so[:, n, :], op=ALU.mult), reads=[hxb, sob], writes=[hfb])
            psH = psbank(5, 128).bitcast(BF16)
            for c in range(2):
                sc.op("pe", lambda e, c=c, hf=hf: e.transpose(psH[:, c * 128:(c + 1) * 128], hf[:, c * 128:(c + 1) * 128], ident_b),
                      reads=[hfb, b_identb], writes=[PB[5]] if c == 0 else (), pwrites=() if c == 0 else [PB[5]], sig=(c == 1))
            sc.op("act", lambda e, hT=hT, tk=tk: e.copy(hT[:, :, tk], psH.rearrange("p (c t) -> p c t", c=2)), reads=[PB[5]], pwrites=[hTb])
            for c in range(2):
                psKV = psbank(6 + c, 257)
                sc.op("pe", lambda e, c=c, kk=kk, v=v, n=n, psKV=psKV: e.matmul(psKV, kk[:, c * 128:(c + 1) * 128], v[:, n, :], start=True, stop=True),
                      reads=[kkb, vb], writes=[PB[6 + c]])
                ct, ctb = ctmp.next()
                sc.op("dve", lambda e, c=c, ct=ct, psKV=psKV: e.tensor_tensor(ct, Cf[:, c, :], psKV, op=ALU.add), reads=[b_Cf, PB[6 + c]], writes=[ctb])
                sc.op("dve", lambda e, c=c, ct=ct, col=col: e.tensor_scalar(Cf[:, c, :], ct, ebL[:, col:col + 1], None, op0=ALU.mult),
                      reads=[ctb, b_ebL], pwrites=[b_Cf])
                sc.op("act", lambda e, c=c, ct=ct, col=col: e.activation(Cb[:, c, :], ct, AF.Copy, scale=ebL[:, col:col + 1]),
                      reads=[ctb, b_ebL], pwrites=[b_Cb])

        def finish():
            sc.dma(hmT_rows[:, 2 * h:2 * h + 2, :], hT, hTb, reads=[hTb])
        return stage1, stage2, finish

    for hp in range(2):
        H = [head_setup(2 * hp), head_setup(2 * hp + 1)]
        cur = [H[i][0](0) for i in range(2)]
        for n in range(NT):
            nxt_ = [H[i][0](n + 1) if n + 1 < NT else None for i in range(2)]
            for i in range(2):
                H[i][1](n, *cur[i])
            cur = nxt_
        for i in range(2):
            H[i][2]()
    sc.barrier()
    A.reset(base_mark)


def _finish(nc, sc, es):
    sc.barrier()

    with nc.Block() as block:
        @block.sync
        def _(e):
            sc.replay("sp", e)

        @block.tensor
        def _(e):
            sc.replay("pe", e)

        @block.scalar
        def _(e):
            sc.replay("act", e)

        @block.vector
        def _(e):
            sc.replay("dve", e)

        @block.gpsimd
        def _(e):
            sc.replay("pool", e)
    es.close()


def rope_tables_T():
    inv = (np.float32(10000.0) ** (-(np.arange(0, 128, 2, dtype=np.float32)) / np.float32(128))).astype(np.float32)
    ang = (np.arange(S, dtype=np.float32)[:, None] * inv[None, :]).astype(np.float32)
    ang = np.concatenate([ang, ang], axis=-1)
    cosT = np.cos(ang).astype(np.float32).T.copy()
    sinT = np.sin(ang).astype(np.float32).T.copy()
    sinT[:64, :] *= -1.0
    return np.ascontiguousarray(cosT), np.ascontiguousarray(sinT)


def _overlap_const():
    n = np.arange(256)[:, None]
    j = np.arange(64)[None, :]
    ov = ((16 * n < 64 * j + 64) & (16 * n + 32 > 64 * j) & (n < 255)).astype(np.float32)
    return np.ascontiguousarray(ov.reshape(2, 128, 64))


def _eall_const():
    j = np.arange(64)[:, None]
    p = np.arange(S)[None, :]
    return np.ascontiguousarray((p // 64 == j).astype(np.float32))


def _kblk(k1, k2):
    kb = np.zeros((128, 256), np.float32)
    kb[0:64, 0:128] = k1.T
    kb[64:128, 128:256] = k2.T
    return kb


def prep_shared(inputs):
    f = np.float32
    w_in = np.asarray(inputs["w_in"][0], dtype=f)

    def kp_layout(w):
        return np.ascontiguousarray(w.reshape(8, 128, w.shape[1]).transpose(1, 0, 2))

    wfm = w_in[:, fm_col_index()]
    w_fm = np.ascontiguousarray(kp_layout(wfm).reshape(128, 8, 60, 128).transpose(2, 0, 1, 3))
    wtm = w_in[:, tm_col_index()]
    w_tm = np.ascontiguousarray(kp_layout(wtm).reshape(128, 8, 5, 512).transpose(2, 0, 1, 3))
    w_sm = kp_layout(w_in[:, sm_col_index()])
    cosT, sinT = rope_tables_T()
    conv_w = np.asarray(inputs["m_conv_w"][0], dtype=f)
    conv_b = np.asarray(inputs["m_conv_b"][0], dtype=f)
    cw = np.concatenate([conv_w.T, conv_b[:, None]], axis=1)
    shared = {
        "g_mix": np.ascontiguousarray(np.broadcast_to(np.asarray(inputs["ln_mix_g"][0], dtype=f)[None, :], (128, D))),
        "w_fm": w_fm, "w_tm": w_tm, "w_sm": w_sm, "cosT": cosT, "sinT": sinT,
        "conv_w": np.ascontiguousarray(cw.reshape(16, 128, 5)),
        "ident": np.eye(128, dtype=f),
        "tri": np.triu(np.ones((128, 128), dtype=f)),
        "ovl": _overlap_const(), "eall": _eall_const(),
        "peer_u": np.ascontiguousarray(np.asarray(inputs["peer_u"][0], dtype=f)),
        "peer_v": np.ascontiguousarray(np.asarray(inputs["peer_v"][0], dtype=f)),
        "wq": kp_layout(np.asarray(inputs["peer_wq"][0], dtype=f)),
        "kblk": _kblk(np.asarray(inputs["peer_k1"][0], dtype=f), np.asarray(inputs["peer_k2"][0], dtype=f)),
        "iota": np.ascontiguousarray(np.broadcast_to(np.arange(128, dtype=f)[None, :], (128, 128))),
        "g_fin": np.ascontiguousarray(np.broadcast_to(np.asarray(inputs["ln_f_g"], dtype=f)[None, :], (128, D))),
        "wsq": np.ascontiguousarray(np.stack([kp_layout(np.asarray(inputs[k][0], dtype=f)) for k in ("w_m_out", "w_n_out", "w_out")])),
        "g_ffn": np.ascontiguousarray(np.broadcast_to(np.asarray(inputs["ln_ffn_g"][0], dtype=f)[None, :], (128, D))),
        "cw1": np.ascontiguousarray(np.stack([np.asarray(inputs[k][0], dtype=f).reshape(32, 128, 128).transpose(1, 0, 2)
                                              for k in ("cmp_k_w1", "cmp_v_w1")])),
        "cw2": np.ascontiguousarray(np.stack([np.asarray(inputs[k][0], dtype=f) for k in ("cmp_k_w2", "cmp_v_w2")])),
        "cpos": np.ascontiguousarray(np.stack([np.asarray(inputs[k][0], dtype=f).T for k in ("cmp_k_pos", "cmp_v_pos")])),
        "m_bias": np.ascontiguousarray(np.broadcast_to(np.concatenate([np.asarray(inputs["m_i_bias"][0], dtype=f),
                                                                       np.asarray(inputs["m_f_bias"][0], dtype=f)])[None, :], (128, 8))),
        "m_norm_g": np.ascontiguousarray(np.broadcast_to(np.asarray(inputs["m_norm_g"][0], dtype=f)[None, :], (128, 1024))),
    }
    return shared


def kernel(**inputs):
    shared = prep_shared(inputs)
    x = np.asarray(inputs["x"], dtype=np.float32)
    nc = build()
    in_maps = []
    for c in range(N_CORES):
        m = dict(shared)
        m["x"] = np.ascontiguousarray(x[c])
        in_maps.append(m)
    res = run_bass_kernel_spmd(nc, in_maps, core_ids=list(range(N_CORES)))
    return np.stack([np.asarray(r["out"], dtype=np.float32) for r in res.results], axis=0)
```

```python
from contextlib import ExitStack
import numpy as np
import ml_dtypes
import concourse.bass as bass
import concourse.mybir as mybir
from concourse.bass_utils import run_bass_kernel_spmd

F32 = mybir.dt.float32
BF16 = mybir.dt.bfloat16
U32 = mybir.dt.uint32
ALU = mybir.AluOpType
AF = mybir.ActivationFunctionType
AX = mybir.AxisListType

S = 4096
D = 1024
NT = 32
EPS = 1e-6
N_CORES = 8
E2_VQ_ACT = False
SAME_ENGINE_WAITS = True


class Buf:
    __slots__ = ("name", "w", "r", "dsem", "dcnt", "excl")

    def __init__(self, name, excl=False):
        self.name = name
        self.excl = excl
        self.w = {}
        self.r = {}
        self.dsem = None
        self.dcnt = 0


class _Rec:
    def __init__(self):
        self.call = None

    def __getattr__(self, name):
        def f(*a, **k):
            self.call = (name, a, k)
            return self
        return f


def _bind(fn):
    rec = _Rec()
    fn(rec)
    name, a, k = rec.call

    def run(e):
        try:
            return getattr(e, name)(*a, **k)
        except Exception:
            print("FAILED OP", name, [getattr(x, "shape", x) for x in a], {kk: getattr(v, "shape", v) for kk, v in k.items()})
            raise
    return run


class Sched:
    ENG = ("pe", "act", "dve", "pool", "sp")

    def __init__(self, nc, es):
        self.nc, self.es = nc, es
        self.q = {e: [] for e in self.ENG}
        self.sems = []
        self.ecnt = {e: 0 for e in self.ENG}
        self.seen = {e: {} for e in self.ENG}
        self.pending = {e: [] for e in self.ENG}
        self.esem = {}
        for e in ("pe", "act", "dve", "pool"):
            self.esem[e] = self.new_sem("s_" + e)
        self.dbufs = []
        self.sem_pool = []
        self.nbuf = 0

    def new_sem(self, name):
        h = self.es.enter_context(self.nc.semaphore(name))
        self.sems.append(h)
        return len(self.sems) - 1

    def buf(self, name="b"):
        self.nbuf += 1
        return Buf("%s%d" % (name, self.nbuf))

    def _waits(self, eng, reads, writes, pwrites):
        need = {}

        def add(evs):
            for s, v in evs.items():
                if need.get(s, 0) < v:
                    need[s] = v

        for b in reads:
            add(b.w)
            if b.excl:
                add(b.r)
        for b in writes:
            add(b.w)
            add(b.r)
        for b in pwrites:
            add(b.w)
            add(b.r)
        out = []
        seen = self.seen[eng]
        own = self.esem.get(eng)
        for s, v in need.items():
            if s == own and not SAME_ENGINE_WAITS:
                continue
            if seen.get(s, 0) < v:
                seen[s] = v
                out.append((s, v))
        return out

    @staticmethod
    def _commit(ev, reads, writes, pwrites):
        s, v = ev
        for b in writes:
            b.w = {s: v}
            b.r = {}
        for b in pwrites:
            if b.w.get(s, 0) < v:
                b.w[s] = v
        for b in reads:
            if b.r.get(s, 0) < v:
                b.r[s] = v

    def op(self, eng, fn, reads=(), writes=(), pwrites=(), sig=True):
        fn = _bind(fn)
        waits = self._waits(eng, reads, writes, pwrites)
        if not sig:
            self.q[eng].append((waits, fn, None))
            self.pending[eng].append((reads, writes, pwrites))
            return
        self.ecnt[eng] += 1
        ev = (self.esem[eng], self.ecnt[eng])
        self.q[eng].append((waits, fn, (ev[0], 1)))
        for (r, w, pw) in self.pending[eng]:
            self._commit(ev, r, w, pw)
        self.pending[eng] = []
        self._commit(ev, reads, writes, pwrites)

    def dma(self, out_ap, in_ap, sb, reads=(), writes=(), pwrites=(), q="sp"):
        waits = self._waits(q, reads, writes, pwrites)
        if sb.dsem is None:
            if self.sem_pool:
                sb.dsem, sb.dcnt = self.sem_pool.pop()
            else:
                sb.dsem = self.new_sem("d_" + sb.name)
            self.dbufs.append(sb)
        sb.dcnt += 16
        ev = (sb.dsem, sb.dcnt)
        self.q[q].append((waits, lambda e: e.dma_start(out=out_ap, in_=in_ap), (sb.dsem, 16)))
        self._commit(ev, reads, writes, pwrites)

    def barrier(self):
        evs = [(self.esem[e], self.ecnt[e]) for e in ("pe", "act", "dve", "pool") if self.ecnt[e] > 0]
        evs += [(b.dsem, b.dcnt) for b in self.dbufs]
        for e in self.ENG:
            seen = self.seen[e]
            ws = []
            for s, v in evs:
                if seen.get(s, 0) < v:
                    seen[s] = v
                    ws.append((s, v))
            if ws:
                self.q[e].append((ws, None, None))
        for b in self.dbufs:
            self.sem_pool.append((b.dsem, b.dcnt))
            b.dsem = None
        self.dbufs = []

    def replay(self, name, eng):
        for waits, fn, inc in self.q[name]:
            for s, v in waits:
                eng.wait_ge(self.sems[s], v)
            if fn is None:
                continue
            ins = fn(eng)
            if inc is not None:
                ins.then_inc(self.sems[inc[0]], inc[1])


class Ring:
    def __init__(self, sc, name, aps, bufs=None):
        self.items = [(ap, sc.buf(name) if bufs is None else bufs[i]) for i, ap in enumerate(aps)]
        self.i = 0

    def next(self):
        it = self.items[self.i % len(self.items)]
        self.i += 1
        return it


class Arena:
    def __init__(self, t, nbytes):
        self.t = t
        self.cap = nbytes
        self.off = 0

    def mark(self):
        return self.off

    def reset(self, m):
        self.off = m

    def alloc(self, shape, dt):
        esz = 4 if dt in (F32, U32) else 2
        n = 1
        for s_ in shape:
            n *= s_
        nb = (n * esz + 31) // 32 * 32
        assert self.off + nb <= self.cap, ("SBUF arena overflow", self.off, nb, self.cap)
        a = self.t[:, self.off // 4:(self.off + nb) // 4]
        self.off += nb
        if esz == 2:
            a = a.bitcast(dt)
        elif dt is not F32:
            a = a.bitcast(dt)
        a = a[:, 0:n]
        if len(shape) == 2:
            return a.rearrange("p (a b) -> p a b", a=shape[0])
        if len(shape) == 3:
            return a.rearrange("p (a b c) -> p a b c", a=shape[0], b=shape[1])
        return a


IN_WIDTHS = (2048, 1024, 1024, 4, 4, 1024, 256, 256, 256, 256, 256, 256, 24, 1024, 1024)
_off = np.cumsum((0,) + IN_WIDTHS)
(O_QK, O_V, O_O, O_I, O_F, O_NQ, O_KC, O_VC, O_KS, O_VS, O_KW, O_VW, O_NG, O_GA, O_GB) = [int(v) for v in _off[:-1]]


def _rot_cols(base, nheads):
    idx = []
    for h in range(nheads):
        for d in range(128):
            idx.append(base + h * 128 + (d + 64) % 128)
    return idx


def fm_col_index():
    cols = list(range(O_QK, O_QK + 2048))
    cols += list(range(O_NQ, O_NQ + 1024)) + _rot_cols(O_NQ, 8)
    cols += list(range(O_KC, O_KC + 256)) + list(range(O_VC, O_VC + 256))
    cols += list(range(O_KS, O_KS + 256)) + _rot_cols(O_KS, 2)
    cols += list(range(O_KW, O_KW + 256)) + _rot_cols(O_KW, 2)
    cols += list(range(O_GA, O_GA + 1024)) + list(range(O_GB, O_GB + 1024))
    return np.asarray(cols)


def tm_col_index():
    cols = list(range(O_V, O_V + 1024)) + list(range(O_O, O_O + 1024))
    cols += list(range(O_VS, O_VS + 256)) + list(range(O_VW, O_VW + 256))
    return np.asarray(cols)


def sm_col_index():
    return np.asarray(list(range(O_I, O_I + 4)) + list(range(O_F, O_F + 4)) + list(range(O_NG, O_NG + 24)))


ZR_QK, ZR_Q, ZR_QR, ZR_KC, ZR_VC, ZR_KSR, ZR_KWR, ZR_GA, ZR_GB = 0, 2048, 3072, 4096, 4352, 4608, 4864, 5120, 6144
ZT_ROWS = 7168


class _Stop(Exception):
    pass


def build(dbg=(), stop=None):
    nc = bass.Bass("TRN2", target_bir_lowering=False)
    es = ExitStack()
    dbg = set(dbg)

    def check(name):
        if stop == name:
            raise _Stop()

    def din(name, shape, dt=F32):
        return nc.dram_tensor(name, list(shape), dt, kind="ExternalInput").ap()

    def dscr(name, shape, dt):
        kind = "ExternalOutput" if name in dbg else "Internal"
        return nc.dram_tensor(name, list(shape), dt, kind=kind).ap()

    x_d = din("x", [S, D])
    gmix_d = din("g_mix", [128, D])
    wfm_d = din("w_fm", [60, 128, 8, 128])
    wtm_d = din("w_tm", [5, 128, 8, 512])
    wsm_d = din("w_sm", [128, 8, 32])
    cos_d = din("cosT", [128, S])
    sin_d = din("sinT", [128, S])
    convw_d = din("conv_w", [16, 128, 5])
    ident_d = din("ident", [128, 128])
    tri_d = din("tri", [128, 128])
    mbias_d = din("m_bias", [128, 8])
    mnormg_d = din("m_norm_g", [128, 1024])
    out_d = nc.dram_tensor("out", [S, D], F32, kind="ExternalOutput").ap()

    zT_d = dscr("zT", [ZT_ROWS, S], BF16)
    ztm_d = dscr("ztm", [S, 2560], BF16)
    zsm_d = dscr("zsm", [S, 32], F32)
    hmT_d = dscr("hmT", [1024, S], BF16)
    onT_d = dscr("onT", [1024, S], BF16)
    h1_d = dscr("h1", [S, D], F32)
    xhT_d = dscr("xhT", [1024, S], BF16)
    wsq_d = din("wsq", [3, 128, 8, 1024])
    u_d = din("peer_u", [16384, 1024])
    v_d = din("peer_v", [16384, 1024])
    uT_d = dscr("uT", [128, 128, 8, 128], BF16)
    vb_d = dscr("vb", [128, 128, 1024], BF16)
    wq_d = din("wq", [128, 8, 1024])
    kblk_d = din("kblk", [128, 256])
    iota_d = din("iota", [128, 128])
    gf_d = din("g_fin", [128, D])
    gffn_d = din("g_ffn", [128, D])
    ovl_d = din("ovl", [2, 128, 64])
    eall_d = din("eall", [64, S])
    cw1_d = din("cw1", [2, 128, 32, 128])
    cw2_d = din("cw2", [2, 128, 128])
    cpos_d = din("cpos", [2, 128, 32])

    ARENA_BYTES = 204 * 1024
    arena_t = es.enter_context(nc.sbuf_tensor("arena", [128, ARENA_BYTES // 4], F32))
    psum_t = es.enter_context(nc.psum_tensor("psum", [128, 4096], F32))
    sc = Sched(nc, es)
    A = Arena(arena_t, ARENA_BYTES)

    def psbank(i, n=512, off=0):
        return psum_t[:, i * 512 + off:i * 512 + off + n]

    PB = [Buf("psb%d" % i, excl=True) for i in range(8)]

    try:
        _body(nc, sc, A, psbank, PB, check, locals())
    except _Stop:
        pass
    _finish(nc, sc, es)
    return nc


def _body(nc, sc, A, psbank, PB, check, L):
    (x_d, gmix_d, wfm_d, wtm_d, wsm_d, cos_d, sin_d, convw_d, ident_d, out_d, zT_d, ztm_d, zsm_d) = [L[k] for k in (
        "x_d", "gmix_d", "wfm_d", "wtm_d", "wsm_d", "cos_d", "sin_d", "convw_d", "ident_d", "out_d", "zT_d", "ztm_d", "zsm_d")]
    tri_d, mbias_d, mnormg_d, hmT_d = L["tri_d"], L["mbias_d"], L["mnormg_d"], L["hmT_d"]
    dbg = L["dbg"]

    def dump(name, ap, b, dt=F32):
        if name in dbg:
            d = nc.dram_tensor(name, [128] + list(ap.shape[1:]), dt, kind="ExternalOutput").ap()
            sc.dma(d, ap, b, reads=[b])

    ident_f = A.alloc([128], F32)
    ident_b = A.alloc([128], BF16)
    b_ident = sc.buf("ident")
    sc.dma(ident_f, ident_d, b_ident, writes=[b_ident])
    b_identb = sc.buf("identb")
    sc.op("dve", lambda e: e.tensor_copy(ident_b, ident_f), reads=[b_ident], writes=[b_identb])
    tri_f = A.alloc([128], F32)
    b_tri = sc.buf("tri")
    sc.dma(tri_f, tri_d, b_tri, writes=[b_tri])
    ones_f = A.alloc([128], F32)
    b_ones = sc.buf("ones")
    sc.op("pool", lambda e: e.memset(ones_f, 1.0), writes=[b_ones])
    base_mark = A.mark()
    check("const")

    xnT = A.alloc([8, S], BF16)
    b_xnT = sc.buf("xnT")
    gmix = A.alloc([D], F32)
    b_gmix = sc.buf("gmix")
    sc.dma(gmix, gmix_d, b_gmix, writes=[b_gmix])
    mA = A.mark()
    xin = Ring(sc, "xin", [A.alloc([D], F32) for _ in range(2)])
    sqr = Ring(sc, "sq", [A.alloc([D], F32) for _ in range(1)])
    xnb = Ring(sc, "xnb", [A.alloc([D], BF16) for _ in range(2)])
    stat = Ring(sc, "stat", [A.alloc([4], F32) for _ in range(2)])
    pst = Ring(sc, "pst", [psbank(i).bitcast(BF16) for i in (6, 7)], [PB[6], PB[7]])
    x_t = x_d.rearrange("(n p) d -> n p d", p=128)
    for t in range(NT):
        xa, xb = xin.next()
        sc.dma(xa, x_t[t], xb, writes=[xb])
        sq, sqb = sqr.next()
        st, stb = stat.next()
        sc.op("act", lambda e, sq=sq, xa=xa: e.activation(sq, xa, AF.Square), reads=[xb], writes=[sqb])
        sc.op("dve", lambda e, st=st, sq=sq: e.reduce_sum(st[:, 0:1], sq, axis=AX.X), reads=[sqb], writes=[stb])
        sc.op("dve", lambda e, st=st: e.tensor_scalar(st[:, 1:2], st[:, 0:1], 1.0 / D, EPS, op0=ALU.mult, op1=ALU.add),
              reads=[stb], pwrites=[stb])
        sc.op("act", lambda e, st=st: e.activation(st[:, 2:3], st[:, 1:2], AF.Ln), reads=[stb], pwrites=[stb])
        sc.op("act", lambda e, st=st: e.activation(st[:, 3:4], st[:, 2:3], AF.Exp, scale=-0.5), reads=[stb], pwrites=[stb])
        xn, xnbuf = xnb.next()
        sc.op("dve", lambda e, xn=xn, xa=xa, st=st: e.scalar_tensor_tensor(
            xn, xa, st[:, 3:4], gmix, op0=ALU.mult, op1=ALU.mult), reads=[xb, stb, b_gmix], writes=[xnbuf])
        if t == 1:
            check("norm1")
        pt, ptb = pst.next()
        for k in range(8):
            sc.op("pe", lambda e, pt=pt, xn=xn, k=k: e.transpose(pt[:, k * 128:(k + 1) * 128], xn[:, k * 128:(k + 1) * 128], ident_b),
                  reads=[xnbuf, b_identb], writes=[ptb] if k == 0 else (), pwrites=() if k == 0 else [ptb], sig=(k == 7))
        if t == 1:
            check("norm2")
        sc.op("act", lambda e, pt=pt, t=t: e.copy(xnT[:, :, t * 128:(t + 1) * 128], pt.rearrange("p (k c) -> p k c", k=8)),
              reads=[ptb], pwrites=[b_xnT])
        check("norm3_%d" % t)
    sc.barrier()
    A.reset(mA)
    check("norm")

    cosT = A.alloc([S], F32)
    sinT = A.alloc([S], F32)
    b_cos, b_sin = sc.buf("cos"), sc.buf("sin")
    sc.dma(cosT, cos_d, b_cos, writes=[b_cos])
    sc.dma(sinT, sin_d, b_sin, writes=[b_sin])

    wf32 = Ring(sc, "wf32", [A.alloc([8, 128], F32) for _ in range(4)])
    wbf = Ring(sc, "wbf", [A.alloc([8, 128], BF16) for _ in range(4)])
    zrow = Ring(sc, "zrow", [A.alloc([S], BF16) for _ in range(3)])
    z32 = Ring(sc, "z32", [A.alloc([S + 4], F32) for _ in range(1)])
    acc = Ring(sc, "acc", [A.alloc([S], F32) for _ in range(1)])
    cw = Ring(sc, "cw", [A.alloc([5], F32) for _ in range(2)])
    t1r = Ring(sc, "t1", [A.alloc([512], F32) for _ in range(2)])
    t2r = Ring(sc, "t2", [A.alloc([512], F32) for _ in range(2)])
    psA = Ring(sc, "psA", [psbank(i) for i in range(6)], PB[0:6])
    evq = [0]

    wmemo = {}

    def load_w(c):
        if c in wmemo:
            return wmemo.pop(c)
        return _load_w(c)

    def prefetch_w(cs):
        for c in cs:
            if c not in wmemo:
                wmemo[c] = _load_w(c)

    def _load_w(c):
        wa, wb = wf32.next()
        sc.dma(wa, wfm_d[c], wb, writes=[wb])
        wba, wbb = wbf.next()
        sc.op("act", lambda e: e.copy(wba, wa), reads=[wb], writes=[wbb])
        return wba, wbb

    def proj_group(wba, wbb, g):
        ps, psb = psA.next()
        for k in range(8):
            sc.op("pe", lambda e, k=k: e.matmul(ps, wba[:, k, :], xnT[:, k, g * 512:(g + 1) * 512], start=(k == 0), stop=(k == 7)),
                  reads=[wbb, b_xnT], writes=[psb] if k == 0 else (), pwrites=() if k == 0 else [psb], sig=(k == 7))
        return ps, psb

    def store_row(zr, zrb, row0):
        sc.dma(zT_d[row0:row0 + 128, :], zr, zrb, reads=[zrb])

    def evac_copy(dst, dstb, ps, psb, func=None):
        evq[0] += 1
        if func is not None:
            sc.op("act", lambda e: e.activation(dst, ps, func), reads=[psb], pwrites=[dstb])
        elif evq[0] % 2 == 0:
            sc.op("act", lambda e: e.copy(dst, ps), reads=[psb], pwrites=[dstb])
        else:
            sc.op("dve", lambda e: e.tensor_copy(dst, ps), reads=[psb], pwrites=[dstb])

    def qk_job(c):
        wba, wbb = load_w(c)
        cwa, cwb = cw.next()
        sc.dma(cwa, convw_d[c], cwb, writes=[cwb])
        za, zb = z32.next()
        sc.op("pool", lambda e, za=za: e.memset(za[:, 0:4], 0.0), writes=[zb])
        for g in range(8):
            ps, psb = proj_group(wba, wbb, g)
            evac_copy(za[:, 4 + g * 512:4 + (g + 1) * 512], zb, ps, psb)
        aa, ab = acc.next()
        sc.op("dve", lambda e, aa=aa, za=za, cwa=cwa: e.tensor_scalar(aa, za[:, 4:4 + S], cwa[:, 3:4], cwa[:, 4:5], op0=ALU.mult, op1=ALU.add),
              reads=[zb, cwb], writes=[ab])
        sc.op("dve", lambda e, aa=aa, za=za, cwa=cwa: e.scalar_tensor_tensor(aa, za[:, 3:3 + S], cwa[:, 2:3], aa, op0=ALU.mult, op1=ALU.add),
              reads=[zb, cwb, ab], pwrites=[ab])
        sc.op("dve", lambda e, aa=aa, za=za, cwa=cwa: e.scalar_tensor_tensor(aa, za[:, 2:2 + S], cwa[:, 1:2], aa, op0=ALU.mult, op1=ALU.add),
              reads=[zb, cwb, ab], pwrites=[ab])
        sc.op("dve", lambda e, aa=aa, za=za, cwa=cwa: e.scalar_tensor_tensor(aa, za[:, 1:1 + S], cwa[:, 0:1], aa, op0=ALU.mult, op1=ALU.add),
              reads=[zb, cwb, ab], pwrites=[ab])
        zr, zrb = zrow.next()
        sc.op("act", lambda e, zr=zr, aa=aa: e.activation(zr, aa, AF.Silu), reads=[ab], writes=[zrb])
        store_row(zr, zrb, ZR_QK + c * 128)

    check("qk")
    def rope_job(c_plain, c_rot, row_plain, row_rot):
        wba, wbb = load_w(c_plain)
        wbr, wbrb = load_w(c_rot)
        if row_plain is not None:
            zp, zpb = zrow.next()
            sc.op("pool", lambda e: e.memset(zp[:, 0:1], 0.0), writes=[zpb])
        zr, zrb = zrow.next()
        sc.op("pool", lambda e: e.memset(zr[:, 0:1], 0.0), writes=[zrb])
        for g in range(8):
            sl = slice(g * 512, (g + 1) * 512)
            ps, psb = proj_group(wba, wbb, g)
            ps2, ps2b = proj_group(wbr, wbrb, g)
            if row_plain is not None:
                sc.op("act", lambda e, ps=ps, sl=sl: e.copy(zp[:, sl], ps), reads=[psb], pwrites=[zpb])
            t1, t1b = t1r.next()
            t2, t2b = t2r.next()
            sc.op("dve", lambda e, t1=t1, ps=ps, sl=sl: e.tensor_tensor(t1, ps, cosT[:, sl], op=ALU.mult), reads=[psb, b_cos], writes=[t1b])
            sc.op("dve", lambda e, t2=t2, ps2=ps2, sl=sl: e.tensor_tensor(t2, ps2, sinT[:, sl], op=ALU.mult), reads=[ps2b, b_sin], writes=[t2b])
            sc.op("pool", lambda e, t1=t1, t2=t2, sl=sl: e.tensor_tensor(zr[:, sl], t1, t2, op=ALU.add), reads=[t1b, t2b], pwrites=[zrb])
        if row_plain is not None:
            store_row(zp, zpb, row_plain)
        store_row(zr, zrb, row_rot)

    jobs = [((c,), (lambda c=c: qk_job(c))) for c in range(16)]
    for h in range(8):
        jobs.append(((16 + h, 24 + h), (lambda h=h: rope_job(16 + h, 24 + h, ZR_Q + h * 128, ZR_QR + h * 128))))
    for gI in range(2):
        jobs.append(((36 + gI, 38 + gI), (lambda gI=gI: rope_job(36 + gI, 38 + gI, None, ZR_KSR + gI * 128))))
        jobs.append(((40 + gI, 42 + gI), (lambda gI=gI: rope_job(40 + gI, 42 + gI, None, ZR_KWR + gI * 128))))

    check("rope")
    def plain_job(c, row0, func=None):
        wba, wbb = load_w(c)
        zr, zrb = zrow.next()
        sc.op("pool", lambda e: e.memset(zr[:, 0:1], 0.0), writes=[zrb])
        for g in range(8):
            ps, psb = proj_group(wba, wbb, g)
            evac_copy(zr[:, g * 512:(g + 1) * 512], zrb, ps, psb, func)
        store_row(zr, zrb, row0)

    for i in range(2):
        jobs.append(((32 + i,), (lambda i=i: plain_job(32 + i, ZR_KC + i * 128))))
        jobs.append(((34 + i,), (lambda i=i: plain_job(34 + i, ZR_VC + i * 128))))
    for i in range(8):
        jobs.append(((44 + i,), (lambda i=i: plain_job(44 + i, ZR_GA + i * 128, AF.Sigmoid))))
        jobs.append(((52 + i,), (lambda i=i: plain_job(52 + i, ZR_GB + i * 128, AF.Sigmoid))))
    prefetch_w(jobs[0][0])
    for ji, (cs, fn) in enumerate(jobs):
        if ji + 1 < len(jobs):
            prefetch_w(jobs[ji + 1][0])
        fn()

    check("fm")
    sc.barrier()
    A.reset(mA)
    wt32 = Ring(sc, "wt32", [A.alloc([8, 512], F32) for _ in range(1)])
    wtbf = Ring(sc, "wtbf", [A.alloc([8, 512], BF16) for _ in range(2)])
    ztile = Ring(sc, "ztile", [A.alloc([512], BF16) for _ in range(3)])
    zstile = Ring(sc, "zstile", [A.alloc([32], F32) for _ in range(3)])
    for blk in range(6):
        small = blk == 5
        n = 32 if small else 512
        wa, wb = wt32.next()
        wba, wbb = wtbf.next()
        if small:
            sc.dma(wa[:, :, 0:32], wsm_d, wb, writes=[wb])
        else:
            sc.dma(wa, wtm_d[blk], wb, writes=[wb])
        sc.op("pool", lambda e, wba=wba, wa=wa, n=n: e.tensor_copy(wba[:, :, 0:n], wa[:, :, 0:n]), reads=[wb], writes=[wbb])
        func = AF.Sigmoid if blk in (2, 3) else None
        for t in range(NT):
            ps, psb = psA.next()
            for k in range(8):
                sc.op("pe", lambda e, ps=ps, wba=wba, k=k, t=t, n=n: e.matmul(ps[:, 0:n], xnT[:, k, t * 128:(t + 1) * 128], wba[:, k, 0:n],
                                                                      start=(k == 0), stop=(k == 7)),
                      reads=[wbb, b_xnT], writes=[psb] if k == 0 else (), pwrites=() if k == 0 else [psb], sig=(k == 7))
            if small:
                zt, ztb = zstile.next()
                sc.op("dve", lambda e, zt=zt, ps=ps: e.tensor_copy(zt, ps[:, 0:32]), reads=[psb], writes=[ztb])
                sc.dma(zsm_d[t * 128:(t + 1) * 128, :], zt, ztb, reads=[ztb])
            else:
                zt, ztb = ztile.next()
                evq[0] += 1
                if func is not None or evq[0] % 2 == 0:
                    sc.op("act", lambda e, zt=zt, ps=ps, func=func: e.activation(zt, ps, func if func is not None else AF.Copy),
                          reads=[psb], writes=[ztb])
                else:
                    sc.op("dve", lambda e, zt=zt, ps=ps: e.tensor_copy(zt, ps), reads=[psb], writes=[ztb])
                sc.dma(ztm_d[t * 128:(t + 1) * 128, blk * 512:(blk + 1) * 512], zt, ztb, reads=[ztb])
    sc.barrier()
    A.reset(base_mark)
    check("A")
    phase_mlstm(**locals())
    check("B")
    phase_nsa(**locals())
    check("C")
    phase_mixout(**locals())
    check("D")
    phase_peer(**locals())
    check("E")


def phase_peer(nc, sc, A, psbank, PB, check, L, dump, ident_f, b_ident, ident_b, b_identb, base_mark, **_):
    xhT_d, h1_d, out_d, uT_d, vb_d = L["xhT_d"], L["h1_d"], L["out_d"], L["uT_d"], L["vb_d"]
    xh_rows = xhT_d.rearrange("(c p) t -> p c t", p=128)
    h1_t = h1_d.rearrange("(n p) d -> n p d", p=128)
    out_t = out_d.rearrange("(n p) d -> n p d", p=128)
    NEGBIG = -1e30

    aT = A.alloc([S], BF16)
    bT = A.alloc([S], BF16)
    wT = A.alloc([S], F32)
    b_aT, b_bT, b_wT = sc.buf("aT"), sc.buf("bT"), sc.buf("wT")
    iota_f = A.alloc([128], F32)
    iota_b = A.alloc([128], BF16)
    b_iota, b_iotab = sc.buf("iota"), sc.buf("iotab")
    sc.dma(iota_f, L["iota_d"], b_iota, writes=[b_iota])
    sc.op("dve", lambda e: e.tensor_copy(iota_b, iota_f), reads=[b_iota], writes=[b_iotab])
    gfin = A.alloc([1024], F32)
    b_gfin = sc.buf("gfin")
    sc.dma(gfin, L["gf_d"], b_gfin, writes=[b_gfin])
    m1 = A.mark()

    wqb = A.alloc([8, 1024], BF16)
    mwq = A.mark()
    wqf = A.alloc([8, 1024], F32)
    b_wqf, b_wqb = sc.buf("wqf"), sc.buf("wqb")
    sc.dma(wqf, L["wq_d"], b_wqf, writes=[b_wqf])
    sc.op("pool", lambda e: e.tensor_copy(wqb, wqf), reads=[b_wqf], writes=[b_wqb])
    sc.barrier()
    A.reset(mwq)
    u_ch = L["u_d"].rearrange("(a p) d -> a p d", p=128)
    v_ch = L["v_d"].rearrange("(a p) d -> a p d", p=128)
    uf = Ring(sc, "uf", [A.alloc([1024], F32) for _ in range(3)])
    ubf = Ring(sc, "ubf", [A.alloc([1024], BF16) for _ in range(2)])
    uTs = Ring(sc, "uTs", [A.alloc([8, 128], BF16) for _ in range(2)])
    vf = Ring(sc, "vf", [A.alloc([1024], F32) for _ in range(3)])
    vbf = Ring(sc, "vbf", [A.alloc([1024], BF16) for _ in range(2)])
    ptE0 = psbank(1).bitcast(BF16)
    e0l = {}

    def e0_load(a):
        ua, uab = uf.next()
        sc.dma(ua, u_ch[a], uab, writes=[uab])
        va, vab = vf.next()
        sc.dma(va, v_ch[a], vab, writes=[vab])
        e0l[a] = (ua, uab, va, vab)

    def e0_chunk(a):
        if a + 2 < 128:
            e0_load(a + 2)
        ua, uab, va, vab = e0l.pop(a)
        ub, ubb = ubf.next()
        sc.op("pool", lambda e: e.tensor_copy(ub, ua), reads=[uab], writes=[ubb])
        for k in range(8):
            sc.op("pe", lambda e, k=k: e.transpose(ptE0[:, k * 128:(k + 1) * 128], ub[:, k * 128:(k + 1) * 128], ident_b),
                  reads=[ubb, b_identb], writes=[PB[1]] if k == 0 else (), pwrites=() if k == 0 else [PB[1]], sig=(k == 7))
        us, usb = uTs.next()
        sc.op("act", lambda e: e.copy(us, ptE0.rearrange("p (k c) -> p k c", k=8)), reads=[PB[1]], writes=[usb])
        sc.dma(uT_d[a], us, usb, reads=[usb])
        vb_, vbb = vbf.next()
        sc.op("act", lambda e: e.copy(vb_, va), reads=[vab], writes=[vbb])
        sc.dma(vb_d[a], vb_, vbb, reads=[vbb])

    kbf = A.alloc([256], F32)
    kbb = A.alloc([256], BF16)
    b_kbf, b_kbb = sc.buf("kbf"), sc.buf("kbb")
    sc.dma(kbf, L["kblk_d"], b_kbf, writes=[b_kbf])
    sc.op("pool", lambda e: e.tensor_copy(kbb, kbf), reads=[b_kbf], writes=[b_kbb])
    xg = Ring(sc, "xg", [A.alloc([8, 512], BF16) for _ in range(2)])
    qTr = Ring(sc, "qTr", [A.alloc([8, 512], BF16) for _ in range(1)])
    S12r = Ring(sc, "S12", [A.alloc([8, 256], F32) for _ in range(2)])
    v12r = Ring(sc, "v12", [A.alloc([16, 16], F32) for _ in range(2)])
    i12r = Ring(sc, "i12", [A.alloc([16, 16], U32) for _ in range(2)])
    i12fr = Ring(sc, "i12f", [A.alloc([16, 16], F32) for _ in range(2)])
    tmpr = Ring(sc, "tmpk", [A.alloc([16, 128], F32) for _ in range(1)])
    candr = Ring(sc, "cand", [A.alloc([8, 256], F32) for _ in range(1)])
    tmpcr = Ring(sc, "tmpc", [A.alloc([8, 256], F32) for _ in range(1)])
    svr = Ring(sc, "sv", [A.alloc([8, 16], F32) for _ in range(2)])
    cir = Ring(sc, "ci", [A.alloc([8, 16], U32) for _ in range(2)])
    hlr = Ring(sc, "hl", [A.alloc([2, 128], U32) for _ in range(1)])
    hlfr = Ring(sc, "hlf", [A.alloc([2, 128], F32) for _ in range(1)])
    eqr = Ring(sc, "eq", [A.alloc([8, 256], F32) for _ in range(1)])
    abw = Ring(sc, "abw", [A.alloc([3, 128], F32) for _ in range(2)])
    smx = Ring(sc, "smx", [A.alloc([128], F32) for _ in range(2)])
    ssr = Ring(sc, "ss", [A.alloc([16], F32) for _ in range(2)])
    psq = Ring(sc, "psq", [psbank(i) for i in (0,)], PB[0:1])
    e0_load(0)
    e0_load(1)
    ps12 = [(psbank(i), PB[i]) for i in (2, 3, 4, 5)]
    for tg in range(8):
        xa, xab = xg.next()
        sc.dma(xa, xh_rows[:, :, tg * 512:(tg + 1) * 512], xab, writes=[xab])
        qT, qTb = qTr.next()
        sc.op("pool", lambda e: e.memset(qT[:, 0, 0:2], 0.0), writes=[qTb])
        for h in range(8):
            ps, psb = psq.next()
            for k in range(8):
                sc.op("pe", lambda e, k=k: e.matmul(ps, wqb[:, k, h * 128:(h + 1) * 128], xa[:, k, :], start=(k == 0), stop=(k == 7)),
                      reads=[b_wqb, xab], writes=[psb] if k == 0 else (), pwrites=() if k == 0 else [psb], sig=(k == 7))
            if h % 2 == 0:
                sc.op("act", lambda e: e.copy(qT[:, h, :], ps), reads=[psb], pwrites=[qTb])
            else:
                sc.op("dve", lambda e: e.tensor_copy(qT[:, h, :], ps), reads=[psb], pwrites=[qTb])
        for tt in range(4):
            t = tg * 4 + tt
            tsl = slice(tt * 128, (tt + 1) * 128)
            s12, s12b = S12r.next()
            for h in range(8):
                ps, psb = ps12[h // 2]
                sc.op("pe", lambda e: e.matmul(ps[:, (h % 2) * 256:(h % 2) * 256 + 256], qT[:, h, tsl], kbb, start=True, stop=True),
                      reads=[qTb, b_kbb], writes=[psb] if h % 2 == 0 else (), pwrites=() if h % 2 == 0 else [psb], sig=(h % 2 == 1))
                if h % 2 == 1:
                    sc.op("act", lambda e: e.copy(s12[:, h - 1:h + 1, :], ps.rearrange("p (h c) -> p h c", h=2)), reads=[psb],
                          writes=[s12b] if h == 1 else (), pwrites=() if h == 1 else [s12b])
            for a_ in range(4 * t, 4 * t + 4):
                e0_chunk(a_)
            v12, v12b = v12r.next()
            i12, i12b = i12r.next()
            tm_all, tmb = tmpr.next()
            rows = [s12[:, r // 2, (r % 2) * 128:(r % 2 + 1) * 128] for r in range(16)]
            for r in range(16):
                sc.op("dve", lambda e: e.max(out=v12[:, r, 0:8], in_=rows[r]), reads=[s12b], writes=[v12b] if r == 0 else (), pwrites=() if r == 0 else [v12b])
            for r in range(16):
                sc.op("dve", lambda e: e.max_index(out=i12[:, r, 0:8], in_max=v12[:, r, 0:8], in_values=rows[r]), reads=[s12b, v12b],
                      writes=[i12b] if r == 0 else (), pwrites=() if r == 0 else [i12b])
            for r in range(16):
                sc.op("dve", lambda e: e.match_replace(out=tm_all[:, r, :], in_to_replace=v12[:, r, 0:8], in_values=rows[r], imm_value=NEGBIG),
                      reads=[s12b, v12b], writes=[tmb] if r == 0 else (), pwrites=() if r == 0 else [tmb])
            for r in range(16):
                sc.op("dve", lambda e: e.max(out=v12[:, r, 8:16], in_=tm_all[:, r, :]), reads=[tmb], pwrites=[v12b])
            for r in range(16):
                sc.op("dve", lambda e: e.max_index(out=i12[:, r, 8:16], in_max=v12[:, r, 8:16], in_values=tm_all[:, r, :]), reads=[tmb, v12b], pwrites=[i12b])
            i12f, i12fb = i12fr.next()
            sc.op("dve", lambda e: e.tensor_copy(i12f, i12), reads=[i12b], writes=[i12fb])
            v4 = v12.rearrange("p (h two) i -> p h two i", two=2)
            i4 = i12f.rearrange("p (h two) i -> p h two i", two=2)
            cand, candb = candr.next()
            c4 = cand.rearrange("p h (i j) -> p h i j", i=16)
            sc.op("dve", lambda e: e.tensor_tensor(c4, v4[:, :, 0, :].unsqueeze(3).to_broadcast([128, 8, 16, 16]),
                                                   v4[:, :, 1, :].unsqueeze(2).to_broadcast([128, 8, 16, 16]), op=ALU.add),
                  reads=[v12b], writes=[candb])
            sv, svb = svr.next()
            ci, cib = cir.next()
            tc_all, tcb = tmpcr.next()
            for h in range(8):
                sc.op("dve", lambda e: e.max(out=sv[:, h, 0:8], in_=cand[:, h, :]), reads=[candb], writes=[svb] if h == 0 else (), pwrites=() if h == 0 else [svb])
            for h in range(8):
                sc.op("dve", lambda e: e.max_index(out=ci[:, h, 0:8], in_max=sv[:, h, 0:8], in_values=cand[:, h, :]), reads=[candb, svb],
                      writes=[cib] if h == 0 else (), pwrites=() if h == 0 else [cib])
            for h in range(8):
                sc.op("dve", lambda e: e.match_replace(out=tc_all[:, h, :], in_to_replace=sv[:, h, 0:8], in_values=cand[:, h, :], imm_value=NEGBIG),
                      reads=[candb, svb], writes=[tcb] if h == 0 else (), pwrites=() if h == 0 else [tcb])
            for h in range(8):
                sc.op("dve", lambda e: e.max(out=sv[:, h, 8:16], in_=tc_all[:, h, :]), reads=[tcb], pwrites=[svb])
            for h in range(8):
                sc.op("dve", lambda e: e.max_index(out=ci[:, h, 8:16], in_max=sv[:, h, 8:16], in_values=tc_all[:, h, :]), reads=[tcb, svb], pwrites=[cib])
            hl, hlb = hlr.next()
            cif = ci.rearrange("p h j -> p (h j)")
            sc.op("dve", lambda e: e.tensor_scalar(hl[:, 0, :], cif, 4, None, op0=ALU.logical_shift_right), reads=[cib], writes=[hlb])
            sc.op("dve", lambda e: e.tensor_scalar(hl[:, 1, :], cif, 15, None, op0=ALU.bitwise_and), reads=[cib], pwrites=[hlb])
            hlf, hlfb = hlfr.next()
            sc.op("dve", lambda e: e.tensor_copy(hlf, hl), reads=[hlb], writes=[hlfb])
            ab, abb = abw.next()
            for which in range(2):
                eq, eqb = eqr.next()
                e4 = eq.rearrange("p h (j i) -> p h j i", j=16)
                sel = hlf[:, which, :].rearrange("p (h j) -> p h j", h=8)
                sc.op("dve", lambda e: e.tensor_tensor(e4, sel.unsqueeze(3).to_broadcast([128, 8, 16, 16]),
                                                       iota_f[:, 0:16].unsqueeze(1).unsqueeze(1).to_broadcast([128, 8, 16, 16]), op=ALU.is_equal),
                      reads=[hlfb, b_iota], writes=[eqb])
                sc.op("dve", lambda e: e.tensor_tensor(e4, e4, i4[:, :, which, :].unsqueeze(2).to_broadcast([128, 8, 16, 16]), op=ALU.mult),
                      reads=[eqb, i12fb], writes=[eqb])
                sc.op("dve", lambda e: e.reduce_sum(ab[:, which, :], e4.rearrange("p h j i -> p (h j) i"), axis=AX.X),
                      reads=[eqb], writes=[abb] if which == 0 else (), pwrites=() if which == 0 else [abb])
            sx, sxb = smx.next()
            sx3 = sx.rearrange("p (h j) -> p h j", h=8)
            ss, ssb = ssr.next()
            sc.op("dve", lambda e: e.tensor_tensor(sx3, sv, sv[:, :, 0:1].to_broadcast([128, 8, 16]), op=ALU.subtract), reads=[svb], writes=[sxb])
            sc.op("act", lambda e: e.activation(sx, sx, AF.Exp), reads=[sxb], writes=[sxb])
            sc.op("dve", lambda e: e.reduce_sum(ss[:, 0:8], sx3, axis=AX.X), reads=[sxb], writes=[ssb])
            sc.op("dve", lambda e: e.reciprocal(ss[:, 8:16], ss[:, 0:8]), reads=[ssb], pwrites=[ssb])
            sc.op("dve", lambda e: e.tensor_tensor(ab[:, 2, :].rearrange("p (h j) -> p h j", h=8), sx3,
                                                   ss[:, 8:16].unsqueeze(2).to_broadcast([128, 8, 16]), op=ALU.mult),
                  reads=[sxb, ssb], pwrites=[abb])
            psT, psTb = psbank(6 + (t % 2), 384), PB[6 + (t % 2)]
            for i in range(3):
                sc.op("pe", lambda e, i=i: e.transpose(psT[:, i * 128:(i + 1) * 128], ab[:, i, :], ident_f),
                      reads=[abb, b_ident], writes=[psTb] if i == 0 else (), pwrites=() if i == 0 else [psTb], sig=(i == 2))
            tk = slice(t * 128, (t + 1) * 128)
            sc.op("act", lambda e: e.copy(aT[:, tk], psT[:, 0:128]), reads=[psTb], pwrites=[b_aT])
            sc.op("act", lambda e: e.copy(bT[:, tk], psT[:, 128:256]), reads=[psTb], pwrites=[b_bT])
            sc.op("act", lambda e: e.copy(wT[:, tk], psT[:, 256:384]), reads=[psTb], pwrites=[b_wT])
    dump("d_aT", aT, b_aT, BF16); dump("d_bT", bT, b_bT, BF16); dump("d_wT", wT, b_wT)
    sc.barrier()
    A.reset(m1)
    check("E1")

    TG = 256
    SB = 16
    GT = A.alloc([TG, 128], BF16)
    b_GT = sc.buf("GT")
    A1r = Ring(sc, "A1", [A.alloc([SB, 128], BF16) for _ in range(2)])
    B1r = Ring(sc, "B1", [A.alloc([SB, 128], BF16) for _ in range(2)])
    B1wr = Ring(sc, "B1w", [A.alloc([SB, 128], BF16) for _ in range(2)])
    ur = Ring(sc, "ur", [A.alloc([8, 128], BF16) for _ in range(7)])
    vr = Ring(sc, "vr", [A.alloc([1024], BF16) for _ in range(7)])
    ger = Ring(sc, "ge", [A.alloc([TG], F32) for _ in range(4)])
    WTr = Ring(sc, "WTe", [A.alloc([TG], BF16) for _ in range(4)])
    xgr = Ring(sc, "xge", [A.alloc([8, TG], BF16) for _ in range(2)])
    h1r = Ring(sc, "h1e", [A.alloc([1024], F32) for _ in range(2)])
    h2r = Ring(sc, "h2e", [A.alloc([1024], F32) for _ in range(1)])
    outr = Ring(sc, "oute", [A.alloc([1024], F32) for _ in range(2)])
    rings = {"sq": Ring(sc, "esq", [A.alloc([1024], F32) for _ in range(1)]), "st": Ring(sc, "est", [A.alloc([4], F32) for _ in range(2)])}
    psA = Ring(sc, "psAct", [psbank(i, TG) for i in (4, 5, 7)], [PB[4], PB[5], PB[7]])
    psG = Ring(sc, "psG", [psbank(i) for i in (6, 7)], PB[6:8])
    for grp in range(S // TG):
        t0 = grp * TG
        xa, xab = xgr.next()
        sc.dma(xa, xh_rows[:, :, t0:t0 + TG], xab, writes=[xab])
        sc.op("pool", lambda e: e.memset(GT[:, 0, 0:2], 0.0), writes=[b_GT])
        def onehots(sb_):
            ts0 = t0 + sb_ * SB
            a1, a1b = A1r.next()
            b1, b1b = B1r.next()
            b1w, b1wb = B1wr.next()
            io3 = iota_b.unsqueeze(1).to_broadcast([128, SB, 128])
            sc.op("dve", lambda e: e.tensor_tensor(a1, io3, aT[:, ts0:ts0 + SB].unsqueeze(2).to_broadcast([128, SB, 128]), op=ALU.is_equal),
                  reads=[b_iotab, b_aT], writes=[a1b])
            sc.op("dve", lambda e: e.tensor_tensor(b1, io3, bT[:, ts0:ts0 + SB].unsqueeze(2).to_broadcast([128, SB, 128]), op=ALU.is_equal),
                  reads=[b_iotab, b_bT], writes=[b1b])
            sc.op("dve", lambda e: e.tensor_tensor(b1w, b1, wT[:, ts0:ts0 + SB].unsqueeze(2).to_broadcast([128, SB, 128]), op=ALU.mult),
                  reads=[b1b, b_wT], writes=[b1wb])
            return a1, a1b, b1w, b1wb

        def gmm(sb_, a1, a1b, b1w, b1wb):
            for q4 in range(SB // 4):
                pg, pgb = psG.next()
                for i in range(4):
                    tl = q4 * 4 + i
                    sc.op("pe", lambda e: e.matmul(pg[:, i * 128:(i + 1) * 128], b1w[:, tl, :], a1[:, tl, :], start=True, stop=True),
                          reads=[b1wb, a1b], writes=[pgb] if i == 0 else (), pwrites=() if i == 0 else [pgb], sig=(i == 3))
                tg0 = sb_ * SB + q4 * 4
                sc.op("act", lambda e: e.copy(GT[:, tg0:tg0 + 4, :], pg.rearrange("p (t a) -> p t a", t=4)), reads=[pgb], pwrites=[b_GT])

        cur_oh = onehots(0)
        for sb_ in range(TG // SB):
            nxt_oh = onehots(sb_ + 1) if sb_ + 1 < TG // SB else None
            gmm(sb_, *cur_oh)
            cur_oh = nxt_oh
        if grp == 0:
            dump("d_GT0", GT, b_GT, BF16)
        NPF = 6
        wl = {}

        def load_uv(a):
            ua, uab = ur.next()
            sc.dma(ua, uT_d[a], uab, writes=[uab])
            va, vab = vr.next()
            sc.dma(va, vb_d[a], vab, writes=[vab], q="act" if E2_VQ_ACT else "sp")
            wl[a] = (ua, uab, va, vab)

        def act_mm(a):
            ua, uab, _, _ = wl[a]
            pa, pab = psA.next()
            for k in range(8):
                sc.op("pe", lambda e, k=k: e.matmul(pa, ua[:, k, :], xa[:, k, :], start=(k == 0), stop=(k == 7)),
                      reads=[uab, xab], writes=[pab] if k == 0 else (), pwrites=() if k == 0 else [pab], sig=(k == 7))
            return pa, pab

        for a in range(min(NPF, 128)):
            load_uv(a)
        pend_act = [act_mm(0), act_mm(1)]
        for a in range(128):
            if a + NPF < 128:
                load_uv(a + NPF)
            if a + 2 < 128:
                pend_act.append(act_mm(a + 2))
            pa, pab = pend_act.pop(0)
            _, _, va, vab = wl.pop(a)
            ge, geb = ger.next()
            sc.op("act", lambda e: e.activation(ge, pa, AF.Gelu), reads=[pab], writes=[geb])
            wt, wtb = WTr.next()
            sc.op("dve", lambda e: e.tensor_tensor(wt, ge, GT[:, :, a], op=ALU.mult), reads=[geb, b_GT], writes=[wtb])
            for i in range(2):
                for half in range(2):
                    bi = i * 2 + half
                    sc.op("pe", lambda e: e.matmul(psbank(bi), wt[:, i * 128:(i + 1) * 128], va[:, half * 512:(half + 1) * 512],
                                                   start=(a == 0), stop=(a == 127)),
                          reads=[wtb, vab], writes=[PB[bi]] if a == 0 else (), pwrites=() if a == 0 else [PB[bi]], sig=(bi == 3))
        for i in range(2):
            t = grp * 2 + i
            h1, h1b = h1r.next()
            sc.dma(h1, h1_t[t], h1b, writes=[h1b])
            h2, h2b = h2r.next()
            for half in range(2):
                bi = i * 2 + half
                sc.op("dve", lambda e: e.tensor_tensor(h2[:, half * 512:(half + 1) * 512], psbank(bi), h1[:, half * 512:(half + 1) * 512], op=ALU.add),
                      reads=[PB[bi], h1b], writes=[h2b] if half == 0 else (), pwrites=() if half == 0 else [h2b])
            if grp == 0 and i == 0:
                dump("d_h2", h2, h2b)
            oo, oob = outr.next()
            rmsnorm_tile(sc, rings, h2, h2b, gfin, b_gfin, oo, oob)
            sc.dma(out_t[t], oo, oob, reads=[oob])
        check("E2_%d" % grp)
    sc.barrier()
    A.reset(base_mark)


def rmsnorm_tile(sc, A_rings, src, srcb, g_bc, b_g, dst, dstb, width=1024):
    sq, sqb = A_rings["sq"].next()
    st, stb = A_rings["st"].next()
    sc.op("act", lambda e: e.activation(sq, src, AF.Square), reads=[srcb], writes=[sqb])
    sc.op("dve", lambda e: e.reduce_sum(st[:, 0:1], sq, axis=AX.X), reads=[sqb], writes=[stb])
    sc.op("dve", lambda e: e.tensor_scalar(st[:, 1:2], st[:, 0:1], 1.0 / width, EPS, op0=ALU.mult, op1=ALU.add), reads=[stb], pwrites=[stb])
    sc.op("act", lambda e: e.activation(st[:, 2:3], st[:, 1:2], AF.Ln), reads=[stb], pwrites=[stb])
    sc.op("act", lambda e: e.activation(st[:, 3:4], st[:, 2:3], AF.Exp, scale=-0.5), reads=[stb], pwrites=[stb])
    sc.op("dve", lambda e: e.scalar_tensor_tensor(dst, src, st[:, 3:4], g_bc, op0=ALU.mult, op1=ALU.mult),
          reads=[srcb, stb, b_g], writes=[dstb])


def phase_mixout(nc, sc, A, psbank, PB, check, L, dump, zT_d, x_d, ident_b, b_identb, base_mark, **_):
    hmT_d, onT_d, h1_d, xhT_d = L["hmT_d"], L["onT_d"], L["h1_d"], L["xhT_d"]
    zt_rows = zT_d.rearrange("(c p) t -> p c t", p=128)
    hm_rows = hmT_d.rearrange("(c p) t -> p c t", p=128)
    on_rows = onT_d.rearrange("(c p) t -> p c t", p=128)
    xh_rows = xhT_d.rearrange("(c p) t -> p c t", p=128)
    W = [(A.alloc([8, 1024], BF16), sc.buf("wsq")) for _ in range(3)]
    mst = A.mark()
    wst = Ring(sc, "wst", [A.alloc([8, 1024], F32) for _ in range(2)])
    for i in range(3):
        wa, wb = wst.next()
        sc.dma(wa, L["wsq_d"][i], wb, writes=[wb])
        sc.op("pool", lambda e: e.tensor_copy(W[i][0], wa), reads=[wb], writes=[W[i][1]])
    sc.barrier()
    A.reset(mst)
    gffn = A.alloc([1024], F32)
    b_gffn = sc.buf("gffn")
    sc.dma(gffn, L["gffn_d"], b_gffn, writes=[b_gffn])
    hin = Ring(sc, "hin", [A.alloc([8, 512], BF16) for _ in range(2)])
    oin = Ring(sc, "oin", [A.alloc([8, 512], BF16) for _ in range(2)])
    gain = Ring(sc, "gain", [A.alloc([8, 512], BF16) for _ in range(2)])
    gbin = Ring(sc, "gbin", [A.alloc([8, 512], BF16) for _ in range(2)])
    mixT = Ring(sc, "mixT", [A.alloc([8, 512], BF16) for _ in range(2)])
    t1r = Ring(sc, "dt1", [A.alloc([512], F32) for _ in range(2)])
    t2r = Ring(sc, "dt2", [A.alloc([512], F32) for _ in range(2)])
    xin = Ring(sc, "dxin", [A.alloc([1024], F32) for _ in range(2)])
    h1r = Ring(sc, "h1", [A.alloc([1024], F32) for _ in range(2)])
    xhb = Ring(sc, "xhb", [A.alloc([1024], BF16) for _ in range(2)])
    xhT = Ring(sc, "xhT", [A.alloc([8, 512], BF16) for _ in range(2)])
    rings = {"sq": Ring(sc, "dsq", [A.alloc([1024], F32) for _ in range(1)]), "st": Ring(sc, "dst", [A.alloc([4], F32) for _ in range(2)])}
    psr = Ring(sc, "psD", [psbank(i) for i in range(6)], PB[0:6])
    pst = Ring(sc, "pstD", [psbank(i).bitcast(BF16) for i in (6, 7)], PB[6:8])
    x_t = x_d.rearrange("(n p) d -> n p d", p=128)
    h1_t = h1_d.rearrange("(n p) d -> n p d", p=128)
    for tg in range(8):
        ts_ = slice(tg * 512, (tg + 1) * 512)
        hi, hib = hin.next()
        sc.dma(hi, hm_rows[:, :, ts_], hib, writes=[hib])
        oi, oib = oin.next()
        sc.dma(oi, on_rows[:, :, ts_], oib, writes=[oib])
        ga, gab = gain.next()
        sc.dma(ga, zt_rows[:, ZR_GA // 128:ZR_GA // 128 + 8, ts_], gab, writes=[gab])
        gb, gbb = gbin.next()
        sc.dma(gb, zt_rows[:, ZR_GB // 128:ZR_GB // 128 + 8, ts_], gbb, writes=[gbb])
        mx, mxb = mixT.next()
        sc.op("pool", lambda e: e.memset(mx[:, 0, 0:2], 0.0), writes=[mxb])
        for dc in range(8):
            psa, psab = psr.next()
            for k in range(8):
                sc.op("pe", lambda e, k=k: e.matmul(psa, W[0][0][:, k, dc * 128:(dc + 1) * 128], hi[:, k, :], start=(k == 0), stop=(k == 7)),
                      reads=[W[0][1], hib], writes=[psab] if k == 0 else (), pwrites=() if k == 0 else [psab], sig=(k == 7))
            psb_, psbb = psr.next()
            for k in range(8):
                sc.op("pe", lambda e, k=k: e.matmul(psb_, W[1][0][:, k, dc * 128:(dc + 1) * 128], oi[:, k, :], start=(k == 0), stop=(k == 7)),
                      reads=[W[1][1], oib], writes=[psbb] if k == 0 else (), pwrites=() if k == 0 else [psbb], sig=(k == 7))
            t1, t1b = t1r.next()
            t2, t2b = t2r.next()
            sc.op("dve", lambda e: e.tensor_tensor(t1, psa, ga[:, dc, :], op=ALU.mult), reads=[psab, gab], writes=[t1b])
            sc.op("dve", lambda e: e.tensor_tensor(t2, psb_, gb[:, dc, :], op=ALU.mult), reads=[psbb, gbb], writes=[t2b])
            sc.op("pool", lambda e: e.tensor_tensor(mx[:, dc, :], t1, t2, op=ALU.add), reads=[t1b, t2b], pwrites=[mxb])
        xt_, xtb = xhT.next()
        sc.op("pool", lambda e: e.memset(xt_[:, 0, 0:2], 0.0), writes=[xtb])
        for tt in range(4):
            t = tg * 4 + tt
            xa, xab = xin.next()
            sc.dma(xa, x_t[t], xab, writes=[xab])
            h1, h1b = h1r.next()
            for half in range(2):
                ps, psb2 = psr.next()
                for k in range(8):
                    sc.op("pe", lambda e, k=k: e.matmul(ps, mx[:, k, tt * 128:(tt + 1) * 128], W[2][0][:, k, half * 512:(half + 1) * 512],
                                                        start=(k == 0), stop=(k == 7)),
                          reads=[W[2][1], mxb], writes=[psb2] if k == 0 else (), pwrites=() if k == 0 else [psb2], sig=(k == 7))
                sc.op("dve", lambda e: e.tensor_tensor(h1[:, half * 512:(half + 1) * 512], ps, xa[:, half * 512:(half + 1) * 512], op=ALU.add),
                      reads=[psb2, xab], writes=[h1b] if half == 0 else (), pwrites=() if half == 0 else [h1b])
            sc.dma(h1_t[t], h1, h1b, reads=[h1b])
            xh, xhbb = xhb.next()
            rmsnorm_tile(sc, rings, h1, h1b, gffn, b_gffn, xh, xhbb)
            pt, ptb = pst.next()
            for k in range(8):
                sc.op("pe", lambda e, k=k: e.transpose(pt[:, k * 128:(k + 1) * 128], xh[:, k * 128:(k + 1) * 128], ident_b),
                      reads=[xhbb, b_identb], writes=[ptb] if k == 0 else (), pwrites=() if k == 0 else [ptb], sig=(k == 7))
            sc.op("act", lambda e: e.copy(xt_[:, :, tt * 128:(tt + 1) * 128], pt.rearrange("p (k c) -> p k c", k=8)), reads=[ptb], pwrites=[xtb])
        sc.dma(xh_rows[:, :, ts_], xt_, xtb, reads=[xtb])
    sc.barrier()
    A.reset(base_mark)


def phase_nsa(nc, sc, A, psbank, PB, check, L, dump, zT_d, ztm_d, zsm_d, ident_b, b_identb, base_mark, **_):
    SC = float(128 ** -0.5)
    NEG = -30000.0
    onT_d = L["onT_d"]
    zt_rows = zT_d.rearrange("(c p) t -> p c t", p=128)
    ztm_t = ztm_d.rearrange("(n p) c -> p n c", p=128)
    onT_rows = onT_d.rearrange("(c p) t -> p c t", p=128)

    sm = A.alloc([NT, 32], F32)
    b_sm = sc.buf("smC")
    sc.dma(sm, zsm_d.rearrange("(n p) c -> p n c", p=128), b_sm, writes=[b_sm])
    gates = A.alloc([NT, 24], F32)
    b_gates = sc.buf("gates")
    sc.op("act", lambda e: e.activation(gates, sm[:, :, 8:32], AF.Sigmoid), reads=[b_sm], writes=[b_gates])

    ovl32 = A.alloc([2, 64], F32)
    b_ovl = sc.buf("ovl")
    sc.dma(ovl32, L["ovl_d"].rearrange("t p j -> p t j"), b_ovl, writes=[b_ovl])
    Eall = A.alloc([S], BF16)
    b_Eall = sc.buf("Eall")
    kcmpT = A.alloc([2, 256], BF16)
    b_kcmpT = sc.buf("kcmpT")
    vcx = A.alloc([2, 2, 193], BF16)
    b_vcx = sc.buf("vcx")
    cmark = A.mark()
    E32 = A.alloc([S], F32)
    b_E32 = sc.buf("E32")
    sc.dma(E32[0:64, :], L["eall_d"], b_E32, writes=[b_E32])
    sc.op("pool", lambda e: e.memset(Eall[64:128, :], 0.0), writes=[b_Eall])
    sc.op("pool", lambda e: e.tensor_copy(Eall[0:64, :], E32[0:64, :]), reads=[b_E32], pwrites=[b_Eall])

    sc.barrier()
    A.reset(cmark)
    c1mark = A.mark()
    sc.op("pool", lambda e: e.memset(kcmpT, 0.0), writes=[b_kcmpT])
    sc.op("pool", lambda e: e.memset(vcx, 0.0), writes=[b_vcx])
    for g in range(2):
        sc.op("pool", lambda e, g=g: e.memset(vcx[:, 0, g, 128:129], 1.0), pwrites=[b_vcx])
        sc.op("pool", lambda e, g=g: e.memset(vcx[0:127, 1, g, 128:129], 1.0), pwrites=[b_vcx])
        sc.op("pool", lambda e, g=g: e.tensor_copy(vcx[:, :, g, 129:193], ovl32), reads=[b_ovl], pwrites=[b_vcx])
    kvc = A.alloc([4, S], BF16)
    b_kvc = sc.buf("kvc")
    sc.dma(kvc, zt_rows[:, ZR_KC // 128:ZR_KC // 128 + 4, :], b_kvc, writes=[b_kvc])
    w1f = A.alloc([32, 128], F32)
    b_w1f = sc.buf("w1f")
    w1b = A.alloc([32, 128], BF16)
    b_w1b = sc.buf("w1b")
    w2f = A.alloc([128], F32)
    w2b = A.alloc([128], BF16)
    b_w2f, b_w2b = sc.buf("w2f"), sc.buf("w2b")
    posf = A.alloc([32], F32)
    posb = A.alloc([32], BF16)
    b_posf, b_posb = sc.buf("posf"), sc.buf("posb")
    cbias = A.alloc([1], F32)
    b_cbias = sc.buf("cbias")
    gT = A.alloc([256], BF16)
    b_gT = sc.buf("gT")
    for kv in range(2):
        sc.dma(w1f, L["cw1_d"][kv], b_w1f, writes=[b_w1f])
        sc.op("pool", lambda e: e.tensor_copy(w1b, w1f), reads=[b_w1f], writes=[b_w1b])
        sc.dma(w2f, L["cw2_d"][kv], b_w2f, writes=[b_w2f])
        sc.op("pool", lambda e: e.tensor_copy(w2b, w2f), reads=[b_w2f], writes=[b_w2b])
        sc.dma(posf, L["cpos_d"][kv], b_posf, writes=[b_posf])
        sc.op("pool", lambda e: e.tensor_copy(posb, posf), reads=[b_posf], writes=[b_posb])
        psb_ = psbank(0, 1)
        for l in range(32):
            sc.op("pe", lambda e, l=l: e.matmul(psb_, w1b[:, l, :], posb[:, l:l + 1], start=(l == 0), stop=(l == 31)),
                  reads=[b_w1b, b_posb], writes=[PB[0]] if l == 0 else (), pwrites=() if l == 0 else [PB[0]], sig=(l == 31))
        sc.op("dve", lambda e: e.tensor_copy(cbias, psb_), reads=[PB[0]], writes=[b_cbias])
        for g in range(2):
            src = kvc[:, kv * 2 + g, :].rearrange("p (n s) -> p n s", s=16)
            psp = psbank(1, 255)
            for l in range(32):
                sc.op("pe", lambda e, l=l, src=src: e.matmul(psp, w1b[:, l, :], src[:, 0:255, l] if l < 16 else src[:, 1:256, l - 16], start=(l == 0), stop=(l == 31)),
                      reads=[b_w1b, b_kvc], writes=[PB[1]] if l == 0 else (), pwrites=() if l == 0 else [PB[1]], sig=(l == 31))
            sc.op("pool", lambda e: e.memset(gT, 0.0), writes=[b_gT])
            sc.op("act", lambda e: e.activation(gT[:, 0:255], psp, AF.Gelu, bias=cbias[:, 0:1]), reads=[PB[1], b_cbias], pwrites=[b_gT])
            if kv == 0:
                pso = psbank(2, 255)
                sc.op("pe", lambda e: e.matmul(pso, w2b, gT[:, 0:255], start=True, stop=True), reads=[b_w2b, b_gT], writes=[PB[2]])
                sc.op("dve", lambda e, g=g: e.tensor_copy(kcmpT[:, g, 0:255], pso), reads=[PB[2]], pwrites=[b_kcmpT])
            else:
                for tt in range(2):
                    nn = 128 if tt == 0 else 127
                    pso = psbank(2 + tt, 128)
                    sc.op("pe", lambda e, tt=tt, nn=nn, pso=pso: e.matmul(pso[0:nn, :], gT[:, tt * 128:tt * 128 + nn], w2b, start=True, stop=True),
                          reads=[b_w2b, b_gT], writes=[PB[2 + tt]])
                    sc.op("dve", lambda e, tt=tt, nn=nn, g=g, pso=pso: e.tensor_copy(vcx[0:nn, tt, g, 0:128], pso[0:nn, :]),
                          reads=[PB[2 + tt]], pwrites=[b_vcx])
    dump("d_kcmpT", kcmpT, b_kcmpT, BF16)
    dump("d_vcx", vcx, b_vcx, BF16)
    sc.barrier()
    A.reset(c1mark)
    check("C1")

    qg = A.alloc([NT, 4, 128], BF16)
    qrg = A.alloc([NT, 4, 128], BF16)
    ksw = A.alloc([2, S], BF16)
    vsx = A.alloc([NT, 129], BF16)
    vwx = A.alloc([NT, 129], BF16)
    onT = A.alloc([4, S], BF16)
    b_qg, b_qrg, b_ksw, b_vsx, b_vwx, b_onT = [sc.buf(n_) for n_ in ("qg", "qrg", "ksw", "vsx", "vwx", "onT")]
    E32r = Ring(sc, "E32r", [A.alloc([512], F32) for _ in range(2)])
    PTr = Ring(sc, "PTr", [A.alloc([512], BF16) for _ in range(4)])
    impr = Ring(sc, "imp", [A.alloc([64], F32) for _ in range(2)])
    m8r = Ring(sc, "m8", [A.alloc([8], F32) for _ in range(2)])
    negr = Ring(sc, "neg", [A.alloc([64], BF16) for _ in range(2)])
    negTr = Ring(sc, "negT", [A.alloc([512], BF16) for _ in range(2)])
    for nT_, nTb_ in negTr.items:
        sc.op("pool", lambda e: e.memset(nT_[64:128, :], 0.0), writes=[nTb_])
    zzr = Ring(sc, "zz", [A.alloc([12], F32) for _ in range(2)])
    cfr = Ring(sc, "cf", [A.alloc([12], F32) for _ in range(2)])
    oacc = Ring(sc, "oacc", [A.alloc([128], F32) for _ in range(8)])
    obf = Ring(sc, "obf", [A.alloc([4, 128], BF16) for _ in range(2)])
    sring = [0]

    SBANKS = (0, 1, 7)

    def s_bank():
        sring[0] += 1
        i = SBANKS[sring[0] % 3]
        return psbank(i), PB[i]

    def pv_accumulate(pt, ptb, vx, vxb, kt_idx, accs, width):
        for hh in range(4):
            bi, co = accs[hh]
            out = psbank(bi, width, co)
            sc.op("pe", lambda e, hh=hh, out=out: e.matmul(out, pt[:, hh * 128:(hh + 1) * 128], vx[:, kt_idx, :], start=False, stop=False,
                                                          skip_group_check=True),
                  reads=[ptb, vxb], pwrites=[PB[bi]], sig=(hh == 3))

    def masked_exp(ps, psb, base, cm, pat, cmp_op):
        ea, eb = E32r.next()
        sc.op("act", lambda e: e.activation(ea, ps, AF.Exp, scale=SC), reads=[psb], writes=[eb])
        pt, ptb = PTr.next()
        sc.op("pool", lambda e: e.affine_select(pt.rearrange("p (h t) -> p h t", h=4), ea.rearrange("p (h t) -> p h t", h=4),
                                               pattern=pat, compare_op=cmp_op, fill=0.0, base=base, channel_multiplier=cm),
              reads=[eb], writes=[ptb])
        return pt, ptb

    def run_branch(kts, s_fn, e_fn, pv_fn):
        LA = 2
        pend = [s_fn(kt) for kt in kts[:LA]]
        for i, kt in enumerate(kts):
            if i + LA < len(kts):
                pend.append(s_fn(kts[i + LA]))
            pt, ptb = e_fn(kt, *pend.pop(0))
            pv_fn(kt, pt, ptb)

    def plain_exp(ps, psb):
        pt, ptb = PTr.next()
        sc.op("act", lambda e: e.activation(pt, ps, AF.Exp, scale=SC), reads=[psb], writes=[ptb])
        return pt, ptb

    for g in range(2):
        for hh in range(4):
            sc.dma(qg[:, :, hh, :], zt_rows[:, ZR_Q // 128 + 4 * g + hh, :].rearrange("p (n t) -> p n t", t=128), b_qg,
                   writes=[b_qg] if hh == 0 else (), pwrites=() if hh == 0 else [b_qg])
            sc.dma(qrg[:, :, hh, :], zt_rows[:, ZR_QR // 128 + 4 * g + hh, :].rearrange("p (n t) -> p n t", t=128), b_qrg,
                   writes=[b_qrg] if hh == 0 else (), pwrites=() if hh == 0 else [b_qrg])
        sc.dma(ksw[:, 0, :], zt_rows[:, ZR_KSR // 128 + g, :], b_ksw, writes=[b_ksw])
        sc.dma(ksw[:, 1, :], zt_rows[:, ZR_KWR // 128 + g, :], b_ksw, pwrites=[b_ksw])
        sc.dma(vsx[:, :, 0:128], ztm_t[:, :, 2048 + g * 128:2048 + (g + 1) * 128], b_vsx, writes=[b_vsx])
        sc.op("pool", lambda e: e.memset(vsx[:, :, 128:129], 1.0), pwrites=[b_vsx])
        sc.dma(vwx[:, :, 0:128], ztm_t[:, :, 2304 + g * 128:2304 + (g + 1) * 128], b_vwx, writes=[b_vwx])
        sc.op("pool", lambda e: e.memset(vwx[:, :, 128:129], 1.0), pwrites=[b_vwx])
        sc.op("pool", lambda e: e.memset(onT[:, 0, 0:2], 0.0), writes=[b_onT])
        for qt in range(NT):
            tq = slice(qt * 128, (qt + 1) * 128)
            qs = qg[:, qt, :, :].rearrange("p h t -> p (h t)")
            qrs = qrg[:, qt, :, :].rearrange("p h t -> p (h t)")
            for bi in (2, 3):
                sc.op("dve", lambda e, bi=bi: e.memset(psbank(bi, 386), 0.0), writes=[PB[bi]])
            accC = [(2, 0), (2, 193), (3, 0), (3, 193)]
            accW = [(4, 0), (4, 129), (5, 0), (5, 129)]
            accS = [(2, 0), (2, 129), (3, 0), (3, 129)]

            def s_cmp(kt):
                ps, psb = s_bank()
                sc.op("pe", lambda e: e.matmul(ps, kcmpT[:, g, kt * 128:(kt + 1) * 128], qs, start=True, stop=True),
                      reads=[b_kcmpT, b_qg], writes=[psb])
                return ps, psb

            run_branch(list(range(2 if qt >= 16 else 1)), s_cmp,
                       lambda kt, ps, psb: masked_exp(ps, psb, base=128 * qt - 31 - 2048 * kt, cm=-16, pat=[[0, 4], [1, 128]], cmp_op=ALU.is_ge),
                       lambda kt, pt, ptb: pv_accumulate(pt, ptb, vcx[:, :, g, :], b_vcx, kt, accC, 193))
            for bi in (4, 5):
                sc.op("dve", lambda e, bi=bi: e.memset(psbank(bi, 258), 0.0), writes=[PB[bi]])
            zz, zzb = zzr.next()
            for hh in range(4):
                bi, co = accC[hh]
                sc.op("dve", lambda e, hh=hh, bi=bi, co=co: e.tensor_scalar(zz[:, hh * 3:hh * 3 + 1], psbank(bi, 1, co + 128), 1e-30, None, op0=ALU.max),
                      reads=[PB[bi]], writes=[zzb] if hh == 0 else (), pwrites=() if hh == 0 else [zzb])
            im, imb = impr.next()
            rzc, rzcb = m8r.next()
            sc.op("dve", lambda e: e.reciprocal(rzc[:, 0:4], zz[:, 0:12:3]), reads=[zzb], writes=[rzcb])
            for hh in range(4):
                bi, co = accC[hh]
                Mh = psbank(bi, 64, co + 129)
                if hh == 0:
                    sc.op("dve", lambda e: e.tensor_scalar(im, Mh, rzc[:, 0:1], None, op0=ALU.mult), reads=[PB[bi], rzcb], writes=[imb])
                else:
                    sc.op("dve", lambda e: e.scalar_tensor_tensor(im, Mh, rzc[:, hh:hh + 1], im, op0=ALU.mult, op1=ALU.add),
                          reads=[PB[bi], rzcb, imb], pwrites=[imb])
            sc.op("pool", lambda e: e.memset(im[:, 2 * qt + 1:64], -1e9), reads=[imb], pwrites=[imb])
            sc.op("pool", lambda e: e.memset(im[:, 0:1], 1e9), pwrites=[imb])
            sc.op("pool", lambda e: e.memset(im[:, 2 * qt:2 * qt + 1], 1e9), pwrites=[imb])
            if qt >= 1:
                sc.op("pool", lambda e: e.memset(im[0:64, 2 * qt - 1:2 * qt], 1e9), pwrites=[imb])
            sc.op("pool", lambda e: e.memset(im[64:128, 2 * qt + 1:2 * qt + 2], 1e9), pwrites=[imb])
            m8, m8b = m8r.next()
            sc.op("dve", lambda e: e.max(out=m8, in_=im), reads=[imb], writes=[m8b])
            ng, ngb = negr.next()
            sc.op("dve", lambda e: e.tensor_scalar(ng, im, m8[:, 7:8], NEG, op0=ALU.is_lt, op1=ALU.mult), reads=[imb, m8b], writes=[ngb])
            dump("d_neg_%d_%d" % (g, qt), ng, ngb, BF16)

            def s_win(kt):
                ps, psb = s_bank()
                sc.op("pe", lambda e: e.matmul(ps, ksw[:, 1, kt * 128:(kt + 1) * 128], qrs, start=True, stop=True),
                      reads=[b_ksw, b_qrg], writes=[psb])
                return ps, psb

            def e_win(kt, ps, psb):
                if kt == qt:
                    return masked_exp(ps, psb, base=0, cm=-1, pat=[[0, 4], [1, 128]], cmp_op=ALU.is_ge)
                if kt == qt - 4:
                    return masked_exp(ps, psb, base=0, cm=1, pat=[[0, 4], [-1, 128]], cmp_op=ALU.is_gt)
                return plain_exp(ps, psb)

            run_branch(list(range(max(0, qt - 4), qt + 1)), s_win, e_win,
                       lambda kt, pt, ptb: pv_accumulate(pt, ptb, vwx, b_vwx, kt, accW, 129))
            psT = psbank(6, 64).bitcast(BF16)
            sc.op("pe", lambda e: e.transpose(psT[0:64, 0:128], ng, ident_b), reads=[ngb, b_identb], writes=[PB[6]])
            nT, nTb = negTr.next()
            sc.op("dve", lambda e: e.tensor_copy(nT[0:64, :].rearrange("p (h t) -> p h t", h=4), psT[0:64, 0:128].unsqueeze(1).to_broadcast([64, 4, 128])),
                  reads=[PB[6]], pwrites=[nTb])
            cf, cfb = cfr.next()
            sc.op("dve", lambda e: e.reciprocal(cf[:, 0:12:3], zz[:, 0:12:3]), reads=[zzb], writes=[cfb])
            sc.op("dve", lambda e: e.tensor_tensor(cf[:, 0:12:3], cf[:, 0:12:3], gates[:, qt, g * 12:g * 12 + 12:3], op=ALU.mult),
                  reads=[cfb, b_gates], pwrites=[cfb])
            oa_list = []
            for hh in range(4):
                bi, co = accC[hh]
                oa, oab = oacc.next()
                oa_list.append((oa, oab))
                sc.op("act", lambda e: e.activation(oa, psbank(bi, 128, co), AF.Copy, scale=cf[:, hh * 3:hh * 3 + 1]),
                      reads=[PB[bi], cfb], writes=[oab])
            for bi in (2, 3):
                sc.op("dve", lambda e, bi=bi: e.memset(psbank(bi, 258), 0.0), writes=[PB[bi]])

            def s_slc(kt):
                ps, psb = s_bank()
                sc.op("pe", lambda e: e.matmul(ps, ksw[:, 0, kt * 128:(kt + 1) * 128], qrs, start=True, stop=False),
                      reads=[b_ksw, b_qrg], writes=[psb], sig=False)
                sc.op("pe", lambda e: e.matmul(ps, Eall[:, kt * 128:(kt + 1) * 128], nT, start=False, stop=True),
                      reads=[b_Eall, nTb], pwrites=[psb])
                return ps, psb

            run_branch(list(range(qt + 1)), s_slc,
                       lambda kt, ps, psb: (masked_exp(ps, psb, base=0, cm=-1, pat=[[0, 4], [1, 128]], cmp_op=ALU.is_ge)
                                            if kt == qt else plain_exp(ps, psb)),
                       lambda kt, pt, ptb: pv_accumulate(pt, ptb, vsx, b_vsx, kt, accS, 129))
            for hh in range(4):
                bi, co = accS[hh]
                sc.op("dve", lambda e: e.tensor_scalar(zz[:, hh * 3 + 1:hh * 3 + 2], psbank(bi, 1, co + 128), 1e-30, None, op0=ALU.max),
                      reads=[PB[bi]], pwrites=[zzb])
                bi, co = accW[hh]
                sc.op("dve", lambda e: e.tensor_scalar(zz[:, hh * 3 + 2:hh * 3 + 3], psbank(bi, 1, co + 128), 1e-30, None, op0=ALU.max),
                      reads=[PB[bi]], pwrites=[zzb])
            for br in (1, 2):
                sc.op("dve", lambda e: e.reciprocal(cf[:, br:12:3], zz[:, br:12:3]), reads=[zzb, cfb], pwrites=[cfb])
                sc.op("dve", lambda e: e.tensor_tensor(cf[:, br:12:3], cf[:, br:12:3], gates[:, qt, g * 12 + br:g * 12 + 12:3], op=ALU.mult),
                      reads=[cfb, b_gates], pwrites=[cfb])
            ob, obb = obf.next()
            for hh in range(4):
                oa, oab = oa_list[hh]
                bs, cs = accS[hh]
                bw, cw_ = accW[hh]
                sc.op("dve", lambda e: e.scalar_tensor_tensor(oa, psbank(bs, 128, cs), cf[:, hh * 3 + 1:hh * 3 + 2], oa, op0=ALU.mult, op1=ALU.add),
                      reads=[PB[bs], cfb, oab], writes=[oab])
                sc.op("dve", lambda e: e.scalar_tensor_tensor(ob[:, hh, :], psbank(bw, 128, cw_), cf[:, hh * 3 + 2:hh * 3 + 3], oa, op0=ALU.mult, op1=ALU.add),
                      reads=[PB[bw], cfb, oab], writes=[obb] if hh == 0 else (), pwrites=() if hh == 0 else [obb])
            psO = psbank(6).bitcast(BF16)[:, 256:768]
            for hh in range(4):
                sc.op("pe", lambda e: e.transpose(psO[:, hh * 128:(hh + 1) * 128], ob[:, hh, :], ident_b),
                      reads=[obb, b_identb], writes=[PB[6]] if hh == 0 else (), pwrites=() if hh == 0 else [PB[6]], sig=(hh == 3))
            sc.op("act", lambda e: e.copy(onT[:, :, tq], psO.rearrange("p (h t) -> p h t", h=4)), reads=[PB[6]], pwrites=[b_onT])
        sc.dma(onT_rows[:, 4 * g:4 * g + 4, :], onT, b_onT, reads=[b_onT])
    sc.barrier()
    A.reset(base_mark)


def phase_mlstm(nc, sc, A, psbank, PB, check, L, dump, zT_d, ztm_d, zsm_d, hmT_d, tri_f, b_tri, ones_f, b_ones,
                ident_b, b_identb, mbias_d, mnormg_d, base_mark, **_):
    LN16 = float(np.log(16.0))
    sm = A.alloc([NT, 32], F32)
    b_sm = sc.buf("sm")
    sc.dma(sm, zsm_d.rearrange("(n p) c -> p n c", p=128), b_sm, writes=[b_sm])
    mbias = A.alloc([8], F32)
    b_mbias = sc.buf("mbias")
    sc.dma(mbias, mbias_d, b_mbias, writes=[b_mbias])
    fpre = A.alloc([NT, 4], F32)
    ipre = A.alloc([NT, 4], F32)
    b_fpre, b_ipre = sc.buf("fpre"), sc.buf("ipre")
    for h in range(4):
        sc.op("dve", lambda e, h=h: e.tensor_scalar(fpre[:, :, h], sm[:, :, 4 + h], mbias[:, 4 + h:5 + h], None, op0=ALU.add),
              reads=[b_sm, b_mbias], pwrites=[b_fpre])
        sc.op("dve", lambda e, h=h: e.tensor_scalar(ipre[:, :, h], sm[:, :, h], mbias[:, h:h + 1], None, op0=ALU.add),
              reads=[b_sm, b_mbias], pwrites=[b_ipre])
    fl = fpre.rearrange("p n h -> p (n h)")
    il = ipre.rearrange("p n h -> p (n h)")
    lf = A.alloc([128], F32)
    b_lf = sc.buf("lf")
    sc.op("act", lambda e: e.activation(lf, fl, AF.Exp, scale=-1.0), reads=[b_fpre], writes=[b_lf])
    sc.op("act", lambda e: e.activation(lf, lf, AF.Ln, bias=1.0), reads=[b_lf], writes=[b_lf])
    sc.op("dve", lambda e: e.tensor_scalar(lf, lf, -1.0, None, op0=ALU.mult), reads=[b_lf], writes=[b_lf])
    bcs = A.alloc([128], F32)
    ebl = A.alloc([128], F32)
    ik = A.alloc([128], F32)
    eks = A.alloc([128], F32)
    ebL = A.alloc([128], F32)
    b_bcs, b_ebl, b_ik, b_eks, b_ebL = [sc.buf(n_) for n_ in ("bcs", "ebl", "ik", "eks", "ebL")]
    ps0 = psbank(0, 128)
    sc.op("pe", lambda e: e.matmul(ps0, tri_f, lf, start=True, stop=True), reads=[b_tri, b_lf], writes=[PB[0]])
    sc.op("dve", lambda e: e.tensor_copy(bcs, ps0), reads=[PB[0]], writes=[b_bcs])
    sc.op("act", lambda e: e.activation(ebl, ps0, AF.Exp), reads=[PB[0]], writes=[b_ebl])
    ps1 = psbank(1, 128)
    sc.op("pe", lambda e: e.matmul(ps1, ones_f, lf, start=True, stop=True), reads=[b_ones, b_lf], writes=[PB[1]])
    sc.op("act", lambda e: e.activation(ebL, ps1, AF.Exp), reads=[PB[1]], writes=[b_ebL])
    sc.op("dve", lambda e: e.tensor_tensor(ik, il, bcs, op=ALU.subtract), reads=[b_ipre, b_bcs], writes=[b_ik])
    sc.op("dve", lambda e: e.tensor_scalar(ik, ik, -LN16, None, op0=ALU.add), reads=[b_ik], writes=[b_ik])
    sc.op("act", lambda e: e.activation(eks, ik, AF.Exp), reads=[b_ik], writes=[b_eks])
    mnormg = A.alloc([1024], F32)
    b_mnormg = sc.buf("mnormg")
    sc.dma(mnormg, mnormg_d, b_mnormg, writes=[b_mnormg])
    dump("d_lf", lf, b_lf); dump("d_bcs", bcs, b_bcs); dump("d_ebl", ebl, b_ebl); dump("d_ik", ik, b_ik)
    dump("d_eks", eks, b_eks); dump("d_ebL", ebL, b_ebL)
    check("Bgates")

    qkT = Ring(sc, "qkT", [A.alloc([4, S], BF16) for _ in range(2)])
    vh = Ring(sc, "vh", [A.alloc([NT, 257], BF16) for _ in range(2)])
    soh = Ring(sc, "soh", [A.alloc([NT, 256], BF16) for _ in range(2)])
    hmT = Ring(sc, "hmT", [A.alloc([2, S], BF16) for _ in range(2)])
    Cstate = Ring(sc, "Cst", [(A.alloc([2, 257], F32), A.alloc([2, 257], BF16), sc.buf("Cf"), sc.buf("Cb")) for _ in range(2)])
    DT = Ring(sc, "DT", [A.alloc([128], F32) for _ in range(2)])
    DTm = Ring(sc, "DTm", [A.alloc([128], F32) for _ in range(2)])
    WT = Ring(sc, "WT", [A.alloc([128], BF16) for _ in range(4)])
    k2 = Ring(sc, "k2", [A.alloc([256], BF16) for _ in range(4)])
    T1 = Ring(sc, "T1", [A.alloc([257], F32) for _ in range(2)])
    NUM = Ring(sc, "NUM", [A.alloc([257], F32) for _ in range(2)])
    hh = Ring(sc, "hh", [A.alloc([256], F32) for _ in range(2)])
    hfin = Ring(sc, "hfin", [A.alloc([256], BF16) for _ in range(2)])
    st8 = Ring(sc, "st8", [A.alloc([8], F32) for _ in range(4)])
    ctmp = Ring(sc, "ctmp", [A.alloc([257], F32) for _ in range(2)])
    zt_rows = zT_d.rearrange("(c p) t -> p c t", p=128)
    ztm_t = ztm_d.rearrange("(n p) c -> p n c", p=128)
    hmT_rows = hmT_d.rearrange("(c p) t -> p c t", p=128)
    def head_setup(h):
        qk, qkb = qkT.next()
        (Cf, Cb, b_Cf, b_Cb), _unused = Cstate.next()
        sc.dma(qk[:, 0:2, :], zt_rows[:, 2 * h:2 * h + 2, :], qkb, writes=[qkb])
        sc.dma(qk[:, 2:4, :], zt_rows[:, 8 + 2 * h:8 + 2 * h + 2, :], qkb, pwrites=[qkb])
        v, vb = vh.next()
        sc.dma(v[:, :, 0:256], ztm_t[:, :, h * 256:(h + 1) * 256], vb, writes=[vb])
        sc.op("pool", lambda e, v=v: e.memset(v[:, :, 256:257], 1.0), pwrites=[vb])
        so, sob = soh.next()
        sc.dma(so, ztm_t[:, :, 1024 + h * 256:1024 + (h + 1) * 256], sob, writes=[sob])
        hT, hTb = hmT.next()
        sc.op("pool", lambda e, hT=hT: e.memset(hT[:, 0, 0:2], 0.0), writes=[hTb])
        sc.op("pool", lambda e: e.memset(Cf, 0.0), writes=[b_Cf])
        sc.op("pool", lambda e: e.memset(Cb, 0.0), writes=[b_Cb])
        def stage1(n):
            col = n * 4 + h
            tk = slice(n * 128, (n + 1) * 128)
            psB = psbank(0, 128)
            sc.op("pe", lambda e, col=col: e.matmul(psB, lf[:, col:col + 1].to_broadcast([128, 128]), tri_f, start=True, stop=True),
                  reads=[b_lf, b_tri], writes=[PB[0]])
            dt, dtb = DT.next()
            sc.op("act", lambda e, dt=dt, col=col: e.activation(dt, psB, AF.Exp, bias=ik[:, col:col + 1]), reads=[PB[0], b_ik], writes=[dtb])
            dm, dmb = DTm.next()
            sc.op("pool", lambda e, dm=dm, dt=dt: e.tensor_tensor(dm, dt, tri_f, op=ALU.mult), reads=[dtb, b_tri], writes=[dmb])
            psS = psbank(1, 128)
            for c in range(2):
                sc.op("pe", lambda e, c=c, qk=qk, tk=tk: e.matmul(psS, qk[:, 2 + c, tk], qk[:, c, tk], start=(c == 0), stop=(c == 1)),
                      reads=[qkb], writes=[PB[1]] if c == 0 else (), pwrites=() if c == 0 else [PB[1]], sig=(c == 1))
            wt, wtb = WT.next()
            sc.op("dve", lambda e, wt=wt, dm=dm: e.tensor_tensor(wt, psS, dm, op=ALU.mult), reads=[PB[1], dmb], writes=[wtb])
            psK = psbank(2, 128).bitcast(BF16)
            for c in range(2):
                sc.op("pe", lambda e, c=c, qk=qk, tk=tk: e.transpose(psK[:, c * 128:(c + 1) * 128], qk[:, 2 + c, tk], ident_b),
                      reads=[qkb, b_identb], writes=[PB[2]] if c == 0 else (), pwrites=() if c == 0 else [PB[2]], sig=(c == 1))
            kk, kkb = k2.next()
            sc.op("act", lambda e, kk=kk, col=col: e.activation(kk, psK, AF.Copy, scale=eks[:, col:col + 1]), reads=[PB[2], b_eks], writes=[kkb])
            return wt, wtb, kk, kkb

        def stage2(n, wt, wtb, kk, kkb):
            col = n * 4 + h
            tk = slice(n * 128, (n + 1) * 128)
            psP1 = psbank(3, 257)
            for c in range(2):
                sc.op("pe", lambda e, c=c, qk=qk, tk=tk: e.matmul(psP1, qk[:, c, tk], Cb[:, c, :], start=(c == 0), stop=(c == 1)),
                      reads=[qkb, b_Cb], writes=[PB[3]] if c == 0 else (), pwrites=() if c == 0 else [PB[3]], sig=(c == 1))
            psP2 = psbank(4, 257)
            sc.op("pe", lambda e, wt=wt, v=v, n=n: e.matmul(psP2, wt, v[:, n, :], start=True, stop=True), reads=[wtb, vb], writes=[PB[4]])
            t1, t1b = T1.next()
            sc.op("act", lambda e, t1=t1, col=col: e.activation(t1, psP1, AF.Copy, scale=ebl[:, col:col + 1]), reads=[PB[3], b_ebl], writes=[t1b])
            nm, nmb = NUM.next()
            sc.op("dve", lambda e, nm=nm, t1=t1: e.tensor_tensor(nm, t1, psP2, op=ALU.add), reads=[t1b, PB[4]], writes=[nmb])
            s8, s8b = st8.next()
            sc.op("dve", lambda e, s8=s8, nm=nm: e.tensor_scalar(s8[:, 6:7], nm[:, 256:257], -1.0, None, op0=ALU.mult),
                  reads=[nmb], writes=[s8b])
            sc.op("dve", lambda e, s8=s8, nm=nm: e.scalar_tensor_tensor(s8[:, 0:1], nm[:, 256:257], 1.0, s8[:, 6:7], op0=ALU.max, op1=ALU.max),
                  reads=[nmb, s8b], pwrites=[s8b])
            sc.op("dve", lambda e, s8=s8: e.reciprocal(s8[:, 1:2], s8[:, 0:1]), reads=[s8b], pwrites=[s8b])
            hx, hxb = hh.next()
            sc.op("dve", lambda e, hx=hx, nm=nm, s8=s8: e.tensor_scalar(hx, nm[:, 0:256], s8[:, 1:2], None, op0=ALU.mult), reads=[nmb, s8b], writes=[hxb])
            hq, hqb = t1[:, 0:256], t1b
            sc.op("act", lambda e, hq=hq, hx=hx: e.activation(hq, hx, AF.Square), reads=[hxb, nmb], writes=[hqb])
            sc.op("dve", lambda e, s8=s8, hq=hq: e.reduce_sum(s8[:, 2:3], hq, axis=AX.X), reads=[hqb], pwrites=[s8b])
            sc.op("dve", lambda e, s8=s8: e.tensor_scalar(s8[:, 3:4], s8[:, 2:3], 1.0 / 256, EPS, op0=ALU.mult, op1=ALU.add), reads=[s8b], pwrites=[s8b])
            sc.op("act", lambda e, s8=s8: e.activation(s8[:, 4:5], s8[:, 3:4], AF.Ln), reads=[s8b], pwrites=[s8b])
            sc.op("act", lambda e, s8=s8: e.activation(s8[:, 5:6], s8[:, 4:5], AF.Exp, scale=-0.5), reads=[s8b], pwrites=[s8b])
            sc.op("dve", lambda e, hx=hx, s8=s8, h=h: e.scalar_tensor_tensor(hx, hx, s8[:, 5:6], mnormg[:, h * 256:(h + 1) * 256], op0=ALU.mult, op1=ALU.mult),
                  reads=[hxb, s8b, b_mnormg], writes=[hxb])
            hf, hfb = hfin.next()
            sc.op("pool", lambda e, hf=hf, hx=hx, so=so, n=n: e.tensor_tensor(hf, hx, so[:, n, :], op=ALU.mult), reads=[hxb, sob], writes=[hfb])
            psH = psbank(5, 128).bitcast(BF16)
            for c in range(2):
                sc.op("pe", lambda e, c=c, hf=hf: e.transpose(psH[:, c * 128:(c + 1) * 128], hf[:, c * 128:(c + 1) * 128], ident_b),
                      reads=[hfb, b_identb], writes=[PB[5]] if c == 0 else (), pwrites=() if c == 0 else [PB[5]], sig=(c == 1))
            sc.op("act", lambda e, hT=hT, tk=tk: e.copy(hT[:, :, tk], psH.rearrange("p (c t) -> p c t", c=2)), reads=[PB[5]], pwrites=[hTb])
            for c in range(2):
                psKV = psbank(6 + c, 257)
                sc.op("pe", lambda e, c=c, kk=kk, v=v, n=n, psKV=psKV: e.matmul(psKV, kk[:, c * 128:(c + 1) * 128], v[:, n, :], start=True, stop=True),
                      reads=[kkb, vb], writes=[PB[6 + c]])
                ct, ctb = ctmp.next()
                sc.op("dve", lambda e, c=c, ct=ct, psKV=psKV: e.tensor_tensor(ct, Cf[:, c, :], psKV, op=ALU.add), reads=[b_Cf, PB[6 + c]], writes=[ctb])
                sc.op("dve", lambda e, c=c, ct=ct, col=col: e.tensor_scalar(Cf[:, c, :], ct, ebL[:, col:col + 1], None, op0=ALU.mult),
                      reads=[ctb, b_ebL], pwrites=[b_Cf])
                sc.op("act", lambda e, c=c, ct=ct, col=col: e.activation(Cb[:, c, :], ct, AF.Copy, scale=ebL[:, col:col + 1]),
                      reads=[ctb, b_ebL], pwrites=[b_Cb])

        def finish():
            sc.dma(hmT_rows[:, 2 * h:2 * h + 2, :], hT, hTb, reads=[hTb])
        return stage1, stage2, finish

    for hp in range(2):
        H = [head_setup(2 * hp), head_setup(2 * hp + 1)]
        cur = [H[i][0](0) for i in range(2)]
        for n in range(NT):
            nxt_ = [H[i][0](n + 1) if n + 1 < NT else None for i in range(2)]
            for i in range(2):
                H[i][1](n, *cur[i])
            cur = nxt_
        for i in range(2):
            H[i][2]()
    sc.barrier()
    A.reset(base_mark)


def _finish(nc, sc, es):
    sc.barrier()

    with nc.Block() as block:
        @block.sync
        def _(e):
            sc.replay("sp", e)

        @block.tensor
        def _(e):
            sc.replay("pe", e)

        @block.scalar
        def _(e):
            sc.replay("act", e)

        @block.vector
        def _(e):
            sc.replay("dve", e)

        @block.gpsimd
        def _(e):
            sc.replay("pool", e)
    es.close()


def rope_tables_T():
    inv = (np.float32(10000.0) ** (-(np.arange(0, 128, 2, dtype=np.float32)) / np.float32(128))).astype(np.float32)
    ang = (np.arange(S, dtype=np.float32)[:, None] * inv[None, :]).astype(np.float32)
    ang = np.concatenate([ang, ang], axis=-1)
    cosT = np.cos(ang).astype(np.float32).T.copy()
    sinT = np.sin(ang).astype(np.float32).T.copy()
    sinT[:64, :] *= -1.0
    return np.ascontiguousarray(cosT), np.ascontiguousarray(sinT)


def _overlap_const():
    n = np.arange(256)[:, None]
    j = np.arange(64)[None, :]
    ov = ((16 * n < 64 * j + 64) & (16 * n + 32 > 64 * j) & (n < 255)).astype(np.float32)
    return np.ascontiguousarray(ov.reshape(2, 128, 64))


def _eall_const():
    j = np.arange(64)[:, None]
    p = np.arange(S)[None, :]
    return np.ascontiguousarray((p // 64 == j).astype(np.float32))


def _kblk(k1, k2):
    kb = np.zeros((128, 256), np.float32)
    kb[0:64, 0:128] = k1.T
    kb[64:128, 128:256] = k2.T
    return kb


def prep_shared(inputs):
    f = np.float32
    w_in = np.asarray(inputs["w_in"][0], dtype=f)

    def kp_layout(w):
        return np.ascontiguousarray(w.reshape(8, 128, w.shape[1]).transpose(1, 0, 2))

    wfm = w_in[:, fm_col_index()]
    w_fm = np.ascontiguousarray(kp_layout(wfm).reshape(128, 8, 60, 128).transpose(2, 0, 1, 3))
    wtm = w_in[:, tm_col_index()]
    w_tm = np.ascontiguousarray(kp_layout(wtm).reshape(128, 8, 5, 512).transpose(2, 0, 1, 3))
    w_sm = kp_layout(w_in[:, sm_col_index()])
    cosT, sinT = rope_tables_T()
    conv_w = np.asarray(inputs["m_conv_w"][0], dtype=f)
    conv_b = np.asarray(inputs["m_conv_b"][0], dtype=f)
    cw = np.concatenate([conv_w.T, conv_b[:, None]], axis=1)
    shared = {
        "g_mix": np.ascontiguousarray(np.broadcast_to(np.asarray(inputs["ln_mix_g"][0], dtype=f)[None, :], (128, D))),
        "w_fm": w_fm, "w_tm": w_tm, "w_sm": w_sm, "cosT": cosT, "sinT": sinT,
        "conv_w": np.ascontiguousarray(cw.reshape(16, 128, 5)),
        "ident": np.eye(128, dtype=f),
        "tri": np.triu(np.ones((128, 128), dtype=f)),
        "ovl": _overlap_const(), "eall": _eall_const(),
        "peer_u": np.ascontiguousarray(np.asarray(inputs["peer_u"][0], dtype=f)),
        "peer_v": np.ascontiguousarray(np.asarray(inputs["peer_v"][0], dtype=f)),
        "wq": kp_layout(np.asarray(inputs["peer_wq"][0], dtype=f)),
        "kblk": _kblk(np.asarray(inputs["peer_k1"][0], dtype=f), np.asarray(inputs["peer_k2"][0], dtype=f)),
        "iota": np.ascontiguousarray(np.broadcast_to(np.arange(128, dtype=f)[None, :], (128, 128))),
        "g_fin": np.ascontiguousarray(np.broadcast_to(np.asarray(inputs["ln_f_g"], dtype=f)[None, :], (128, D))),
        "wsq": np.ascontiguousarray(np.stack([kp_layout(np.asarray(inputs[k][0], dtype=f)) for k in ("w_m_out", "w_n_out", "w_out")])),
        "g_ffn": np.ascontiguousarray(np.broadcast_to(np.asarray(inputs["ln_ffn_g"][0], dtype=f)[None, :], (128, D))),
        "cw1": np.ascontiguousarray(np.stack([np.asarray(inputs[k][0], dtype=f).reshape(32, 128, 128).transpose(1, 0, 2)
                                              for k in ("cmp_k_w1", "cmp_v_w1")])),
        "cw2": np.ascontiguousarray(np.stack([np.asarray(inputs[k][0], dtype=f) for k in ("cmp_k_w2", "cmp_v_w2")])),
        "cpos": np.ascontiguousarray(np.stack([np.asarray(inputs[k][0], dtype=f).T for k in ("cmp_k_pos", "cmp_v_pos")])),
        "m_bias": np.ascontiguousarray(np.broadcast_to(np.concatenate([np.asarray(inputs["m_i_bias"][0], dtype=f),
                                                                       np.asarray(inputs["m_f_bias"][0], dtype=f)])[None, :], (128, 8))),
        "m_norm_g": np.ascontiguousarray(np.broadcast_to(np.asarray(inputs["m_norm_g"][0], dtype=f)[None, :], (128, 1024))),
    }
    return shared


def kernel(**inputs):
    shared = prep_shared(inputs)
    x = np.asarray(inputs["x"], dtype=np.float32)
    nc = build()
    in_maps = []
    for c in range(N_CORES):
        m = dict(shared)
        m["x"] = np.ascontiguousarray(x[c])
        in_maps.append(m)
    res = run_bass_kernel_spmd(nc, in_maps, core_ids=list(range(N_CORES)))
    return np.stack([np.asarray(r["out"], dtype=np.float32) for r in res.results], axis=0)
```
